# Optimizing a Trainium2 kernel written in Bass

```python
import math
import jax, jax.numpy as jnp
from jax import lax
import numpy as np

D_MODEL = 2048
BATCH = 8
SEQ = 4096
DEPTH = 1

CHUNK = 64
A_HEADS = 8
A_HEAD_DIM = 128
A_LEFT_CHUNKS = 8
REL_CLIP_LEFT = 256
REL_CLIP_RIGHT = CHUNK - 1
REL_SIZE = REL_CLIP_LEFT + REL_CLIP_RIGHT + 1
B_HEADS = 8
Q_LORA = 512
KV_LORA = 512
QK_NOPE = 128
QK_ROPE = 64
V_HEAD = 128
ROPE_THETA = 10000.0
Q_BLOCK = 128
N_KEYS = 128
N_EXPERTS = N_KEYS * N_KEYS
PEER_HEADS = 8
PEER_TOPK = 16
PEER_QDIM = 256
PEER_HALF = PEER_QDIM // 2
TOKEN_BLOCK = 128
DN_ALPHA = (2 * DEPTH) ** 0.25
DN_BETA = (8 * DEPTH) ** -0.25
LN_EPS = 1e-5
RMS_EPS = 1e-6
NEG = -1e30

A_WIDTH = A_HEADS * A_HEAD_DIM
B_WIDTH = B_HEADS * V_HEAD
IN_SIZES = [A_WIDTH, A_WIDTH, A_WIDTH, Q_LORA, KV_LORA, QK_ROPE, D_MODEL, D_MODEL]
IN_COLS = sum(IN_SIZES)

kernel_name = "hybrid_chunkattn_mla_peer_deepnorm_adaln"


def layer_norm(x, g, b):
    xf = x.astype(jnp.float32)
    mu = jnp.mean(xf, axis=-1, keepdims=True)
    var = jnp.mean(jnp.square(xf - mu), axis=-1, keepdims=True)
    y = (xf - mu) * lax.rsqrt(var + LN_EPS)
    return (y * g.astype(jnp.float32) + b.astype(jnp.float32)).astype(x.dtype)


def rms_norm(x, g):
    xf = x.astype(jnp.float32)
    y = xf * lax.rsqrt(jnp.mean(jnp.square(xf), axis=-1, keepdims=True) + RMS_EPS)
    return (y * g.astype(jnp.float32)).astype(x.dtype)


def rope(x, cos, sin):
    half = x.shape[-1] // 2
    x1, x2 = x[..., :half], x[..., half:]
    return jnp.concatenate([x1 * cos - x2 * sin, x1 * sin + x2 * cos], axis=-1)


def chunk_band_attention(q, k, v, rel_bias):
    bsz, s, h, dh = q.shape
    nc = s // CHUNK
    nb = A_LEFT_CHUNKS + 1
    qc = q.reshape(bsz, nc, CHUNK, h, dh)
    pad = ((0, 0), (A_LEFT_CHUNKS, 0), (0, 0), (0, 0), (0, 0))
    kp = jnp.pad(k.reshape(bsz, nc, CHUNK, h, dh), pad)
    vp = jnp.pad(v.reshape(bsz, nc, CHUNK, h, dh), pad)
    band = jnp.arange(nc)[:, None] + jnp.arange(nb)[None, :]
    kb = kp[:, band].reshape(bsz, nc, nb * CHUNK, h, dh)
    vb = vp[:, band].reshape(bsz, nc, nb * CHUNK, h, dh)
    valid = jnp.repeat(band >= A_LEFT_CHUNKS, CHUNK, axis=1)
    t = jnp.arange(CHUNK)
    kpos = ((jnp.arange(nb)[:, None] - A_LEFT_CHUNKS) * CHUNK + t[None, :]).reshape(-1)
    rel = t[:, None] - kpos[None, :]
    ridx = jnp.clip(rel, -REL_CLIP_RIGHT, REL_CLIP_LEFT) + REL_CLIP_RIGHT
    bias = rel_bias[:, ridx].astype(jnp.float32)
    sc = jnp.einsum('bnqhd,bnkhd->bhnqk', qc, kb).astype(jnp.float32) * (dh ** -0.5)
    sc = sc + bias[None, :, None]
    sc = jnp.where(valid[None, None, :, None, :], sc, NEG)
    p = jax.nn.softmax(sc, axis=-1).astype(v.dtype)
    o = jnp.einsum('bhnqk,bnkhd->bnqhd', p, vb)
    return o.reshape(bsz, s, h * dh)


def chunk_causal_attention(q, k, v):
    bsz, s, h, dq = q.shape
    dv = v.shape[-1]
    nqb = s // Q_BLOCK
    qb = q.reshape(bsz, nqb, Q_BLOCK, h, dq).transpose(1, 0, 2, 3, 4)
    kchunk = jnp.arange(s) // CHUNK
    scale = dq ** -0.5

    def block(args):
        q_blk, i = args
        qchunk = (i * Q_BLOCK + jnp.arange(Q_BLOCK)) // CHUNK
        mask = kchunk[None, :] <= qchunk[:, None]
        sc = jnp.einsum('bqhd,bkhd->bhqk', q_blk, k).astype(jnp.float32) * scale
        sc = jnp.where(mask[None, None], sc, NEG)
        p = jax.nn.softmax(sc, axis=-1).astype(v.dtype)
        return jnp.einsum('bhqk,bkhd->bqhd', p, v)

    o = lax.map(block, (qb, jnp.arange(nqb)))
    return o.transpose(1, 0, 2, 3, 4).reshape(bsz, s, h * dv)


def peer(h, wq, keys, u_tab, v_tab):
    bsz, s, d = h.shape
    q = (h @ wq).reshape(bsz, s, PEER_HEADS, 2, PEER_HALF)
    sc = jnp.einsum('bshpc,hpnc->bshpn', q, keys).astype(jnp.float32)
    sv, si = lax.top_k(sc, PEER_TOPK)
    comb = (sv[..., 0, :, None] + sv[..., 1, None, :]).reshape(bsz, s, PEER_HEADS, PEER_TOPK * PEER_TOPK)
    fv, fi = lax.top_k(comb, PEER_TOPK)
    e1 = jnp.take_along_axis(si[..., 0, :], fi // PEER_TOPK, axis=-1)
    e2 = jnp.take_along_axis(si[..., 1, :], fi % PEER_TOPK, axis=-1)
    experts = e1 * N_KEYS + e2
    g = jax.nn.softmax(fv, axis=-1).astype(h.dtype)
    nblk = (bsz * s) // TOKEN_BLOCK
    hb = h.reshape(nblk, TOKEN_BLOCK, d)
    eb = experts.reshape(nblk, TOKEN_BLOCK, PEER_HEADS, PEER_TOPK)
    gb = g.reshape(nblk, TOKEN_BLOCK, PEER_HEADS, PEER_TOPK)

    def block(args):
        xt, et, gt = args
        a = jnp.einsum('td,thkd->thk', xt, u_tab[et])
        w = gt * jax.nn.gelu(a, approximate=False)
        return jnp.einsum('thk,thkd->td', w, v_tab[et])

    out = lax.map(block, (hb, eb, gb))
    return out.reshape(bsz, s, d)


def setup_inputs(seed: int = 0) -> dict:
    key = jax.random.key(seed)
    ks = jax.random.split(key, 24)
    f32 = jnp.float32
    L = DEPTH
    D = D_MODEL

    def nrm(k, shape, scale):
        return jax.random.normal(k, shape, f32) * scale

    x = nrm(ks[0], (BATCH, SEQ, D), 1.0)
    c = nrm(ks[1], (BATCH, D), 1.0)
    offset = jax.random.randint(ks[2], (BATCH, 1), 0, 4096, dtype=jnp.int32)
    positions = offset + jnp.arange(SEQ, dtype=jnp.int32)[None, :]
    return {
        "x": x,
        "c": c,
        "positions": positions,
        "w_ada": nrm(ks[3], (L, D, 6 * D), 0.5 * D ** -0.5),
        "b_ada": nrm(ks[4], (L, 6 * D), 0.02),
        "w_in": nrm(ks[5], (L, D, IN_COLS), D ** -0.5),
        "b_in": nrm(ks[6], (L, IN_COLS), 0.02),
        "rel_bias": nrm(ks[7], (L, A_HEADS, REL_SIZE), 0.5),
        "q_norm_g": 1.0 + nrm(ks[8], (L, Q_LORA), 0.02),
        "kv_norm_g": 1.0 + nrm(ks[9], (L, KV_LORA), 0.02),
        "w_uq": nrm(ks[10], (L, Q_LORA, B_HEADS * (QK_NOPE + QK_ROPE)), Q_LORA ** -0.5),
        "w_ukv": nrm(ks[11], (L, KV_LORA, B_HEADS * (QK_NOPE + V_HEAD)), KV_LORA ** -0.5),
        "w_pa": nrm(ks[12], (L, A_WIDTH, D), A_WIDTH ** -0.5),
        "w_pb": nrm(ks[13], (L, B_WIDTH, D), B_WIDTH ** -0.5),
        "w_o": nrm(ks[14], (L, D, D), DN_BETA * D ** -0.5),
        "ln1_g": 1.0 + nrm(ks[15], (L, D), 0.02),
        "ln1_b": nrm(ks[16], (L, D), 0.02),
        "peer_wq": nrm(ks[17], (L, D, PEER_HEADS * PEER_QDIM), D ** -0.5),
        "peer_keys": nrm(ks[18], (L, PEER_HEADS, 2, N_KEYS, PEER_HALF), PEER_HALF ** -0.5),
        "peer_u": nrm(ks[19], (L, N_EXPERTS, D), D ** -0.5),
        "peer_v": nrm(ks[20], (L, N_EXPERTS, D), DN_BETA),
        "ln2_g": 1.0 + nrm(ks[21], (L, D), 0.02),
        "ln2_b": nrm(ks[22], (L, D), 0.02),
    }


def reference(x, c, positions, w_ada, b_ada, w_in, b_in, rel_bias, q_norm_g, kv_norm_g, w_uq, w_ukv,
              w_pa, w_pb, w_o, ln1_g, ln1_b, peer_wq, peer_keys, peer_u, peer_v, ln2_g, ln2_b):
    bsz, s, d = x.shape
    split_at = [int(i) for i in np.cumsum(IN_SIZES)[:-1]]
    inv_freq = ROPE_THETA ** (-jnp.arange(0, QK_ROPE, 2, dtype=jnp.float32) / QK_ROPE)
    ang = positions.astype(jnp.float32)[..., None] * inv_freq
    cos = jnp.cos(ang).astype(x.dtype)[:, :, None, :]
    sin = jnp.sin(ang).astype(x.dtype)[:, :, None, :]
    cond = jax.nn.silu(c)
    for l in range(DEPTH):
        mod = (cond @ w_ada[l] + b_ada[l]).reshape(bsz, 6, d)
        sh1, sc1, g1 = mod[:, 0, None, :], mod[:, 1, None, :], mod[:, 2, None, :]
        sh2, sc2, g2 = mod[:, 3, None, :], mod[:, 4, None, :], mod[:, 5, None, :]

        h = x * (1.0 + sc1) + sh1
        z = h @ w_in[l] + b_in[l]
        qa, ka, va, cq, ckv, kr, gate_a, gate_b = jnp.split(z, split_at, axis=-1)

        a_shape = (bsz, s, A_HEADS, A_HEAD_DIM)
        ya = chunk_band_attention(qa.reshape(a_shape), ka.reshape(a_shape), va.reshape(a_shape), rel_bias[l])
        ya = ya @ w_pa[l]

        qb = (rms_norm(cq, q_norm_g[l]) @ w_uq[l]).reshape(bsz, s, B_HEADS, QK_NOPE + QK_ROPE)
        q_nope, q_rope = qb[..., :QK_NOPE], rope(qb[..., QK_NOPE:], cos, sin)
        kv = (rms_norm(ckv, kv_norm_g[l]) @ w_ukv[l]).reshape(bsz, s, B_HEADS, QK_NOPE + V_HEAD)
        k_nope, vb = kv[..., :QK_NOPE], kv[..., QK_NOPE:]
        k_rope = jnp.broadcast_to(rope(kr[:, :, None, :], cos, sin), (bsz, s, B_HEADS, QK_ROPE))
        yb = chunk_causal_attention(jnp.concatenate([q_nope, q_rope], axis=-1),
                                    jnp.concatenate([k_nope, k_rope], axis=-1), vb)
        yb = yb @ w_pb[l]

        y = (jax.nn.sigmoid(gate_a) * ya + jax.nn.sigmoid(gate_b) * yb) @ w_o[l]
        x = layer_norm(DN_ALPHA * x + g1 * y, ln1_g[l], ln1_b[l])

        h2 = x * (1.0 + sc2) + sh2
        p = peer(h2, peer_wq[l], peer_keys[l], peer_u[l], peer_v[l])
        x = layer_norm(DN_ALPHA * x + g2 * p, ln2_g[l], ln2_b[l])
    return x
```

```python
import math
from contextlib import ExitStack

import numpy as np
import concourse.bass as bass
import concourse.mybir as mybir
from concourse.bass_utils import run_bass_kernel_spmd

F32 = mybir.dt.float32
BF16 = mybir.dt.bfloat16
I32 = mybir.dt.int32
AF = mybir.ActivationFunctionType
ALU = mybir.AluOpType
AX = mybir.AxisListType

NCORES = 8
S = 4096
D = 2048
NT = S // 128
DN_ALPHA = 2.0 ** 0.25
LN_EPS = 1e-5
RMS_EPS = 1e-6
SCALE_A = 128.0 ** -0.5
SCALE_B = 192.0 ** -0.5
NEGM = -30000.0
MAGIC = 12582912.0
TWO_PI = 2.0 * math.pi
C1 = 6.28125
C2 = TWO_PI - C1
PI_SAFE = 3.14159

SAME_ENG_SYNC = True


class Buf:
    __slots__ = ("w", "r")

    def __init__(self):
        self.w = {}
        self.r = {}


class T:
    def __init__(self, handle):
        self.t = handle
        self.b = Buf()

    def __getitem__(self, k):
        return self.t[k]


class Ctx:
    def __init__(self, nc, es):
        self.nc = nc
        self.eng = {"pe": nc.tensor, "act": nc.scalar, "dve": nc.vector, "pool": nc.gpsimd, "sp": nc.sync}
        self.sems = {}
        self.tot = {}
        for e in ("pe", "act", "dve", "pool"):
            self.sems[e] = es.enter_context(nc.semaphore("s_" + e))
            self.tot[e] = 0
        self.dq = {}
        for q, n in (("sp", 16), ("pool", 8)):
            lst = []
            for i in range(n):
                k = "d_%s%d" % (q, i)
                self.sems[k] = es.enter_context(nc.semaphore(k))
                self.tot[k] = 0
                lst.append(k)
            self.dq[q] = [lst, 0]
        self.seen = {e: {} for e in self.eng}
        self.nins = 0

    def _wait(self, e, deps):
        own = e if e in ("pe", "act", "dve", "pool") else None
        seen = self.seen[e]
        for k, v in deps.items():
            if v <= 0:
                continue
            if k == own and (own == "pe" or not SAME_ENG_SYNC):
                continue
            if seen.get(k, 0) >= v:
                continue
            self.eng[e].wait_ge(self.sems[k], v)
            seen[k] = v

    @staticmethod
    def _merge(d, k, v):
        if d.get(k, 0) < v:
            d[k] = v

    def _deps(self, reads, writes, pwrites):
        deps = {}
        for t in reads:
            for k, v in t.b.w.items():
                self._merge(deps, k, v)
        for t in writes:
            for k, v in t.b.w.items():
                self._merge(deps, k, v)
            for k, v in t.b.r.items():
                self._merge(deps, k, v)
        for t in pwrites:
            for k, v in t.b.r.items():
                self._merge(deps, k, v)
        return deps

    def _update(self, tok, reads, writes, pwrites):
        k, v = tok
        for t in writes:
            t.b.w = {k: v}
            t.b.r = {}
        for t in pwrites:
            if t.b.r:
                t.b.w = {k: v}
                t.b.r = {}
            else:
                self._merge(t.b.w, k, v)
        for t in reads:
            self._merge(t.b.r, k, v)

    def op(self, e, fn, reads=(), writes=(), pwrites=()):
        self._wait(e, self._deps(reads, writes, pwrites))
        ins = fn()
        self.tot[e] += 1
        ins.then_inc(self.sems[e], 1)
        self.nins += 1
        self._update((e, self.tot[e]), reads, writes, pwrites)
        return ins

    def dma(self, q, out, in_, reads=(), writes=(), pwrites=()):
        lst, i = self.dq[q]
        k = lst[i % len(lst)]
        self.dq[q][1] = i + 1
        deps = self._deps(reads, writes, pwrites)
        self._merge(deps, k, self.tot[k])
        self._wait(q, deps)
        ins = self.eng[q].dma_start(out=out, in_=in_)
        self.tot[k] += 16
        ins.then_inc(self.sems[k], 16)
        self.nins += 1
        self._update((k, self.tot[k]), reads, writes, pwrites)
        return ins

    def barrier(self, engines=("pe", "act", "dve", "pool", "sp")):
        for e in engines:
            self._wait(e, dict(self.tot))


def bcast(ap, axis, shape):
    return ap.unsqueeze(axis).broadcast_to(shape)


def build(upto=99, dbg=()):
    nc = bass.Bass("TRN2", target_bir_lowering=False)

    def din(name, shape, dt=F32):
        return nc.dram_tensor(name, list(shape), dt, kind="ExternalInput").ap()

    def dscr(name, shape, dt):
        kind = "ExternalOutput" if name in dbg else "Internal"
        return nc.dram_tensor(name, list(shape), dt, kind=kind).ap()

    x_d = din("x", [S, D])
    cT_d = din("cT", [128, 16])
    pos_d = din("posb", [64, S], I32)
    invf_d = din("invf", [64, 2])
    wada_d = din("w_ada", [D, 6 * D])
    badaT_d = din("b_adaT", [128, 96])
    badar_d = din("b_ada_row", [1, 6 * D])
    win_d = din("w_in_l", [65, 128, 2048])
    binT_d = din("b_inT", [128, 65])
    biasT_d = din("biasT", [128, 8, 640])
    maskA_d = din("maskA", [128, 640])
    gq_d = din("gqT", [128, 4])
    gkv_d = din("gkvT", [128, 4])
    wuq_d = din("w_uq_l", [128, 4, 2048])
    wuk_d = din("w_uk_l", [128, 4, 1024])
    wuv_d = din("w_uv_l", [128, 4, 1024])
    wpa_d = din("w_pa", [1024, D])
    wpb_d = din("w_pb", [1024, D])
    wo_d = din("w_o", [D, D])
    ln_d = din("ln_bc", [4, 128, D])
    wq_d = din("peer_wq", [D, D])
    keysT_d = din("keysT", [128, 16, 128])
    ut_d = din("ut_l", [16384, 2048])
    v_d = din("peer_v", [16384, D])
    ident_d = din("ident", [128, 128])

    out_d = nc.dram_tensor("out", [S, D], F32, kind="ExternalOutput").ap()

    featT_d = dscr("featT", [65, 128, S], BF16)
    yT_d = dscr("yT", [16, 128, S], BF16)
    qnT_d = dscr("qnT", [8, 128, S], BF16)
    qrT_d = dscr("qrT", [8, 64, S], BF16)
    knT_d = dscr("knT", [8, 128, S], BF16)
    krT_d = dscr("krT", [64, S], BF16)
    vbs_d = dscr("vbs", [S, 1024], BF16)
    yfT_d = dscr("yfT", [16, 128, S], BF16)
    x1_d = dscr("x1s", [S, D], F32)
    h2T_d = dscr("h2T", [16, 128, S], BF16)
    scs_d = dscr("scs", [S, 2048], F32)
    utb_d = dscr("utb", [16384, 2048], BF16)
    vb16_d = dscr("vb16", [16384, D], BF16)
    dbgmod_d = dscr("dbgmod", [128, 96 + 2], F32) if "dbgmod" in dbg else None
    dbgg_d = dscr("dbgg", [128, 2 * D], F32) if "dbgg" in dbg else None
    dbgprm_d = dscr("dbgprm", [128, NT, 16], F32) if "dbgprm" in dbg else None

    with ExitStack() as es:
        cx = Ctx(nc, es)

        uid = [0]

        def sb(scope, name, shape, dt=F32):
            uid[0] += 1
            return T(scope.enter_context(nc.sbuf_tensor("sb%d_%s" % (uid[0], name), list(shape), dt)))

        def ps(scope, name, shape, dt=F32):
            uid[0] += 1
            return T(scope.enter_context(nc.psum_tensor("ps%d_%s" % (uid[0], name), list(shape), dt)))

        ident = sb(es, "ident", [128, 128])
        identb = sb(es, "identb", [128, 128], BF16)
        ones_f = sb(es, "ones_f", [128, 128])
        ones_b = sb(es, "ones_b", [128, 128], BF16)
        s1 = sb(es, "s1", [128, 16])
        b1 = sb(es, "b1", [128, 16])
        s2 = sb(es, "s2", [128, 16])
        b2 = sb(es, "b2", [128, 16])
        g1bc = sb(es, "g1bc", [128, D])
        g2bc = sb(es, "g2bc", [128, D])
        prm = sb(es, "prm", [128, NT, 16])

        cx.dma("sp", ident[:, :], ident_d, writes=[ident])
        cx.op("dve", lambda: nc.vector.tensor_copy(identb[:, :], ident[:, :]), reads=[ident], writes=[identb])
        cx.op("dve", lambda: nc.vector.memset(ones_f[:, :], 1.0), writes=[ones_f])
        cx.op("dve", lambda: nc.vector.memset(ones_b[:, :], 1.0), writes=[ones_b])

        if upto >= 6:
            utb_t = T(None)
            vb16_t = T(None)
            for i in range(32):
                cx.dma("pool", utb_d[i * 512:(i + 1) * 512, :], ut_d[i * 512:(i + 1) * 512, :], pwrites=[utb_t])
            for i in range(32):
                cx.dma("pool", vb16_d[i * 512:(i + 1) * 512, :], v_d[i * 512:(i + 1) * 512, :], pwrites=[vb16_t])

        with ExitStack() as ph:
            cT = sb(ph, "cT", [128, 16])
            scT = sb(ph, "scT", [128, 16])
            badaT = sb(ph, "badaT", [128, 96])
            badar = sb(ph, "badar", [1, 6 * D])
            modT = sb(ph, "modT", [128, 96])
            wst = [sb(ph, "wst%d" % i, [128, 16, 512]) for i in range(2)]
            rowsb = [sb(ph, "rowsb%d" % i, [1, 512]) for i in range(2)]
            pm = ps(ph, "pm", [128, 512])
            pr = [ps(ph, "pr%d" % i, [128, 512]) for i in range(2)]
            pbc = [ps(ph, "pbc%d" % i, [128, 512]) for i in range(2)]
            cx.dma("sp", cT[:, :], cT_d, writes=[cT])
            cx.dma("sp", badaT[:, :], badaT_d, writes=[badaT])
            cx.dma("sp", badar[:, :], badar_d, writes=[badar])
            cx.op("act", lambda: nc.scalar.activation(scT[:, :], cT[:, :], AF.Silu), reads=[cT], writes=[scT])
            nrow = 0
            for gi in range(24):
                m = gi // 4
                w = wst[gi % 2]
                cx.dma("sp", w[:, :, :], wada_d[:, gi * 512:(gi + 1) * 512].rearrange("(k p) c -> p k c", p=128), writes=[w])
                if m in (0, 1, 3, 4):
                    for cc in range(4):
                        col = m * 16 + (gi % 4) * 4 + cc
                        for kc in range(16):
                            cx.op("pe", lambda kc=kc, cc=cc, col=col, w=w: nc.tensor.matmul(
                                pm[:, col:col + 1], w[:, kc, cc * 128:(cc + 1) * 128], scT[:, kc:kc + 1],
                                start=(kc == 0), stop=(kc == 15)), reads=[w, scT], pwrites=[pm])
                else:
                    p_r = pr[nrow % 2]
                    rs = rowsb[nrow % 2]
                    p_b = pbc[nrow % 2]
                    nrow += 1
                    for kc in range(16):
                        cx.op("pe", lambda kc=kc, w=w, p_r=p_r: nc.tensor.matmul(
                            p_r[0:1, :], scT[:, kc:kc + 1], w[:, kc, :], start=(kc == 0), stop=(kc == 15)),
                            reads=[w, scT], pwrites=[p_r])
                    cx.op("dve", lambda p_r=p_r, rs=rs, gi=gi: nc.vector.tensor_tensor(
                        out=rs[0:1, :], in0=p_r[0:1, :], in1=badar[0:1, gi * 512:(gi + 1) * 512], op=ALU.add),
                        reads=[p_r, badar], writes=[rs])
                    cx.op("pe", lambda rs=rs, p_b=p_b: nc.tensor.matmul(
                        p_b[:, :], ones_f[0:1, :], rs[0:1, :], start=True, stop=True), reads=[rs, ones_f], pwrites=[p_b])
                    dst = g1bc if m == 2 else g2bc
                    cx.op("act", lambda dst=dst, p_b=p_b, gi=gi: nc.scalar.copy(
                        dst[:, (gi % 4) * 512:(gi % 4 + 1) * 512], p_b[:, :]), reads=[p_b], pwrites=[dst])
            cx.op("dve", lambda: nc.vector.tensor_tensor(out=modT[:, :], in0=pm[:, 0:96], in1=badaT[:, :], op=ALU.add),
                  reads=[pm, badaT], writes=[modT])
            cx.op("dve", lambda: nc.vector.tensor_scalar(out=s1[:, :], in0=modT[:, 16:32], scalar1=1.0, scalar2=None, op0=ALU.add),
                  reads=[modT], writes=[s1])
            cx.op("dve", lambda: nc.vector.tensor_copy(b1[:, :], modT[:, 0:16]), reads=[modT], writes=[b1])
            cx.op("dve", lambda: nc.vector.tensor_scalar(out=s2[:, :], in0=modT[:, 64:80], scalar1=1.0, scalar2=None, op0=ALU.add),
                  reads=[modT], writes=[s2])
            cx.op("dve", lambda: nc.vector.tensor_copy(b2[:, :], modT[:, 48:64]), reads=[modT], writes=[b2])
            if dbgmod_d is not None:
                cx.dma("sp", dbgmod_d[:, 0:96], modT[:, :], reads=[modT])
            if dbgg_d is not None:
                cx.dma("sp", dbgg_d[:, 0:D], g1bc[:, :], reads=[g1bc])
                cx.dma("sp", dbgg_d[:, D:2 * D], g2bc[:, :], reads=[g2bc])
            cx.barrier()

        if upto >= 1:
          with ExitStack() as ph12:
            hT = sb(ph12, "hT", [128, 16, S], BF16)
            with ExitStack() as ph:
                xs = [sb(ph, "xs%d" % i, [128, 2, D]) for i in range(2)]
                ptr = [ps(ph, "ptr%d" % i, [128, 512]) for i in range(4)]
                n = 0
                for g in range(16):
                    xb = xs[g % 2]
                    cx.dma("sp", xb[:, :, :], x_d[g * 256:(g + 1) * 256, :].rearrange("(j p) d -> p j d", p=128), writes=[xb])
                    for dc in range(16):
                        pt = ptr[n % 4]
                        n += 1
                        for j in range(2):
                            cx.op("pe", lambda pt=pt, xb=xb, j=j, dc=dc: nc.tensor.transpose(
                                pt[:, j * 128:(j + 1) * 128], xb[:, j, dc * 128:(dc + 1) * 128], ident[:, :]),
                                reads=[xb, ident], pwrites=[pt])
                        cx.op("act", lambda pt=pt, g=g, dc=dc: nc.scalar.activation(
                            hT[:, dc, g * 256:(g + 1) * 256], pt[:, 0:256], AF.Identity,
                            bias=b1[:, dc:dc + 1], scale=s1[:, dc:dc + 1]), reads=[pt, s1, b1], pwrites=[hT])
                cx.barrier()
            with ExitStack() as ph:
                wc = [sb(ph, "wc%d" % i, [128, 16, 128], BF16) for i in range(3)]
                ost = [sb(ph, "ost%d" % i, [128, S], BF16) for i in range(2)]
                binT = sb(ph, "binT", [128, 65])
                pz = [ps(ph, "pz%d" % i, [128, 512]) for i in range(4)]
                cx.dma("sp", binT[:, :], binT_d, writes=[binT])
                n = 0
                for ch in range(65):
                    w = wc[ch % 3]
                    cx.dma("pool", w[:, :, :], win_d[ch].rearrange("p (k c) -> p k c", c=128), writes=[w])
                    o = ost[ch % 2]
                    func = AF.Sigmoid if ch >= 33 else AF.Identity
                    for g in range(8):
                        p = pz[n % 4]
                        n += 1
                        for kc in range(16):
                            cx.op("pe", lambda p=p, w=w, kc=kc, g=g: nc.tensor.matmul(
                                p[:, :], w[:, kc, :], hT[:, kc, g * 512:(g + 1) * 512], start=(kc == 0), stop=(kc == 15)),
                                reads=[w, hT], pwrites=[p])
                        cx.op("act", lambda p=p, o=o, g=g, ch=ch, func=func: nc.scalar.activation(
                            o[:, g * 512:(g + 1) * 512], p[:, :], func, bias=binT[:, ch:ch + 1]),
                            reads=[p, binT], pwrites=[o])
                    cx.dma("sp", featT_d[ch], o[:, :], reads=[o])
                cx.barrier()

        if upto >= 3:
          with ExitStack() as ph:
            biasT = sb(ph, "biasT", [128, 8, 640])
            maskA = sb(ph, "maskA", [128, 640])
            qTs = [sb(ph, "qT%d" % i, [128, S], BF16) for i in range(2)]
            kTs = [sb(ph, "kT%d" % i, [128, S], BF16) for i in range(2)]
            vTs = [sb(ph, "vT%d" % i, [128, S], BF16) for i in range(2)]
            vas = [sb(ph, "va%d" % i, [128, NT, 128], BF16) for i in range(2)]
            ybs = [sb(ph, "yb%d" % i, [128, S], BF16) for i in range(2)]
            t1s = [sb(ph, "t1_%d" % i, [128, 640]) for i in range(2)]
            pTs = [sb(ph, "pT%d" % i, [128, 640], BF16) for i in range(2)]
            rds = [sb(ph, "rd%d" % i, [128, 128]) for i in range(2)]
            pss = [ps(ph, "psA%d" % i, [128, 1024]) for i in range(2)]
            pos_ = [ps(ph, "poA%d" % i, [128, 512]) for i in range(2)]
            ptr = [ps(ph, "ptA%d" % i, [128, 1024], BF16) for i in range(2)]
            cx.dma("sp", biasT[:, :, :], biasT_d, writes=[biasT])
            cx.dma("sp", maskA[:, :], maskA_d, writes=[maskA])
            for h in range(8):
                cx.op("dve", lambda h=h: nc.vector.tensor_tensor(out=biasT[:, h, :], in0=biasT[:, h, :], in1=maskA[:, :], op=ALU.add),
                      reads=[maskA], writes=[biasT])
            nt = 0
            for h in range(8):
                qT, kT, vT, va, yb = qTs[h % 2], kTs[h % 2], vTs[h % 2], vas[h % 2], ybs[h % 2]
                cx.dma("sp", qT[:, :], featT_d[h], writes=[qT])
                cx.dma("sp", kT[:, :], featT_d[8 + h], writes=[kT])
                cx.dma("sp", vT[:, :], featT_d[16 + h], writes=[vT])
                for blk in range(4):
                    pt = ptr[nt % 2]
                    nt += 1
                    for i in range(8):
                        tl = blk * 8 + i
                        cx.op("pe", lambda pt=pt, vT=vT, i=i, tl=tl: nc.tensor.transpose(
                            pt[:, i * 128:(i + 1) * 128], vT[:, tl * 128:(tl + 1) * 128], identb[:, :]),
                            reads=[vT, identb], pwrites=[pt])
                    cx.op("act", lambda pt=pt, va=va, blk=blk: nc.scalar.copy(
                        va[:, blk * 8:(blk + 1) * 8, :], pt[:, :].rearrange("p (a b) -> p a b", b=128)),
                        reads=[pt], pwrites=[va])
                for m in range(NT):
                    j0 = max(0, 4 - m)
                    lo = j0 * 128
                    psm, t1, pT, po, rd = pss[m % 2], t1s[m % 2], pTs[m % 2], pos_[m % 2], rds[m % 2]
                    for j in range(j0, 5):
                        kt = m - 4 + j
                        cx.op("pe", lambda psm=psm, kT=kT, qT=qT, j=j, kt=kt, m=m: nc.tensor.matmul(
                            psm[:, j * 128:(j + 1) * 128], kT[:, kt * 128:(kt + 1) * 128], qT[:, m * 128:(m + 1) * 128],
                            start=True, stop=True), reads=[kT, qT], pwrites=[psm])
                    cx.op("dve", lambda psm=psm, t1=t1, h=h, lo=lo: nc.vector.scalar_tensor_tensor(
                        out=t1[:, lo:640], in0=psm[:, lo:640], scalar=SCALE_A, in1=biasT[:, h, lo:640],
                        op0=ALU.mult, op1=ALU.add), reads=[psm, biasT], writes=[t1])
                    cx.op("act", lambda t1=t1, pT=pT, lo=lo: nc.scalar.activation(pT[:, lo:640], t1[:, lo:640], AF.Exp),
                          reads=[t1], writes=[pT])
                    for j in range(j0, 5):
                        kt = m - 4 + j
                        cx.op("pe", lambda po=po, va=va, pT=pT, j=j, kt=kt, j0=j0: nc.tensor.matmul(
                            po[:, 0:128], va[:, kt, :], pT[:, j * 128:(j + 1) * 128], start=(j == j0), stop=(j == 4)),
                            reads=[va, pT], pwrites=[po])
                    for j in range(j0, 5):
                        cx.op("pe", lambda po=po, pT=pT, j=j, j0=j0: nc.tensor.matmul(
                            po[:, 128:256], ones_b[:, :], pT[:, j * 128:(j + 1) * 128], start=(j == j0), stop=(j == 4)),
                            reads=[ones_b, pT], pwrites=[po])
                    cx.op("dve", lambda rd=rd, po=po: nc.vector.reciprocal(rd[:, :], po[:, 128:256]), reads=[po], writes=[rd])
                    cx.op("dve", lambda yb=yb, po=po, rd=rd, m=m: nc.vector.tensor_tensor(
                        out=yb[:, m * 128:(m + 1) * 128], in0=po[:, 0:128], in1=rd[:, :], op=ALU.mult),
                        reads=[po, rd], pwrites=[yb])
                cx.dma("sp", yT_d[h], yb[:, :], reads=[yb])
            cx.barrier()

        if upto >= 4:
          with ExitStack() as ph:
            cos2 = sb(ph, "cos2", [64, S])
            sinS = sb(ph, "sinS", [64, S])
            invf = sb(ph, "invf", [64, 2])
            cx.dma("sp", invf[:, :], invf_d, writes=[invf])
            with ExitStack() as ph2:
                posi = sb(ph2, "posi", [64, S], I32)
                ang = sb(ph2, "ang", [64, S])
                ta = sb(ph2, "ta", [64, S])
                tb = sb(ph2, "tb", [64, S])
                cx.dma("sp", posi[:, :], pos_d, writes=[posi])
                cx.op("dve", lambda: nc.vector.tensor_copy(ta[:, :], posi[:, :]), reads=[posi], writes=[ta])
                cx.op("dve", lambda: nc.vector.tensor_scalar(out=ang[:, :], in0=ta[:, :], scalar1=invf[:, 0:1], scalar2=None, op0=ALU.mult),
                      reads=[ta, invf], writes=[ang])
                for dst, shift, use_sgn in ((sinS, 0.0, True), (cos2, math.pi / 2.0, False)):
                    src = ang
                    if shift != 0.0:
                        cx.op("dve", lambda: nc.vector.tensor_scalar(out=ta[:, :], in0=ang[:, :], scalar1=shift, scalar2=None, op0=ALU.add),
                              reads=[ang], writes=[ta])
                        src = ta
                    else:
                        cx.op("dve", lambda: nc.vector.tensor_copy(ta[:, :], ang[:, :]), reads=[ang], writes=[ta])
                        src = ta
                    cx.op("dve", lambda: nc.vector.tensor_scalar(out=tb[:, :], in0=ta[:, :], scalar1=1.0 / TWO_PI, scalar2=None, op0=ALU.mult),
                          reads=[ta], writes=[tb])
                    cx.op("dve", lambda: nc.vector.tensor_scalar(out=tb[:, :], in0=tb[:, :], scalar1=MAGIC, scalar2=None, op0=ALU.add),
                          reads=[tb], writes=[tb])
                    cx.op("dve", lambda: nc.vector.tensor_scalar(out=tb[:, :], in0=tb[:, :], scalar1=-MAGIC, scalar2=None, op0=ALU.add),
                          reads=[tb], writes=[tb])
                    cx.op("dve", lambda: nc.vector.scalar_tensor_tensor(out=ta[:, :], in0=tb[:, :], scalar=-C1, in1=ta[:, :], op0=ALU.mult, op1=ALU.add),
                          reads=[tb, ta], writes=[ta])
                    cx.op("dve", lambda: nc.vector.scalar_tensor_tensor(out=ta[:, :], in0=tb[:, :], scalar=-C2, in1=ta[:, :], op0=ALU.mult, op1=ALU.add),
                          reads=[tb, ta], writes=[ta])
                    cx.op("dve", lambda: nc.vector.tensor_scalar(out=ta[:, :], in0=ta[:, :], scalar1=PI_SAFE, scalar2=-PI_SAFE, op0=ALU.min, op1=ALU.max),
                          reads=[ta], writes=[ta])
                    if use_sgn:
                        cx.op("act", lambda dst=dst: nc.scalar.activation(dst[:, :], ta[:, :], AF.Sin, scale=invf[:, 1:2]),
                              reads=[ta, invf], writes=[dst])
                    else:
                        cx.op("act", lambda dst=dst: nc.scalar.activation(dst[:, :], ta[:, :], AF.Sin), reads=[ta], writes=[dst])
                cx.barrier()

            with ExitStack() as ph2:
                lat = sb(ph2, "lat", [128, 4, S], BF16)
                sq = [sb(ph2, "sq%d" % i, [128, 4, 512], BF16) for i in range(2)]
                tmpf = [sb(ph2, "tmpf%d" % i, [128, 512]) for i in range(2)]
                rstd = sb(ph2, "rstd", [128, S])
                gn = sb(ph2, "gn", [128, 8])
                wuq = sb(ph2, "wuq", [128, 4, 2048], BF16)
                wuk = sb(ph2, "wuk", [128, 4, 1024], BF16)
                wuv = sb(ph2, "wuv", [128, 4, 1024], BF16)
                qn_st = [sb(ph2, "qn_st%d" % i, [128, S], BF16) for i in range(2)]
                qr_st = [sb(ph2, "qr_st%d" % i, [64, S], BF16) for i in range(2)]
                v_st = [sb(ph2, "v_st%d" % i, [128, 2, 1024], BF16) for i in range(2)]
                rta = [sb(ph2, "rta%d" % i, [64, 512]) for i in range(2)]
                rtb = [sb(ph2, "rtb%d" % i, [64, 512]) for i in range(2)]
                kr_a, kr_b = qr_st[0], qr_st[1]
                pp = [ps(ph2, "pp%d" % i, [128, 512]) for i in range(6)]
                npp = [0]

                def nextp():
                    p = pp[npp[0] % 6]
                    npp[0] += 1
                    return p

                cx.dma("sp", gn[:, 0:4], gq_d, pwrites=[gn])
                cx.dma("sp", gn[:, 4:8], gkv_d, pwrites=[gn])
                cx.dma("pool", wuq[:, :, :], wuq_d, writes=[wuq])
                cx.dma("pool", wuk[:, :, :], wuk_d, writes=[wuk])
                cx.dma("pool", wuv[:, :, :], wuv_d, writes=[wuv])

                def load_norm(first_chunk, goff):
                    for c in range(4):
                        cx.dma("sp", lat[:, c, :], featT_d[first_chunk + c], pwrites=[lat])
                    for g in range(8):
                        sqb, tf = sq[g % 2], tmpf[g % 2]
                        cx.op("act", lambda sqb=sqb, g=g: nc.scalar.activation(sqb[:, :, :], lat[:, :, g * 512:(g + 1) * 512], AF.Square),
                              reads=[lat], writes=[sqb])
                        p = nextp()
                        for c in range(4):
                            cx.op("pe", lambda p=p, sqb=sqb, c=c: nc.tensor.matmul(p[:, :], ones_b[:, :], sqb[:, c, :], start=(c == 0), stop=(c == 3)),
                                  reads=[sqb, ones_b], pwrites=[p])
                        cx.op("act", lambda p=p, tf=tf: nc.scalar.activation(tf[:, :], p[:, :], AF.Sqrt, scale=1.0 / 512.0, bias=RMS_EPS),
                              reads=[p], writes=[tf])
                        cx.op("dve", lambda tf=tf, g=g: nc.vector.reciprocal(rstd[:, g * 512:(g + 1) * 512], tf[:, :]), reads=[tf], pwrites=[rstd])
                    for c in range(4):
                        for g in range(4):
                            cx.op("dve", lambda c=c, g=g: nc.vector.scalar_tensor_tensor(
                                out=lat[:, c, g * 1024:(g + 1) * 1024], in0=lat[:, c, g * 1024:(g + 1) * 1024],
                                scalar=gn[:, goff + c:goff + c + 1], in1=rstd[:, g * 1024:(g + 1) * 1024], op0=ALU.mult, op1=ALU.mult),
                                reads=[rstd, gn], writes=[lat])

                load_norm(28, 4)
                for h in range(8):
                    st = qn_st[h % 2]
                    for g in range(8):
                        p = nextp()
                        for c in range(4):
                            cx.op("pe", lambda p=p, c=c, h=h, g=g: nc.tensor.matmul(
                                p[:, :], wuk[:, c, h * 128:(h + 1) * 128], lat[:, c, g * 512:(g + 1) * 512], start=(c == 0), stop=(c == 3)),
                                reads=[wuk, lat], pwrites=[p])
                        cx.op("act", lambda p=p, st=st, g=g: nc.scalar.copy(st[:, g * 512:(g + 1) * 512], p[:, :]), reads=[p], pwrites=[st])
                    cx.dma("sp", knT_d[h], st[:, :], reads=[st])
                for tq in range(16):
                    st = v_st[tq % 2]
                    for ti in range(2):
                        tt = tq * 2 + ti
                        for half in range(2):
                            p = nextp()
                            for c in range(4):
                                cx.op("pe", lambda p=p, c=c, tt=tt, half=half: nc.tensor.matmul(
                                    p[:, :], lat[:, c, tt * 128:(tt + 1) * 128], wuv[:, c, half * 512:(half + 1) * 512], start=(c == 0), stop=(c == 3)),
                                    reads=[wuv, lat], pwrites=[p])
                            cx.op("act", lambda p=p, st=st, ti=ti, half=half: nc.scalar.copy(st[:, ti, half * 512:(half + 1) * 512], p[:, :]),
                                  reads=[p], pwrites=[st])
                    cx.dma("sp", vbs_d[tq * 256:(tq + 1) * 256, :].rearrange("(a p) c -> p a c", p=128), st[:, :, :], reads=[st])
                cx.dma("sp", kr_a[:, :], featT_d[32][0:64, :], writes=[kr_a])
                cx.dma("sp", kr_b[:, :], featT_d[32][64:128, :], writes=[kr_b])
                for g in range(8):
                    ra, rb = rta[g % 2], rtb[g % 2]
                    sl = slice(g * 512, (g + 1) * 512)
                    cx.op("dve", lambda ra=ra, sl=sl: nc.vector.tensor_tensor(out=ra[:, :], in0=kr_a[:, sl], in1=cos2[:, sl], op=ALU.mult),
                          reads=[kr_a, cos2], writes=[ra])
                    cx.op("dve", lambda rb=rb, sl=sl: nc.vector.tensor_tensor(out=rb[:, :], in0=kr_b[:, sl], in1=sinS[:, sl], op=ALU.mult),
                          reads=[kr_b, sinS], writes=[rb])
                    cx.op("dve", lambda ra=ra, rb=rb, sl=sl: nc.vector.tensor_tensor(out=kr_a[:, sl], in0=ra[:, :], in1=rb[:, :], op=ALU.add),
                          reads=[ra, rb], writes=[kr_a])
                cx.dma("sp", krT_d, kr_a[:, :], reads=[kr_a])

                load_norm(24, 0)
                for h in range(8):
                    stn, strp = qn_st[h % 2], qr_st[h % 2]
                    for g in range(8):
                        sl = slice(g * 512, (g + 1) * 512)
                        p = nextp()
                        for c in range(4):
                            cx.op("pe", lambda p=p, c=c, h=h, sl=sl: nc.tensor.matmul(
                                p[:, :], wuq[:, c, h * 256:h * 256 + 128], lat[:, c, sl], start=(c == 0), stop=(c == 3)),
                                reads=[wuq, lat], pwrites=[p])
                        cx.op("act", lambda p=p, stn=stn, sl=sl: nc.scalar.copy(stn[:, sl], p[:, :]), reads=[p], pwrites=[stn])
                        p2 = nextp()
                        p3 = nextp()
                        for c in range(4):
                            cx.op("pe", lambda p2=p2, c=c, h=h, sl=sl: nc.tensor.matmul(
                                p2[0:64, :], wuq[:, c, h * 256 + 128:h * 256 + 192], lat[:, c, sl], start=(c == 0), stop=(c == 3)),
                                reads=[wuq, lat], pwrites=[p2])
                        for c in range(4):
                            cx.op("pe", lambda p3=p3, c=c, h=h, sl=sl: nc.tensor.matmul(
                                p3[0:64, :], wuq[:, c, h * 256 + 192:h * 256 + 256], lat[:, c, sl], start=(c == 0), stop=(c == 3)),
                                reads=[wuq, lat], pwrites=[p3])
                        ra, rb = rta[g % 2], rtb[g % 2]
                        cx.op("dve", lambda ra=ra, p2=p2, sl=sl: nc.vector.tensor_tensor(out=ra[:, :], in0=p2[0:64, :], in1=cos2[:, sl], op=ALU.mult),
                              reads=[p2, cos2], writes=[ra])
                        cx.op("dve", lambda rb=rb, p3=p3, sl=sl: nc.vector.tensor_tensor(out=rb[:, :], in0=p3[0:64, :], in1=sinS[:, sl], op=ALU.mult),
                              reads=[p3, sinS], writes=[rb])
                        cx.op("dve", lambda ra=ra, rb=rb, strp=strp, sl=sl: nc.vector.tensor_tensor(out=strp[:, sl], in0=ra[:, :], in1=rb[:, :], op=ALU.add),
                              reads=[ra, rb], pwrites=[strp])
                    cx.dma("sp", qnT_d[h], stn[:, :], reads=[stn])
                    cx.dma("sp", qrT_d[h], strp[:, :], reads=[strp])
                cx.barrier()
            cx.barrier()

        if upto >= 5:
          with ExitStack() as ph:
            knT = sb(ph, "knT", [128, 8, S], BF16)
            vb = sb(ph, "vb", [128, NT, 1024], BF16)
            krT = sb(ph, "krT", [64, S], BF16)
            qns = [sb(ph, "qn%d" % i, [128, 512], BF16) for i in range(2)]
            qrs = [sb(ph, "qr%d" % i, [64, 512], BF16) for i in range(2)]
            pTs = [sb(ph, "pTb%d" % i, [128, 512], BF16) for i in range(3)]
            rds = [sb(ph, "rdb%d" % i, [128, 512]) for i in range(2)]
            ybs = [sb(ph, "ybb%d" % i, [128, S], BF16) for i in range(2)]
            pss = [ps(ph, "psB%d" % i, [128, 512]) for i in range(2)]
            pos_ = [ps(ph, "poB%d" % i, [128, 512]) for i in range(2)]
            pds = [ps(ph, "pdB%d" % i, [128, 512]) for i in range(2)]
            for h in range(8):
                cx.dma("sp", knT[:, h, :], knT_d[h], pwrites=[knT])
            for a in range(4):
                cx.dma("sp", vb[:, a * 8:(a + 1) * 8, :], vbs_d[a * 1024:(a + 1) * 1024, :].rearrange("(a p) c -> p a c", p=128), pwrites=[vb])
            cx.dma("sp", krT[:, :], krT_d, writes=[krT])
            it = 0
            cnt = 0
            for h in range(8):
                yb = ybs[h % 2]
                for Q in range(8):
                    qn, qr, po, pd, rd = qns[it % 2], qrs[it % 2], pos_[it % 2], pds[it % 2], rds[it % 2]
                    it += 1
                    cx.dma("sp", qn[:, :], qnT_d[h][:, Q * 512:(Q + 1) * 512], writes=[qn])
                    cx.dma("sp", qr[:, :], qrT_d[h][:, Q * 512:(Q + 1) * 512], writes=[qr])
                    nk = 4 * (Q + 1)
                    for kt in range(nk):
                        jj = kt - 4 * Q
                        c0 = max(jj, 0) * 128
                        psm = pss[cnt % 2]
                        pT = pTs[cnt % 3]
                        cnt += 1
                        ks = slice(kt * 128, (kt + 1) * 128)
                        cx.op("pe", lambda psm=psm, qn=qn, h=h, ks=ks, c0=c0: nc.tensor.matmul(
                            psm[:, c0:512], knT[:, h, ks], qn[:, c0:512], start=True, stop=False), reads=[knT, qn], pwrites=[psm])
                        cx.op("pe", lambda psm=psm, qr=qr, ks=ks, c0=c0: nc.tensor.matmul(
                            psm[:, c0:512], krT[:, ks], qr[:, c0:512], start=False, stop=True), reads=[krT, qr], pwrites=[psm])
                        cx.op("act", lambda psm=psm, pT=pT, c0=c0: nc.scalar.activation(pT[:, c0:512], psm[:, c0:512], AF.Exp, scale=SCALE_B),
                              reads=[psm], writes=[pT])
                        if jj >= 0:
                            cx.op("dve", lambda pT=pT, c0=c0: nc.vector.memset(pT[64:128, c0:c0 + 64], 0.0), writes=[pT])
                        cx.op("pe", lambda po=po, pT=pT, h=h, kt=kt, c0=c0, nk=nk: nc.tensor.matmul(
                            po[:, c0:512], vb[:, kt, h * 128:(h + 1) * 128], pT[:, c0:512], start=(kt == 0), stop=(kt == nk - 1)),
                            reads=[vb, pT], pwrites=[po])
                        cx.op("pe", lambda pd=pd, pT=pT, kt=kt, c0=c0, nk=nk: nc.tensor.matmul(
                            pd[:, c0:512], ones_b[:, :], pT[:, c0:512], start=(kt == 0), stop=(kt == nk - 1)),
                            reads=[ones_b, pT], pwrites=[pd])
                    cx.op("dve", lambda rd=rd, pd=pd: nc.vector.reciprocal(rd[:, :], pd[:, :]), reads=[pd], writes=[rd])
                    cx.op("dve", lambda yb=yb, po=po, rd=rd, Q=Q: nc.vector.tensor_tensor(
                        out=yb[:, Q * 512:(Q + 1) * 512], in0=po[:, :], in1=rd[:, :], op=ALU.mult), reads=[po, rd], pwrites=[yb])
                cx.dma("sp", yT_d[8 + h], yb[:, :], reads=[yb])
            cx.barrier()

        if upto >= 6:
          with ExitStack() as ph:
            wpa = sb(ph, "wpa", [128, 8, D], BF16)
            wpb = sb(ph, "wpb", [128, 8, D], BF16)
            yas = [sb(ph, "ya%d" % i, [128, 8, 256], BF16) for i in range(2)]
            ybs = [sb(ph, "ybm%d" % i, [128, 8, 256], BF16) for i in range(2)]
            gts = [sb(ph, "gt%d" % i, [128, 32, 256], BF16) for i in range(2)]
            yfs = [sb(ph, "yf%d" % i, [128, 16, 256], BF16) for i in range(2)]
            tas = [sb(ph, "tam%d" % i, [128, 256]) for i in range(2)]
            tbs = [sb(ph, "tbm%d" % i, [128, 256]) for i in range(2)]
            ppa = [ps(ph, "ppa%d" % i, [128, 512]) for i in range(2)]
            ppb = [ps(ph, "ppb%d" % i, [128, 512]) for i in range(2)]
            for hh in range(2):
                cx.dma("pool", wpa[:, hh * 4:(hh + 1) * 4, :], wpa_d[hh * 512:(hh + 1) * 512, :].rearrange("(h p) c -> p h c", p=128), pwrites=[wpa])
                cx.dma("pool", wpb[:, hh * 4:(hh + 1) * 4, :], wpb_d[hh * 512:(hh + 1) * 512, :].rearrange("(h p) c -> p h c", p=128), pwrites=[wpb])
            n = 0
            for g in range(16):
                ya, yb, gt, yf = yas[g % 2], ybs[g % 2], gts[g % 2], yfs[g % 2]
                ts = slice(g * 256, (g + 1) * 256)
                cx.dma("sp", ya[:, :, :], yT_d[0:8, :, ts].rearrange("h p t -> p h t"), writes=[ya])
                cx.dma("sp", yb[:, :, :], yT_d[8:16, :, ts].rearrange("h p t -> p h t"), writes=[yb])
                for a in range(4):
                    cx.dma("sp", gt[:, a * 8:(a + 1) * 8, :], featT_d[33 + a * 8:33 + (a + 1) * 8, :, ts].rearrange("c p t -> p c t"), pwrites=[gt])
                for oc in range(16):
                    pa, pb, ta, tb = ppa[n % 2], ppb[n % 2], tas[n % 2], tbs[n % 2]
                    n += 1
                    os_ = slice(oc * 128, (oc + 1) * 128)
                    for h in range(8):
                        cx.op("pe", lambda pa=pa, ya=ya, h=h, os_=os_: nc.tensor.matmul(
                            pa[:, 0:256], wpa[:, h, os_], ya[:, h, :], start=(h == 0), stop=(h == 7)), reads=[wpa, ya], pwrites=[pa])
                    for h in range(8):
                        cx.op("pe", lambda pb=pb, yb=yb, h=h, os_=os_: nc.tensor.matmul(
                            pb[:, 0:256], wpb[:, h, os_], yb[:, h, :], start=(h == 0), stop=(h == 7)), reads=[wpb, yb], pwrites=[pb])
                    cx.op("dve", lambda ta=ta, pa=pa, gt=gt, oc=oc: nc.vector.tensor_tensor(out=ta[:, :], in0=pa[:, 0:256], in1=gt[:, oc, :], op=ALU.mult),
                          reads=[pa, gt], writes=[ta])
                    cx.op("dve", lambda tb=tb, pb=pb, gt=gt, oc=oc: nc.vector.tensor_tensor(out=tb[:, :], in0=pb[:, 0:256], in1=gt[:, 16 + oc, :], op=ALU.mult),
                          reads=[pb, gt], writes=[tb])
                    cx.op("pool", lambda yf=yf, ta=ta, tb=tb, oc=oc: nc.gpsimd.tensor_tensor(out=yf[:, oc, :], in0=ta[:, :], in1=tb[:, :], op=ALU.add),
                          reads=[ta, tb], pwrites=[yf])
                for a in range(2):
                    cx.dma("sp", yfT_d[a * 8:(a + 1) * 8, :, ts].rearrange("c p t -> p c t"), yf[:, a * 8:(a + 1) * 8, :], reads=[yf])
            cx.barrier()

        def layer_norm_tile(r, lng, lnb, dst, stats, mv, rs_t):
            for k in range(4):
                cx.op("dve", lambda k=k: nc.vector.bn_stats(stats[:, k, :], r[:, k * 512:(k + 1) * 512]), reads=[r], pwrites=[stats])
            cx.op("dve", lambda: nc.vector.bn_aggr(mv[:, :], stats[:, :, :].rearrange("p a b -> p (a b)")), reads=[stats], writes=[mv])
            cx.op("act", lambda: nc.scalar.activation(rs_t[:, 0:1], mv[:, 1:2], AF.Sqrt, bias=LN_EPS), reads=[mv], writes=[rs_t])
            cx.op("dve", lambda: nc.vector.reciprocal(rs_t[:, 1:2], rs_t[:, 0:1]), writes=[rs_t])
            cx.op("dve", lambda: nc.vector.tensor_scalar(out=r[:, :], in0=r[:, :], scalar1=mv[:, 0:1], scalar2=rs_t[:, 1:2],
                                                         op0=ALU.subtract, op1=ALU.mult), reads=[mv, rs_t], writes=[r])
            cx.op("pool", lambda: nc.gpsimd.tensor_tensor(out=r[:, :], in0=r[:, :], in1=lng[:, :], op=ALU.mult), reads=[lng], writes=[r])
            cx.op("pool", lambda: nc.gpsimd.tensor_tensor(out=dst[:, :], in0=r[:, :], in1=lnb[:, :], op=ALU.add), reads=[r, lnb], writes=[dst])

        if upto >= 7:
          with ExitStack() as ph:
            wo = sb(ph, "wo", [128, 16, D], BF16)
            lng = sb(ph, "ln1g", [128, D])
            lnb = sb(ph, "ln1b", [128, D])
            yfs = [sb(ph, "yfo%d" % i, [128, 16, 512], BF16) for i in range(2)]
            xts = [sb(ph, "xt%d" % i, [128, D]) for i in range(2)]
            rts = [sb(ph, "rt%d" % i, [128, D]) for i in range(1)]
            x1s = [sb(ph, "x1t%d" % i, [128, D]) for i in range(2)]
            h2s = [sb(ph, "h2s%d" % i, [128, 16, 512], BF16) for i in range(1)]
            stats = sb(ph, "stats", [128, 4, 6])
            mv = sb(ph, "mv", [128, 2])
            rs_t = sb(ph, "rs_t", [128, 2])
            pob = [ps(ph, "pob%d" % i, [128, 512]) for i in range(4)]
            ptb = [ps(ph, "ptb%d" % i, [128, 512]) for i in range(4)]
            for a in range(4):
                cx.dma("pool", wo[:, a * 4:(a + 1) * 4, :], wo_d[a * 512:(a + 1) * 512, :].rearrange("(k p) c -> p k c", p=128), pwrites=[wo])
            cx.dma("sp", lng[:, :], ln_d[0], writes=[lng])
            cx.dma("sp", lnb[:, :], ln_d[1], writes=[lnb])
            npt = 0
            for g in range(8):
                yf, h2b = yfs[g % 2], h2s[0]
                for a in range(2):
                    cx.dma("sp", yf[:, a * 8:(a + 1) * 8, :], yfT_d[a * 8:(a + 1) * 8, :, g * 512:(g + 1) * 512].rearrange("c p t -> p c t"), pwrites=[yf])
                for tl in range(4):
                    tt = g * 4 + tl
                    xt, r, x1t = xts[tt % 2], rts[0], x1s[tt % 2]
                    cx.dma("sp", xt[:, :], x_d[tt * 128:(tt + 1) * 128, :], writes=[xt])
                    for cg in range(4):
                        for oc in range(16):
                            cx.op("pe", lambda cg=cg, oc=oc, yf=yf, tl=tl: nc.tensor.matmul(
                                pob[cg][:, :], yf[:, oc, tl * 128:(tl + 1) * 128], wo[:, oc, cg * 512:(cg + 1) * 512],
                                start=(oc == 0), stop=(oc == 15)), reads=[yf, wo], pwrites=[pob[cg]])
                        cx.op("dve", lambda cg=cg, r=r: nc.vector.tensor_tensor(
                            out=r[:, cg * 512:(cg + 1) * 512], in0=pob[cg][:, :], in1=g1bc[:, cg * 512:(cg + 1) * 512], op=ALU.mult),
                            reads=[pob[cg], g1bc], pwrites=[r])
                    cx.op("dve", lambda r=r, xt=xt: nc.vector.scalar_tensor_tensor(
                        out=r[:, :], in0=xt[:, :], scalar=DN_ALPHA, in1=r[:, :], op0=ALU.mult, op1=ALU.add), reads=[xt], writes=[r])
                    layer_norm_tile(r, lng, lnb, x1t, stats, mv, rs_t)
                    cx.dma("sp", x1_d[tt * 128:(tt + 1) * 128, :], x1t[:, :], reads=[x1t])
                    for q4 in range(4):
                        pt = ptb[npt % 4]
                        npt += 1
                        for i in range(4):
                            dc = q4 * 4 + i
                            cx.op("pe", lambda pt=pt, x1t=x1t, i=i, dc=dc: nc.tensor.transpose(
                                pt[:, i * 128:(i + 1) * 128], x1t[:, dc * 128:(dc + 1) * 128], ident[:, :]), reads=[x1t, ident], pwrites=[pt])
                        for i in range(4):
                            dc = q4 * 4 + i
                            cx.op("act", lambda pt=pt, h2b=h2b, i=i, dc=dc, tl=tl: nc.scalar.activation(
                                h2b[:, dc, tl * 128:(tl + 1) * 128], pt[:, i * 128:(i + 1) * 128], AF.Identity,
                                bias=b2[:, dc:dc + 1], scale=s2[:, dc:dc + 1]), reads=[pt, s2, b2], pwrites=[h2b])
                for a in range(2):
                    cx.dma("sp", h2T_d[a * 8:(a + 1) * 8, :, g * 512:(g + 1) * 512].rearrange("c p t -> p c t"), h2b[:, a * 8:(a + 1) * 8, :], reads=[h2b])
            cx.barrier()

        if upto >= 8:
          with ExitStack() as ph:
            wq = sb(ph, "wq", [128, 16, D], BF16)
            keysT = sb(ph, "keysT", [128, 16, 128], BF16)
            h2g = [sb(ph, "h2g%d" % i, [128, 16, 512], BF16) for i in range(2)]
            qTs = [sb(ph, "qTs%d" % i, [128, 16, 512], BF16) for i in range(2)]
            scb = [sb(ph, "scb%d" % i, [128, 8, 2, 128]) for i in range(2)]
            sv = sb(ph, "sv", [128, 8, 2, 16])
            tmp = sb(ph, "tk_tmp", [128, 128])
            c16 = sb(ph, "c16", [128, 8, 16, 16])
            c8 = sb(ph, "c8", [128, 8, 24])
            tmp2 = sb(ph, "tk_tmp2", [128, 256])
            tmp3 = sb(ph, "tk_tmp3", [128, 256])
            d16 = sb(ph, "d16", [128, 8, 16])
            zz = sb(ph, "zz", [128, 8])
            lz = sb(ph, "lz", [128, 8])
            pq = [ps(ph, "pq%d" % i, [128, 512]) for i in range(4)]
            psc = [ps(ph, "psc%d" % i, [128, 512]) for i in range(4)]
            for a in range(4):
                cx.dma("pool", wq[:, a * 4:(a + 1) * 4, :], wq_d[a * 512:(a + 1) * 512, :].rearrange("(k p) c -> p k c", p=128), pwrites=[wq])
            cx.dma("pool", keysT[:, :, :], keysT_d, writes=[keysT])
            npq = 0
            for g in range(8):
                hg, qt = h2g[g % 2], qTs[g % 2]
                for a in range(2):
                    cx.dma("sp", hg[:, a * 8:(a + 1) * 8, :], h2T_d[a * 8:(a + 1) * 8, :, g * 512:(g + 1) * 512].rearrange("c p t -> p c t"), pwrites=[hg])
                for hp in range(16):
                    p = pq[npq % 4]
                    npq += 1
                    for dc in range(16):
                        cx.op("pe", lambda p=p, hg=hg, dc=dc, hp=hp: nc.tensor.matmul(
                            p[:, :], wq[:, dc, hp * 128:(hp + 1) * 128], hg[:, dc, :], start=(dc == 0), stop=(dc == 15)),
                            reads=[wq, hg], pwrites=[p])
                    cx.op("act", lambda p=p, qt=qt, hp=hp: nc.scalar.copy(qt[:, hp, :], p[:, :]), reads=[p], pwrites=[qt])
                for tl in range(4):
                    tt = g * 4 + tl
                    sc = scb[tt % 2]
                    for bk in range(4):
                        for i in range(4):
                            hp = bk * 4 + i
                            cx.op("pe", lambda bk=bk, i=i, hp=hp, qt=qt, tl=tl: nc.tensor.matmul(
                                psc[bk][:, i * 128:(i + 1) * 128], qt[:, hp, tl * 128:(tl + 1) * 128], keysT[:, hp, :], start=True, stop=True),
                                reads=[qt, keysT], pwrites=[psc[bk]])
                        cx.op("act", lambda bk=bk, sc=sc: nc.scalar.copy(
                            sc[:, bk * 2:(bk + 1) * 2, :, :], psc[bk][:, :].rearrange("p (a b c) -> p a b c", a=2, b=2)),
                            reads=[psc[bk]], pwrites=[sc])
                    cx.dma("sp", scs_d[tt * 128:(tt + 1) * 128, :], sc[:, :, :, :].rearrange("p a b c -> p (a b c)"), reads=[sc])
                    for h in range(8):
                        for p_ in range(2):
                            cx.op("dve", lambda h=h, p_=p_, sc=sc: nc.vector.max(out=sv[:, h, p_, 0:8], in_=sc[:, h, p_, :]), reads=[sc], pwrites=[sv])
                            cx.op("dve", lambda h=h, p_=p_, sc=sc: nc.vector.match_replace(
                                out=tmp[:, :], in_to_replace=sv[:, h, p_, 0:8], in_values=sc[:, h, p_, :], imm_value=-1e30), reads=[sc, sv], writes=[tmp])
                            cx.op("dve", lambda h=h, p_=p_: nc.vector.max(out=sv[:, h, p_, 8:16], in_=tmp[:, :]), reads=[tmp], pwrites=[sv])
                    cx.op("dve", lambda: nc.vector.tensor_tensor(
                        out=c16[:, :, :, :], in0=bcast(sv[:, :, 0, :], 3, [128, 8, 16, 16]), in1=bcast(sv[:, :, 1, :], 2, [128, 8, 16, 16]), op=ALU.add),
                        reads=[sv], writes=[c16])
                    for h in range(8):
                        cflat = c16[:, h, :, :].rearrange("p a b -> p (a b)")
                        cx.op("dve", lambda h=h, cflat=cflat: nc.vector.max(out=c8[:, h, 0:8], in_=cflat), reads=[c16], pwrites=[c8])
                        cx.op("dve", lambda h=h, cflat=cflat: nc.vector.match_replace(
                            out=tmp2[:, :], in_to_replace=c8[:, h, 0:8], in_values=cflat, imm_value=-1e30), reads=[c16, c8], writes=[tmp2])
                        cx.op("dve", lambda h=h: nc.vector.max(out=c8[:, h, 8:16], in_=tmp2[:, :]), reads=[tmp2], pwrites=[c8])
                        cx.op("dve", lambda h=h: nc.vector.match_replace(
                            out=tmp3[:, :], in_to_replace=c8[:, h, 8:16], in_values=tmp2[:, :], imm_value=-1e30), reads=[tmp2, c8], writes=[tmp3])
                        cx.op("dve", lambda h=h: nc.vector.max(out=c8[:, h, 16:24], in_=tmp3[:, :]), reads=[tmp3], pwrites=[c8])
                    cx.op("dve", lambda: nc.vector.tensor_tensor(out=zz[:, :], in0=c8[:, :, 15], in1=c8[:, :, 16], op=ALU.add),
                          reads=[c8], writes=[zz])
                    cx.op("dve", lambda tt=tt: nc.vector.tensor_scalar(out=prm[:, tt, 0:8], in0=zz[:, :], scalar1=0.5, scalar2=None, op0=ALU.mult),
                          reads=[zz], pwrites=[prm])
                    cx.op("dve", lambda: nc.vector.tensor_tensor(out=d16[:, :, :], in0=c8[:, :, 0:16], in1=bcast(c8[:, :, 0], 2, [128, 8, 16]), op=ALU.subtract),
                          reads=[c8], writes=[d16])
                    cx.op("act", lambda: nc.scalar.activation(d16[:, :, :], d16[:, :, :], AF.Exp), writes=[d16])
                    cx.op("dve", lambda: nc.vector.tensor_reduce(out=zz[:, :], in_=d16[:, :, :], axis=AX.X, op=ALU.add), reads=[d16], writes=[zz])
                    cx.op("act", lambda: nc.scalar.activation(lz[:, :], zz[:, :], AF.Ln), reads=[zz], writes=[lz])
                    cx.op("dve", lambda tt=tt: nc.vector.scalar_tensor_tensor(
                        out=prm[:, tt, 8:16], in0=c8[:, :, 0], scalar=-1.0, in1=lz[:, :], op0=ALU.mult, op1=ALU.subtract),
                        reads=[c8, lz], pwrites=[prm])
            if dbgprm_d is not None:
                cx.dma("sp", dbgprm_d, prm[:, :, :], reads=[prm])
            cx.barrier()

        if upto >= 9:
          with ExitStack() as ph:
            lng = sb(ph, "ln2g", [128, D])
            lnb = sb(ph, "ln2b", [128, D])
            h2gs = [sb(ph, "h2p%d" % i, [128, 16, 128], BF16) for i in range(2)]
            scbs = [sb(ph, "scp%d" % i, [128, 8, 2, 128]) for i in range(2)]
            x1ts = [sb(ph, "x1p%d" % i, [128, D]) for i in range(1)]
            rt = sb(ph, "rtp", [128, D])
            ot = sb(ph, "otp", [128, D])
            cbs = [sb(ph, "cb%d" % i, [128, 16, 128]) for i in range(2)]
            ees = [sb(ph, "ee%d" % i, [128, 2048], BF16) for i in range(2)]
            gms = [sb(ph, "gm%d" % i, [128, 2048], BF16) for i in range(2)]
            gqs = [sb(ph, "gq%d" % i, [128, 2048], BF16) for i in range(2)]
            uts = [sb(ph, "ut%d" % i, [128, 16, 512], BF16) for i in range(2)]
            vcs = [sb(ph, "vc%d" % i, [128, 4, D], BF16) for i in range(2)]
            gas = [sb(ph, "ga%d" % i, [128, 512], BF16) for i in range(2)]
            wws = [sb(ph, "ww%d" % i, [128, 512], BF16) for i in range(2)]
            wTs = [sb(ph, "wT%d" % i, [128, 4, 128], BF16) for i in range(2)]
            stats = sb(ph, "stats2", [128, 4, 6])
            mv = sb(ph, "mv2", [128, 2])
            rs_t = sb(ph, "rs_t2", [128, 2])
            pop = [ps(ph, "pop%d" % i, [128, 512]) for i in range(4)]
            pap = [ps(ph, "pap%d" % i, [128, 512]) for i in range(2)]
            pwp = [ps(ph, "pwp%d" % i, [128, 1024], BF16) for i in range(2)]
            cx.dma("sp", lng[:, :], ln_d[2], writes=[lng])
            cx.dma("sp", lnb[:, :], ln_d[3], writes=[lnb])
            kq = 0
            ne = 0
            for tt in range(NT):
                tl = 0
                hg = h2gs[tt % 2]
                for a in range(2):
                    cx.dma("sp", hg[:, a * 8:(a + 1) * 8, :], h2T_d[a * 8:(a + 1) * 8, :, tt * 128:(tt + 1) * 128].rearrange("c p t -> p c t"), pwrites=[hg])
                sc, x1t = scbs[tt % 2], x1ts[0]
                cx.dma("sp", sc[:, :, :, :].rearrange("p a b c -> p (a b c)"), scs_d[tt * 128:(tt + 1) * 128, :], writes=[sc])
                cx.dma("sp", x1t[:, :], x1_d[tt * 128:(tt + 1) * 128, :], writes=[x1t])
                for e8 in range(8):
                    gq = gqs[(tt * 8 + e8) % 2]
                    for h in range(8):
                        cb, ee, gm = cbs[kq % 2], ees[kq % 2], gms[kq % 2]
                        kq += 1
                        cx.op("dve", lambda cb=cb, sc=sc, h=h, e8=e8: nc.vector.tensor_tensor(
                            out=cb[:, :, :], in0=bcast(sc[:, h, 0, e8 * 16:(e8 + 1) * 16], 2, [128, 16, 128]),
                            in1=bcast(sc[:, h, 1, :], 1, [128, 16, 128]), op=ALU.add), reads=[sc], writes=[cb])
                        cx.op("act", lambda cb=cb, ee=ee, h=h, tt=tt: nc.scalar.activation(
                            ee[:, :], cb[:, :, :].rearrange("p a b -> p (a b)"), AF.Exp, bias=prm[:, tt, 8 + h:9 + h]),
                            reads=[cb, prm], writes=[ee])
                        dst = gq if h == 0 else gm
                        cx.op("dve", lambda cb=cb, ee=ee, dst=dst, h=h, tt=tt: nc.vector.scalar_tensor_tensor(
                            out=dst[:, :], in0=cb[:, :, :].rearrange("p a b -> p (a b)"), scalar=prm[:, tt, h:h + 1], in1=ee[:, :],
                            op0=ALU.is_ge, op1=ALU.mult), reads=[cb, ee, prm], writes=[dst])
                        if h > 0:
                            cx.op("pool", lambda gq=gq, gm=gm: nc.gpsimd.tensor_tensor(out=gq[:, :], in0=gq[:, :], in1=gm[:, :], op=ALU.add),
                                  reads=[gm], writes=[gq])
                    for e4 in range(4):
                        eg = e8 * 4 + e4
                        ut, vc, pa, ga, ww, pw, wT = uts[ne % 2], vcs[ne % 2], pap[ne % 2], gas[ne % 2], wws[ne % 2], pwp[ne % 2], wTs[ne % 2]
                        ne += 1
                        cx.dma("sp", ut[:, :, :], utb_d[eg * 512:(eg + 1) * 512, :].rearrange("(p k) c -> p (k c)", k=4).rearrange("p (k c) -> p k c", c=512),
                               reads=[utb_t] if tt == 0 else (), writes=[ut])
                        cx.dma("sp", vc[:, :, :], vb16_d[eg * 512:(eg + 1) * 512, :].rearrange("(k p) d -> p k d", p=128),
                               reads=[vb16_t] if tt == 0 else (), writes=[vc])
                        for dc in range(16):
                            cx.op("pe", lambda pa=pa, hg=hg, ut=ut, dc=dc, tl=tl: nc.tensor.matmul(
                                pa[:, :], hg[:, dc, tl * 128:(tl + 1) * 128], ut[:, dc, :], start=(dc == 0), stop=(dc == 15)),
                                reads=[hg, ut], pwrites=[pa])
                        cx.op("act", lambda pa=pa, ga=ga: nc.scalar.activation(ga[:, :], pa[:, :], AF.Gelu), reads=[pa], writes=[ga])
                        cx.op("dve", lambda ga=ga, ww=ww, gq=gq, e4=e4: nc.vector.tensor_tensor(
                            out=ww[:, :], in0=ga[:, :], in1=gq[:, e4 * 512:(e4 + 1) * 512], op=ALU.mult), reads=[ga, gq], writes=[ww])
                        for k in range(4):
                            cx.op("pe", lambda pw=pw, ww=ww, k=k: nc.tensor.transpose(
                                pw[:, k * 128:(k + 1) * 128], ww[:, k * 128:(k + 1) * 128], identb[:, :]), reads=[ww, identb], pwrites=[pw])
                        cx.op("act", lambda pw=pw, wT=wT: nc.scalar.copy(wT[:, :, :], pw[:, 0:512].rearrange("p (a b) -> p a b", b=128)),
                              reads=[pw], writes=[wT])
                        for k in range(4):
                            for dq in range(4):
                                cx.op("pe", lambda wT=wT, vc=vc, k=k, dq=dq, eg=eg: nc.tensor.matmul(
                                    pop[dq][:, :], wT[:, k, :], vc[:, k, dq * 512:(dq + 1) * 512],
                                    start=(eg == 0 and k == 0), stop=(eg == 31 and k == 3)), reads=[wT, vc], pwrites=[pop[dq]])
                for dq in range(4):
                    cx.op("dve", lambda dq=dq: nc.vector.tensor_tensor(
                        out=rt[:, dq * 512:(dq + 1) * 512], in0=pop[dq][:, :], in1=g2bc[:, dq * 512:(dq + 1) * 512], op=ALU.mult),
                        reads=[pop[dq], g2bc], pwrites=[rt])
                cx.op("dve", lambda x1t=x1t: nc.vector.scalar_tensor_tensor(
                    out=rt[:, :], in0=x1t[:, :], scalar=DN_ALPHA, in1=rt[:, :], op0=ALU.mult, op1=ALU.add), reads=[x1t], writes=[rt])
                layer_norm_tile(rt, lng, lnb, ot, stats, mv, rs_t)
                cx.dma("sp", out_d[tt * 128:(tt + 1) * 128, :], ot[:, :], reads=[ot])
            cx.barrier()
        cx.barrier()
        print("[kernel] instructions emitted:", cx.nins)
    return nc


def host_layout(inputs):
    f = lambda k: np.asarray(inputs[k])
    shared = {}
    w_in = f("w_in")[0]
    b_in = f("b_in")[0]
    perm = np.concatenate([np.arange(32, 64), np.arange(0, 32)])
    cols = np.concatenate([np.arange(0, 4160), 4096 + perm, np.arange(4160, 8256)])
    wext = w_in[:, cols]
    shared["w_in_l"] = np.ascontiguousarray(wext.reshape(16, 128, 65, 128).transpose(2, 1, 0, 3).reshape(65, 128, 2048))
    shared["b_inT"] = np.ascontiguousarray(b_in[cols].reshape(65, 128).T)
    shared["w_ada"] = np.ascontiguousarray(f("w_ada")[0])
    b_ada = f("b_ada")[0]
    shared["b_adaT"] = np.ascontiguousarray(b_ada.reshape(96, 128).T)
    shared["b_ada_row"] = np.ascontiguousarray(b_ada.reshape(1, -1))
    rb = f("rel_bias")[0]
    p = np.arange(128)[:, None, None]
    j = np.arange(5)[None, :, None]
    c = np.arange(128)[None, None, :]
    rel = 128 * (4 - j) + c - p
    idx = np.clip(rel, -63, 256) + 63
    shared["biasT"] = np.ascontiguousarray(rb[:, idx].transpose(1, 0, 2, 3).reshape(128, 8, 640))
    mask = np.zeros((128, 5, 128), np.float32)
    mask[:64, 0, 64:] = NEGM
    mask[64:, 4, :64] = NEGM
    shared["maskA"] = mask.reshape(128, 640)
    shared["gqT"] = np.ascontiguousarray(f("q_norm_g")[0].reshape(4, 128).T)
    shared["gkvT"] = np.ascontiguousarray(f("kv_norm_g")[0].reshape(4, 128).T)
    w_uq = f("w_uq")[0]
    qcols = []
    for h in range(8):
        base = h * 192
        qcols += [np.arange(base, base + 128), np.arange(base + 128, base + 192), base + 128 + perm]
    qcols = np.concatenate(qcols)
    shared["w_uq_l"] = np.ascontiguousarray(w_uq[:, qcols].reshape(4, 128, 2048).transpose(1, 0, 2))
    w_ukv = f("w_ukv")[0].reshape(512, 8, 256)
    shared["w_uk_l"] = np.ascontiguousarray(w_ukv[:, :, :128].reshape(4, 128, 1024).transpose(1, 0, 2))
    shared["w_uv_l"] = np.ascontiguousarray(w_ukv[:, :, 128:].reshape(4, 128, 1024).transpose(1, 0, 2))
    shared["w_pa"] = np.ascontiguousarray(f("w_pa")[0])
    shared["w_pb"] = np.ascontiguousarray(f("w_pb")[0])
    shared["w_o"] = np.ascontiguousarray(f("w_o")[0])
    shared["ln_bc"] = np.ascontiguousarray(np.stack([np.broadcast_to(f(k)[0][None, :], (128, D)) for k in ("ln1_g", "ln1_b", "ln2_g", "ln2_b")]))
    shared["peer_wq"] = np.ascontiguousarray(f("peer_wq")[0])
    keys = f("peer_keys")[0]
    shared["keysT"] = np.ascontiguousarray(keys.reshape(16, 128, 128).transpose(2, 0, 1))
    U = f("peer_u")[0]
    shared["ut_l"] = np.ascontiguousarray(U.reshape(32, 512, 16, 128).transpose(0, 3, 2, 1)).reshape(16384, 2048)
    shared["peer_v"] = np.ascontiguousarray(f("peer_v")[0])
    shared["ident"] = np.eye(128, dtype=np.float32)
    inv_freq = (10000.0 ** (-np.arange(0, 64, 2, dtype=np.float32) / 64.0)).astype(np.float32)
    invf = np.zeros((64, 2), np.float32)
    invf[:, 0] = np.concatenate([inv_freq, inv_freq])
    invf[:32, 1] = -1.0
    invf[32:, 1] = 1.0
    shared["invf"] = invf
    per_core = []
    x = f("x")
    cc = f("c")
    pos = f("positions")
    for b in range(NCORES):
        m = dict(shared)
        m["x"] = np.ascontiguousarray(x[b])
        m["cT"] = np.ascontiguousarray(cc[b].reshape(16, 128).T)
        m["posb"] = np.ascontiguousarray(np.broadcast_to(pos[b][None, :].astype(np.int32), (64, S)))
        per_core.append(m)
    return per_core


_NC_CACHE = {}


def kernel(**inputs):
    maps = host_layout(inputs)
    if "full" not in _NC_CACHE:
        _NC_CACHE["full"] = build()
    nc = _NC_CACHE["full"]
    res = run_bass_kernel_spmd(nc, maps, core_ids=list(range(NCORES)))
    out = np.stack([np.asarray(r["out"]) for r in res.results], axis=0)
    return out.astype(np.float32)
```

```python
import math
from contextlib import ExitStack

import numpy as np
import concourse.bass as bass
import concourse.mybir as mybir
from concourse.bass_utils import run_bass_kernel_spmd

F32 = mybir.dt.float32
BF16 = mybir.dt.bfloat16
I32 = mybir.dt.int32
AF = mybir.ActivationFunctionType
ALU = mybir.AluOpType
AX = mybir.AxisListType

NCORES = 8
S = 4096
D = 2048
NT = S // 128
DN_ALPHA = 2.0 ** 0.25
LN_EPS = 1e-5
RMS_EPS = 1e-6
SCALE_A = 128.0 ** -0.5
SCALE_B = 192.0 ** -0.5
NEGM = -30000.0
MAGIC = 12582912.0
TWO_PI = 2.0 * math.pi
C1 = 6.28125
C2 = TWO_PI - C1
PI_SAFE = 3.14159

SAME_ENG_SYNC = True
CB_ON_POOL = False


class Buf:
    __slots__ = ("w", "r")

    def __init__(self):
        self.w = {}
        self.r = {}


class T:
    def __init__(self, handle):
        self.t = handle
        self.b = Buf()

    def __getitem__(self, k):
        return self.t[k]


class Ctx:
    def __init__(self, nc, es):
        self.nc = nc
        self.eng = {"pe": nc.tensor, "act": nc.scalar, "dve": nc.vector, "pool": nc.gpsimd, "sp": nc.sync}
        self.sems = {}
        self.tot = {}
        for e in ("pe", "act", "dve", "pool"):
            self.sems[e] = es.enter_context(nc.semaphore("s_" + e))
            self.tot[e] = 0
        self.dq = {}
        for q, n in (("sp", 16), ("pool", 8)):
            lst = []
            for i in range(n):
                k = "d_%s%d" % (q, i)
                self.sems[k] = es.enter_context(nc.semaphore(k))
                self.tot[k] = 0
                lst.append(k)
            self.dq[q] = [lst, 0]
        self.seen = {e: {} for e in self.eng}
        self.nins = 0

    def _wait(self, e, deps):
        own = e if e in ("pe", "act", "dve", "pool") else None
        seen = self.seen[e]
        for k, v in deps.items():
            if v <= 0:
                continue
            if k == own and (own == "pe" or not SAME_ENG_SYNC):
                continue
            if seen.get(k, 0) >= v:
                continue
            self.eng[e].wait_ge(self.sems[k], v)
            seen[k] = v

    @staticmethod
    def _merge(d, k, v):
        if d.get(k, 0) < v:
            d[k] = v

    def _deps(self, reads, writes, pwrites):
        deps = {}
        for t in reads:
            for k, v in t.b.w.items():
                self._merge(deps, k, v)
        for t in writes:
            for k, v in t.b.w.items():
                self._merge(deps, k, v)
            for k, v in t.b.r.items():
                self._merge(deps, k, v)
        for t in pwrites:
            for k, v in t.b.r.items():
                self._merge(deps, k, v)
        return deps

    def _update(self, tok, reads, writes, pwrites):
        k, v = tok
        for t in writes:
            t.b.w = {k: v}
            t.b.r = {}
        for t in pwrites:
            if t.b.r:
                t.b.w = {k: v}
                t.b.r = {}
            else:
                self._merge(t.b.w, k, v)
        for t in reads:
            self._merge(t.b.r, k, v)

    def op(self, e, fn, reads=(), writes=(), pwrites=()):
        self._wait(e, self._deps(reads, writes, pwrites))
        ins = fn()
        self.tot[e] += 1
        ins.then_inc(self.sems[e], 1)
        self.nins += 1
        self._update((e, self.tot[e]), reads, writes, pwrites)
        return ins

    def dma(self, q, out, in_, reads=(), writes=(), pwrites=()):
        lst, i = self.dq[q]
        k = lst[i % len(lst)]
        self.dq[q][1] = i + 1
        deps = self._deps(reads, writes, pwrites)
        self._merge(deps, k, self.tot[k])
        self._wait(q, deps)
        ins = self.eng[q].dma_start(out=out, in_=in_)
        self.tot[k] += 16
        ins.then_inc(self.sems[k], 16)
        self.nins += 1
        self._update((k, self.tot[k]), reads, writes, pwrites)
        return ins

    def barrier(self, engines=("pe", "act", "dve", "pool", "sp")):
        for e in engines:
            self._wait(e, dict(self.tot))


def bcast(ap, axis, shape):
    return ap.unsqueeze(axis).broadcast_to(shape)


def build(upto=99, dbg=()):
    nc = bass.Bass("TRN2", target_bir_lowering=False)

    def din(name, shape, dt=F32):
        return nc.dram_tensor(name, list(shape), dt, kind="ExternalInput").ap()

    def dscr(name, shape, dt):
        kind = "ExternalOutput" if name in dbg else "Internal"
        return nc.dram_tensor(name, list(shape), dt, kind=kind).ap()

    x_d = din("x", [S, D])
    cT_d = din("cT", [128, 16])
    pos_d = din("posb", [64, S], I32)
    invf_d = din("invf", [64, 2])
    wada_d = din("w_ada", [D, 6 * D])
    badaT_d = din("b_adaT", [128, 96])
    badar_d = din("b_ada_row", [1, 6 * D])
    win_d = din("w_in_l", [65, 128, 2048])
    binT_d = din("b_inT", [128, 65])
    biasT_d = din("biasT", [128, 8, 640])
    maskA_d = din("maskA", [128, 640])
    gq_d = din("gqT", [128, 4])
    gkv_d = din("gkvT", [128, 4])
    wuq_d = din("w_uq_l", [128, 4, 2048])
    wuk_d = din("w_uk_l", [128, 4, 1024])
    wuv_d = din("w_uv_l", [128, 4, 1024])
    wpa_d = din("w_pa", [1024, D])
    wpb_d = din("w_pb", [1024, D])
    wo_d = din("w_o", [D, D])
    ln_d = din("ln_bc", [4, 128, D])
    wq_d = din("peer_wq", [D, D])
    keysT_d = din("keysT", [128, 16, 128])
    ut_d = din("ut_l", [16384, 2048])
    v_d = din("peer_v", [16384, D])
    ident_d = din("ident", [128, 128])
    iota_d = din("iota", [128, 128])

    out_d = nc.dram_tensor("out", [S, D], F32, kind="ExternalOutput").ap()

    featT_d = dscr("featT", [65, 128, S], BF16)
    yT_d = dscr("yT", [16, 128, S], BF16)
    qnT_d = dscr("qnT", [8, 128, S], BF16)
    qrT_d = dscr("qrT", [8, 64, S], BF16)
    knT_d = dscr("knT", [8, 128, S], BF16)
    krT_d = dscr("krT", [64, S], BF16)
    vbs_d = dscr("vbs", [S, 1024], BF16)
    yfT_d = dscr("yfT", [16, 128, S], BF16)
    x1_d = dscr("x1s", [S, D], F32)
    h2T_d = dscr("h2T", [16, 128, S], BF16)
    scs_d = dscr("scs", [S, 2048], F32)
    pk_d = dscr("pk", [S, 272], F32)
    utb_d = dscr("utb", [16384, 2048], BF16)
    vb16_d = dscr("vb16", [16384, D], BF16)
    dbgmod_d = dscr("dbgmod", [128, 96 + 2], F32) if "dbgmod" in dbg else None
    dbgg_d = dscr("dbgg", [128, 2 * D], F32) if "dbgg" in dbg else None
    dbgprm_d = dscr("dbgprm", [128, NT, 16], F32) if "dbgprm" in dbg else None

    with ExitStack() as es:
        cx = Ctx(nc, es)

        uid = [0]

        def sb(scope, name, shape, dt=F32):
            uid[0] += 1
            return T(scope.enter_context(nc.sbuf_tensor("sb%d_%s" % (uid[0], name), list(shape), dt)))

        def ps(scope, name, shape, dt=F32):
            uid[0] += 1
            return T(scope.enter_context(nc.psum_tensor("ps%d_%s" % (uid[0], name), list(shape), dt)))

        CB_ENG = "pool" if CB_ON_POOL else "dve"
        CB_OBJ = nc.gpsimd if CB_ON_POOL else nc.vector
        ident = sb(es, "ident", [128, 128])
        identb = sb(es, "identb", [128, 128], BF16)
        ones_f = sb(es, "ones_f", [128, 128])
        ones_b = sb(es, "ones_b", [128, 128], BF16)
        s1 = sb(es, "s1", [128, 16])
        b1 = sb(es, "b1", [128, 16])
        s2 = sb(es, "s2", [128, 16])
        b2 = sb(es, "b2", [128, 16])
        g1bc = sb(es, "g1bc", [128, D])
        g2bc = sb(es, "g2bc", [128, D])
        prm = sb(es, "prm", [128, NT, 16])

        cx.dma("sp", ident[:, :], ident_d, writes=[ident])
        cx.op("dve", lambda: nc.vector.tensor_copy(identb[:, :], ident[:, :]), reads=[ident], writes=[identb])
        cx.op("dve", lambda: nc.vector.memset(ones_f[:, :], 1.0), writes=[ones_f])
        cx.op("dve", lambda: nc.vector.memset(ones_b[:, :], 1.0), writes=[ones_b])

        if upto >= 6:
            utb_t = T(None)
            vb16_t = T(None)
            for i in range(32):
                cx.dma("pool", utb_d[i * 512:(i + 1) * 512, :], ut_d[i * 512:(i + 1) * 512, :], pwrites=[utb_t])
            for i in range(32):
                cx.dma("pool", vb16_d[i * 512:(i + 1) * 512, :], v_d[i * 512:(i + 1) * 512, :], pwrites=[vb16_t])

        with ExitStack() as ph:
            cT = sb(ph, "cT", [128, 16])
            scT = sb(ph, "scT", [128, 16])
            badaT = sb(ph, "badaT", [128, 96])
            badar = sb(ph, "badar", [1, 6 * D])
            modT = sb(ph, "modT", [128, 96])
            wst = [sb(ph, "wst%d" % i, [128, 16, 512]) for i in range(2)]
            rowsb = [sb(ph, "rowsb%d" % i, [1, 512]) for i in range(2)]
            pm = ps(ph, "pm", [128, 512])
            pr = [ps(ph, "pr%d" % i, [128, 512]) for i in range(2)]
            pbc = [ps(ph, "pbc%d" % i, [128, 512]) for i in range(2)]
            cx.dma("sp", cT[:, :], cT_d, writes=[cT])
            cx.dma("sp", badaT[:, :], badaT_d, writes=[badaT])
            cx.dma("sp", badar[:, :], badar_d, writes=[badar])
            cx.op("act", lambda: nc.scalar.activation(scT[:, :], cT[:, :], AF.Silu), reads=[cT], writes=[scT])
            nrow = 0
            for gi in range(24):
                m = gi // 4
                w = wst[gi % 2]
                cx.dma("sp", w[:, :, :], wada_d[:, gi * 512:(gi + 1) * 512].rearrange("(k p) c -> p k c", p=128), writes=[w])
                if m in (0, 1, 3, 4):
                    for cc in range(4):
                        col = m * 16 + (gi % 4) * 4 + cc
                        for kc in range(16):
                            cx.op("pe", lambda kc=kc, cc=cc, col=col, w=w: nc.tensor.matmul(
                                pm[:, col:col + 1], w[:, kc, cc * 128:(cc + 1) * 128], scT[:, kc:kc + 1],
                                start=(kc == 0), stop=(kc == 15)), reads=[w, scT], pwrites=[pm])
                else:
                    p_r = pr[nrow % 2]
                    rs = rowsb[nrow % 2]
                    p_b = pbc[nrow % 2]
                    nrow += 1
                    for kc in range(16):
                        cx.op("pe", lambda kc=kc, w=w, p_r=p_r: nc.tensor.matmul(
                            p_r[0:1, :], scT[:, kc:kc + 1], w[:, kc, :], start=(kc == 0), stop=(kc == 15)),
                            reads=[w, scT], pwrites=[p_r])
                    cx.op("dve", lambda p_r=p_r, rs=rs, gi=gi: nc.vector.tensor_tensor(
                        out=rs[0:1, :], in0=p_r[0:1, :], in1=badar[0:1, gi * 512:(gi + 1) * 512], op=ALU.add),
                        reads=[p_r, badar], writes=[rs])
                    cx.op("pe", lambda rs=rs, p_b=p_b: nc.tensor.matmul(
                        p_b[:, :], ones_f[0:1, :], rs[0:1, :], start=True, stop=True), reads=[rs, ones_f], pwrites=[p_b])
                    dst = g1bc if m == 2 else g2bc
                    cx.op("act", lambda dst=dst, p_b=p_b, gi=gi: nc.scalar.copy(
                        dst[:, (gi % 4) * 512:(gi % 4 + 1) * 512], p_b[:, :]), reads=[p_b], pwrites=[dst])
            cx.op("dve", lambda: nc.vector.tensor_tensor(out=modT[:, :], in0=pm[:, 0:96], in1=badaT[:, :], op=ALU.add),
                  reads=[pm, badaT], writes=[modT])
            cx.op("dve", lambda: nc.vector.tensor_scalar(out=s1[:, :], in0=modT[:, 16:32], scalar1=1.0, scalar2=None, op0=ALU.add),
                  reads=[modT], writes=[s1])
            cx.op("dve", lambda: nc.vector.tensor_copy(b1[:, :], modT[:, 0:16]), reads=[modT], writes=[b1])
            cx.op("dve", lambda: nc.vector.tensor_scalar(out=s2[:, :], in0=modT[:, 64:80], scalar1=1.0, scalar2=None, op0=ALU.add),
                  reads=[modT], writes=[s2])
            cx.op("dve", lambda: nc.vector.tensor_copy(b2[:, :], modT[:, 48:64]), reads=[modT], writes=[b2])
            if dbgmod_d is not None:
                cx.dma("sp", dbgmod_d[:, 0:96], modT[:, :], reads=[modT])
            if dbgg_d is not None:
                cx.dma("sp", dbgg_d[:, 0:D], g1bc[:, :], reads=[g1bc])
                cx.dma("sp", dbgg_d[:, D:2 * D], g2bc[:, :], reads=[g2bc])
            cx.barrier()

        if upto >= 1:
          with ExitStack() as ph12:
            hT = sb(ph12, "hT", [128, 16, S], BF16)
            with ExitStack() as ph:
                xs = [sb(ph, "xs%d" % i, [128, 2, D]) for i in range(2)]
                ptr = [ps(ph, "ptr%d" % i, [128, 512]) for i in range(4)]
                n = 0
                for g in range(16):
                    xb = xs[g % 2]
                    cx.dma("sp", xb[:, :, :], x_d[g * 256:(g + 1) * 256, :].rearrange("(j p) d -> p j d", p=128), writes=[xb])
                    for dc in range(16):
                        pt = ptr[n % 4]
                        n += 1
                        for j in range(2):
                            cx.op("pe", lambda pt=pt, xb=xb, j=j, dc=dc: nc.tensor.transpose(
                                pt[:, j * 128:(j + 1) * 128], xb[:, j, dc * 128:(dc + 1) * 128], ident[:, :]),
                                reads=[xb, ident], pwrites=[pt])
                        cx.op("act", lambda pt=pt, g=g, dc=dc: nc.scalar.activation(
                            hT[:, dc, g * 256:(g + 1) * 256], pt[:, 0:256], AF.Identity,
                            bias=b1[:, dc:dc + 1], scale=s1[:, dc:dc + 1]), reads=[pt, s1, b1], pwrites=[hT])
                cx.barrier()
            with ExitStack() as ph:
                wc = [sb(ph, "wc%d" % i, [128, 16, 128], BF16) for i in range(3)]
                ost = [sb(ph, "ost%d" % i, [128, S], BF16) for i in range(2)]
                binT = sb(ph, "binT", [128, 65])
                pz = [ps(ph, "pz%d" % i, [128, 512]) for i in range(4)]
                cx.dma("sp", binT[:, :], binT_d, writes=[binT])
                n = 0
                for ch in range(65):
                    w = wc[ch % 3]
                    cx.dma("pool", w[:, :, :], win_d[ch].rearrange("p (k c) -> p k c", c=128), writes=[w])
                    o = ost[ch % 2]
                    func = AF.Sigmoid if ch >= 33 else AF.Identity
                    for g in range(8):
                        p = pz[n % 4]
                        n += 1
                        for kc in range(16):
                            cx.op("pe", lambda p=p, w=w, kc=kc, g=g: nc.tensor.matmul(
                                p[:, :], w[:, kc, :], hT[:, kc, g * 512:(g + 1) * 512], start=(kc == 0), stop=(kc == 15)),
                                reads=[w, hT], pwrites=[p])
                        cx.op("act", lambda p=p, o=o, g=g, ch=ch, func=func: nc.scalar.activation(
                            o[:, g * 512:(g + 1) * 512], p[:, :], func, bias=binT[:, ch:ch + 1]),
                            reads=[p, binT], pwrites=[o])
                    cx.dma("sp", featT_d[ch], o[:, :], reads=[o])
                cx.barrier()

        if upto >= 3:
          with ExitStack() as ph:
            biasT = sb(ph, "biasT", [128, 8, 640])
            maskA = sb(ph, "maskA", [128, 640])
            qTs = [sb(ph, "qT%d" % i, [128, S], BF16) for i in range(2)]
            kTs = [sb(ph, "kT%d" % i, [128, S], BF16) for i in range(2)]
            vTs = [sb(ph, "vT%d" % i, [128, S], BF16) for i in range(2)]
            vas = [sb(ph, "va%d" % i, [128, NT, 128], BF16) for i in range(2)]
            ybs = [sb(ph, "yb%d" % i, [128, S], BF16) for i in range(2)]
            t1s = [sb(ph, "t1_%d" % i, [128, 640]) for i in range(2)]
            pTs = [sb(ph, "pT%d" % i, [128, 640], BF16) for i in range(2)]
            rds = [sb(ph, "rd%d" % i, [128, 128]) for i in range(2)]
            pss = [ps(ph, "psA%d" % i, [128, 1024]) for i in range(2)]
            pos_ = [ps(ph, "poA%d" % i, [128, 512]) for i in range(2)]
            ptr = [ps(ph, "ptA%d" % i, [128, 1024], BF16) for i in range(2)]
            cx.dma("sp", biasT[:, :, :], biasT_d, writes=[biasT])
            cx.dma("sp", maskA[:, :], maskA_d, writes=[maskA])
            for h in range(8):
                cx.op("dve", lambda h=h: nc.vector.tensor_tensor(out=biasT[:, h, :], in0=biasT[:, h, :], in1=maskA[:, :], op=ALU.add),
                      reads=[maskA], writes=[biasT])
            nt = 0
            for h in range(8):
                qT, kT, vT, va, yb = qTs[h % 2], kTs[h % 2], vTs[h % 2], vas[h % 2], ybs[h % 2]
                cx.dma("sp", qT[:, :], featT_d[h], writes=[qT])
                cx.dma("sp", kT[:, :], featT_d[8 + h], writes=[kT])
                cx.dma("sp", vT[:, :], featT_d[16 + h], writes=[vT])
                for blk in range(4):
                    pt = ptr[nt % 2]
                    nt += 1
                    for i in range(8):
                        tl = blk * 8 + i
                        cx.op("pe", lambda pt=pt, vT=vT, i=i, tl=tl: nc.tensor.transpose(
                            pt[:, i * 128:(i + 1) * 128], vT[:, tl * 128:(tl + 1) * 128], identb[:, :]),
                            reads=[vT, identb], pwrites=[pt])
                    cx.op("act", lambda pt=pt, va=va, blk=blk: nc.scalar.copy(
                        va[:, blk * 8:(blk + 1) * 8, :], pt[:, :].rearrange("p (a b) -> p a b", b=128)),
                        reads=[pt], pwrites=[va])
                for m in range(NT):
                    j0 = max(0, 4 - m)
                    lo = j0 * 128
                    psm, t1, pT, po, rd = pss[m % 2], t1s[m % 2], pTs[m % 2], pos_[m % 2], rds[m % 2]
                    for j in range(j0, 5):
                        kt = m - 4 + j
                        cx.op("pe", lambda psm=psm, kT=kT, qT=qT, j=j, kt=kt, m=m: nc.tensor.matmul(
                            psm[:, j * 128:(j + 1) * 128], kT[:, kt * 128:(kt + 1) * 128], qT[:, m * 128:(m + 1) * 128],
                            start=True, stop=True), reads=[kT, qT], pwrites=[psm])
                    cx.op("dve", lambda psm=psm, t1=t1, h=h, lo=lo: nc.vector.scalar_tensor_tensor(
                        out=t1[:, lo:640], in0=psm[:, lo:640], scalar=SCALE_A, in1=biasT[:, h, lo:640],
                        op0=ALU.mult, op1=ALU.add), reads=[psm, biasT], writes=[t1])
                    cx.op("act", lambda t1=t1, pT=pT, lo=lo: nc.scalar.activation(pT[:, lo:640], t1[:, lo:640], AF.Exp),
                          reads=[t1], writes=[pT])
                    for j in range(j0, 5):
                        kt = m - 4 + j
                        cx.op("pe", lambda po=po, va=va, pT=pT, j=j, kt=kt, j0=j0: nc.tensor.matmul(
                            po[:, 0:128], va[:, kt, :], pT[:, j * 128:(j + 1) * 128], start=(j == j0), stop=(j == 4)),
                            reads=[va, pT], pwrites=[po])
                    for j in range(j0, 5):
                        cx.op("pe", lambda po=po, pT=pT, j=j, j0=j0: nc.tensor.matmul(
                            po[:, 128:256], ones_b[:, :], pT[:, j * 128:(j + 1) * 128], start=(j == j0), stop=(j == 4)),
                            reads=[ones_b, pT], pwrites=[po])
                    cx.op("dve", lambda rd=rd, po=po: nc.vector.reciprocal(rd[:, :], po[:, 128:256]), reads=[po], writes=[rd])
                    cx.op("dve", lambda yb=yb, po=po, rd=rd, m=m: nc.vector.tensor_tensor(
                        out=yb[:, m * 128:(m + 1) * 128], in0=po[:, 0:128], in1=rd[:, :], op=ALU.mult),
                        reads=[po, rd], pwrites=[yb])
                cx.dma("sp", yT_d[h], yb[:, :], reads=[yb])
            cx.barrier()

        if upto >= 4:
          with ExitStack() as ph:
            cos2 = sb(ph, "cos2", [64, S])
            sinS = sb(ph, "sinS", [64, S])
            invf = sb(ph, "invf", [64, 2])
            cx.dma("sp", invf[:, :], invf_d, writes=[invf])
            with ExitStack() as ph2:
                posi = sb(ph2, "posi", [64, S], I32)
                ang = sb(ph2, "ang", [64, S])
                ta = sb(ph2, "ta", [64, S])
                tb = sb(ph2, "tb", [64, S])
                cx.dma("sp", posi[:, :], pos_d, writes=[posi])
                cx.op("dve", lambda: nc.vector.tensor_copy(ta[:, :], posi[:, :]), reads=[posi], writes=[ta])
                cx.op("dve", lambda: nc.vector.tensor_scalar(out=ang[:, :], in0=ta[:, :], scalar1=invf[:, 0:1], scalar2=None, op0=ALU.mult),
                      reads=[ta, invf], writes=[ang])
                for dst, shift, use_sgn in ((sinS, 0.0, True), (cos2, math.pi / 2.0, False)):
                    src = ang
                    if shift != 0.0:
                        cx.op("dve", lambda: nc.vector.tensor_scalar(out=ta[:, :], in0=ang[:, :], scalar1=shift, scalar2=None, op0=ALU.add),
                              reads=[ang], writes=[ta])
                        src = ta
                    else:
                        cx.op("dve", lambda: nc.vector.tensor_copy(ta[:, :], ang[:, :]), reads=[ang], writes=[ta])
                        src = ta
                    cx.op("dve", lambda: nc.vector.tensor_scalar(out=tb[:, :], in0=ta[:, :], scalar1=1.0 / TWO_PI, scalar2=None, op0=ALU.mult),
                          reads=[ta], writes=[tb])
                    cx.op("dve", lambda: nc.vector.tensor_scalar(out=tb[:, :], in0=tb[:, :], scalar1=MAGIC, scalar2=None, op0=ALU.add),
                          reads=[tb], writes=[tb])
                    cx.op("dve", lambda: nc.vector.tensor_scalar(out=tb[:, :], in0=tb[:, :], scalar1=-MAGIC, scalar2=None, op0=ALU.add),
                          reads=[tb], writes=[tb])
                    cx.op("dve", lambda: nc.vector.scalar_tensor_tensor(out=ta[:, :], in0=tb[:, :], scalar=-C1, in1=ta[:, :], op0=ALU.mult, op1=ALU.add),
                          reads=[tb, ta], writes=[ta])
                    cx.op("dve", lambda: nc.vector.scalar_tensor_tensor(out=ta[:, :], in0=tb[:, :], scalar=-C2, in1=ta[:, :], op0=ALU.mult, op1=ALU.add),
                          reads=[tb, ta], writes=[ta])
                    cx.op("dve", lambda: nc.vector.tensor_scalar(out=ta[:, :], in0=ta[:, :], scalar1=PI_SAFE, scalar2=-PI_SAFE, op0=ALU.min, op1=ALU.max),
                          reads=[ta], writes=[ta])
                    if use_sgn:
                        cx.op("act", lambda dst=dst: nc.scalar.activation(dst[:, :], ta[:, :], AF.Sin, scale=invf[:, 1:2]),
                              reads=[ta, invf], writes=[dst])
                    else:
                        cx.op("act", lambda dst=dst: nc.scalar.activation(dst[:, :], ta[:, :], AF.Sin), reads=[ta], writes=[dst])
                cx.barrier()

            with ExitStack() as ph2:
                lat = sb(ph2, "lat", [128, 4, S], BF16)
                sq = [sb(ph2, "sq%d" % i, [128, 4, 512], BF16) for i in range(2)]
                tmpf = [sb(ph2, "tmpf%d" % i, [128, 512]) for i in range(2)]
                rstd = sb(ph2, "rstd", [128, S])
                gn = sb(ph2, "gn", [128, 8])
                wuq = sb(ph2, "wuq", [128, 4, 2048], BF16)
                wuk = sb(ph2, "wuk", [128, 4, 1024], BF16)
                wuv = sb(ph2, "wuv", [128, 4, 1024], BF16)
                qn_st = [sb(ph2, "qn_st%d" % i, [128, S], BF16) for i in range(2)]
                qr_st = [sb(ph2, "qr_st%d" % i, [64, S], BF16) for i in range(2)]
                v_st = [sb(ph2, "v_st%d" % i, [128, 2, 1024], BF16) for i in range(2)]
                rta = [sb(ph2, "rta%d" % i, [64, 512]) for i in range(2)]
                rtb = [sb(ph2, "rtb%d" % i, [64, 512]) for i in range(2)]
                kr_a, kr_b = qr_st[0], qr_st[1]
                pp = [ps(ph2, "pp%d" % i, [128, 512]) for i in range(6)]
                npp = [0]

                def nextp():
                    p = pp[npp[0] % 6]
                    npp[0] += 1
                    return p

                cx.dma("sp", gn[:, 0:4], gq_d, pwrites=[gn])
                cx.dma("sp", gn[:, 4:8], gkv_d, pwrites=[gn])
                cx.dma("pool", wuq[:, :, :], wuq_d, writes=[wuq])
                cx.dma("pool", wuk[:, :, :], wuk_d, writes=[wuk])
                cx.dma("pool", wuv[:, :, :], wuv_d, writes=[wuv])

                def load_norm(first_chunk, goff):
                    for c in range(4):
                        cx.dma("sp", lat[:, c, :], featT_d[first_chunk + c], pwrites=[lat])
                    for g in range(8):
                        sqb, tf = sq[g % 2], tmpf[g % 2]
                        cx.op("act", lambda sqb=sqb, g=g: nc.scalar.activation(sqb[:, :, :], lat[:, :, g * 512:(g + 1) * 512], AF.Square),
                              reads=[lat], writes=[sqb])
                        p = nextp()
                        for c in range(4):
                            cx.op("pe", lambda p=p, sqb=sqb, c=c: nc.tensor.matmul(p[:, :], ones_b[:, :], sqb[:, c, :], start=(c == 0), stop=(c == 3)),
                                  reads=[sqb, ones_b], pwrites=[p])
                        cx.op("act", lambda p=p, tf=tf: nc.scalar.activation(tf[:, :], p[:, :], AF.Sqrt, scale=1.0 / 512.0, bias=RMS_EPS),
                              reads=[p], writes=[tf])
                        cx.op("dve", lambda tf=tf, g=g: nc.vector.reciprocal(rstd[:, g * 512:(g + 1) * 512], tf[:, :]), reads=[tf], pwrites=[rstd])
                    for c in range(4):
                        for g in range(4):
                            cx.op("dve", lambda c=c, g=g: nc.vector.scalar_tensor_tensor(
                                out=lat[:, c, g * 1024:(g + 1) * 1024], in0=lat[:, c, g * 1024:(g + 1) * 1024],
                                scalar=gn[:, goff + c:goff + c + 1], in1=rstd[:, g * 1024:(g + 1) * 1024], op0=ALU.mult, op1=ALU.mult),
                                reads=[rstd, gn], writes=[lat])

                load_norm(28, 4)
                for h in range(8):
                    st = qn_st[h % 2]
                    for g in range(8):
                        p = nextp()
                        for c in range(4):
                            cx.op("pe", lambda p=p, c=c, h=h, g=g: nc.tensor.matmul(
                                p[:, :], wuk[:, c, h * 128:(h + 1) * 128], lat[:, c, g * 512:(g + 1) * 512], start=(c == 0), stop=(c == 3)),
                                reads=[wuk, lat], pwrites=[p])
                        cx.op("act", lambda p=p, st=st, g=g: nc.scalar.copy(st[:, g * 512:(g + 1) * 512], p[:, :]), reads=[p], pwrites=[st])
                    cx.dma("sp", knT_d[h], st[:, :], reads=[st])
                for tq in range(16):
                    st = v_st[tq % 2]
                    for ti in range(2):
                        tt = tq * 2 + ti
                        for half in range(2):
                            p = nextp()
                            for c in range(4):
                                cx.op("pe", lambda p=p, c=c, tt=tt, half=half: nc.tensor.matmul(
                                    p[:, :], lat[:, c, tt * 128:(tt + 1) * 128], wuv[:, c, half * 512:(half + 1) * 512], start=(c == 0), stop=(c == 3)),
                                    reads=[wuv, lat], pwrites=[p])
                            cx.op("act", lambda p=p, st=st, ti=ti, half=half: nc.scalar.copy(st[:, ti, half * 512:(half + 1) * 512], p[:, :]),
                                  reads=[p], pwrites=[st])
                    cx.dma("sp", vbs_d[tq * 256:(tq + 1) * 256, :].rearrange("(a p) c -> p a c", p=128), st[:, :, :], reads=[st])
                cx.dma("sp", kr_a[:, :], featT_d[32][0:64, :], writes=[kr_a])
                cx.dma("sp", kr_b[:, :], featT_d[32][64:128, :], writes=[kr_b])
                for g in range(8):
                    ra, rb = rta[g % 2], rtb[g % 2]
                    sl = slice(g * 512, (g + 1) * 512)
                    cx.op("dve", lambda ra=ra, sl=sl: nc.vector.tensor_tensor(out=ra[:, :], in0=kr_a[:, sl], in1=cos2[:, sl], op=ALU.mult),
                          reads=[kr_a, cos2], writes=[ra])
                    cx.op("dve", lambda rb=rb, sl=sl: nc.vector.tensor_tensor(out=rb[:, :], in0=kr_b[:, sl], in1=sinS[:, sl], op=ALU.mult),
                          reads=[kr_b, sinS], writes=[rb])
                    cx.op("dve", lambda ra=ra, rb=rb, sl=sl: nc.vector.tensor_tensor(out=kr_a[:, sl], in0=ra[:, :], in1=rb[:, :], op=ALU.add),
                          reads=[ra, rb], writes=[kr_a])
                cx.dma("sp", krT_d, kr_a[:, :], reads=[kr_a])

                load_norm(24, 0)
                for h in range(8):
                    stn, strp = qn_st[h % 2], qr_st[h % 2]
                    for g in range(8):
                        sl = slice(g * 512, (g + 1) * 512)
                        p = nextp()
                        for c in range(4):
                            cx.op("pe", lambda p=p, c=c, h=h, sl=sl: nc.tensor.matmul(
                                p[:, :], wuq[:, c, h * 256:h * 256 + 128], lat[:, c, sl], start=(c == 0), stop=(c == 3)),
                                reads=[wuq, lat], pwrites=[p])
                        cx.op("act", lambda p=p, stn=stn, sl=sl: nc.scalar.copy(stn[:, sl], p[:, :]), reads=[p], pwrites=[stn])
                        p2 = nextp()
                        p3 = nextp()
                        for c in range(4):
                            cx.op("pe", lambda p2=p2, c=c, h=h, sl=sl: nc.tensor.matmul(
                                p2[0:64, :], wuq[:, c, h * 256 + 128:h * 256 + 192], lat[:, c, sl], start=(c == 0), stop=(c == 3)),
                                reads=[wuq, lat], pwrites=[p2])
                        for c in range(4):
                            cx.op("pe", lambda p3=p3, c=c, h=h, sl=sl: nc.tensor.matmul(
                                p3[0:64, :], wuq[:, c, h * 256 + 192:h * 256 + 256], lat[:, c, sl], start=(c == 0), stop=(c == 3)),
                                reads=[wuq, lat], pwrites=[p3])
                        ra, rb = rta[g % 2], rtb[g % 2]
                        cx.op("dve", lambda ra=ra, p2=p2, sl=sl: nc.vector.tensor_tensor(out=ra[:, :], in0=p2[0:64, :], in1=cos2[:, sl], op=ALU.mult),
                              reads=[p2, cos2], writes=[ra])
                        cx.op("dve", lambda rb=rb, p3=p3, sl=sl: nc.vector.tensor_tensor(out=rb[:, :], in0=p3[0:64, :], in1=sinS[:, sl], op=ALU.mult),
                              reads=[p3, sinS], writes=[rb])
                        cx.op("dve", lambda ra=ra, rb=rb, strp=strp, sl=sl: nc.vector.tensor_tensor(out=strp[:, sl], in0=ra[:, :], in1=rb[:, :], op=ALU.add),
                              reads=[ra, rb], pwrites=[strp])
                    cx.dma("sp", qnT_d[h], stn[:, :], reads=[stn])
                    cx.dma("sp", qrT_d[h], strp[:, :], reads=[strp])
                cx.barrier()
            cx.barrier()

        if upto >= 5:
          with ExitStack() as ph:
            knT = sb(ph, "knT", [128, 8, S], BF16)
            vb = sb(ph, "vb", [128, NT, 1024], BF16)
            krT = sb(ph, "krT", [64, S], BF16)
            qns = [sb(ph, "qn%d" % i, [128, 512], BF16) for i in range(2)]
            qrs = [sb(ph, "qr%d" % i, [64, 512], BF16) for i in range(2)]
            pTs = [sb(ph, "pTb%d" % i, [128, 512], BF16) for i in range(3)]
            rds = [sb(ph, "rdb%d" % i, [128, 512]) for i in range(2)]
            ybs = [sb(ph, "ybb%d" % i, [128, S], BF16) for i in range(2)]
            pss = [ps(ph, "psB%d" % i, [128, 512]) for i in range(2)]
            pos_ = [ps(ph, "poB%d" % i, [128, 512]) for i in range(2)]
            pds = [ps(ph, "pdB%d" % i, [128, 512]) for i in range(2)]
            for h in range(8):
                cx.dma("sp", knT[:, h, :], knT_d[h], pwrites=[knT])
            for a in range(4):
                cx.dma("sp", vb[:, a * 8:(a + 1) * 8, :], vbs_d[a * 1024:(a + 1) * 1024, :].rearrange("(a p) c -> p a c", p=128), pwrites=[vb])
            cx.dma("sp", krT[:, :], krT_d, writes=[krT])
            it = 0
            cnt = 0
            for h in range(8):
                yb = ybs[h % 2]
                for Q in range(8):
                    qn, qr, po, pd, rd = qns[it % 2], qrs[it % 2], pos_[it % 2], pds[it % 2], rds[it % 2]
                    it += 1
                    cx.dma("sp", qn[:, :], qnT_d[h][:, Q * 512:(Q + 1) * 512], writes=[qn])
                    cx.dma("sp", qr[:, :], qrT_d[h][:, Q * 512:(Q + 1) * 512], writes=[qr])
                    nk = 4 * (Q + 1)
                    for kt in range(nk):
                        jj = kt - 4 * Q
                        c0 = max(jj, 0) * 128
                        psm = pss[cnt % 2]
                        pT = pTs[cnt % 3]
                        cnt += 1
                        ks = slice(kt * 128, (kt + 1) * 128)
                        cx.op("pe", lambda psm=psm, qn=qn, h=h, ks=ks, c0=c0: nc.tensor.matmul(
                            psm[:, c0:512], knT[:, h, ks], qn[:, c0:512], start=True, stop=False), reads=[knT, qn], pwrites=[psm])
                        cx.op("pe", lambda psm=psm, qr=qr, ks=ks, c0=c0: nc.tensor.matmul(
                            psm[:, c0:512], krT[:, ks], qr[:, c0:512], start=False, stop=True), reads=[krT, qr], pwrites=[psm])
                        cx.op("act", lambda psm=psm, pT=pT, c0=c0: nc.scalar.activation(pT[:, c0:512], psm[:, c0:512], AF.Exp, scale=SCALE_B),
                              reads=[psm], writes=[pT])
                        if jj >= 0:
                            cx.op("dve", lambda pT=pT, c0=c0: nc.vector.memset(pT[64:128, c0:c0 + 64], 0.0), writes=[pT])
                        cx.op("pe", lambda po=po, pT=pT, h=h, kt=kt, c0=c0, nk=nk: nc.tensor.matmul(
                            po[:, c0:512], vb[:, kt, h * 128:(h + 1) * 128], pT[:, c0:512], start=(kt == 0), stop=(kt == nk - 1)),
                            reads=[vb, pT], pwrites=[po])
                        cx.op("pe", lambda pd=pd, pT=pT, kt=kt, c0=c0, nk=nk: nc.tensor.matmul(
                            pd[:, c0:512], ones_b[:, :], pT[:, c0:512], start=(kt == 0), stop=(kt == nk - 1)),
                            reads=[ones_b, pT], pwrites=[pd])
                    cx.op("dve", lambda rd=rd, pd=pd: nc.vector.reciprocal(rd[:, :], pd[:, :]), reads=[pd], writes=[rd])
                    cx.op("dve", lambda yb=yb, po=po, rd=rd, Q=Q: nc.vector.tensor_tensor(
                        out=yb[:, Q * 512:(Q + 1) * 512], in0=po[:, :], in1=rd[:, :], op=ALU.mult), reads=[po, rd], pwrites=[yb])
                cx.dma("sp", yT_d[8 + h], yb[:, :], reads=[yb])
            cx.barrier()

        if upto >= 6:
          with ExitStack() as ph:
            wpa = sb(ph, "wpa", [128, 8, D], BF16)
            wpb = sb(ph, "wpb", [128, 8, D], BF16)
            yas = [sb(ph, "ya%d" % i, [128, 8, 256], BF16) for i in range(2)]
            ybs = [sb(ph, "ybm%d" % i, [128, 8, 256], BF16) for i in range(2)]
            gts = [sb(ph, "gt%d" % i, [128, 32, 256], BF16) for i in range(2)]
            yfs = [sb(ph, "yf%d" % i, [128, 16, 256], BF16) for i in range(2)]
            tas = [sb(ph, "tam%d" % i, [128, 256]) for i in range(2)]
            tbs = [sb(ph, "tbm%d" % i, [128, 256]) for i in range(2)]
            ppa = [ps(ph, "ppa%d" % i, [128, 512]) for i in range(2)]
            ppb = [ps(ph, "ppb%d" % i, [128, 512]) for i in range(2)]
            for hh in range(2):
                cx.dma("pool", wpa[:, hh * 4:(hh + 1) * 4, :], wpa_d[hh * 512:(hh + 1) * 512, :].rearrange("(h p) c -> p h c", p=128), pwrites=[wpa])
                cx.dma("pool", wpb[:, hh * 4:(hh + 1) * 4, :], wpb_d[hh * 512:(hh + 1) * 512, :].rearrange("(h p) c -> p h c", p=128), pwrites=[wpb])
            n = 0
            for g in range(16):
                ya, yb, gt, yf = yas[g % 2], ybs[g % 2], gts[g % 2], yfs[g % 2]
                ts = slice(g * 256, (g + 1) * 256)
                cx.dma("sp", ya[:, :, :], yT_d[0:8, :, ts].rearrange("h p t -> p h t"), writes=[ya])
                cx.dma("sp", yb[:, :, :], yT_d[8:16, :, ts].rearrange("h p t -> p h t"), writes=[yb])
                for a in range(4):
                    cx.dma("sp", gt[:, a * 8:(a + 1) * 8, :], featT_d[33 + a * 8:33 + (a + 1) * 8, :, ts].rearrange("c p t -> p c t"), pwrites=[gt])
                for oc in range(16):
                    pa, pb, ta, tb = ppa[n % 2], ppb[n % 2], tas[n % 2], tbs[n % 2]
                    n += 1
                    os_ = slice(oc * 128, (oc + 1) * 128)
                    for h in range(8):
                        cx.op("pe", lambda pa=pa, ya=ya, h=h, os_=os_: nc.tensor.matmul(
                            pa[:, 0:256], wpa[:, h, os_], ya[:, h, :], start=(h == 0), stop=(h == 7)), reads=[wpa, ya], pwrites=[pa])
                    for h in range(8):
                        cx.op("pe", lambda pb=pb, yb=yb, h=h, os_=os_: nc.tensor.matmul(
                            pb[:, 0:256], wpb[:, h, os_], yb[:, h, :], start=(h == 0), stop=(h == 7)), reads=[wpb, yb], pwrites=[pb])
                    cx.op("dve", lambda ta=ta, pa=pa, gt=gt, oc=oc: nc.vector.tensor_tensor(out=ta[:, :], in0=pa[:, 0:256], in1=gt[:, oc, :], op=ALU.mult),
                          reads=[pa, gt], writes=[ta])
                    cx.op("dve", lambda tb=tb, pb=pb, gt=gt, oc=oc: nc.vector.tensor_tensor(out=tb[:, :], in0=pb[:, 0:256], in1=gt[:, 16 + oc, :], op=ALU.mult),
                          reads=[pb, gt], writes=[tb])
                    cx.op("pool", lambda yf=yf, ta=ta, tb=tb, oc=oc: nc.gpsimd.tensor_tensor(out=yf[:, oc, :], in0=ta[:, :], in1=tb[:, :], op=ALU.add),
                          reads=[ta, tb], pwrites=[yf])
                for a in range(2):
                    cx.dma("sp", yfT_d[a * 8:(a + 1) * 8, :, ts].rearrange("c p t -> p c t"), yf[:, a * 8:(a + 1) * 8, :], reads=[yf])
            cx.barrier()

        def layer_norm_tile(r, lng, lnb, dst, stats, mv, rs_t):
            for k in range(4):
                cx.op("dve", lambda k=k: nc.vector.bn_stats(stats[:, k, :], r[:, k * 512:(k + 1) * 512]), reads=[r], pwrites=[stats])
            cx.op("dve", lambda: nc.vector.bn_aggr(mv[:, :], stats[:, :, :].rearrange("p a b -> p (a b)")), reads=[stats], writes=[mv])
            cx.op("act", lambda: nc.scalar.activation(rs_t[:, 0:1], mv[:, 1:2], AF.Sqrt, bias=LN_EPS), reads=[mv], writes=[rs_t])
            cx.op("dve", lambda: nc.vector.reciprocal(rs_t[:, 1:2], rs_t[:, 0:1]), writes=[rs_t])
            cx.op("dve", lambda: nc.vector.tensor_scalar(out=r[:, :], in0=r[:, :], scalar1=mv[:, 0:1], scalar2=rs_t[:, 1:2],
                                                         op0=ALU.subtract, op1=ALU.mult), reads=[mv, rs_t], writes=[r])
            cx.op("pool", lambda: nc.gpsimd.tensor_tensor(out=r[:, :], in0=r[:, :], in1=lng[:, :], op=ALU.mult), reads=[lng], writes=[r])
            cx.op("pool", lambda: nc.gpsimd.tensor_tensor(out=dst[:, :], in0=r[:, :], in1=lnb[:, :], op=ALU.add), reads=[r, lnb], writes=[dst])

        if upto >= 7:
          with ExitStack() as ph:
            wo = sb(ph, "wo", [128, 16, D], BF16)
            lng = sb(ph, "ln1g", [128, D])
            lnb = sb(ph, "ln1b", [128, D])
            yfs = [sb(ph, "yfo%d" % i, [128, 16, 512], BF16) for i in range(2)]
            xts = [sb(ph, "xt%d" % i, [128, D]) for i in range(2)]
            rts = [sb(ph, "rt%d" % i, [128, D]) for i in range(1)]
            x1s = [sb(ph, "x1t%d" % i, [128, D]) for i in range(2)]
            h2s = [sb(ph, "h2s%d" % i, [128, 16, 512], BF16) for i in range(1)]
            stats = sb(ph, "stats", [128, 4, 6])
            mv = sb(ph, "mv", [128, 2])
            rs_t = sb(ph, "rs_t", [128, 2])
            pob = [ps(ph, "pob%d" % i, [128, 512]) for i in range(4)]
            ptb = [ps(ph, "ptb%d" % i, [128, 512]) for i in range(4)]
            for a in range(4):
                cx.dma("pool", wo[:, a * 4:(a + 1) * 4, :], wo_d[a * 512:(a + 1) * 512, :].rearrange("(k p) c -> p k c", p=128), pwrites=[wo])
            cx.dma("sp", lng[:, :], ln_d[0], writes=[lng])
            cx.dma("sp", lnb[:, :], ln_d[1], writes=[lnb])
            npt = 0
            for g in range(8):
                yf, h2b = yfs[g % 2], h2s[0]
                for a in range(2):
                    cx.dma("sp", yf[:, a * 8:(a + 1) * 8, :], yfT_d[a * 8:(a + 1) * 8, :, g * 512:(g + 1) * 512].rearrange("c p t -> p c t"), pwrites=[yf])
                for tl in range(4):
                    tt = g * 4 + tl
                    xt, r, x1t = xts[tt % 2], rts[0], x1s[tt % 2]
                    cx.dma("sp", xt[:, :], x_d[tt * 128:(tt + 1) * 128, :], writes=[xt])
                    for cg in range(4):
                        for oc in range(16):
                            cx.op("pe", lambda cg=cg, oc=oc, yf=yf, tl=tl: nc.tensor.matmul(
                                pob[cg][:, :], yf[:, oc, tl * 128:(tl + 1) * 128], wo[:, oc, cg * 512:(cg + 1) * 512],
                                start=(oc == 0), stop=(oc == 15)), reads=[yf, wo], pwrites=[pob[cg]])
                        cx.op("dve", lambda cg=cg, r=r: nc.vector.tensor_tensor(
                            out=r[:, cg * 512:(cg + 1) * 512], in0=pob[cg][:, :], in1=g1bc[:, cg * 512:(cg + 1) * 512], op=ALU.mult),
                            reads=[pob[cg], g1bc], pwrites=[r])
                    cx.op("dve", lambda r=r, xt=xt: nc.vector.scalar_tensor_tensor(
                        out=r[:, :], in0=xt[:, :], scalar=DN_ALPHA, in1=r[:, :], op0=ALU.mult, op1=ALU.add), reads=[xt], writes=[r])
                    layer_norm_tile(r, lng, lnb, x1t, stats, mv, rs_t)
                    cx.dma("sp", x1_d[tt * 128:(tt + 1) * 128, :], x1t[:, :], reads=[x1t])
                    for q4 in range(4):
                        pt = ptb[npt % 4]
                        npt += 1
                        for i in range(4):
                            dc = q4 * 4 + i
                            cx.op("pe", lambda pt=pt, x1t=x1t, i=i, dc=dc: nc.tensor.transpose(
                                pt[:, i * 128:(i + 1) * 128], x1t[:, dc * 128:(dc + 1) * 128], ident[:, :]), reads=[x1t, ident], pwrites=[pt])
                        for i in range(4):
                            dc = q4 * 4 + i
                            cx.op("act", lambda pt=pt, h2b=h2b, i=i, dc=dc, tl=tl: nc.scalar.activation(
                                h2b[:, dc, tl * 128:(tl + 1) * 128], pt[:, i * 128:(i + 1) * 128], AF.Identity,
                                bias=b2[:, dc:dc + 1], scale=s2[:, dc:dc + 1]), reads=[pt, s2, b2], pwrites=[h2b])
                for a in range(2):
                    cx.dma("sp", h2T_d[a * 8:(a + 1) * 8, :, g * 512:(g + 1) * 512].rearrange("c p t -> p c t"), h2b[:, a * 8:(a + 1) * 8, :], reads=[h2b])
            cx.barrier()

        if upto >= 8:
          with ExitStack() as ph:
            wq = sb(ph, "wq", [128, 16, D], BF16)
            keysT = sb(ph, "keysT", [128, 16, 128], BF16)
            h2g = [sb(ph, "h2g%d" % i, [128, 16, 512], BF16) for i in range(2)]
            qTs = [sb(ph, "qTs%d" % i, [128, 16, 512], BF16) for i in range(2)]
            scb = [sb(ph, "scb%d" % i, [128, 8, 2, 128]) for i in range(2)]
            sv = sb(ph, "sv", [128, 8, 2, 16])
            tmp = sb(ph, "tk_tmp", [128, 128])
            c16 = sb(ph, "c16", [128, 8, 16, 16])
            c8 = sb(ph, "c8", [128, 8, 24])
            tmp2 = sb(ph, "tk_tmp2", [128, 256])
            tmp3 = sb(ph, "tk_tmp3", [128, 256])
            d16 = sb(ph, "d16", [128, 8, 16])
            zz = sb(ph, "zz", [128, 8])
            lz = sb(ph, "lz", [128, 8])
            idx = sb(ph, "idx", [128, 8, 16], mybir.dt.uint32)
            pkb = [sb(ph, "pkb%d" % i, [128, 272]) for i in range(2)]
            pq = [ps(ph, "pq%d" % i, [128, 512]) for i in range(4)]
            psc = [ps(ph, "psc%d" % i, [128, 512]) for i in range(4)]
            for a in range(4):
                cx.dma("pool", wq[:, a * 4:(a + 1) * 4, :], wq_d[a * 512:(a + 1) * 512, :].rearrange("(k p) c -> p k c", p=128), pwrites=[wq])
            cx.dma("pool", keysT[:, :, :], keysT_d, writes=[keysT])
            npq = 0
            for g in range(8):
                hg, qt = h2g[g % 2], qTs[g % 2]
                for a in range(2):
                    cx.dma("sp", hg[:, a * 8:(a + 1) * 8, :], h2T_d[a * 8:(a + 1) * 8, :, g * 512:(g + 1) * 512].rearrange("c p t -> p c t"), pwrites=[hg])
                for hp in range(16):
                    p = pq[npq % 4]
                    npq += 1
                    for dc in range(16):
                        cx.op("pe", lambda p=p, hg=hg, dc=dc, hp=hp: nc.tensor.matmul(
                            p[:, :], wq[:, dc, hp * 128:(hp + 1) * 128], hg[:, dc, :], start=(dc == 0), stop=(dc == 15)),
                            reads=[wq, hg], pwrites=[p])
                    cx.op("act", lambda p=p, qt=qt, hp=hp: nc.scalar.copy(qt[:, hp, :], p[:, :]), reads=[p], pwrites=[qt])
                for tl in range(4):
                    tt = g * 4 + tl
                    sc = scb[tt % 2]
                    for bk in range(4):
                        for i in range(4):
                            hp = bk * 4 + i
                            cx.op("pe", lambda bk=bk, i=i, hp=hp, qt=qt, tl=tl: nc.tensor.matmul(
                                psc[bk][:, i * 128:(i + 1) * 128], qt[:, hp, tl * 128:(tl + 1) * 128], keysT[:, hp, :], start=True, stop=True),
                                reads=[qt, keysT], pwrites=[psc[bk]])
                        cx.op("act", lambda bk=bk, sc=sc: nc.scalar.copy(
                            sc[:, bk * 2:(bk + 1) * 2, :, :], psc[bk][:, :].rearrange("p (a b c) -> p a b c", a=2, b=2)),
                            reads=[psc[bk]], pwrites=[sc])
                    cx.dma("sp", scs_d[tt * 128:(tt + 1) * 128, :], sc[:, :, :, :].rearrange("p a b c -> p (a b c)"), reads=[sc])
                    for h in range(8):
                        for p_ in range(2):
                            cx.op("dve", lambda h=h, p_=p_, sc=sc: nc.vector.max(out=sv[:, h, p_, 0:8], in_=sc[:, h, p_, :]), reads=[sc], pwrites=[sv])
                            cx.op("dve", lambda h=h, p_=p_, sc=sc: nc.vector.match_replace(
                                out=tmp[:, :], in_to_replace=sv[:, h, p_, 0:8], in_values=sc[:, h, p_, :], imm_value=-1e30), reads=[sc, sv], writes=[tmp])
                            cx.op("dve", lambda h=h, p_=p_: nc.vector.max(out=sv[:, h, p_, 8:16], in_=tmp[:, :]), reads=[tmp], pwrites=[sv])
                    for h in range(8):
                        for r8 in range(2):
                            cx.op("dve", lambda h=h, r8=r8, sc=sc: nc.vector.max_index(
                                out=idx[:, h, r8 * 8:(r8 + 1) * 8], in_max=sv[:, h, 0, r8 * 8:(r8 + 1) * 8], in_values=sc[:, h, 0, :]),
                                reads=[sv, sc], pwrites=[idx])
                    cx.op("dve", lambda: nc.vector.tensor_tensor(
                        out=c16[:, :, :, :], in0=bcast(sv[:, :, 0, :], 3, [128, 8, 16, 16]), in1=bcast(sv[:, :, 1, :], 2, [128, 8, 16, 16]), op=ALU.add),
                        reads=[sv], writes=[c16])
                    for h in range(8):
                        cflat = c16[:, h, :, :].rearrange("p a b -> p (a b)")
                        cx.op("dve", lambda h=h, cflat=cflat: nc.vector.max(out=c8[:, h, 0:8], in_=cflat), reads=[c16], pwrites=[c8])
                        cx.op("dve", lambda h=h, cflat=cflat: nc.vector.match_replace(
                            out=tmp2[:, :], in_to_replace=c8[:, h, 0:8], in_values=cflat, imm_value=-1e30), reads=[c16, c8], writes=[tmp2])
                        cx.op("dve", lambda h=h: nc.vector.max(out=c8[:, h, 8:16], in_=tmp2[:, :]), reads=[tmp2], pwrites=[c8])
                        cx.op("dve", lambda h=h: nc.vector.match_replace(
                            out=tmp3[:, :], in_to_replace=c8[:, h, 8:16], in_values=tmp2[:, :], imm_value=-1e30), reads=[tmp2, c8], writes=[tmp3])
                        cx.op("dve", lambda h=h: nc.vector.max(out=c8[:, h, 16:24], in_=tmp3[:, :]), reads=[tmp3], pwrites=[c8])
                    cx.op("dve", lambda: nc.vector.tensor_tensor(out=zz[:, :], in0=c8[:, :, 15], in1=c8[:, :, 16], op=ALU.add),
                          reads=[c8], writes=[zz])
                    cx.op("dve", lambda tt=tt: nc.vector.tensor_scalar(out=prm[:, tt, 0:8], in0=zz[:, :], scalar1=0.5, scalar2=None, op0=ALU.mult),
                          reads=[zz], pwrites=[prm])
                    cx.op("dve", lambda: nc.vector.tensor_tensor(out=d16[:, :, :], in0=c8[:, :, 0:16], in1=bcast(c8[:, :, 0], 2, [128, 8, 16]), op=ALU.subtract),
                          reads=[c8], writes=[d16])
                    cx.op("act", lambda: nc.scalar.activation(d16[:, :, :], d16[:, :, :], AF.Exp), writes=[d16])
                    cx.op("dve", lambda: nc.vector.tensor_reduce(out=zz[:, :], in_=d16[:, :, :], axis=AX.X, op=ALU.add), reads=[d16], writes=[zz])
                    cx.op("act", lambda: nc.scalar.activation(lz[:, :], zz[:, :], AF.Ln), reads=[zz], writes=[lz])
                    cx.op("dve", lambda tt=tt: nc.vector.scalar_tensor_tensor(
                        out=prm[:, tt, 8:16], in0=c8[:, :, 0], scalar=-1.0, in1=lz[:, :], op0=ALU.mult, op1=ALU.subtract),
                        reads=[c8, lz], pwrites=[prm])
                    pk = pkb[tt % 2]
                    cx.op("dve", lambda pk=pk, tt=tt: nc.vector.tensor_tensor(
                        out=pk[:, 0:128].rearrange("p (h k) -> p h k", h=8), in0=sv[:, :, 0, :], in1=bcast(prm[:, tt, 8:16], 2, [128, 8, 16]), op=ALU.add),
                        reads=[sv, prm], pwrites=[pk])
                    cx.op("dve", lambda pk=pk: nc.vector.tensor_copy(pk[:, 128:256].rearrange("p (h k) -> p h k", h=8), idx[:, :, :]),
                          reads=[idx], pwrites=[pk])
                    cx.op("dve", lambda pk=pk, tt=tt: nc.vector.tensor_tensor(out=pk[:, 256:264], in0=prm[:, tt, 0:8], in1=prm[:, tt, 8:16], op=ALU.add),
                          reads=[prm], pwrites=[pk])
                    cx.op("dve", lambda pk=pk: nc.vector.memset(pk[:, 264:272], 0.0), pwrites=[pk])
                    cx.dma("sp", pk_d[tt * 128:(tt + 1) * 128, :], pk[:, :], reads=[pk])
            if dbgprm_d is not None:
                cx.dma("sp", dbgprm_d, prm[:, :, :], reads=[prm])
            cx.barrier()

        if upto >= 9:
          with ExitStack() as ph:
            lng = sb(ph, "ln2g", [128, D])
            lnb = sb(ph, "ln2b", [128, D])
            iota = sb(ph, "iota", [128, 128])
            hgs = [sb(ph, "h2p%d" % i, [128, 16, 128], BF16) for i in range(2)]
            s2bs = [sb(ph, "s2b%d" % i, [128, 8, 128]) for i in range(2)]
            pks = [sb(ph, "pkp%d" % i, [128, 272]) for i in range(2)]
            x1t = sb(ph, "x1p", [128, D])
            rt = sb(ph, "rtp", [128, D])
            cmb = sb(ph, "cmb", [128, 128, 16])
            ee = sb(ph, "eeR", [128, 2048], BF16)
            Rp = sb(ph, "Rp", [128, 128, 16], BF16)
            RT = sb(ph, "RT", [128, 128, 128], BF16)
            si1T = sb(ph, "si1T", [128, 128])
            O1T = sb(ph, "O1T", [128, 128, 32], BF16)
            GTq = sb(ph, "GTq", [128, 32, 128], BF16)
            gqs = [sb(ph, "gq%d" % i, [128, 4096], BF16) for i in range(2)]
            uts = [sb(ph, "ut%d" % i, [128, 16, 512], BF16) for i in range(2)]
            vcs = [sb(ph, "vc%d" % i, [128, 2, D], BF16) for i in range(2)]
            gas = [sb(ph, "ga%d" % i, [128, 512], BF16) for i in range(2)]
            wws = [sb(ph, "ww%d" % i, [128, 512], BF16) for i in range(2)]
            wTs = [sb(ph, "wT%d" % i, [128, 4, 128], BF16) for i in range(2)]
            stats = sb(ph, "stats2", [128, 4, 6])
            mv = sb(ph, "mv2", [128, 2])
            rs_t = sb(ph, "rs_t2", [128, 2])
            pop = [ps(ph, "pop%d" % i, [128, 512]) for i in range(4)]
            pap = [ps(ph, "pap%d" % i, [128, 512]) for i in range(2)]
            pgf = ps(ph, "pgf", [128, 512])
            pgb = ps(ph, "pgb", [128, 1024], BF16)
            cx.dma("sp", lng[:, :], ln_d[2], writes=[lng])
            cx.dma("sp", lnb[:, :], ln_d[3], writes=[lnb])
            cx.dma("sp", iota[:, :], iota_d, writes=[iota])

            def load_tile(tt):
                hg, s2b, pk = hgs[tt % 2], s2bs[tt % 2], pks[tt % 2]
                for a in range(2):
                    cx.dma("sp", hg[:, a * 8:(a + 1) * 8, :], h2T_d[a * 8:(a + 1) * 8, :, tt * 128:(tt + 1) * 128].rearrange("c p t -> p c t"), pwrites=[hg])
                cx.dma("sp", s2b[:, :, :], scs_d[tt * 128:(tt + 1) * 128, :].rearrange("t (h p n) -> t h p n", h=8, p=2)[:, :, 1, :], writes=[s2b])
                cx.dma("sp", pk[:, :], pk_d[tt * 128:(tt + 1) * 128, :], writes=[pk])

            def prep_items(tt):
                s2b, pk = s2bs[tt % 2], pks[tt % 2]
                items = []
                for p_ in range(8):
                    j0 = p_ * 16

                    def elem(j0=j0):
                        cx.op("dve", lambda: nc.vector.tensor_tensor(
                            out=cmb[:, :, :].rearrange("p (h k) j -> p h k j", h=8),
                            in0=bcast(pk[:, 0:128].rearrange("p (h k) -> p h k", h=8), 3, [128, 8, 16, 16]),
                            in1=bcast(s2b[:, :, j0:j0 + 16], 2, [128, 8, 16, 16]), op=ALU.add), reads=[pk, s2b], writes=[cmb])
                        cx.op("act", lambda: nc.scalar.activation(ee[:, :], cmb[:, :, :].rearrange("p a b -> p (a b)"), AF.Exp),
                              reads=[cmb], writes=[ee])
                        cx.op("dve", lambda: nc.vector.tensor_tensor(
                            out=Rp[:, :, :].rearrange("p (h k) j -> p h (k j)", h=8), in0=cmb[:, :, :].rearrange("p (h k) j -> p h (k j)", h=8),
                            in1=bcast(pk[:, 256:264], 2, [128, 8, 256]), op=ALU.is_ge), reads=[cmb, pk], writes=[Rp])
                        cx.op("pool", lambda: nc.gpsimd.tensor_tensor(out=Rp[:, :, :].rearrange("p a b -> p (a b)"),
                                                                      in0=Rp[:, :, :].rearrange("p a b -> p (a b)"), in1=ee[:, :], op=ALU.mult),
                              reads=[ee], writes=[Rp])
                    items.append(elem)
                    for b_ in range(2):
                        def rb(j0=j0, b_=b_):
                            for i in range(8):
                                cx.op("pe", lambda i=i: nc.tensor.transpose(pgb[:, i * 128:(i + 1) * 128], Rp[:, :, b_ * 8 + i], identb[:, :]),
                                      reads=[Rp, identb], pwrites=[pgb])
                            cx.op("act", lambda: nc.scalar.copy(
                                RT[:, :, j0 + b_ * 8:j0 + b_ * 8 + 8].rearrange("p t j -> p j t"), pgb[:, :].rearrange("p (j t) -> p j t", t=128)),
                                reads=[pgb], pwrites=[RT])
                        items.append(rb)

                def s_item():
                    cx.op("pe", lambda: nc.tensor.transpose(pgf[:, 0:128], pk[:, 128:256], ident[:, :]), reads=[pk, ident], pwrites=[pgf])
                    cx.op("act", lambda: nc.scalar.copy(si1T[:, :], pgf[:, 0:128]), reads=[pgf], writes=[si1T])
                items.append(s_item)
                return items

            def quarter_items(tt, q):
                gq = gqs[(tt * 4 + q) % 2]
                items = []

                def o1():
                    cx.op("dve", lambda: nc.vector.tensor_tensor(
                        out=O1T[:, :, :], in0=bcast(si1T[:, :], 2, [128, 128, 32]), in1=bcast(iota[:, q * 32:(q + 1) * 32], 1, [128, 128, 32]),
                        op=ALU.is_equal), reads=[si1T, iota], writes=[O1T])
                items.append(o1)
                for tb in range(8):
                    def pt(tb=tb):
                        for tl in range(16):
                            t = tb * 16 + tl
                            cx.op("pe", lambda tl=tl, t=t: nc.tensor.matmul(pgf[:, tl * 32:(tl + 1) * 32], RT[:, t, :], O1T[:, t, :], start=True, stop=True),
                                  reads=[RT, O1T], pwrites=[pgf])
                        cx.op("act", lambda: nc.scalar.copy(
                            GTq[:, :, tb * 16:(tb + 1) * 16].rearrange("p i t -> p t i"), pgf[:, :].rearrange("p (t i) -> p t i", i=32)),
                            reads=[pgf], pwrites=[GTq])
                    items.append(pt)
                for gb in range(4):
                    def gt(gb=gb):
                        for il in range(8):
                            cx.op("pe", lambda il=il: nc.tensor.transpose(pgb[:, il * 128:(il + 1) * 128], GTq[:, gb * 8 + il, :], identb[:, :]),
                                  reads=[GTq, identb], pwrites=[pgb])
                        cx.op("act", lambda: nc.scalar.copy(gq[:, gb * 1024:(gb + 1) * 1024], pgb[:, :]), reads=[pgb], pwrites=[gq])
                    items.append(gt)
                return items

            NEG = NT * 32

            def coords(EG):
                tt, eg = EG // 32, EG % 32
                return tt, eg // 8, eg % 8, eg

            def load_ut(EG):
                tt, q, e4, eg = coords(EG)
                ut = uts[EG % 2]
                cx.dma("sp", ut[:, :, :], utb_d[eg * 512:(eg + 1) * 512, :].rearrange("(p k) c -> p (k c)", k=4).rearrange("p (k c) -> p k c", c=512),
                       reads=[utb_t] if EG < 2 else (), writes=[ut])

            def load_vc(EG):
                tt, q, e4, eg = coords(EG)
                for half in range(2):
                    vc = vcs[half]
                    r0 = eg * 512 + half * 256
                    cx.dma("sp", vc[:, :, :], vb16_d[r0:r0 + 256, :].rearrange("(k p) d -> p k d", p=128),
                           reads=[vb16_t] if EG < 1 else (), writes=[vc])

            def a_mm(EG):
                tt, q, e4, eg = coords(EG)
                hg, ut, pa, ga, ww = hgs[tt % 2], uts[EG % 2], pap[EG % 2], gas[EG % 2], wws[EG % 2]
                gq = gqs[(tt * 4 + q) % 2]
                for dc in range(16):
                    cx.op("pe", lambda dc=dc: nc.tensor.matmul(pa[:, :], hg[:, dc, :], ut[:, dc, :], start=(dc == 0), stop=(dc == 15)),
                          reads=[hg, ut], pwrites=[pa])
                cx.op("act", lambda: nc.scalar.activation(ga[:, :], pa[:, :], AF.Gelu), reads=[pa], writes=[ga])
                cx.op("dve", lambda: nc.vector.tensor_tensor(out=ww[:, :], in0=ga[:, :], in1=gq[:, e4 * 512:(e4 + 1) * 512], op=ALU.mult),
                      reads=[ga, gq], writes=[ww])

            def wtr(EG):
                ww, wT = wws[EG % 2], wTs[EG % 2]
                for k in range(4):
                    cx.op("pe", lambda k=k: nc.tensor.transpose(pgb[:, k * 128:(k + 1) * 128], ww[:, k * 128:(k + 1) * 128], identb[:, :]),
                          reads=[ww, identb], pwrites=[pgb])
                cx.op("act", lambda: nc.scalar.copy(wT[:, :, :], pgb[:, 0:512].rearrange("p (a b) -> p a b", b=128)), reads=[pgb], writes=[wT])

            def ph2(EG):
                tt, q, e4, eg = coords(EG)
                wT = wTs[EG % 2]
                for k in range(4):
                    vc = vcs[k // 2]
                    for dq in range(4):
                        cx.op("pe", lambda k=k, dq=dq, vc=vc: nc.tensor.matmul(
                            pop[dq][:, :], wT[:, k, :], vc[:, k % 2, dq * 512:(dq + 1) * 512],
                            start=(eg == 0 and k == 0), stop=(eg == 31 and k == 3)), reads=[wT, vc], pwrites=[pop[dq]])

            def epilogue(tt):
                for dq in range(4):
                    cx.op("dve", lambda dq=dq: nc.vector.tensor_tensor(
                        out=rt[:, dq * 512:(dq + 1) * 512], in0=pop[dq][:, :], in1=g2bc[:, dq * 512:(dq + 1) * 512], op=ALU.mult),
                        reads=[pop[dq], g2bc], pwrites=[rt])
                cx.op("dve", lambda: nc.vector.scalar_tensor_tensor(
                    out=rt[:, :], in0=x1t[:, :], scalar=DN_ALPHA, in1=rt[:, :], op0=ALU.mult, op1=ALU.add), reads=[x1t], writes=[rt])
                layer_norm_tile(rt, lng, lnb, x1t, stats, mv, rs_t)
                cx.dma("sp", out_d[tt * 128:(tt + 1) * 128, :], x1t[:, :], reads=[x1t])

            load_tile(0)
            for it in prep_items(0):
                it()
            for it in quarter_items(0, 0):
                it()
            cx.dma("sp", x1t[:, :], x1_d[0:128, :], writes=[x1t])
            load_ut(0)
            load_ut(1)
            load_vc(0)
            a_mm(0)
            for EG in range(NEG):
                tt, q, e4, eg = coords(EG)
                if e4 == 0:
                    queue = []
                    if q < 2:
                        queue = quarter_items(tt, q + 1)
                    elif q == 2:
                        queue = quarter_items(tt, 3)
                        if tt + 1 < NT:
                            load_tile(tt + 1)
                            nxt = prep_items(tt + 1)
                            queue = queue + nxt[:12]
                            carry = nxt[12:]
                    else:
                        if tt + 1 < NT:
                            queue = carry + quarter_items(tt + 1, 0)
                    per_slot = -(-len(queue) // 8)
                n_pop = per_slot if e4 < 7 else len(queue)
                for _ in range(min(n_pop, len(queue))):
                    queue.pop(0)()
                if EG + 2 < NEG:
                    load_ut(EG + 2)
                if EG + 1 < NEG:
                    a_mm(EG + 1)
                wtr(EG)
                if EG >= 1:
                    ph2(EG - 1)
                    if (EG - 1) % 32 == 31:
                        epilogue((EG - 1) // 32)
                        cx.dma("sp", x1t[:, :], x1_d[tt * 128:(tt + 1) * 128, :], writes=[x1t])
                load_vc(EG) if EG >= 1 else None
            ph2(NEG - 1)
            epilogue(NT - 1)
            cx.barrier()
        cx.barrier()
        print("[kernel] instructions emitted:", cx.nins)
    return nc


def host_layout(inputs):
    f = lambda k: np.asarray(inputs[k])
    shared = {}
    w_in = f("w_in")[0]
    b_in = f("b_in")[0]
    perm = np.concatenate([np.arange(32, 64), np.arange(0, 32)])
    cols = np.concatenate([np.arange(0, 4160), 4096 + perm, np.arange(4160, 8256)])
    wext = w_in[:, cols]
    shared["w_in_l"] = np.ascontiguousarray(wext.reshape(16, 128, 65, 128).transpose(2, 1, 0, 3).reshape(65, 128, 2048))
    shared["b_inT"] = np.ascontiguousarray(b_in[cols].reshape(65, 128).T)
    shared["w_ada"] = np.ascontiguousarray(f("w_ada")[0])
    b_ada = f("b_ada")[0]
    shared["b_adaT"] = np.ascontiguousarray(b_ada.reshape(96, 128).T)
    shared["b_ada_row"] = np.ascontiguousarray(b_ada.reshape(1, -1))
    rb = f("rel_bias")[0]
    p = np.arange(128)[:, None, None]
    j = np.arange(5)[None, :, None]
    c = np.arange(128)[None, None, :]
    rel = 128 * (4 - j) + c - p
    idx = np.clip(rel, -63, 256) + 63
    shared["biasT"] = np.ascontiguousarray(rb[:, idx].transpose(1, 0, 2, 3).reshape(128, 8, 640))
    mask = np.zeros((128, 5, 128), np.float32)
    mask[:64, 0, 64:] = NEGM
    mask[64:, 4, :64] = NEGM
    shared["maskA"] = mask.reshape(128, 640)
    shared["gqT"] = np.ascontiguousarray(f("q_norm_g")[0].reshape(4, 128).T)
    shared["gkvT"] = np.ascontiguousarray(f("kv_norm_g")[0].reshape(4, 128).T)
    w_uq = f("w_uq")[0]
    qcols = []
    for h in range(8):
        base = h * 192
        qcols += [np.arange(base, base + 128), np.arange(base + 128, base + 192), base + 128 + perm]
    qcols = np.concatenate(qcols)
    shared["w_uq_l"] = np.ascontiguousarray(w_uq[:, qcols].reshape(4, 128, 2048).transpose(1, 0, 2))
    w_ukv = f("w_ukv")[0].reshape(512, 8, 256)
    shared["w_uk_l"] = np.ascontiguousarray(w_ukv[:, :, :128].reshape(4, 128, 1024).transpose(1, 0, 2))
    shared["w_uv_l"] = np.ascontiguousarray(w_ukv[:, :, 128:].reshape(4, 128, 1024).transpose(1, 0, 2))
    shared["w_pa"] = np.ascontiguousarray(f("w_pa")[0])
    shared["w_pb"] = np.ascontiguousarray(f("w_pb")[0])
    shared["w_o"] = np.ascontiguousarray(f("w_o")[0])
    shared["ln_bc"] = np.ascontiguousarray(np.stack([np.broadcast_to(f(k)[0][None, :], (128, D)) for k in ("ln1_g", "ln1_b", "ln2_g", "ln2_b")]))
    shared["peer_wq"] = np.ascontiguousarray(f("peer_wq")[0])
    keys = f("peer_keys")[0]
    shared["keysT"] = np.ascontiguousarray(keys.reshape(16, 128, 128).transpose(2, 0, 1))
    U = f("peer_u")[0]
    shared["ut_l"] = np.ascontiguousarray(U.reshape(32, 512, 16, 128).transpose(0, 3, 2, 1)).reshape(16384, 2048)
    shared["peer_v"] = np.ascontiguousarray(f("peer_v")[0])
    shared["ident"] = np.eye(128, dtype=np.float32)
    shared["iota"] = np.ascontiguousarray(np.broadcast_to(np.arange(128, dtype=np.float32)[None, :], (128, 128)))
    inv_freq = (10000.0 ** (-np.arange(0, 64, 2, dtype=np.float32) / 64.0)).astype(np.float32)
    invf = np.zeros((64, 2), np.float32)
    invf[:, 0] = np.concatenate([inv_freq, inv_freq])
    invf[:32, 1] = -1.0
    invf[32:, 1] = 1.0
    shared["invf"] = invf
    per_core = []
    x = f("x")
    cc = f("c")
    pos = f("positions")
    for b in range(NCORES):
        m = dict(shared)
        m["x"] = np.ascontiguousarray(x[b])
        m["cT"] = np.ascontiguousarray(cc[b].reshape(16, 128).T)
        m["posb"] = np.ascontiguousarray(np.broadcast_to(pos[b][None, :].astype(np.int32), (64, S)))
        per_core.append(m)
    return per_core


_NC_CACHE = {}


def kernel(**inputs):
    maps = host_layout(inputs)
    if "full" not in _NC_CACHE:
        _NC_CACHE["full"] = build()
    nc = _NC_CACHE["full"]
    res = run_bass_kernel_spmd(nc, maps, core_ids=list(range(NCORES)))
    out = np.stack([np.asarray(r["out"]) for r in res.results], axis=0)
    return out.astype(np.float32)
```

```python
import math
from contextlib import ExitStack

import numpy as np
import concourse.bass as bass
import concourse.mybir as mybir
from concourse.bass_utils import run_bass_kernel_spmd

F32 = mybir.dt.float32
BF16 = mybir.dt.bfloat16
I32 = mybir.dt.int32
AF = mybir.ActivationFunctionType
ALU = mybir.AluOpType
AX = mybir.AxisListType

NCORES = 8
S = 4096
D = 2048
NT = S // 128
DN_ALPHA = 2.0 ** 0.25
LN_EPS = 1e-5
RMS_EPS = 1e-6
SCALE_A = 128.0 ** -0.5
SCALE_B = 192.0 ** -0.5
NEGM = -30000.0
MAGIC = 12582912.0
TWO_PI = 2.0 * math.pi
C1 = 6.28125
C2 = TWO_PI - C1
PI_SAFE = 3.14159

SAME_ENG_SYNC = True
CB_ON_POOL = False


class Buf:
    __slots__ = ("w", "r")

    def __init__(self):
        self.w = {}
        self.r = {}


class T:
    def __init__(self, handle):
        self.t = handle
        self.b = Buf()

    def __getitem__(self, k):
        return self.t[k]


class Ctx:
    def __init__(self, nc, es):
        self.nc = nc
        self.eng = {"pe": nc.tensor, "act": nc.scalar, "dve": nc.vector, "pool": nc.gpsimd, "sp": nc.sync}
        self.sems = {}
        self.tot = {}
        for e in ("pe", "act", "dve", "pool"):
            self.sems[e] = es.enter_context(nc.semaphore("s_" + e))
            self.tot[e] = 0
        self.dq = {}
        for q, n in (("sp", 16), ("pool", 8)):
            lst = []
            for i in range(n):
                k = "d_%s%d" % (q, i)
                self.sems[k] = es.enter_context(nc.semaphore(k))
                self.tot[k] = 0
                lst.append(k)
            self.dq[q] = [lst, 0]
        self.seen = {e: {} for e in self.eng}
        self.nins = 0

    def _wait(self, e, deps):
        own = e if e in ("pe", "act", "dve", "pool") else None
        seen = self.seen[e]
        for k, v in deps.items():
            if v <= 0:
                continue
            if k == own and (own == "pe" or not SAME_ENG_SYNC):
                continue
            if seen.get(k, 0) >= v:
                continue
            self.eng[e].wait_ge(self.sems[k], v)
            seen[k] = v

    @staticmethod
    def _merge(d, k, v):
        if d.get(k, 0) < v:
            d[k] = v

    def _deps(self, reads, writes, pwrites):
        deps = {}
        for t in reads:
            for k, v in t.b.w.items():
                self._merge(deps, k, v)
        for t in writes:
            for k, v in t.b.w.items():
                self._merge(deps, k, v)
            for k, v in t.b.r.items():
                self._merge(deps, k, v)
        for t in pwrites:
            for k, v in t.b.r.items():
                self._merge(deps, k, v)
        return deps

    def _update(self, tok, reads, writes, pwrites):
        k, v = tok
        for t in writes:
            t.b.w = {k: v}
            t.b.r = {}
        for t in pwrites:
            if t.b.r:
                t.b.w = {k: v}
                t.b.r = {}
            else:
                self._merge(t.b.w, k, v)
        for t in reads:
            self._merge(t.b.r, k, v)

    def op(self, e, fn, reads=(), writes=(), pwrites=()):
        self._wait(e, self._deps(reads, writes, pwrites))
        ins = fn()
        self.tot[e] += 1
        ins.then_inc(self.sems[e], 1)
        self.nins += 1
        self._update((e, self.tot[e]), reads, writes, pwrites)
        return ins

    def dma(self, q, out, in_, reads=(), writes=(), pwrites=()):
        lst, i = self.dq[q]
        k = lst[i % len(lst)]
        self.dq[q][1] = i + 1
        deps = self._deps(reads, writes, pwrites)
        self._merge(deps, k, self.tot[k])
        self._wait(q, deps)
        ins = self.eng[q].dma_start(out=out, in_=in_)
        self.tot[k] += 16
        ins.then_inc(self.sems[k], 16)
        self.nins += 1
        self._update((k, self.tot[k]), reads, writes, pwrites)
        return ins

    def barrier(self, engines=("pe", "act", "dve", "pool", "sp")):
        for e in engines:
            self._wait(e, dict(self.tot))


def bcast(ap, axis, shape):
    return ap.unsqueeze(axis).broadcast_to(shape)


def build(upto=99, dbg=()):
    nc = bass.Bass("TRN2", target_bir_lowering=False)

    def din(name, shape, dt=F32):
        return nc.dram_tensor(name, list(shape), dt, kind="ExternalInput").ap()

    def dscr(name, shape, dt):
        kind = "ExternalOutput" if name in dbg else "Internal"
        return nc.dram_tensor(name, list(shape), dt, kind=kind).ap()

    x_d = din("x", [S, D])
    cT_d = din("cT", [128, 16])
    pos_d = din("posb", [64, S], I32)
    invf_d = din("invf", [64, 2])
    wada_d = din("w_ada", [D, 6 * D])
    badaT_d = din("b_adaT", [128, 96])
    badar_d = din("b_ada_row", [1, 6 * D])
    win_d = din("w_in_l", [65, 128, 2048])
    binT_d = din("b_inT", [128, 65])
    biasT_d = din("biasT", [128, 8, 640])
    maskA_d = din("maskA", [128, 640])
    gq_d = din("gqT", [128, 4])
    gkv_d = din("gkvT", [128, 4])
    wuq_d = din("w_uq_l", [128, 4, 2048])
    wuk_d = din("w_uk_l", [128, 4, 1024])
    wuv_d = din("w_uv_l", [128, 4, 1024])
    wpa_d = din("w_pa", [1024, D])
    wpb_d = din("w_pb", [1024, D])
    wo_d = din("w_o", [D, D])
    ln_d = din("ln_bc", [4, 128, D])
    wq_d = din("peer_wq", [D, D])
    keysT_d = din("keysT", [128, 16, 128])
    ut_d = din("ut_l", [16384, 2048])
    v_d = din("peer_v", [16384, D])
    ident_d = din("ident", [128, 128])
    iota_d = din("iota", [128, 128])

    out_d = nc.dram_tensor("out", [S, D], F32, kind="ExternalOutput").ap()

    featT_d = dscr("featT", [65, 128, S], BF16)
    yT_d = dscr("yT", [16, 128, S], BF16)
    qnT_d = dscr("qnT", [8, 128, S], BF16)
    qrT_d = dscr("qrT", [8, 64, S], BF16)
    knT_d = dscr("knT", [8, 128, S], BF16)
    krT_d = dscr("krT", [64, S], BF16)
    vbs_d = dscr("vbs", [S, 1024], BF16)
    yfT_d = dscr("yfT", [16, 128, S], BF16)
    x1_d = dscr("x1s", [S, D], F32)
    h2T_d = dscr("h2T", [16, 128, S], BF16)
    scs_d = dscr("scs", [S, 2048], F32)
    pk_d = dscr("pk", [S, 272], F32)
    utb_d = dscr("utb", [16384, 2048], BF16)
    vb16_d = dscr("vb16", [16384, D], BF16)
    dbgmod_d = dscr("dbgmod", [128, 96 + 2], F32) if "dbgmod" in dbg else None
    dbgg_d = dscr("dbgg", [128, 2 * D], F32) if "dbgg" in dbg else None
    dbgprm_d = dscr("dbgprm", [128, NT, 16], F32) if "dbgprm" in dbg else None

    with ExitStack() as es:
        cx = Ctx(nc, es)

        uid = [0]

        def sb(scope, name, shape, dt=F32):
            uid[0] += 1
            return T(scope.enter_context(nc.sbuf_tensor("sb%d_%s" % (uid[0], name), list(shape), dt)))

        def ps(scope, name, shape, dt=F32):
            uid[0] += 1
            return T(scope.enter_context(nc.psum_tensor("ps%d_%s" % (uid[0], name), list(shape), dt)))

        CB_ENG = "pool" if CB_ON_POOL else "dve"
        CB_OBJ = nc.gpsimd if CB_ON_POOL else nc.vector
        ident = sb(es, "ident", [128, 128])
        identb = sb(es, "identb", [128, 128], BF16)
        ones_f = sb(es, "ones_f", [128, 128])
        ones_b = sb(es, "ones_b", [128, 128], BF16)
        s1 = sb(es, "s1", [128, 16])
        b1 = sb(es, "b1", [128, 16])
        s2 = sb(es, "s2", [128, 16])
        b2 = sb(es, "b2", [128, 16])
        g1bc = sb(es, "g1bc", [128, D])
        g2bc = sb(es, "g2bc", [128, D])
        prm = sb(es, "prm", [128, NT, 16])

        cx.dma("sp", ident[:, :], ident_d, writes=[ident])
        cx.op("dve", lambda: nc.vector.tensor_copy(identb[:, :], ident[:, :]), reads=[ident], writes=[identb])
        cx.op("dve", lambda: nc.vector.memset(ones_f[:, :], 1.0), writes=[ones_f])
        cx.op("dve", lambda: nc.vector.memset(ones_b[:, :], 1.0), writes=[ones_b])

        utb_t = T(None)
        vb16_t = T(None)

        def cast_tables(i):
            if upto < 6 or i >= 64:
                return
            if i < 32:
                cx.dma("pool", utb_d[i * 512:(i + 1) * 512, :], ut_d[i * 512:(i + 1) * 512, :], pwrites=[utb_t])
            else:
                i -= 32
                cx.dma("pool", vb16_d[i * 512:(i + 1) * 512, :], v_d[i * 512:(i + 1) * 512, :], pwrites=[vb16_t])

        with ExitStack() as ph:
            cT = sb(ph, "cT", [128, 16])
            scT = sb(ph, "scT", [128, 16])
            badaT = sb(ph, "badaT", [128, 96])
            badar = sb(ph, "badar", [1, 6 * D])
            modT = sb(ph, "modT", [128, 96])
            wst = [sb(ph, "wst%d" % i, [128, 16, 512]) for i in range(2)]
            rowsb = [sb(ph, "rowsb%d" % i, [1, 512]) for i in range(2)]
            pm = ps(ph, "pm", [128, 512])
            pr = [ps(ph, "pr%d" % i, [128, 512]) for i in range(2)]
            pbc = [ps(ph, "pbc%d" % i, [128, 512]) for i in range(2)]
            cx.dma("sp", cT[:, :], cT_d, writes=[cT])
            cx.dma("sp", badaT[:, :], badaT_d, writes=[badaT])
            cx.dma("sp", badar[:, :], badar_d, writes=[badar])
            cx.op("act", lambda: nc.scalar.activation(scT[:, :], cT[:, :], AF.Silu), reads=[cT], writes=[scT])
            nrow = 0
            for gi in range(24):
                m = gi // 4
                w = wst[gi % 2]
                cx.dma("sp", w[:, :, :], wada_d[:, gi * 512:(gi + 1) * 512].rearrange("(k p) c -> p k c", p=128), writes=[w])
                if m in (0, 1, 3, 4):
                    for cc in range(4):
                        col = m * 16 + (gi % 4) * 4 + cc
                        for kc in range(16):
                            cx.op("pe", lambda kc=kc, cc=cc, col=col, w=w: nc.tensor.matmul(
                                pm[:, col:col + 1], w[:, kc, cc * 128:(cc + 1) * 128], scT[:, kc:kc + 1],
                                start=(kc == 0), stop=(kc == 15)), reads=[w, scT], pwrites=[pm])
                else:
                    p_r = pr[nrow % 2]
                    rs = rowsb[nrow % 2]
                    p_b = pbc[nrow % 2]
                    nrow += 1
                    for kc in range(16):
                        cx.op("pe", lambda kc=kc, w=w, p_r=p_r: nc.tensor.matmul(
                            p_r[0:1, :], scT[:, kc:kc + 1], w[:, kc, :], start=(kc == 0), stop=(kc == 15)),
                            reads=[w, scT], pwrites=[p_r])
                    cx.op("dve", lambda p_r=p_r, rs=rs, gi=gi: nc.vector.tensor_tensor(
                        out=rs[0:1, :], in0=p_r[0:1, :], in1=badar[0:1, gi * 512:(gi + 1) * 512], op=ALU.add),
                        reads=[p_r, badar], writes=[rs])
                    cx.op("pe", lambda rs=rs, p_b=p_b: nc.tensor.matmul(
                        p_b[:, :], ones_f[0:1, :], rs[0:1, :], start=True, stop=True), reads=[rs, ones_f], pwrites=[p_b])
                    dst = g1bc if m == 2 else g2bc
                    cx.op("act", lambda dst=dst, p_b=p_b, gi=gi: nc.scalar.copy(
                        dst[:, (gi % 4) * 512:(gi % 4 + 1) * 512], p_b[:, :]), reads=[p_b], pwrites=[dst])
            cx.op("dve", lambda: nc.vector.tensor_tensor(out=modT[:, :], in0=pm[:, 0:96], in1=badaT[:, :], op=ALU.add),
                  reads=[pm, badaT], writes=[modT])
            cx.op("dve", lambda: nc.vector.tensor_scalar(out=s1[:, :], in0=modT[:, 16:32], scalar1=1.0, scalar2=None, op0=ALU.add),
                  reads=[modT], writes=[s1])
            cx.op("dve", lambda: nc.vector.tensor_copy(b1[:, :], modT[:, 0:16]), reads=[modT], writes=[b1])
            cx.op("dve", lambda: nc.vector.tensor_scalar(out=s2[:, :], in0=modT[:, 64:80], scalar1=1.0, scalar2=None, op0=ALU.add),
                  reads=[modT], writes=[s2])
            cx.op("dve", lambda: nc.vector.tensor_copy(b2[:, :], modT[:, 48:64]), reads=[modT], writes=[b2])
            if dbgmod_d is not None:
                cx.dma("sp", dbgmod_d[:, 0:96], modT[:, :], reads=[modT])
            if dbgg_d is not None:
                cx.dma("sp", dbgg_d[:, 0:D], g1bc[:, :], reads=[g1bc])
                cx.dma("sp", dbgg_d[:, D:2 * D], g2bc[:, :], reads=[g2bc])
            cx.barrier()

        if upto >= 1:
          with ExitStack() as ph12:
            hT = sb(ph12, "hT", [128, 16, S], BF16)
            with ExitStack() as ph:
                xs = [sb(ph, "xs%d" % i, [128, 2, D]) for i in range(2)]
                ptr = [ps(ph, "ptr%d" % i, [128, 512]) for i in range(4)]
                n = 0
                for g in range(16):
                    xb = xs[g % 2]
                    cx.dma("sp", xb[:, :, :], x_d[g * 256:(g + 1) * 256, :].rearrange("(j p) d -> p j d", p=128), writes=[xb])
                    for dc in range(16):
                        pt = ptr[n % 4]
                        n += 1
                        for j in range(2):
                            cx.op("pe", lambda pt=pt, xb=xb, j=j, dc=dc: nc.tensor.transpose(
                                pt[:, j * 128:(j + 1) * 128], xb[:, j, dc * 128:(dc + 1) * 128], ident[:, :]),
                                reads=[xb, ident], pwrites=[pt])
                        cx.op("act", lambda pt=pt, g=g, dc=dc: nc.scalar.activation(
                            hT[:, dc, g * 256:(g + 1) * 256], pt[:, 0:256], AF.Identity,
                            bias=b1[:, dc:dc + 1], scale=s1[:, dc:dc + 1]), reads=[pt, s1, b1], pwrites=[hT])
                cx.barrier()
            with ExitStack() as ph:
                wc = [sb(ph, "wc%d" % i, [128, 16, 128], BF16) for i in range(3)]
                ost = [sb(ph, "ost%d" % i, [128, S], BF16) for i in range(2)]
                binT = sb(ph, "binT", [128, 65])
                pz = [ps(ph, "pz%d" % i, [128, 512]) for i in range(4)]
                cx.dma("sp", binT[:, :], binT_d, writes=[binT])
                n = 0
                for ch in range(65):
                    w = wc[ch % 3]
                    cx.dma("pool", w[:, :, :], win_d[ch].rearrange("p (k c) -> p k c", c=128), writes=[w])
                    if ch >= 2:
                        cast_tables(ch - 2)
                        if ch == 64:
                            cast_tables(63)
                    o = ost[ch % 2]
                    func = AF.Sigmoid if ch >= 33 else AF.Identity
                    for g in range(8):
                        p = pz[n % 4]
                        n += 1
                        for kc in range(16):
                            cx.op("pe", lambda p=p, w=w, kc=kc, g=g: nc.tensor.matmul(
                                p[:, :], w[:, kc, :], hT[:, kc, g * 512:(g + 1) * 512], start=(kc == 0), stop=(kc == 15)),
                                reads=[w, hT], pwrites=[p])
                        cx.op("act", lambda p=p, o=o, g=g, ch=ch, func=func: nc.scalar.activation(
                            o[:, g * 512:(g + 1) * 512], p[:, :], func, bias=binT[:, ch:ch + 1]),
                            reads=[p, binT], pwrites=[o])
                    cx.dma("sp", featT_d[ch], o[:, :], reads=[o])
                cx.barrier()

        if upto >= 3:
          with ExitStack() as ph:
            biasT = sb(ph, "biasT", [128, 8, 640])
            maskA = sb(ph, "maskA", [128, 640])
            qTs = [sb(ph, "qT%d" % i, [128, S], BF16) for i in range(2)]
            kTs = [sb(ph, "kT%d" % i, [128, S], BF16) for i in range(2)]
            vTs = [sb(ph, "vT%d" % i, [128, S], BF16) for i in range(2)]
            vas = [sb(ph, "va%d" % i, [128, NT, 128], BF16) for i in range(2)]
            ybs = [sb(ph, "yb%d" % i, [128, S], BF16) for i in range(2)]
            t1s = [sb(ph, "t1_%d" % i, [128, 640]) for i in range(2)]
            pTs = [sb(ph, "pT%d" % i, [128, 640], BF16) for i in range(2)]
            rds = [sb(ph, "rd%d" % i, [128, 128]) for i in range(2)]
            pss = [ps(ph, "psA%d" % i, [128, 1024]) for i in range(2)]
            pos_ = [ps(ph, "poA%d" % i, [128, 512]) for i in range(2)]
            ptr = [ps(ph, "ptA%d" % i, [128, 1024], BF16) for i in range(2)]
            cx.dma("sp", biasT[:, :, :], biasT_d, writes=[biasT])
            cx.dma("sp", maskA[:, :], maskA_d, writes=[maskA])
            for h in range(8):
                cx.op("dve", lambda h=h: nc.vector.tensor_tensor(out=biasT[:, h, :], in0=biasT[:, h, :], in1=maskA[:, :], op=ALU.add),
                      reads=[maskA], writes=[biasT])
            nt = 0
            for h in range(8):
                qT, kT, vT, va, yb = qTs[h % 2], kTs[h % 2], vTs[h % 2], vas[h % 2], ybs[h % 2]
                cx.dma("sp", qT[:, :], featT_d[h], writes=[qT])
                cx.dma("sp", kT[:, :], featT_d[8 + h], writes=[kT])
                cx.dma("sp", vT[:, :], featT_d[16 + h], writes=[vT])
                for blk in range(4):
                    pt = ptr[nt % 2]
                    nt += 1
                    for i in range(8):
                        tl = blk * 8 + i
                        cx.op("pe", lambda pt=pt, vT=vT, i=i, tl=tl: nc.tensor.transpose(
                            pt[:, i * 128:(i + 1) * 128], vT[:, tl * 128:(tl + 1) * 128], identb[:, :]),
                            reads=[vT, identb], pwrites=[pt])
                    cx.op("act", lambda pt=pt, va=va, blk=blk: nc.scalar.copy(
                        va[:, blk * 8:(blk + 1) * 8, :], pt[:, :].rearrange("p (a b) -> p a b", b=128)),
                        reads=[pt], pwrites=[va])
                for m in range(NT):
                    j0 = max(0, 4 - m)
                    lo = j0 * 128
                    psm, t1, pT, po, rd = pss[m % 2], t1s[m % 2], pTs[m % 2], pos_[m % 2], rds[m % 2]
                    for j in range(j0, 5):
                        kt = m - 4 + j
                        cx.op("pe", lambda psm=psm, kT=kT, qT=qT, j=j, kt=kt, m=m: nc.tensor.matmul(
                            psm[:, j * 128:(j + 1) * 128], kT[:, kt * 128:(kt + 1) * 128], qT[:, m * 128:(m + 1) * 128],
                            start=True, stop=True), reads=[kT, qT], pwrites=[psm])
                    cx.op("dve", lambda psm=psm, t1=t1, h=h, lo=lo: nc.vector.scalar_tensor_tensor(
                        out=t1[:, lo:640], in0=psm[:, lo:640], scalar=SCALE_A, in1=biasT[:, h, lo:640],
                        op0=ALU.mult, op1=ALU.add), reads=[psm, biasT], writes=[t1])
                    cx.op("act", lambda t1=t1, pT=pT, lo=lo: nc.scalar.activation(pT[:, lo:640], t1[:, lo:640], AF.Exp),
                          reads=[t1], writes=[pT])
                    for j in range(j0, 5):
                        kt = m - 4 + j
                        cx.op("pe", lambda po=po, va=va, pT=pT, j=j, kt=kt, j0=j0: nc.tensor.matmul(
                            po[:, 0:128], va[:, kt, :], pT[:, j * 128:(j + 1) * 128], start=(j == j0), stop=(j == 4)),
                            reads=[va, pT], pwrites=[po])
                    for j in range(j0, 5):
                        cx.op("pe", lambda po=po, pT=pT, j=j, j0=j0: nc.tensor.matmul(
                            po[:, 128:256], ones_b[:, :], pT[:, j * 128:(j + 1) * 128], start=(j == j0), stop=(j == 4)),
                            reads=[ones_b, pT], pwrites=[po])
                    cx.op("dve", lambda rd=rd, po=po: nc.vector.reciprocal(rd[:, :], po[:, 128:256]), reads=[po], writes=[rd])
                    cx.op("dve", lambda yb=yb, po=po, rd=rd, m=m: nc.vector.tensor_tensor(
                        out=yb[:, m * 128:(m + 1) * 128], in0=po[:, 0:128], in1=rd[:, :], op=ALU.mult),
                        reads=[po, rd], pwrites=[yb])
                cx.dma("sp", yT_d[h], yb[:, :], reads=[yb])
            cx.barrier()

        if upto >= 4:
          with ExitStack() as ph:
            cos2 = sb(ph, "cos2", [64, S])
            sinS = sb(ph, "sinS", [64, S])
            invf = sb(ph, "invf", [64, 2])
            cx.dma("sp", invf[:, :], invf_d, writes=[invf])
            with ExitStack() as ph2:
                posi = sb(ph2, "posi", [64, S], I32)
                ang = sb(ph2, "ang", [64, S])
                ta = sb(ph2, "ta", [64, S])
                tb = sb(ph2, "tb", [64, S])
                cx.dma("sp", posi[:, :], pos_d, writes=[posi])
                cx.op("dve", lambda: nc.vector.tensor_copy(ta[:, :], posi[:, :]), reads=[posi], writes=[ta])
                cx.op("dve", lambda: nc.vector.tensor_scalar(out=ang[:, :], in0=ta[:, :], scalar1=invf[:, 0:1], scalar2=None, op0=ALU.mult),
                      reads=[ta, invf], writes=[ang])
                for dst, shift, use_sgn in ((sinS, 0.0, True), (cos2, math.pi / 2.0, False)):
                    src = ang
                    if shift != 0.0:
                        cx.op("dve", lambda: nc.vector.tensor_scalar(out=ta[:, :], in0=ang[:, :], scalar1=shift, scalar2=None, op0=ALU.add),
                              reads=[ang], writes=[ta])
                        src = ta
                    else:
                        cx.op("dve", lambda: nc.vector.tensor_copy(ta[:, :], ang[:, :]), reads=[ang], writes=[ta])
                        src = ta
                    cx.op("dve", lambda: nc.vector.tensor_scalar(out=tb[:, :], in0=ta[:, :], scalar1=1.0 / TWO_PI, scalar2=None, op0=ALU.mult),
                          reads=[ta], writes=[tb])
                    cx.op("dve", lambda: nc.vector.tensor_scalar(out=tb[:, :], in0=tb[:, :], scalar1=MAGIC, scalar2=None, op0=ALU.add),
                          reads=[tb], writes=[tb])
                    cx.op("dve", lambda: nc.vector.tensor_scalar(out=tb[:, :], in0=tb[:, :], scalar1=-MAGIC, scalar2=None, op0=ALU.add),
                          reads=[tb], writes=[tb])
                    cx.op("dve", lambda: nc.vector.scalar_tensor_tensor(out=ta[:, :], in0=tb[:, :], scalar=-C1, in1=ta[:, :], op0=ALU.mult, op1=ALU.add),
                          reads=[tb, ta], writes=[ta])
                    cx.op("dve", lambda: nc.vector.scalar_tensor_tensor(out=ta[:, :], in0=tb[:, :], scalar=-C2, in1=ta[:, :], op0=ALU.mult, op1=ALU.add),
                          reads=[tb, ta], writes=[ta])
                    cx.op("dve", lambda: nc.vector.tensor_scalar(out=ta[:, :], in0=ta[:, :], scalar1=PI_SAFE, scalar2=-PI_SAFE, op0=ALU.min, op1=ALU.max),
                          reads=[ta], writes=[ta])
                    if use_sgn:
                        cx.op("act", lambda dst=dst: nc.scalar.activation(dst[:, :], ta[:, :], AF.Sin, scale=invf[:, 1:2]),
                              reads=[ta, invf], writes=[dst])
                    else:
                        cx.op("act", lambda dst=dst: nc.scalar.activation(dst[:, :], ta[:, :], AF.Sin), reads=[ta], writes=[dst])
                cx.barrier()

            with ExitStack() as ph2:
                lat = sb(ph2, "lat", [128, 4, S], BF16)
                sq = [sb(ph2, "sq%d" % i, [128, 4, 512], BF16) for i in range(2)]
                tmpf = [sb(ph2, "tmpf%d" % i, [128, 512]) for i in range(2)]
                rstd = sb(ph2, "rstd", [128, S])
                gn = sb(ph2, "gn", [128, 8])
                wuq = sb(ph2, "wuq", [128, 4, 2048], BF16)
                wuk = sb(ph2, "wuk", [128, 4, 1024], BF16)
                wuv = sb(ph2, "wuv", [128, 4, 1024], BF16)
                qn_st = [sb(ph2, "qn_st%d" % i, [128, S], BF16) for i in range(2)]
                qr_st = [sb(ph2, "qr_st%d" % i, [64, S], BF16) for i in range(2)]
                v_st = [sb(ph2, "v_st%d" % i, [128, 2, 1024], BF16) for i in range(2)]
                rta = [sb(ph2, "rta%d" % i, [64, 512]) for i in range(2)]
                rtb = [sb(ph2, "rtb%d" % i, [64, 512]) for i in range(2)]
                kr_a, kr_b = qr_st[0], qr_st[1]
                pp = [ps(ph2, "pp%d" % i, [128, 512]) for i in range(6)]
                npp = [0]

                def nextp():
                    p = pp[npp[0] % 6]
                    npp[0] += 1
                    return p

                cx.dma("sp", gn[:, 0:4], gq_d, pwrites=[gn])
                cx.dma("sp", gn[:, 4:8], gkv_d, pwrites=[gn])
                cx.dma("pool", wuq[:, :, :], wuq_d, writes=[wuq])
                cx.dma("pool", wuk[:, :, :], wuk_d, writes=[wuk])
                cx.dma("pool", wuv[:, :, :], wuv_d, writes=[wuv])

                def load_norm(first_chunk, goff):
                    for c in range(4):
                        cx.dma("sp", lat[:, c, :], featT_d[first_chunk + c], pwrites=[lat])
                    for g in range(8):
                        sqb, tf = sq[g % 2], tmpf[g % 2]
                        cx.op("act", lambda sqb=sqb, g=g: nc.scalar.activation(sqb[:, :, :], lat[:, :, g * 512:(g + 1) * 512], AF.Square),
                              reads=[lat], writes=[sqb])
                        p = nextp()
                        for c in range(4):
                            cx.op("pe", lambda p=p, sqb=sqb, c=c: nc.tensor.matmul(p[:, :], ones_b[:, :], sqb[:, c, :], start=(c == 0), stop=(c == 3)),
                                  reads=[sqb, ones_b], pwrites=[p])
                        cx.op("act", lambda p=p, tf=tf: nc.scalar.activation(tf[:, :], p[:, :], AF.Sqrt, scale=1.0 / 512.0, bias=RMS_EPS),
                              reads=[p], writes=[tf])
                        cx.op("dve", lambda tf=tf, g=g: nc.vector.reciprocal(rstd[:, g * 512:(g + 1) * 512], tf[:, :]), reads=[tf], pwrites=[rstd])
                    for c in range(4):
                        for g in range(4):
                            cx.op("dve", lambda c=c, g=g: nc.vector.scalar_tensor_tensor(
                                out=lat[:, c, g * 1024:(g + 1) * 1024], in0=lat[:, c, g * 1024:(g + 1) * 1024],
                                scalar=gn[:, goff + c:goff + c + 1], in1=rstd[:, g * 1024:(g + 1) * 1024], op0=ALU.mult, op1=ALU.mult),
                                reads=[rstd, gn], writes=[lat])

                load_norm(28, 4)
                for h in range(8):
                    st = qn_st[h % 2]
                    for g in range(8):
                        p = nextp()
                        for c in range(4):
                            cx.op("pe", lambda p=p, c=c, h=h, g=g: nc.tensor.matmul(
                                p[:, :], wuk[:, c, h * 128:(h + 1) * 128], lat[:, c, g * 512:(g + 1) * 512], start=(c == 0), stop=(c == 3)),
                                reads=[wuk, lat], pwrites=[p])
                        cx.op("act", lambda p=p, st=st, g=g: nc.scalar.copy(st[:, g * 512:(g + 1) * 512], p[:, :]), reads=[p], pwrites=[st])
                    cx.dma("sp", knT_d[h], st[:, :], reads=[st])
                for tq in range(16):
                    st = v_st[tq % 2]
                    for ti in range(2):
                        tt = tq * 2 + ti
                        for half in range(2):
                            p = nextp()
                            for c in range(4):
                                cx.op("pe", lambda p=p, c=c, tt=tt, half=half: nc.tensor.matmul(
                                    p[:, :], lat[:, c, tt * 128:(tt + 1) * 128], wuv[:, c, half * 512:(half + 1) * 512], start=(c == 0), stop=(c == 3)),
                                    reads=[wuv, lat], pwrites=[p])
                            cx.op("act", lambda p=p, st=st, ti=ti, half=half: nc.scalar.copy(st[:, ti, half * 512:(half + 1) * 512], p[:, :]),
                                  reads=[p], pwrites=[st])
                    cx.dma("sp", vbs_d[tq * 256:(tq + 1) * 256, :].rearrange("(a p) c -> p a c", p=128), st[:, :, :], reads=[st])
                cx.dma("sp", kr_a[:, :], featT_d[32][0:64, :], writes=[kr_a])
                cx.dma("sp", kr_b[:, :], featT_d[32][64:128, :], writes=[kr_b])
                for g in range(8):
                    ra, rb = rta[g % 2], rtb[g % 2]
                    sl = slice(g * 512, (g + 1) * 512)
                    cx.op("dve", lambda ra=ra, sl=sl: nc.vector.tensor_tensor(out=ra[:, :], in0=kr_a[:, sl], in1=cos2[:, sl], op=ALU.mult),
                          reads=[kr_a, cos2], writes=[ra])
                    cx.op("dve", lambda rb=rb, sl=sl: nc.vector.tensor_tensor(out=rb[:, :], in0=kr_b[:, sl], in1=sinS[:, sl], op=ALU.mult),
                          reads=[kr_b, sinS], writes=[rb])
                    cx.op("dve", lambda ra=ra, rb=rb, sl=sl: nc.vector.tensor_tensor(out=kr_a[:, sl], in0=ra[:, :], in1=rb[:, :], op=ALU.add),
                          reads=[ra, rb], writes=[kr_a])
                cx.dma("sp", krT_d, kr_a[:, :], reads=[kr_a])

                load_norm(24, 0)
                for h in range(8):
                    stn, strp = qn_st[h % 2], qr_st[h % 2]
                    for g in range(8):
                        sl = slice(g * 512, (g + 1) * 512)
                        p = nextp()
                        for c in range(4):
                            cx.op("pe", lambda p=p, c=c, h=h, sl=sl: nc.tensor.matmul(
                                p[:, :], wuq[:, c, h * 256:h * 256 + 128], lat[:, c, sl], start=(c == 0), stop=(c == 3)),
                                reads=[wuq, lat], pwrites=[p])
                        cx.op("act", lambda p=p, stn=stn, sl=sl: nc.scalar.copy(stn[:, sl], p[:, :]), reads=[p], pwrites=[stn])
                        p2 = nextp()
                        p3 = nextp()
                        for c in range(4):
                            cx.op("pe", lambda p2=p2, c=c, h=h, sl=sl: nc.tensor.matmul(
                                p2[0:64, :], wuq[:, c, h * 256 + 128:h * 256 + 192], lat[:, c, sl], start=(c == 0), stop=(c == 3)),
                                reads=[wuq, lat], pwrites=[p2])
                        for c in range(4):
                            cx.op("pe", lambda p3=p3, c=c, h=h, sl=sl: nc.tensor.matmul(
                                p3[0:64, :], wuq[:, c, h * 256 + 192:h * 256 + 256], lat[:, c, sl], start=(c == 0), stop=(c == 3)),
                                reads=[wuq, lat], pwrites=[p3])
                        ra, rb = rta[g % 2], rtb[g % 2]
                        cx.op("dve", lambda ra=ra, p2=p2, sl=sl: nc.vector.tensor_tensor(out=ra[:, :], in0=p2[0:64, :], in1=cos2[:, sl], op=ALU.mult),
                              reads=[p2, cos2], writes=[ra])
                        cx.op("dve", lambda rb=rb, p3=p3, sl=sl: nc.vector.tensor_tensor(out=rb[:, :], in0=p3[0:64, :], in1=sinS[:, sl], op=ALU.mult),
                              reads=[p3, sinS], writes=[rb])
                        cx.op("dve", lambda ra=ra, rb=rb, strp=strp, sl=sl: nc.vector.tensor_tensor(out=strp[:, sl], in0=ra[:, :], in1=rb[:, :], op=ALU.add),
                              reads=[ra, rb], pwrites=[strp])
                    cx.dma("sp", qnT_d[h], stn[:, :], reads=[stn])
                    cx.dma("sp", qrT_d[h], strp[:, :], reads=[strp])
                cx.barrier()
            cx.barrier()

        if upto >= 5:
          with ExitStack() as ph:
            knT = sb(ph, "knT", [128, 8, S], BF16)
            vb = sb(ph, "vb", [128, NT, 1024], BF16)
            krT = sb(ph, "krT", [64, S], BF16)
            qns = [sb(ph, "qn%d" % i, [128, 512], BF16) for i in range(2)]
            qrs = [sb(ph, "qr%d" % i, [64, 512], BF16) for i in range(2)]
            pTs = [sb(ph, "pTb%d" % i, [128, 512], BF16) for i in range(3)]
            rds = [sb(ph, "rdb%d" % i, [128, 512]) for i in range(2)]
            ybs = [sb(ph, "ybb%d" % i, [128, S], BF16) for i in range(2)]
            pss = [ps(ph, "psB%d" % i, [128, 512]) for i in range(2)]
            pos_ = [ps(ph, "poB%d" % i, [128, 512]) for i in range(2)]
            pds = [ps(ph, "pdB%d" % i, [128, 512]) for i in range(2)]
            for h in range(8):
                cx.dma("sp", knT[:, h, :], knT_d[h], pwrites=[knT])
            for a in range(4):
                cx.dma("sp", vb[:, a * 8:(a + 1) * 8, :], vbs_d[a * 1024:(a + 1) * 1024, :].rearrange("(a p) c -> p a c", p=128), pwrites=[vb])
            cx.dma("sp", krT[:, :], krT_d, writes=[krT])
            it = 0
            cnt = 0
            for h in range(8):
                yb = ybs[h % 2]
                for Q in range(8):
                    qn, qr, po, pd, rd = qns[it % 2], qrs[it % 2], pos_[it % 2], pds[it % 2], rds[it % 2]
                    it += 1
                    cx.dma("sp", qn[:, :], qnT_d[h][:, Q * 512:(Q + 1) * 512], writes=[qn])
                    cx.dma("sp", qr[:, :], qrT_d[h][:, Q * 512:(Q + 1) * 512], writes=[qr])
                    nk = 4 * (Q + 1)

                    def s_step(kt, cnt_):
                        jj = kt - 4 * Q
                        c0 = max(jj, 0) * 128
                        psm = pss[cnt_ % 2]
                        pT = pTs[cnt_ % 3]
                        ks = slice(kt * 128, (kt + 1) * 128)
                        cx.op("pe", lambda: nc.tensor.matmul(
                            psm[:, c0:512], knT[:, h, ks], qn[:, c0:512], start=True, stop=False), reads=[knT, qn], pwrites=[psm])
                        cx.op("pe", lambda: nc.tensor.matmul(
                            psm[:, c0:512], krT[:, ks], qr[:, c0:512], start=False, stop=True), reads=[krT, qr], pwrites=[psm])
                        cx.op("act", lambda: nc.scalar.activation(pT[:, c0:512], psm[:, c0:512], AF.Exp, scale=SCALE_B),
                              reads=[psm], writes=[pT])
                        if jj >= 0:
                            cx.op("dve", lambda: nc.vector.memset(pT[64:128, c0:c0 + 64], 0.0), writes=[pT])

                    def v_step(kt, cnt_):
                        jj = kt - 4 * Q
                        c0 = max(jj, 0) * 128
                        pT = pTs[cnt_ % 3]
                        cx.op("pe", lambda: nc.tensor.matmul(
                            po[:, c0:512], vb[:, kt, h * 128:(h + 1) * 128], pT[:, c0:512], start=(kt == 0), stop=(kt == nk - 1)),
                            reads=[vb, pT], pwrites=[po])
                        cx.op("pe", lambda: nc.tensor.matmul(
                            pd[:, c0:512], ones_b[:, :], pT[:, c0:512], start=(kt == 0), stop=(kt == nk - 1)),
                            reads=[ones_b, pT], pwrites=[pd])

                    s_step(0, cnt)
                    for kt in range(nk):
                        if kt + 1 < nk:
                            s_step(kt + 1, cnt + kt + 1)
                        v_step(kt, cnt + kt)
                    cnt += nk
                    cx.op("dve", lambda rd=rd, pd=pd: nc.vector.reciprocal(rd[:, :], pd[:, :]), reads=[pd], writes=[rd])
                    cx.op("dve", lambda yb=yb, po=po, rd=rd, Q=Q: nc.vector.tensor_tensor(
                        out=yb[:, Q * 512:(Q + 1) * 512], in0=po[:, :], in1=rd[:, :], op=ALU.mult), reads=[po, rd], pwrites=[yb])
                cx.dma("sp", yT_d[8 + h], yb[:, :], reads=[yb])
            cx.barrier()

        if upto >= 6:
          with ExitStack() as ph:
            wpa = sb(ph, "wpa", [128, 8, D], BF16)
            wpb = sb(ph, "wpb", [128, 8, D], BF16)
            yas = [sb(ph, "ya%d" % i, [128, 8, 256], BF16) for i in range(2)]
            ybs = [sb(ph, "ybm%d" % i, [128, 8, 256], BF16) for i in range(2)]
            gts = [sb(ph, "gt%d" % i, [128, 32, 256], BF16) for i in range(2)]
            yfs = [sb(ph, "yf%d" % i, [128, 16, 256], BF16) for i in range(2)]
            tas = [sb(ph, "tam%d" % i, [128, 256]) for i in range(2)]
            tbs = [sb(ph, "tbm%d" % i, [128, 256]) for i in range(2)]
            ppa = [ps(ph, "ppa%d" % i, [128, 512]) for i in range(2)]
            ppb = [ps(ph, "ppb%d" % i, [128, 512]) for i in range(2)]
            for hh in range(2):
                cx.dma("pool", wpa[:, hh * 4:(hh + 1) * 4, :], wpa_d[hh * 512:(hh + 1) * 512, :].rearrange("(h p) c -> p h c", p=128), pwrites=[wpa])
                cx.dma("pool", wpb[:, hh * 4:(hh + 1) * 4, :], wpb_d[hh * 512:(hh + 1) * 512, :].rearrange("(h p) c -> p h c", p=128), pwrites=[wpb])
            n = 0
            for g in range(16):
                ya, yb, gt, yf = yas[g % 2], ybs[g % 2], gts[g % 2], yfs[g % 2]
                ts = slice(g * 256, (g + 1) * 256)
                cx.dma("sp", ya[:, :, :], yT_d[0:8, :, ts].rearrange("h p t -> p h t"), writes=[ya])
                cx.dma("sp", yb[:, :, :], yT_d[8:16, :, ts].rearrange("h p t -> p h t"), writes=[yb])
                for a in range(4):
                    cx.dma("sp", gt[:, a * 8:(a + 1) * 8, :], featT_d[33 + a * 8:33 + (a + 1) * 8, :, ts].rearrange("c p t -> p c t"), pwrites=[gt])
                for oc in range(16):
                    pa, pb, ta, tb = ppa[n % 2], ppb[n % 2], tas[n % 2], tbs[n % 2]
                    n += 1
                    os_ = slice(oc * 128, (oc + 1) * 128)
                    for h in range(8):
                        cx.op("pe", lambda pa=pa, ya=ya, h=h, os_=os_: nc.tensor.matmul(
                            pa[:, 0:256], wpa[:, h, os_], ya[:, h, :], start=(h == 0), stop=(h == 7)), reads=[wpa, ya], pwrites=[pa])
                    for h in range(8):
                        cx.op("pe", lambda pb=pb, yb=yb, h=h, os_=os_: nc.tensor.matmul(
                            pb[:, 0:256], wpb[:, h, os_], yb[:, h, :], start=(h == 0), stop=(h == 7)), reads=[wpb, yb], pwrites=[pb])
                    cx.op("dve", lambda ta=ta, pa=pa, gt=gt, oc=oc: nc.vector.tensor_tensor(out=ta[:, :], in0=pa[:, 0:256], in1=gt[:, oc, :], op=ALU.mult),
                          reads=[pa, gt], writes=[ta])
                    cx.op("dve", lambda tb=tb, pb=pb, gt=gt, oc=oc: nc.vector.tensor_tensor(out=tb[:, :], in0=pb[:, 0:256], in1=gt[:, 16 + oc, :], op=ALU.mult),
                          reads=[pb, gt], writes=[tb])
                    cx.op("pool", lambda yf=yf, ta=ta, tb=tb, oc=oc: nc.gpsimd.tensor_tensor(out=yf[:, oc, :], in0=ta[:, :], in1=tb[:, :], op=ALU.add),
                          reads=[ta, tb], pwrites=[yf])
                for a in range(2):
                    cx.dma("sp", yfT_d[a * 8:(a + 1) * 8, :, ts].rearrange("c p t -> p c t"), yf[:, a * 8:(a + 1) * 8, :], reads=[yf])
            cx.barrier()

        def layer_norm_tile(r, lng, lnb, dst, stats, mv, rs_t):
            for k in range(4):
                cx.op("dve", lambda k=k: nc.vector.bn_stats(stats[:, k, :], r[:, k * 512:(k + 1) * 512]), reads=[r], pwrites=[stats])
            cx.op("dve", lambda: nc.vector.bn_aggr(mv[:, :], stats[:, :, :].rearrange("p a b -> p (a b)")), reads=[stats], writes=[mv])
            cx.op("act", lambda: nc.scalar.activation(rs_t[:, 0:1], mv[:, 1:2], AF.Sqrt, bias=LN_EPS), reads=[mv], writes=[rs_t])
            cx.op("dve", lambda: nc.vector.reciprocal(rs_t[:, 1:2], rs_t[:, 0:1]), writes=[rs_t])
            cx.op("dve", lambda: nc.vector.tensor_scalar(out=r[:, :], in0=r[:, :], scalar1=mv[:, 0:1], scalar2=rs_t[:, 1:2],
                                                         op0=ALU.subtract, op1=ALU.mult), reads=[mv, rs_t], writes=[r])
            cx.op("pool", lambda: nc.gpsimd.tensor_tensor(out=r[:, :], in0=r[:, :], in1=lng[:, :], op=ALU.mult), reads=[lng], writes=[r])
            cx.op("pool", lambda: nc.gpsimd.tensor_tensor(out=dst[:, :], in0=r[:, :], in1=lnb[:, :], op=ALU.add), reads=[r, lnb], writes=[dst])

        if upto >= 7:
          with ExitStack() as ph:
            wo = sb(ph, "wo", [128, 16, D], BF16)
            lng = sb(ph, "ln1g", [128, D])
            lnb = sb(ph, "ln1b", [128, D])
            yfs = [sb(ph, "yfo%d" % i, [128, 16, 512], BF16) for i in range(2)]
            xts = [sb(ph, "xt%d" % i, [128, D]) for i in range(2)]
            rts = [sb(ph, "rt%d" % i, [128, D]) for i in range(1)]
            x1s = [sb(ph, "x1t%d" % i, [128, D]) for i in range(2)]
            h2s = [sb(ph, "h2s%d" % i, [128, 16, 512], BF16) for i in range(1)]
            stats = sb(ph, "stats", [128, 4, 6])
            mv = sb(ph, "mv", [128, 2])
            rs_t = sb(ph, "rs_t", [128, 2])
            pob = [ps(ph, "pob%d" % i, [128, 512]) for i in range(4)]
            ptb = [ps(ph, "ptb%d" % i, [128, 512]) for i in range(4)]
            for a in range(4):
                cx.dma("pool", wo[:, a * 4:(a + 1) * 4, :], wo_d[a * 512:(a + 1) * 512, :].rearrange("(k p) c -> p k c", p=128), pwrites=[wo])
            cx.dma("sp", lng[:, :], ln_d[0], writes=[lng])
            cx.dma("sp", lnb[:, :], ln_d[1], writes=[lnb])
            npt = 0
            for g in range(8):
                yf, h2b = yfs[g % 2], h2s[0]
                for a in range(2):
                    cx.dma("sp", yf[:, a * 8:(a + 1) * 8, :], yfT_d[a * 8:(a + 1) * 8, :, g * 512:(g + 1) * 512].rearrange("c p t -> p c t"), pwrites=[yf])
                for tl in range(4):
                    tt = g * 4 + tl
                    xt, r, x1t = xts[tt % 2], rts[0], x1s[tt % 2]
                    cx.dma("sp", xt[:, :], x_d[tt * 128:(tt + 1) * 128, :], writes=[xt])
                    for cg in range(4):
                        for oc in range(16):
                            cx.op("pe", lambda cg=cg, oc=oc, yf=yf, tl=tl: nc.tensor.matmul(
                                pob[cg][:, :], yf[:, oc, tl * 128:(tl + 1) * 128], wo[:, oc, cg * 512:(cg + 1) * 512],
                                start=(oc == 0), stop=(oc == 15)), reads=[yf, wo], pwrites=[pob[cg]])
                        cx.op("dve", lambda cg=cg, r=r: nc.vector.tensor_tensor(
                            out=r[:, cg * 512:(cg + 1) * 512], in0=pob[cg][:, :], in1=g1bc[:, cg * 512:(cg + 1) * 512], op=ALU.mult),
                            reads=[pob[cg], g1bc], pwrites=[r])
                    cx.op("dve", lambda r=r, xt=xt: nc.vector.scalar_tensor_tensor(
                        out=r[:, :], in0=xt[:, :], scalar=DN_ALPHA, in1=r[:, :], op0=ALU.mult, op1=ALU.add), reads=[xt], writes=[r])
                    layer_norm_tile(r, lng, lnb, x1t, stats, mv, rs_t)
                    cx.dma("sp", x1_d[tt * 128:(tt + 1) * 128, :], x1t[:, :], reads=[x1t])
                    for q4 in range(4):
                        pt = ptb[npt % 4]
                        npt += 1
                        for i in range(4):
                            dc = q4 * 4 + i
                            cx.op("pe", lambda pt=pt, x1t=x1t, i=i, dc=dc: nc.tensor.transpose(
                                pt[:, i * 128:(i + 1) * 128], x1t[:, dc * 128:(dc + 1) * 128], ident[:, :]), reads=[x1t, ident], pwrites=[pt])
                        for i in range(4):
                            dc = q4 * 4 + i
                            cx.op("act", lambda pt=pt, h2b=h2b, i=i, dc=dc, tl=tl: nc.scalar.activation(
                                h2b[:, dc, tl * 128:(tl + 1) * 128], pt[:, i * 128:(i + 1) * 128], AF.Identity,
                                bias=b2[:, dc:dc + 1], scale=s2[:, dc:dc + 1]), reads=[pt, s2, b2], pwrites=[h2b])
                for a in range(2):
                    cx.dma("sp", h2T_d[a * 8:(a + 1) * 8, :, g * 512:(g + 1) * 512].rearrange("c p t -> p c t"), h2b[:, a * 8:(a + 1) * 8, :], reads=[h2b])
            cx.barrier()

        if upto >= 8:
          with ExitStack() as ph:
            wq = sb(ph, "wq", [128, 16, D], BF16)
            keysT = sb(ph, "keysT", [128, 16, 128], BF16)
            h2g = [sb(ph, "h2g%d" % i, [128, 16, 512], BF16) for i in range(2)]
            qTs = [sb(ph, "qTs%d" % i, [128, 16, 512], BF16) for i in range(2)]
            scb = [sb(ph, "scb%d" % i, [128, 8, 2, 128]) for i in range(2)]
            sv = sb(ph, "sv", [128, 8, 2, 16])
            tmp = sb(ph, "tk_tmp", [128, 128])
            c16 = sb(ph, "c16", [128, 8, 16, 16])
            c8 = sb(ph, "c8", [128, 8, 24])
            tmp2 = sb(ph, "tk_tmp2", [128, 256])
            tmp3 = sb(ph, "tk_tmp3", [128, 256])
            d16 = sb(ph, "d16", [128, 8, 16])
            zz = sb(ph, "zz", [128, 8])
            lz = sb(ph, "lz", [128, 8])
            idx = sb(ph, "idx", [128, 8, 16], mybir.dt.uint32)
            pkb = [sb(ph, "pkb%d" % i, [128, 272]) for i in range(2)]
            pq = [ps(ph, "pq%d" % i, [128, 512]) for i in range(4)]
            psc = [ps(ph, "psc%d" % i, [128, 512]) for i in range(4)]
            for a in range(4):
                cx.dma("pool", wq[:, a * 4:(a + 1) * 4, :], wq_d[a * 512:(a + 1) * 512, :].rearrange("(k p) c -> p k c", p=128), pwrites=[wq])
            cx.dma("pool", keysT[:, :, :], keysT_d, writes=[keysT])
            npq = 0
            for g in range(8):
                hg, qt = h2g[g % 2], qTs[g % 2]
                for a in range(2):
                    cx.dma("sp", hg[:, a * 8:(a + 1) * 8, :], h2T_d[a * 8:(a + 1) * 8, :, g * 512:(g + 1) * 512].rearrange("c p t -> p c t"), pwrites=[hg])
                for hp in range(16):
                    p = pq[npq % 4]
                    npq += 1
                    for dc in range(16):
                        cx.op("pe", lambda p=p, hg=hg, dc=dc, hp=hp: nc.tensor.matmul(
                            p[:, :], wq[:, dc, hp * 128:(hp + 1) * 128], hg[:, dc, :], start=(dc == 0), stop=(dc == 15)),
                            reads=[wq, hg], pwrites=[p])
                    cx.op("act", lambda p=p, qt=qt, hp=hp: nc.scalar.copy(qt[:, hp, :], p[:, :]), reads=[p], pwrites=[qt])
                for tl in range(4):
                    tt = g * 4 + tl
                    sc = scb[tt % 2]
                    for bk in range(4):
                        for i in range(4):
                            hp = bk * 4 + i
                            cx.op("pe", lambda bk=bk, i=i, hp=hp, qt=qt, tl=tl: nc.tensor.matmul(
                                psc[bk][:, i * 128:(i + 1) * 128], qt[:, hp, tl * 128:(tl + 1) * 128], keysT[:, hp, :], start=True, stop=True),
                                reads=[qt, keysT], pwrites=[psc[bk]])
                        cx.op("act", lambda bk=bk, sc=sc: nc.scalar.copy(
                            sc[:, bk * 2:(bk + 1) * 2, :, :], psc[bk][:, :].rearrange("p (a b c) -> p a b c", a=2, b=2)),
                            reads=[psc[bk]], pwrites=[sc])
                    cx.dma("sp", scs_d[tt * 128:(tt + 1) * 128, :], sc[:, :, :, :].rearrange("p a b c -> p (a b c)"), reads=[sc])
                    for h in range(8):
                        for p_ in range(2):
                            cx.op("dve", lambda h=h, p_=p_, sc=sc: nc.vector.max(out=sv[:, h, p_, 0:8], in_=sc[:, h, p_, :]), reads=[sc], pwrites=[sv])
                            cx.op("dve", lambda h=h, p_=p_, sc=sc: nc.vector.match_replace(
                                out=tmp[:, :], in_to_replace=sv[:, h, p_, 0:8], in_values=sc[:, h, p_, :], imm_value=-1e30), reads=[sc, sv], writes=[tmp])
                            cx.op("dve", lambda h=h, p_=p_: nc.vector.max(out=sv[:, h, p_, 8:16], in_=tmp[:, :]), reads=[tmp], pwrites=[sv])
                    for h in range(8):
                        for r8 in range(2):
                            cx.op("dve", lambda h=h, r8=r8, sc=sc: nc.vector.max_index(
                                out=idx[:, h, r8 * 8:(r8 + 1) * 8], in_max=sv[:, h, 0, r8 * 8:(r8 + 1) * 8], in_values=sc[:, h, 0, :]),
                                reads=[sv, sc], pwrites=[idx])
                    cx.op("dve", lambda: nc.vector.tensor_tensor(
                        out=c16[:, :, :, :], in0=bcast(sv[:, :, 0, :], 3, [128, 8, 16, 16]), in1=bcast(sv[:, :, 1, :], 2, [128, 8, 16, 16]), op=ALU.add),
                        reads=[sv], writes=[c16])
                    for h in range(8):
                        cflat = c16[:, h, :, :].rearrange("p a b -> p (a b)")
                        cx.op("dve", lambda h=h, cflat=cflat: nc.vector.max(out=c8[:, h, 0:8], in_=cflat), reads=[c16], pwrites=[c8])
                        cx.op("dve", lambda h=h, cflat=cflat: nc.vector.match_replace(
                            out=tmp2[:, :], in_to_replace=c8[:, h, 0:8], in_values=cflat, imm_value=-1e30), reads=[c16, c8], writes=[tmp2])
                        cx.op("dve", lambda h=h: nc.vector.max(out=c8[:, h, 8:16], in_=tmp2[:, :]), reads=[tmp2], pwrites=[c8])
                        cx.op("dve", lambda h=h: nc.vector.match_replace(
                            out=tmp3[:, :], in_to_replace=c8[:, h, 8:16], in_values=tmp2[:, :], imm_value=-1e30), reads=[tmp2, c8], writes=[tmp3])
                        cx.op("dve", lambda h=h: nc.vector.max(out=c8[:, h, 16:24], in_=tmp3[:, :]), reads=[tmp3], pwrites=[c8])
                    cx.op("dve", lambda: nc.vector.tensor_tensor(out=zz[:, :], in0=c8[:, :, 15], in1=c8[:, :, 16], op=ALU.add),
                          reads=[c8], writes=[zz])
                    cx.op("dve", lambda tt=tt: nc.vector.tensor_scalar(out=prm[:, tt, 0:8], in0=zz[:, :], scalar1=0.5, scalar2=None, op0=ALU.mult),
                          reads=[zz], pwrites=[prm])
                    cx.op("dve", lambda: nc.vector.tensor_tensor(out=d16[:, :, :], in0=c8[:, :, 0:16], in1=bcast(c8[:, :, 0], 2, [128, 8, 16]), op=ALU.subtract),
                          reads=[c8], writes=[d16])
                    cx.op("act", lambda: nc.scalar.activation(d16[:, :, :], d16[:, :, :], AF.Exp), writes=[d16])
                    cx.op("dve", lambda: nc.vector.tensor_reduce(out=zz[:, :], in_=d16[:, :, :], axis=AX.X, op=ALU.add), reads=[d16], writes=[zz])
                    cx.op("act", lambda: nc.scalar.activation(lz[:, :], zz[:, :], AF.Ln), reads=[zz], writes=[lz])
                    cx.op("dve", lambda tt=tt: nc.vector.scalar_tensor_tensor(
                        out=prm[:, tt, 8:16], in0=c8[:, :, 0], scalar=-1.0, in1=lz[:, :], op0=ALU.mult, op1=ALU.subtract),
                        reads=[c8, lz], pwrites=[prm])
                    pk = pkb[tt % 2]
                    cx.op("dve", lambda pk=pk, tt=tt: nc.vector.tensor_tensor(
                        out=pk[:, 0:128].rearrange("p (h k) -> p h k", h=8), in0=sv[:, :, 0, :], in1=bcast(prm[:, tt, 8:16], 2, [128, 8, 16]), op=ALU.add),
                        reads=[sv, prm], pwrites=[pk])
                    cx.op("dve", lambda pk=pk: nc.vector.tensor_copy(pk[:, 128:256].rearrange("p (h k) -> p h k", h=8), idx[:, :, :]),
                          reads=[idx], pwrites=[pk])
                    cx.op("dve", lambda pk=pk, tt=tt: nc.vector.tensor_tensor(out=pk[:, 256:264], in0=prm[:, tt, 0:8], in1=prm[:, tt, 8:16], op=ALU.add),
                          reads=[prm], pwrites=[pk])
                    cx.op("dve", lambda pk=pk: nc.vector.memset(pk[:, 264:272], 0.0), pwrites=[pk])
                    cx.dma("sp", pk_d[tt * 128:(tt + 1) * 128, :], pk[:, :], reads=[pk])
            if dbgprm_d is not None:
                cx.dma("sp", dbgprm_d, prm[:, :, :], reads=[prm])
            cx.barrier()

        if upto >= 9:
          with ExitStack() as ph:
            lng = sb(ph, "ln2g", [128, D])
            lnb = sb(ph, "ln2b", [128, D])
            iota = sb(ph, "iota", [128, 128])
            hgs = [sb(ph, "h2p%d" % i, [128, 16, 128], BF16) for i in range(2)]
            s2bs = [sb(ph, "s2b%d" % i, [128, 8, 128]) for i in range(2)]
            pks = [sb(ph, "pkp%d" % i, [128, 272]) for i in range(2)]
            x1t = sb(ph, "x1p", [128, D])
            rt = sb(ph, "rtp", [128, D])
            cmb = sb(ph, "cmb", [128, 128, 16])
            ee = sb(ph, "eeR", [128, 2048], BF16)
            Rp = sb(ph, "Rp", [128, 128, 16], BF16)
            RT = sb(ph, "RT", [128, 128, 128], BF16)
            si1T = sb(ph, "si1T", [128, 128])
            O1T = sb(ph, "O1T", [128, 128, 32], BF16)
            GTq = sb(ph, "GTq", [128, 128, 32], BF16)
            gqs = [sb(ph, "gq%d" % i, [128, 4096], BF16) for i in range(2)]
            uts = [sb(ph, "ut%d" % i, [128, 16, 512], BF16) for i in range(2)]
            vcs = [sb(ph, "vc%d" % i, [128, 2, D], BF16) for i in range(2)]
            gas = [sb(ph, "ga%d" % i, [128, 512], BF16) for i in range(2)]
            wws = [sb(ph, "ww%d" % i, [128, 512], BF16) for i in range(2)]
            wTs = [sb(ph, "wT%d" % i, [128, 4, 128], BF16) for i in range(2)]
            stats = sb(ph, "stats2", [128, 4, 6])
            mv = sb(ph, "mv2", [128, 2])
            rs_t = sb(ph, "rs_t2", [128, 2])
            pop = [ps(ph, "pop%d" % i, [128, 512]) for i in range(4)]
            pap = [ps(ph, "pap%d" % i, [128, 512]) for i in range(2)]
            pgf = ps(ph, "pgf", [128, 512])
            pgb = ps(ph, "pgb", [128, 1024], BF16)
            cx.dma("sp", lng[:, :], ln_d[2], writes=[lng])
            cx.dma("sp", lnb[:, :], ln_d[3], writes=[lnb])
            cx.dma("sp", iota[:, :], iota_d, writes=[iota])

            def load_tile(tt):
                hg, s2b, pk = hgs[tt % 2], s2bs[tt % 2], pks[tt % 2]
                for a in range(2):
                    cx.dma("sp", hg[:, a * 8:(a + 1) * 8, :], h2T_d[a * 8:(a + 1) * 8, :, tt * 128:(tt + 1) * 128].rearrange("c p t -> p c t"), pwrites=[hg])
                cx.dma("sp", s2b[:, :, :], scs_d[tt * 128:(tt + 1) * 128, :].rearrange("t (h p n) -> t h p n", h=8, p=2)[:, :, 1, :], writes=[s2b])
                cx.dma("sp", pk[:, :], pk_d[tt * 128:(tt + 1) * 128, :], writes=[pk])

            def prep_items(tt):
                s2b, pk = s2bs[tt % 2], pks[tt % 2]
                items = []
                for p_ in range(8):
                    j0 = p_ * 16

                    def elem(j0=j0):
                        cx.op("dve", lambda: nc.vector.tensor_tensor(
                            out=cmb[:, :, :].rearrange("p (h k) j -> p h k j", h=8),
                            in0=bcast(pk[:, 0:128].rearrange("p (h k) -> p h k", h=8), 3, [128, 8, 16, 16]),
                            in1=bcast(s2b[:, :, j0:j0 + 16], 2, [128, 8, 16, 16]), op=ALU.add), reads=[pk, s2b], writes=[cmb])
                        cx.op("act", lambda: nc.scalar.activation(ee[:, :], cmb[:, :, :].rearrange("p a b -> p (a b)"), AF.Exp),
                              reads=[cmb], writes=[ee])
                        cx.op("dve", lambda: nc.vector.tensor_tensor(
                            out=Rp[:, :, :].rearrange("p (h k) j -> p h (k j)", h=8), in0=cmb[:, :, :].rearrange("p (h k) j -> p h (k j)", h=8),
                            in1=bcast(pk[:, 256:264], 2, [128, 8, 256]), op=ALU.is_ge), reads=[cmb, pk], writes=[Rp])
                        cx.op("pool", lambda: nc.gpsimd.tensor_tensor(out=Rp[:, :, :].rearrange("p a b -> p (a b)"),
                                                                      in0=Rp[:, :, :].rearrange("p a b -> p (a b)"), in1=ee[:, :], op=ALU.mult),
                              reads=[ee], writes=[Rp])
                    items.append(elem)
                    for b_ in range(2):
                        def rb(j0=j0, b_=b_):
                            for i in range(8):
                                cx.op("pe", lambda i=i: nc.tensor.transpose(pgb[:, i * 128:(i + 1) * 128], Rp[:, :, b_ * 8 + i], identb[:, :]),
                                      reads=[Rp, identb], pwrites=[pgb])
                            cx.op("dve", lambda: nc.vector.tensor_copy(
                                RT[:, :, j0 + b_ * 8:j0 + b_ * 8 + 8].rearrange("p t j -> p j t"), pgb[:, :].rearrange("p (j t) -> p j t", t=128)),
                                reads=[pgb], pwrites=[RT])
                        items.append(rb)

                def s_item():
                    cx.op("pe", lambda: nc.tensor.transpose(pgf[:, 0:128], pk[:, 128:256], ident[:, :]), reads=[pk, ident], pwrites=[pgf])
                    cx.op("dve", lambda: nc.vector.tensor_copy(si1T[:, :], pgf[:, 0:128]), reads=[pgf], writes=[si1T])
                items.append(s_item)
                return items

            def quarter_items(tt, q):
                gq = gqs[(tt * 4 + q) % 2]
                items = []

                def o1():
                    cx.op("dve", lambda: nc.vector.tensor_tensor(
                        out=O1T[:, :, :], in0=bcast(si1T[:, :], 2, [128, 128, 32]), in1=bcast(iota[:, q * 32:(q + 1) * 32], 1, [128, 128, 32]),
                        op=ALU.is_equal), reads=[si1T, iota], writes=[O1T])
                items.append(o1)
                for tb in range(8):
                    def pt(tb=tb):
                        for tl in range(16):
                            t = tb * 16 + tl
                            cx.op("pe", lambda tl=tl, t=t: nc.tensor.matmul(pgf[:, tl * 32:(tl + 1) * 32], RT[:, t, :], O1T[:, t, :], start=True, stop=True),
                                  reads=[RT, O1T], pwrites=[pgf])
                        cx.op("dve", lambda: nc.vector.tensor_copy(
                            GTq[:, tb * 16:(tb + 1) * 16, :], pgf[:, :].rearrange("p (t i) -> p t i", i=32)),
                            reads=[pgf], pwrites=[GTq])
                    items.append(pt)
                for gb in range(4):
                    def gt(gb=gb):
                        for il in range(8):
                            cx.op("pe", lambda il=il: nc.tensor.transpose(pgb[:, il * 128:(il + 1) * 128], GTq[:, :, gb * 8 + il], identb[:, :]),
                                  reads=[GTq, identb], pwrites=[pgb])
                        cx.op("act", lambda: nc.scalar.copy(gq[:, gb * 1024:(gb + 1) * 1024], pgb[:, :]), reads=[pgb], pwrites=[gq])
                    items.append(gt)
                return items

            NEG = NT * 32

            def coords(EG):
                tt, eg = EG // 32, EG % 32
                return tt, eg // 8, eg % 8, eg

            def load_ut(EG):
                tt, q, e4, eg = coords(EG)
                ut = uts[EG % 2]
                cx.dma("sp", ut[:, :, :], utb_d[eg * 512:(eg + 1) * 512, :].rearrange("(p k) c -> p (k c)", k=4).rearrange("p (k c) -> p k c", c=512),
                       reads=[utb_t] if EG < 2 else (), writes=[ut])

            def load_vc(EG):
                tt, q, e4, eg = coords(EG)
                for half in range(2):
                    vc = vcs[half]
                    r0 = eg * 512 + half * 256
                    cx.dma("sp", vc[:, :, :], vb16_d[r0:r0 + 256, :].rearrange("(k p) d -> p k d", p=128),
                           reads=[vb16_t] if EG < 1 else (), writes=[vc])

            def a_mm(EG, part):
                tt, q, e4, eg = coords(EG)
                hg, ut, pa, ga, ww = hgs[tt % 2], uts[EG % 2], pap[EG % 2], gas[EG % 2], wws[EG % 2]
                gq = gqs[(tt * 4 + q) % 2]
                for dc in range(part * 8, part * 8 + 8):
                    cx.op("pe", lambda dc=dc: nc.tensor.matmul(pa[:, :], hg[:, dc, :], ut[:, dc, :], start=(dc == 0), stop=(dc == 15)),
                          reads=[hg, ut], pwrites=[pa])
                if part == 0:
                    return
                cx.op("act", lambda: nc.scalar.activation(ga[:, :], pa[:, :], AF.Gelu), reads=[pa], writes=[ga])
                cx.op("dve", lambda: nc.vector.tensor_tensor(out=ww[:, :], in0=ga[:, :], in1=gq[:, e4 * 512:(e4 + 1) * 512], op=ALU.mult),
                      reads=[ga, gq], writes=[ww])

            def wtr(EG):
                ww, wT = wws[EG % 2], wTs[EG % 2]
                for k in range(4):
                    cx.op("pe", lambda k=k: nc.tensor.transpose(pgb[:, k * 128:(k + 1) * 128], ww[:, k * 128:(k + 1) * 128], identb[:, :]),
                          reads=[ww, identb], pwrites=[pgb])
                cx.op("act", lambda: nc.scalar.copy(wT[:, :, :], pgb[:, 0:512].rearrange("p (a b) -> p a b", b=128)), reads=[pgb], writes=[wT])

            def ph2(EG, part):
                tt, q, e4, eg = coords(EG)
                wT = wTs[EG % 2]
                for k in range(part * 2, part * 2 + 2):
                    vc = vcs[k // 2]
                    for dq in range(4):
                        cx.op("pe", lambda k=k, dq=dq, vc=vc: nc.tensor.matmul(
                            pop[dq][:, :], wT[:, k, :], vc[:, k % 2, dq * 512:(dq + 1) * 512],
                            start=(eg == 0 and k == 0), stop=(eg == 31 and k == 3)), reads=[wT, vc], pwrites=[pop[dq]])

            def epilogue(tt):
                for dq in range(4):
                    cx.op("dve", lambda dq=dq: nc.vector.tensor_tensor(
                        out=rt[:, dq * 512:(dq + 1) * 512], in0=pop[dq][:, :], in1=g2bc[:, dq * 512:(dq + 1) * 512], op=ALU.mult),
                        reads=[pop[dq], g2bc], pwrites=[rt])
                cx.op("dve", lambda: nc.vector.scalar_tensor_tensor(
                    out=rt[:, :], in0=x1t[:, :], scalar=DN_ALPHA, in1=rt[:, :], op0=ALU.mult, op1=ALU.add), reads=[x1t], writes=[rt])
                layer_norm_tile(rt, lng, lnb, x1t, stats, mv, rs_t)
                cx.dma("sp", out_d[tt * 128:(tt + 1) * 128, :], x1t[:, :], reads=[x1t])

            load_tile(0)
            for it in prep_items(0):
                it()
            for it in quarter_items(0, 0):
                it()
            cx.dma("sp", x1t[:, :], x1_d[0:128, :], writes=[x1t])
            load_ut(0)
            load_ut(1)
            load_vc(0)
            a_mm(0, 0)
            a_mm(0, 1)
            queue = []
            carry = []
            per_slot = 0

            def pop_items(n):
                for _ in range(min(n, len(queue))):
                    queue.pop(0)()

            for EG in range(NEG):
                tt, q, e4, eg = coords(EG)
                if e4 == 0:
                    if q < 2:
                        queue.extend(quarter_items(tt, q + 1))
                    elif q == 2:
                        queue.extend(quarter_items(tt, 3))
                        if tt + 1 < NT:
                            load_tile(tt + 1)
                            nxt = prep_items(tt + 1)
                            queue.extend(nxt[:12])
                            carry = nxt[12:]
                    else:
                        if tt + 1 < NT:
                            queue.extend(carry)
                            queue.extend(quarter_items(tt + 1, 0))
                    per_slot = -(-len(queue) // 8)
                n_left = per_slot
                k1 = (n_left + 3) // 4
                if EG + 2 < NEG:
                    load_ut(EG + 2)
                pop_items(k1); n_left -= k1
                if EG + 1 < NEG:
                    a_mm(EG + 1, 0)
                if e4 == 7:
                    pop_items(len(queue))
                else:
                    pop_items(k1); n_left -= k1
                if EG + 1 < NEG:
                    a_mm(EG + 1, 1)
                wtr(EG)
                if EG >= 1:
                    ph2(EG - 1, 0)
                if e4 != 7:
                    pop_items(k1); n_left -= k1
                if EG >= 1:
                    ph2(EG - 1, 1)
                    if (EG - 1) % 32 == 31:
                        epilogue((EG - 1) // 32)
                        cx.dma("sp", x1t[:, :], x1_d[tt * 128:(tt + 1) * 128, :], writes=[x1t])
                    load_vc(EG)
                if e4 != 7:
                    pop_items(max(n_left, 0))
            ph2(NEG - 1, 0)
            ph2(NEG - 1, 1)
            epilogue(NT - 1)
            cx.barrier()
        cx.barrier()
        print("[kernel] instructions emitted:", cx.nins)
    return nc


def host_layout(inputs):
    f = lambda k: np.asarray(inputs[k])
    shared = {}
    w_in = f("w_in")[0]
    b_in = f("b_in")[0]
    perm = np.concatenate([np.arange(32, 64), np.arange(0, 32)])
    cols = np.concatenate([np.arange(0, 4160), 4096 + perm, np.arange(4160, 8256)])
    wext = w_in[:, cols]
    shared["w_in_l"] = np.ascontiguousarray(wext.reshape(16, 128, 65, 128).transpose(2, 1, 0, 3).reshape(65, 128, 2048))
    shared["b_inT"] = np.ascontiguousarray(b_in[cols].reshape(65, 128).T)
    shared["w_ada"] = np.ascontiguousarray(f("w_ada")[0])
    b_ada = f("b_ada")[0]
    shared["b_adaT"] = np.ascontiguousarray(b_ada.reshape(96, 128).T)
    shared["b_ada_row"] = np.ascontiguousarray(b_ada.reshape(1, -1))
    rb = f("rel_bias")[0]
    p = np.arange(128)[:, None, None]
    j = np.arange(5)[None, :, None]
    c = np.arange(128)[None, None, :]
    rel = 128 * (4 - j) + c - p
    idx = np.clip(rel, -63, 256) + 63
    shared["biasT"] = np.ascontiguousarray(rb[:, idx].transpose(1, 0, 2, 3).reshape(128, 8, 640))
    mask = np.zeros((128, 5, 128), np.float32)
    mask[:64, 0, 64:] = NEGM
    mask[64:, 4, :64] = NEGM
    shared["maskA"] = mask.reshape(128, 640)
    shared["gqT"] = np.ascontiguousarray(f("q_norm_g")[0].reshape(4, 128).T)
    shared["gkvT"] = np.ascontiguousarray(f("kv_norm_g")[0].reshape(4, 128).T)
    w_uq = f("w_uq")[0]
    qcols = []
    for h in range(8):
        base = h * 192
        qcols += [np.arange(base, base + 128), np.arange(base + 128, base + 192), base + 128 + perm]
    qcols = np.concatenate(qcols)
    shared["w_uq_l"] = np.ascontiguousarray(w_uq[:, qcols].reshape(4, 128, 2048).transpose(1, 0, 2))
    w_ukv = f("w_ukv")[0].reshape(512, 8, 256)
    shared["w_uk_l"] = np.ascontiguousarray(w_ukv[:, :, :128].reshape(4, 128, 1024).transpose(1, 0, 2))
    shared["w_uv_l"] = np.ascontiguousarray(w_ukv[:, :, 128:].reshape(4, 128, 1024).transpose(1, 0, 2))
    shared["w_pa"] = np.ascontiguousarray(f("w_pa")[0])
    shared["w_pb"] = np.ascontiguousarray(f("w_pb")[0])
    shared["w_o"] = np.ascontiguousarray(f("w_o")[0])
    shared["ln_bc"] = np.ascontiguousarray(np.stack([np.broadcast_to(f(k)[0][None, :], (128, D)) for k in ("ln1_g", "ln1_b", "ln2_g", "ln2_b")]))
    shared["peer_wq"] = np.ascontiguousarray(f("peer_wq")[0])
    keys = f("peer_keys")[0]
    shared["keysT"] = np.ascontiguousarray(keys.reshape(16, 128, 128).transpose(2, 0, 1))
    U = f("peer_u")[0]
    shared["ut_l"] = np.ascontiguousarray(U.reshape(32, 512, 16, 128).transpose(0, 3, 2, 1)).reshape(16384, 2048)
    shared["peer_v"] = np.ascontiguousarray(f("peer_v")[0])
    shared["ident"] = np.eye(128, dtype=np.float32)
    shared["iota"] = np.ascontiguousarray(np.broadcast_to(np.arange(128, dtype=np.float32)[None, :], (128, 128)))
    inv_freq = (10000.0 ** (-np.arange(0, 64, 2, dtype=np.float32) / 64.0)).astype(np.float32)
    invf = np.zeros((64, 2), np.float32)
    invf[:, 0] = np.concatenate([inv_freq, inv_freq])
    invf[:32, 1] = -1.0
    invf[32:, 1] = 1.0
    shared["invf"] = invf
    per_core = []
    x = f("x")
    cc = f("c")
    pos = f("positions")
    for b in range(NCORES):
        m = dict(shared)
        m["x"] = np.ascontiguousarray(x[b])
        m["cT"] = np.ascontiguousarray(cc[b].reshape(16, 128).T)
        m["posb"] = np.ascontiguousarray(np.broadcast_to(pos[b][None, :].astype(np.int32), (64, S)))
        per_core.append(m)
    return per_core


_NC_CACHE = {}


def kernel(**inputs):
    maps = host_layout(inputs)
    if "full" not in _NC_CACHE:
        _NC_CACHE["full"] = build()
    nc = _NC_CACHE["full"]
    res = run_bass_kernel_spmd(nc, maps, core_ids=list(range(NCORES)))
    out = np.stack([np.asarray(r["out"]) for r in res.results], axis=0)
    return out.astype(np.float32)
```

```python
import math
from contextlib import ExitStack

import numpy as np
import concourse.bass as bass
import concourse.mybir as mybir
from concourse.bass_utils import run_bass_kernel_spmd

F32 = mybir.dt.float32
BF16 = mybir.dt.bfloat16
I32 = mybir.dt.int32
AF = mybir.ActivationFunctionType
ALU = mybir.AluOpType
AX = mybir.AxisListType

NCORES = 8
S = 4096
D = 2048
NT = S // 128
DN_ALPHA = 2.0 ** 0.25
LN_EPS = 1e-5
RMS_EPS = 1e-6
SCALE_A = 128.0 ** -0.5
SCALE_B = 192.0 ** -0.5
NEGM = -30000.0
MAGIC = 12582912.0
TWO_PI = 2.0 * math.pi
C1 = 6.28125
C2 = TWO_PI - C1
PI_SAFE = 3.14159

SAME_ENG_SYNC = True
CB_ON_POOL = False


class Buf:
    __slots__ = ("w", "r")

    def __init__(self):
        self.w = {}
        self.r = {}


class T:
    def __init__(self, handle):
        self.t = handle
        self.b = Buf()

    def __getitem__(self, k):
        return self.t[k]


class Ctx:
    def __init__(self, nc, es):
        self.nc = nc
        self.eng = {"pe": nc.tensor, "act": nc.scalar, "dve": nc.vector, "pool": nc.gpsimd, "sp": nc.sync}
        self.sems = {}
        self.tot = {}
        for e in ("pe", "act", "dve", "pool"):
            self.sems[e] = es.enter_context(nc.semaphore("s_" + e))
            self.tot[e] = 0
        self.dq = {}
        for q, n in (("sp", 16), ("pool", 8)):
            lst = []
            for i in range(n):
                k = "d_%s%d" % (q, i)
                self.sems[k] = es.enter_context(nc.semaphore(k))
                self.tot[k] = 0
                lst.append(k)
            self.dq[q] = [lst, 0]
        self.seen = {e: {} for e in self.eng}
        self.nins = 0

    def _wait(self, e, deps):
        own = e if e in ("pe", "act", "dve", "pool") else None
        seen = self.seen[e]
        for k, v in deps.items():
            if v <= 0:
                continue
            if k == own and (own == "pe" or not SAME_ENG_SYNC):
                continue
            if seen.get(k, 0) >= v:
                continue
            self.eng[e].wait_ge(self.sems[k], v)
            seen[k] = v

    @staticmethod
    def _merge(d, k, v):
        if d.get(k, 0) < v:
            d[k] = v

    def _deps(self, reads, writes, pwrites):
        deps = {}
        for t in reads:
            for k, v in t.b.w.items():
                self._merge(deps, k, v)
        for t in writes:
            for k, v in t.b.w.items():
                self._merge(deps, k, v)
            for k, v in t.b.r.items():
                self._merge(deps, k, v)
        for t in pwrites:
            for k, v in t.b.r.items():
                self._merge(deps, k, v)
        return deps

    def _update(self, tok, reads, writes, pwrites):
        k, v = tok
        for t in writes:
            t.b.w = {k: v}
            t.b.r = {}
        for t in pwrites:
            if t.b.r:
                t.b.w = {k: v}
                t.b.r = {}
            else:
                self._merge(t.b.w, k, v)
        for t in reads:
            self._merge(t.b.r, k, v)

    def op(self, e, fn, reads=(), writes=(), pwrites=()):
        self._wait(e, self._deps(reads, writes, pwrites))
        ins = fn()
        self.tot[e] += 1
        ins.then_inc(self.sems[e], 1)
        self.nins += 1
        self._update((e, self.tot[e]), reads, writes, pwrites)
        return ins

    def dma(self, q, out, in_, reads=(), writes=(), pwrites=()):
        lst, i = self.dq[q]
        k = lst[i % len(lst)]
        self.dq[q][1] = i + 1
        deps = self._deps(reads, writes, pwrites)
        self._merge(deps, k, self.tot[k])
        self._wait(q, deps)
        ins = self.eng[q].dma_start(out=out, in_=in_)
        self.tot[k] += 16
        ins.then_inc(self.sems[k], 16)
        self.nins += 1
        self._update((k, self.tot[k]), reads, writes, pwrites)
        return ins

    def barrier(self, engines=("pe", "act", "dve", "pool", "sp")):
        for e in engines:
            self._wait(e, dict(self.tot))


def bcast(ap, axis, shape):
    return ap.unsqueeze(axis).broadcast_to(shape)


def build(upto=99, dbg=()):
    nc = bass.Bass("TRN2", target_bir_lowering=False)

    def din(name, shape, dt=F32):
        return nc.dram_tensor(name, list(shape), dt, kind="ExternalInput").ap()

    def dscr(name, shape, dt):
        kind = "ExternalOutput" if name in dbg else "Internal"
        return nc.dram_tensor(name, list(shape), dt, kind=kind).ap()

    x_d = din("x", [S, D])
    cT_d = din("cT", [128, 16])
    pos_d = din("posb", [64, S], I32)
    invf_d = din("invf", [64, 2])
    wada_d = din("w_ada", [D, 6 * D])
    badaT_d = din("b_adaT", [128, 96])
    badar_d = din("b_ada_row", [1, 6 * D])
    win_d = din("w_in_l", [65, 128, 2048])
    binT_d = din("b_inT", [128, 65])
    biasT_d = din("biasT", [128, 8, 640])
    maskA_d = din("maskA", [128, 640])
    gq_d = din("gqT", [128, 4])
    gkv_d = din("gkvT", [128, 4])
    wuq_d = din("w_uq_l", [128, 4, 2048])
    wuk_d = din("w_uk_l", [128, 4, 1024])
    wuv_d = din("w_uv_l", [128, 4, 1024])
    wpa_d = din("w_pa", [1024, D])
    wpb_d = din("w_pb", [1024, D])
    wo_d = din("w_o", [D, D])
    ln_d = din("ln_bc", [4, 128, D])
    wq_d = din("peer_wq", [D, D])
    keysT_d = din("keysT", [128, 16, 128])
    ut_d = din("ut_l", [16384, 2048])
    v_d = din("peer_v", [16384, D])
    ident_d = din("ident", [128, 128])
    iota_d = din("iota", [128, 128])

    out_d = nc.dram_tensor("out", [S, D], F32, kind="ExternalOutput").ap()

    featT_d = dscr("featT", [65, 128, S], BF16)
    yT_d = dscr("yT", [16, 128, S], BF16)
    qnT_d = dscr("qnT", [8, 128, S], BF16)
    qrT_d = dscr("qrT", [8, 64, S], BF16)
    knT_d = dscr("knT", [8, 128, S], BF16)
    krT_d = dscr("krT", [64, S], BF16)
    vbs_d = dscr("vbs", [S, 1024], BF16)
    yfT_d = dscr("yfT", [16, 128, S], BF16)
    x1_d = dscr("x1s", [S, D], F32)
    h2T_d = dscr("h2T", [16, 128, S], BF16)
    scs_d = dscr("scs", [S, 2048], F32)
    pk_d = dscr("pk", [S, 272], F32)
    utb_d = dscr("utb", [16384, 2048], BF16)
    vb16_d = dscr("vb16", [16384, D], BF16)
    dbgmod_d = dscr("dbgmod", [128, 96 + 2], F32) if "dbgmod" in dbg else None
    dbgg_d = dscr("dbgg", [128, 2 * D], F32) if "dbgg" in dbg else None
    dbgprm_d = dscr("dbgprm", [128, NT, 16], F32) if "dbgprm" in dbg else None

    with ExitStack() as es:
        cx = Ctx(nc, es)

        uid = [0]

        def sb(scope, name, shape, dt=F32):
            uid[0] += 1
            return T(scope.enter_context(nc.sbuf_tensor("sb%d_%s" % (uid[0], name), list(shape), dt)))

        def ps(scope, name, shape, dt=F32):
            uid[0] += 1
            return T(scope.enter_context(nc.psum_tensor("ps%d_%s" % (uid[0], name), list(shape), dt)))

        CB_ENG = "pool" if CB_ON_POOL else "dve"
        CB_OBJ = nc.gpsimd if CB_ON_POOL else nc.vector
        ident = sb(es, "ident", [128, 128])
        identb = sb(es, "identb", [128, 128], BF16)
        ones_f = sb(es, "ones_f", [128, 128])
        ones_b = sb(es, "ones_b", [128, 128], BF16)
        s1 = sb(es, "s1", [128, 16])
        b1 = sb(es, "b1", [128, 16])
        s2 = sb(es, "s2", [128, 16])
        b2 = sb(es, "b2", [128, 16])
        g2bc = sb(es, "g2bc", [128, D])
        prm = sb(es, "prm", [128, NT, 16])
        es_g1 = ExitStack()
        g1bc = sb(es_g1, "g1bc", [128, D])

        cx.dma("sp", ident[:, :], ident_d, writes=[ident])
        cx.op("dve", lambda: nc.vector.tensor_copy(identb[:, :], ident[:, :]), reads=[ident], writes=[identb])
        cx.op("dve", lambda: nc.vector.memset(ones_f[:, :], 1.0), writes=[ones_f])
        cx.op("dve", lambda: nc.vector.memset(ones_b[:, :], 1.0), writes=[ones_b])

        utb_t = T(None)
        vb16_t = T(None)

        def cast_tables(i):
            if upto < 6 or i >= 64:
                return
            if i < 32:
                cx.dma("pool", utb_d[i * 512:(i + 1) * 512, :], ut_d[i * 512:(i + 1) * 512, :], pwrites=[utb_t])
            else:
                i -= 32
                cx.dma("pool", vb16_d[i * 512:(i + 1) * 512, :], v_d[i * 512:(i + 1) * 512, :], pwrites=[vb16_t])

        with ExitStack() as ph:
            cT = sb(ph, "cT", [128, 16])
            scT = sb(ph, "scT", [128, 16])
            badaT = sb(ph, "badaT", [128, 96])
            badar = sb(ph, "badar", [1, 6 * D])
            modT = sb(ph, "modT", [128, 96])
            wst = [sb(ph, "wst%d" % i, [128, 16, 512]) for i in range(2)]
            rowsb = [sb(ph, "rowsb%d" % i, [1, 512]) for i in range(2)]
            pm = ps(ph, "pm", [128, 512])
            pr = [ps(ph, "pr%d" % i, [128, 512]) for i in range(2)]
            pbc = [ps(ph, "pbc%d" % i, [128, 512]) for i in range(2)]
            cx.dma("sp", cT[:, :], cT_d, writes=[cT])
            cx.dma("sp", badaT[:, :], badaT_d, writes=[badaT])
            cx.dma("sp", badar[:, :], badar_d, writes=[badar])
            cx.op("act", lambda: nc.scalar.activation(scT[:, :], cT[:, :], AF.Silu), reads=[cT], writes=[scT])
            nrow = 0
            for gi in range(24):
                m = gi // 4
                w = wst[gi % 2]
                cx.dma("sp", w[:, :, :], wada_d[:, gi * 512:(gi + 1) * 512].rearrange("(k p) c -> p k c", p=128), writes=[w])
                if m in (0, 1, 3, 4):
                    for cc in range(4):
                        col = m * 16 + (gi % 4) * 4 + cc
                        for kc in range(16):
                            cx.op("pe", lambda kc=kc, cc=cc, col=col, w=w: nc.tensor.matmul(
                                pm[:, col:col + 1], w[:, kc, cc * 128:(cc + 1) * 128], scT[:, kc:kc + 1],
                                start=(kc == 0), stop=(kc == 15)), reads=[w, scT], pwrites=[pm])
                else:
                    p_r = pr[nrow % 2]
                    rs = rowsb[nrow % 2]
                    p_b = pbc[nrow % 2]
                    nrow += 1
                    for kc in range(16):
                        cx.op("pe", lambda kc=kc, w=w, p_r=p_r: nc.tensor.matmul(
                            p_r[0:1, :], scT[:, kc:kc + 1], w[:, kc, :], start=(kc == 0), stop=(kc == 15)),
                            reads=[w, scT], pwrites=[p_r])
                    cx.op("dve", lambda p_r=p_r, rs=rs, gi=gi: nc.vector.tensor_tensor(
                        out=rs[0:1, :], in0=p_r[0:1, :], in1=badar[0:1, gi * 512:(gi + 1) * 512], op=ALU.add),
                        reads=[p_r, badar], writes=[rs])
                    cx.op("pe", lambda rs=rs, p_b=p_b: nc.tensor.matmul(
                        p_b[:, :], ones_f[0:1, :], rs[0:1, :], start=True, stop=True), reads=[rs, ones_f], pwrites=[p_b])
                    dst = g1bc if m == 2 else g2bc
                    cx.op("act", lambda dst=dst, p_b=p_b, gi=gi: nc.scalar.copy(
                        dst[:, (gi % 4) * 512:(gi % 4 + 1) * 512], p_b[:, :]), reads=[p_b], pwrites=[dst])
            cx.op("dve", lambda: nc.vector.tensor_tensor(out=modT[:, :], in0=pm[:, 0:96], in1=badaT[:, :], op=ALU.add),
                  reads=[pm, badaT], writes=[modT])
            cx.op("dve", lambda: nc.vector.tensor_scalar(out=s1[:, :], in0=modT[:, 16:32], scalar1=1.0, scalar2=None, op0=ALU.add),
                  reads=[modT], writes=[s1])
            cx.op("dve", lambda: nc.vector.tensor_copy(b1[:, :], modT[:, 0:16]), reads=[modT], writes=[b1])
            cx.op("dve", lambda: nc.vector.tensor_scalar(out=s2[:, :], in0=modT[:, 64:80], scalar1=1.0, scalar2=None, op0=ALU.add),
                  reads=[modT], writes=[s2])
            cx.op("dve", lambda: nc.vector.tensor_copy(b2[:, :], modT[:, 48:64]), reads=[modT], writes=[b2])
            if dbgmod_d is not None:
                cx.dma("sp", dbgmod_d[:, 0:96], modT[:, :], reads=[modT])
            if dbgg_d is not None:
                cx.dma("sp", dbgg_d[:, 0:D], g1bc[:, :], reads=[g1bc])
                cx.dma("sp", dbgg_d[:, D:2 * D], g2bc[:, :], reads=[g2bc])
            cx.barrier()

        if upto >= 1:
          with ExitStack() as ph12:
            hT = sb(ph12, "hT", [128, 16, S], BF16)
            with ExitStack() as ph:
                xs = [sb(ph, "xs%d" % i, [128, 2, D]) for i in range(2)]
                ptr = [ps(ph, "ptr%d" % i, [128, 512]) for i in range(4)]
                n = 0
                for g in range(16):
                    xb = xs[g % 2]
                    cx.dma("sp", xb[:, :, :], x_d[g * 256:(g + 1) * 256, :].rearrange("(j p) d -> p j d", p=128), writes=[xb])
                    for dc in range(16):
                        pt = ptr[n % 4]
                        n += 1
                        for j in range(2):
                            cx.op("pe", lambda pt=pt, xb=xb, j=j, dc=dc: nc.tensor.transpose(
                                pt[:, j * 128:(j + 1) * 128], xb[:, j, dc * 128:(dc + 1) * 128], ident[:, :]),
                                reads=[xb, ident], pwrites=[pt])
                        cx.op("act", lambda pt=pt, g=g, dc=dc: nc.scalar.activation(
                            hT[:, dc, g * 256:(g + 1) * 256], pt[:, 0:256], AF.Identity,
                            bias=b1[:, dc:dc + 1], scale=s1[:, dc:dc + 1]), reads=[pt, s1, b1], pwrites=[hT])
                cx.barrier()
            with ExitStack() as ph:
                wc = [sb(ph, "wc%d" % i, [128, 16, 128], BF16) for i in range(3)]
                ost = [sb(ph, "ost%d" % i, [128, S], BF16) for i in range(2)]
                binT = sb(ph, "binT", [128, 65])
                pz = [ps(ph, "pz%d" % i, [128, 512]) for i in range(4)]
                cx.dma("sp", binT[:, :], binT_d, writes=[binT])
                n = 0
                for ch in range(65):
                    w = wc[ch % 3]
                    cx.dma("pool", w[:, :, :], win_d[ch].rearrange("p (k c) -> p k c", c=128), writes=[w])
                    if ch >= 2:
                        cast_tables(ch - 2)
                        if ch == 64:
                            cast_tables(63)
                    o = ost[ch % 2]
                    func = AF.Sigmoid if ch >= 33 else AF.Identity
                    for g in range(8):
                        p = pz[n % 4]
                        n += 1
                        for kc in range(16):
                            cx.op("pe", lambda p=p, w=w, kc=kc, g=g: nc.tensor.matmul(
                                p[:, :], w[:, kc, :], hT[:, kc, g * 512:(g + 1) * 512], start=(kc == 0), stop=(kc == 15)),
                                reads=[w, hT], pwrites=[p])
                        cx.op("act", lambda p=p, o=o, g=g, ch=ch, func=func: nc.scalar.activation(
                            o[:, g * 512:(g + 1) * 512], p[:, :], func, bias=binT[:, ch:ch + 1]),
                            reads=[p, binT], pwrites=[o])
                    cx.dma("sp", featT_d[ch], o[:, :], reads=[o])
                cx.barrier()

        if upto >= 3:
          with ExitStack() as ph:
            biasT = sb(ph, "biasT", [128, 8, 640])
            maskA = sb(ph, "maskA", [128, 640])
            qTs = [sb(ph, "qT%d" % i, [128, S], BF16) for i in range(2)]
            kTs = [sb(ph, "kT%d" % i, [128, S], BF16) for i in range(2)]
            vTs = [sb(ph, "vT%d" % i, [128, S], BF16) for i in range(2)]
            vas = [sb(ph, "va%d" % i, [128, NT, 128], BF16) for i in range(2)]
            ybs = [sb(ph, "yb%d" % i, [128, S], BF16) for i in range(2)]
            t1s = [sb(ph, "t1_%d" % i, [128, 640]) for i in range(2)]
            pTs = [sb(ph, "pT%d" % i, [128, 640], BF16) for i in range(2)]
            rds = [sb(ph, "rd%d" % i, [128, 128]) for i in range(2)]
            pss = [ps(ph, "psA%d" % i, [128, 1024]) for i in range(2)]
            pos_ = [ps(ph, "poA%d" % i, [128, 512]) for i in range(2)]
            ptr = [ps(ph, "ptA%d" % i, [128, 1024], BF16) for i in range(2)]
            cx.dma("sp", biasT[:, :, :], biasT_d, writes=[biasT])
            cx.dma("sp", maskA[:, :], maskA_d, writes=[maskA])
            for h in range(8):
                cx.op("dve", lambda h=h: nc.vector.tensor_tensor(out=biasT[:, h, :], in0=biasT[:, h, :], in1=maskA[:, :], op=ALU.add),
                      reads=[maskA], writes=[biasT])
            nt = 0
            for h in range(8):
                qT, kT, vT, va, yb = qTs[h % 2], kTs[h % 2], vTs[h % 2], vas[h % 2], ybs[h % 2]
                cx.dma("sp", qT[:, :], featT_d[h], writes=[qT])
                cx.dma("sp", kT[:, :], featT_d[8 + h], writes=[kT])
                cx.dma("sp", vT[:, :], featT_d[16 + h], writes=[vT])
                for blk in range(4):
                    pt = ptr[nt % 2]
                    nt += 1
                    for i in range(8):
                        tl = blk * 8 + i
                        cx.op("pe", lambda pt=pt, vT=vT, i=i, tl=tl: nc.tensor.transpose(
                            pt[:, i * 128:(i + 1) * 128], vT[:, tl * 128:(tl + 1) * 128], identb[:, :]),
                            reads=[vT, identb], pwrites=[pt])
                    cx.op("act", lambda pt=pt, va=va, blk=blk: nc.scalar.copy(
                        va[:, blk * 8:(blk + 1) * 8, :], pt[:, :].rearrange("p (a b) -> p a b", b=128)),
                        reads=[pt], pwrites=[va])
                for m in range(NT):
                    j0 = max(0, 4 - m)
                    lo = j0 * 128
                    psm, t1, pT, po, rd = pss[m % 2], t1s[m % 2], pTs[m % 2], pos_[m % 2], rds[m % 2]
                    for j in range(j0, 5):
                        kt = m - 4 + j
                        cx.op("pe", lambda psm=psm, kT=kT, qT=qT, j=j, kt=kt, m=m: nc.tensor.matmul(
                            psm[:, j * 128:(j + 1) * 128], kT[:, kt * 128:(kt + 1) * 128], qT[:, m * 128:(m + 1) * 128],
                            start=True, stop=True), reads=[kT, qT], pwrites=[psm])
                    cx.op("dve", lambda psm=psm, t1=t1, h=h, lo=lo: nc.vector.scalar_tensor_tensor(
                        out=t1[:, lo:640], in0=psm[:, lo:640], scalar=SCALE_A, in1=biasT[:, h, lo:640],
                        op0=ALU.mult, op1=ALU.add), reads=[psm, biasT], writes=[t1])
                    cx.op("act", lambda t1=t1, pT=pT, lo=lo: nc.scalar.activation(pT[:, lo:640], t1[:, lo:640], AF.Exp),
                          reads=[t1], writes=[pT])
                    for j in range(j0, 5):
                        kt = m - 4 + j
                        cx.op("pe", lambda po=po, va=va, pT=pT, j=j, kt=kt, j0=j0: nc.tensor.matmul(
                            po[:, 0:128], va[:, kt, :], pT[:, j * 128:(j + 1) * 128], start=(j == j0), stop=(j == 4)),
                            reads=[va, pT], pwrites=[po])
                    for j in range(j0, 5):
                        cx.op("pe", lambda po=po, pT=pT, j=j, j0=j0: nc.tensor.matmul(
                            po[:, 128:256], ones_b[:, :], pT[:, j * 128:(j + 1) * 128], start=(j == j0), stop=(j == 4)),
                            reads=[ones_b, pT], pwrites=[po])
                    cx.op("dve", lambda rd=rd, po=po: nc.vector.reciprocal(rd[:, :], po[:, 128:256]), reads=[po], writes=[rd])
                    cx.op("dve", lambda yb=yb, po=po, rd=rd, m=m: nc.vector.tensor_tensor(
                        out=yb[:, m * 128:(m + 1) * 128], in0=po[:, 0:128], in1=rd[:, :], op=ALU.mult),
                        reads=[po, rd], pwrites=[yb])
                cx.dma("sp", yT_d[h], yb[:, :], reads=[yb])
            cx.barrier()

        if upto >= 4:
          with ExitStack() as ph:
            cos2 = sb(ph, "cos2", [64, S])
            sinS = sb(ph, "sinS", [64, S])
            invf = sb(ph, "invf", [64, 2])
            cx.dma("sp", invf[:, :], invf_d, writes=[invf])
            with ExitStack() as ph2:
                posi = sb(ph2, "posi", [64, S], I32)
                ang = sb(ph2, "ang", [64, S])
                ta = sb(ph2, "ta", [64, S])
                tb = sb(ph2, "tb", [64, S])
                cx.dma("sp", posi[:, :], pos_d, writes=[posi])
                cx.op("dve", lambda: nc.vector.tensor_copy(ta[:, :], posi[:, :]), reads=[posi], writes=[ta])
                cx.op("dve", lambda: nc.vector.tensor_scalar(out=ang[:, :], in0=ta[:, :], scalar1=invf[:, 0:1], scalar2=None, op0=ALU.mult),
                      reads=[ta, invf], writes=[ang])
                for dst, shift, use_sgn in ((sinS, 0.0, True), (cos2, math.pi / 2.0, False)):
                    src = ang
                    if shift != 0.0:
                        cx.op("dve", lambda: nc.vector.tensor_scalar(out=ta[:, :], in0=ang[:, :], scalar1=shift, scalar2=None, op0=ALU.add),
                              reads=[ang], writes=[ta])
                        src = ta
                    else:
                        cx.op("dve", lambda: nc.vector.tensor_copy(ta[:, :], ang[:, :]), reads=[ang], writes=[ta])
                        src = ta
                    cx.op("dve", lambda: nc.vector.tensor_scalar(out=tb[:, :], in0=ta[:, :], scalar1=1.0 / TWO_PI, scalar2=None, op0=ALU.mult),
                          reads=[ta], writes=[tb])
                    cx.op("dve", lambda: nc.vector.tensor_scalar(out=tb[:, :], in0=tb[:, :], scalar1=MAGIC, scalar2=None, op0=ALU.add),
                          reads=[tb], writes=[tb])
                    cx.op("dve", lambda: nc.vector.tensor_scalar(out=tb[:, :], in0=tb[:, :], scalar1=-MAGIC, scalar2=None, op0=ALU.add),
                          reads=[tb], writes=[tb])
                    cx.op("dve", lambda: nc.vector.scalar_tensor_tensor(out=ta[:, :], in0=tb[:, :], scalar=-C1, in1=ta[:, :], op0=ALU.mult, op1=ALU.add),
                          reads=[tb, ta], writes=[ta])
                    cx.op("dve", lambda: nc.vector.scalar_tensor_tensor(out=ta[:, :], in0=tb[:, :], scalar=-C2, in1=ta[:, :], op0=ALU.mult, op1=ALU.add),
                          reads=[tb, ta], writes=[ta])
                    cx.op("dve", lambda: nc.vector.tensor_scalar(out=ta[:, :], in0=ta[:, :], scalar1=PI_SAFE, scalar2=-PI_SAFE, op0=ALU.min, op1=ALU.max),
                          reads=[ta], writes=[ta])
                    if use_sgn:
                        cx.op("act", lambda dst=dst: nc.scalar.activation(dst[:, :], ta[:, :], AF.Sin, scale=invf[:, 1:2]),
                              reads=[ta, invf], writes=[dst])
                    else:
                        cx.op("act", lambda dst=dst: nc.scalar.activation(dst[:, :], ta[:, :], AF.Sin), reads=[ta], writes=[dst])
                cx.barrier()

            with ExitStack() as ph2:
                lat = sb(ph2, "lat", [128, 4, S], BF16)
                sq = [sb(ph2, "sq%d" % i, [128, 4, 512], BF16) for i in range(2)]
                tmpf = [sb(ph2, "tmpf%d" % i, [128, 512]) for i in range(2)]
                rstd = sb(ph2, "rstd", [128, S])
                gn = sb(ph2, "gn", [128, 8])
                wuq = sb(ph2, "wuq", [128, 4, 2048], BF16)
                wuk = sb(ph2, "wuk", [128, 4, 1024], BF16)
                wuv = sb(ph2, "wuv", [128, 4, 1024], BF16)
                qn_st = [sb(ph2, "qn_st%d" % i, [128, S], BF16) for i in range(2)]
                qr_st = [sb(ph2, "qr_st%d" % i, [64, S], BF16) for i in range(2)]
                v_st = [sb(ph2, "v_st%d" % i, [128, 2, 1024], BF16) for i in range(2)]
                rta = [sb(ph2, "rta%d" % i, [64, 512]) for i in range(2)]
                rtb = [sb(ph2, "rtb%d" % i, [64, 512]) for i in range(2)]
                kr_a, kr_b = qr_st[0], qr_st[1]
                pp = [ps(ph2, "pp%d" % i, [128, 512]) for i in range(6)]
                npp = [0]

                def nextp():
                    p = pp[npp[0] % 6]
                    npp[0] += 1
                    return p

                cx.dma("sp", gn[:, 0:4], gq_d, pwrites=[gn])
                cx.dma("sp", gn[:, 4:8], gkv_d, pwrites=[gn])
                cx.dma("pool", wuq[:, :, :], wuq_d, writes=[wuq])
                cx.dma("pool", wuk[:, :, :], wuk_d, writes=[wuk])
                cx.dma("pool", wuv[:, :, :], wuv_d, writes=[wuv])

                def load_norm(first_chunk, goff):
                    for c in range(4):
                        cx.dma("sp", lat[:, c, :], featT_d[first_chunk + c], pwrites=[lat])
                    for g in range(8):
                        sqb, tf = sq[g % 2], tmpf[g % 2]
                        cx.op("act", lambda sqb=sqb, g=g: nc.scalar.activation(sqb[:, :, :], lat[:, :, g * 512:(g + 1) * 512], AF.Square),
                              reads=[lat], writes=[sqb])
                        p = nextp()
                        for c in range(4):
                            cx.op("pe", lambda p=p, sqb=sqb, c=c: nc.tensor.matmul(p[:, :], ones_b[:, :], sqb[:, c, :], start=(c == 0), stop=(c == 3)),
                                  reads=[sqb, ones_b], pwrites=[p])
                        cx.op("act", lambda p=p, tf=tf: nc.scalar.activation(tf[:, :], p[:, :], AF.Sqrt, scale=1.0 / 512.0, bias=RMS_EPS),
                              reads=[p], writes=[tf])
                        cx.op("dve", lambda tf=tf, g=g: nc.vector.reciprocal(rstd[:, g * 512:(g + 1) * 512], tf[:, :]), reads=[tf], pwrites=[rstd])
                    for c in range(4):
                        for g in range(4):
                            cx.op("dve", lambda c=c, g=g: nc.vector.scalar_tensor_tensor(
                                out=lat[:, c, g * 1024:(g + 1) * 1024], in0=lat[:, c, g * 1024:(g + 1) * 1024],
                                scalar=gn[:, goff + c:goff + c + 1], in1=rstd[:, g * 1024:(g + 1) * 1024], op0=ALU.mult, op1=ALU.mult),
                                reads=[rstd, gn], writes=[lat])

                load_norm(28, 4)
                for h in range(8):
                    st = qn_st[h % 2]
                    for g in range(8):
                        p = nextp()
                        for c in range(4):
                            cx.op("pe", lambda p=p, c=c, h=h, g=g: nc.tensor.matmul(
                                p[:, :], wuk[:, c, h * 128:(h + 1) * 128], lat[:, c, g * 512:(g + 1) * 512], start=(c == 0), stop=(c == 3)),
                                reads=[wuk, lat], pwrites=[p])
                        cx.op("act", lambda p=p, st=st, g=g: nc.scalar.copy(st[:, g * 512:(g + 1) * 512], p[:, :]), reads=[p], pwrites=[st])
                    cx.dma("sp", knT_d[h], st[:, :], reads=[st])
                for tq in range(16):
                    st = v_st[tq % 2]
                    for ti in range(2):
                        tt = tq * 2 + ti
                        for half in range(2):
                            p = nextp()
                            for c in range(4):
                                cx.op("pe", lambda p=p, c=c, tt=tt, half=half: nc.tensor.matmul(
                                    p[:, :], lat[:, c, tt * 128:(tt + 1) * 128], wuv[:, c, half * 512:(half + 1) * 512], start=(c == 0), stop=(c == 3)),
                                    reads=[wuv, lat], pwrites=[p])
                            cx.op("act", lambda p=p, st=st, ti=ti, half=half: nc.scalar.copy(st[:, ti, half * 512:(half + 1) * 512], p[:, :]),
                                  reads=[p], pwrites=[st])
                    cx.dma("sp", vbs_d[tq * 256:(tq + 1) * 256, :].rearrange("(a p) c -> p a c", p=128), st[:, :, :], reads=[st])
                cx.dma("sp", kr_a[:, :], featT_d[32][0:64, :], writes=[kr_a])
                cx.dma("sp", kr_b[:, :], featT_d[32][64:128, :], writes=[kr_b])
                for g in range(8):
                    ra, rb = rta[g % 2], rtb[g % 2]
                    sl = slice(g * 512, (g + 1) * 512)
                    cx.op("dve", lambda ra=ra, sl=sl: nc.vector.tensor_tensor(out=ra[:, :], in0=kr_a[:, sl], in1=cos2[:, sl], op=ALU.mult),
                          reads=[kr_a, cos2], writes=[ra])
                    cx.op("dve", lambda rb=rb, sl=sl: nc.vector.tensor_tensor(out=rb[:, :], in0=kr_b[:, sl], in1=sinS[:, sl], op=ALU.mult),
                          reads=[kr_b, sinS], writes=[rb])
                    cx.op("dve", lambda ra=ra, rb=rb, sl=sl: nc.vector.tensor_tensor(out=kr_a[:, sl], in0=ra[:, :], in1=rb[:, :], op=ALU.add),
                          reads=[ra, rb], writes=[kr_a])
                cx.dma("sp", krT_d, kr_a[:, :], reads=[kr_a])

                load_norm(24, 0)
                for h in range(8):
                    stn, strp = qn_st[h % 2], qr_st[h % 2]
                    for g in range(8):
                        sl = slice(g * 512, (g + 1) * 512)
                        p = nextp()
                        for c in range(4):
                            cx.op("pe", lambda p=p, c=c, h=h, sl=sl: nc.tensor.matmul(
                                p[:, :], wuq[:, c, h * 256:h * 256 + 128], lat[:, c, sl], start=(c == 0), stop=(c == 3)),
                                reads=[wuq, lat], pwrites=[p])
                        cx.op("act", lambda p=p, stn=stn, sl=sl: nc.scalar.copy(stn[:, sl], p[:, :]), reads=[p], pwrites=[stn])
                        p2 = nextp()
                        p3 = nextp()
                        for c in range(4):
                            cx.op("pe", lambda p2=p2, c=c, h=h, sl=sl: nc.tensor.matmul(
                                p2[0:64, :], wuq[:, c, h * 256 + 128:h * 256 + 192], lat[:, c, sl], start=(c == 0), stop=(c == 3)),
                                reads=[wuq, lat], pwrites=[p2])
                        for c in range(4):
                            cx.op("pe", lambda p3=p3, c=c, h=h, sl=sl: nc.tensor.matmul(
                                p3[0:64, :], wuq[:, c, h * 256 + 192:h * 256 + 256], lat[:, c, sl], start=(c == 0), stop=(c == 3)),
                                reads=[wuq, lat], pwrites=[p3])
                        ra, rb = rta[g % 2], rtb[g % 2]
                        cx.op("dve", lambda ra=ra, p2=p2, sl=sl: nc.vector.tensor_tensor(out=ra[:, :], in0=p2[0:64, :], in1=cos2[:, sl], op=ALU.mult),
                              reads=[p2, cos2], writes=[ra])
                        cx.op("dve", lambda rb=rb, p3=p3, sl=sl: nc.vector.tensor_tensor(out=rb[:, :], in0=p3[0:64, :], in1=sinS[:, sl], op=ALU.mult),
                              reads=[p3, sinS], writes=[rb])
                        cx.op("dve", lambda ra=ra, rb=rb, strp=strp, sl=sl: nc.vector.tensor_tensor(out=strp[:, sl], in0=ra[:, :], in1=rb[:, :], op=ALU.add),
                              reads=[ra, rb], pwrites=[strp])
                    cx.dma("sp", qnT_d[h], stn[:, :], reads=[stn])
                    cx.dma("sp", qrT_d[h], strp[:, :], reads=[strp])
                cx.barrier()
            cx.barrier()

        if upto >= 5:
          with ExitStack() as ph:
            knT = sb(ph, "knT", [128, 8, S], BF16)
            vb = sb(ph, "vb", [128, NT, 1024], BF16)
            krT = sb(ph, "krT", [64, S], BF16)
            qns = [sb(ph, "qn%d" % i, [128, 512], BF16) for i in range(2)]
            qrs = [sb(ph, "qr%d" % i, [64, 512], BF16) for i in range(2)]
            pTs = [sb(ph, "pTb%d" % i, [128, 512], BF16) for i in range(3)]
            rds = [sb(ph, "rdb%d" % i, [128, 512]) for i in range(2)]
            ybs = [sb(ph, "ybb%d" % i, [128, S], BF16) for i in range(2)]
            pss = [ps(ph, "psB%d" % i, [128, 512]) for i in range(2)]
            pos_ = [ps(ph, "poB%d" % i, [128, 512]) for i in range(2)]
            pds = [ps(ph, "pdB%d" % i, [128, 512]) for i in range(2)]
            for h in range(8):
                cx.dma("sp", knT[:, h, :], knT_d[h], pwrites=[knT])
            for a in range(4):
                cx.dma("sp", vb[:, a * 8:(a + 1) * 8, :], vbs_d[a * 1024:(a + 1) * 1024, :].rearrange("(a p) c -> p a c", p=128), pwrites=[vb])
            cx.dma("sp", krT[:, :], krT_d, writes=[krT])
            it = 0
            cnt = 0
            for h in range(8):
                yb = ybs[h % 2]
                for Q in range(8):
                    qn, qr, po, pd, rd = qns[it % 2], qrs[it % 2], pos_[it % 2], pds[it % 2], rds[it % 2]
                    it += 1
                    cx.dma("sp", qn[:, :], qnT_d[h][:, Q * 512:(Q + 1) * 512], writes=[qn])
                    cx.dma("sp", qr[:, :], qrT_d[h][:, Q * 512:(Q + 1) * 512], writes=[qr])
                    nk = 4 * (Q + 1)

                    def s_step(kt, cnt_):
                        jj = kt - 4 * Q
                        c0 = max(jj, 0) * 128
                        psm = pss[cnt_ % 2]
                        pT = pTs[cnt_ % 3]
                        ks = slice(kt * 128, (kt + 1) * 128)
                        cx.op("pe", lambda: nc.tensor.matmul(
                            psm[:, c0:512], knT[:, h, ks], qn[:, c0:512], start=True, stop=False), reads=[knT, qn], pwrites=[psm])
                        cx.op("pe", lambda: nc.tensor.matmul(
                            psm[:, c0:512], krT[:, ks], qr[:, c0:512], start=False, stop=True), reads=[krT, qr], pwrites=[psm])
                        cx.op("act", lambda: nc.scalar.activation(pT[:, c0:512], psm[:, c0:512], AF.Exp, scale=SCALE_B),
                              reads=[psm], writes=[pT])
                        if jj >= 0:
                            cx.op("dve", lambda: nc.vector.memset(pT[64:128, c0:c0 + 64], 0.0), writes=[pT])

                    def v_step(kt, cnt_):
                        jj = kt - 4 * Q
                        c0 = max(jj, 0) * 128
                        pT = pTs[cnt_ % 3]
                        cx.op("pe", lambda: nc.tensor.matmul(
                            po[:, c0:512], vb[:, kt, h * 128:(h + 1) * 128], pT[:, c0:512], start=(kt == 0), stop=(kt == nk - 1)),
                            reads=[vb, pT], pwrites=[po])
                        cx.op("pe", lambda: nc.tensor.matmul(
                            pd[:, c0:512], ones_b[:, :], pT[:, c0:512], start=(kt == 0), stop=(kt == nk - 1)),
                            reads=[ones_b, pT], pwrites=[pd])

                    s_step(0, cnt)
                    for kt in range(nk):
                        if kt + 1 < nk:
                            s_step(kt + 1, cnt + kt + 1)
                        v_step(kt, cnt + kt)
                    cnt += nk
                    cx.op("dve", lambda rd=rd, pd=pd: nc.vector.reciprocal(rd[:, :], pd[:, :]), reads=[pd], writes=[rd])
                    cx.op("dve", lambda yb=yb, po=po, rd=rd, Q=Q: nc.vector.tensor_tensor(
                        out=yb[:, Q * 512:(Q + 1) * 512], in0=po[:, :], in1=rd[:, :], op=ALU.mult), reads=[po, rd], pwrites=[yb])
                cx.dma("sp", yT_d[8 + h], yb[:, :], reads=[yb])
            cx.barrier()

        if upto >= 6:
          with ExitStack() as ph:
            wpa = sb(ph, "wpa", [128, 8, D], BF16)
            wpb = sb(ph, "wpb", [128, 8, D], BF16)
            yas = [sb(ph, "ya%d" % i, [128, 8, 256], BF16) for i in range(2)]
            ybs = [sb(ph, "ybm%d" % i, [128, 8, 256], BF16) for i in range(2)]
            gts = [sb(ph, "gt%d" % i, [128, 32, 256], BF16) for i in range(2)]
            yfs = [sb(ph, "yf%d" % i, [128, 16, 256], BF16) for i in range(2)]
            tas = [sb(ph, "tam%d" % i, [128, 256]) for i in range(2)]
            tbs = [sb(ph, "tbm%d" % i, [128, 256]) for i in range(2)]
            ppa = [ps(ph, "ppa%d" % i, [128, 512]) for i in range(2)]
            ppb = [ps(ph, "ppb%d" % i, [128, 512]) for i in range(2)]
            for hh in range(2):
                cx.dma("pool", wpa[:, hh * 4:(hh + 1) * 4, :], wpa_d[hh * 512:(hh + 1) * 512, :].rearrange("(h p) c -> p h c", p=128), pwrites=[wpa])
                cx.dma("pool", wpb[:, hh * 4:(hh + 1) * 4, :], wpb_d[hh * 512:(hh + 1) * 512, :].rearrange("(h p) c -> p h c", p=128), pwrites=[wpb])
            n = 0
            for g in range(16):
                ya, yb, gt, yf = yas[g % 2], ybs[g % 2], gts[g % 2], yfs[g % 2]
                ts = slice(g * 256, (g + 1) * 256)
                cx.dma("sp", ya[:, :, :], yT_d[0:8, :, ts].rearrange("h p t -> p h t"), writes=[ya])
                cx.dma("sp", yb[:, :, :], yT_d[8:16, :, ts].rearrange("h p t -> p h t"), writes=[yb])
                for a in range(4):
                    cx.dma("sp", gt[:, a * 8:(a + 1) * 8, :], featT_d[33 + a * 8:33 + (a + 1) * 8, :, ts].rearrange("c p t -> p c t"), pwrites=[gt])
                for oc in range(16):
                    pa, pb, ta, tb = ppa[n % 2], ppb[n % 2], tas[n % 2], tbs[n % 2]
                    n += 1
                    os_ = slice(oc * 128, (oc + 1) * 128)
                    for h in range(8):
                        cx.op("pe", lambda pa=pa, ya=ya, h=h, os_=os_: nc.tensor.matmul(
                            pa[:, 0:256], wpa[:, h, os_], ya[:, h, :], start=(h == 0), stop=(h == 7)), reads=[wpa, ya], pwrites=[pa])
                    for h in range(8):
                        cx.op("pe", lambda pb=pb, yb=yb, h=h, os_=os_: nc.tensor.matmul(
                            pb[:, 0:256], wpb[:, h, os_], yb[:, h, :], start=(h == 0), stop=(h == 7)), reads=[wpb, yb], pwrites=[pb])
                    cx.op("dve", lambda ta=ta, pa=pa, gt=gt, oc=oc: nc.vector.tensor_tensor(out=ta[:, :], in0=pa[:, 0:256], in1=gt[:, oc, :], op=ALU.mult),
                          reads=[pa, gt], writes=[ta])
                    cx.op("dve", lambda tb=tb, pb=pb, gt=gt, oc=oc: nc.vector.tensor_tensor(out=tb[:, :], in0=pb[:, 0:256], in1=gt[:, 16 + oc, :], op=ALU.mult),
                          reads=[pb, gt], writes=[tb])
                    cx.op("pool", lambda yf=yf, ta=ta, tb=tb, oc=oc: nc.gpsimd.tensor_tensor(out=yf[:, oc, :], in0=ta[:, :], in1=tb[:, :], op=ALU.add),
                          reads=[ta, tb], pwrites=[yf])
                for a in range(2):
                    cx.dma("sp", yfT_d[a * 8:(a + 1) * 8, :, ts].rearrange("c p t -> p c t"), yf[:, a * 8:(a + 1) * 8, :], reads=[yf])
            cx.barrier()

        def layer_norm_tile(r, lng, lnb, dst, stats, mv, rs_t):
            for k in range(4):
                cx.op("dve", lambda k=k: nc.vector.bn_stats(stats[:, k, :], r[:, k * 512:(k + 1) * 512]), reads=[r], pwrites=[stats])
            cx.op("dve", lambda: nc.vector.bn_aggr(mv[:, :], stats[:, :, :].rearrange("p a b -> p (a b)")), reads=[stats], writes=[mv])
            cx.op("act", lambda: nc.scalar.activation(rs_t[:, 0:1], mv[:, 1:2], AF.Sqrt, bias=LN_EPS), reads=[mv], writes=[rs_t])
            cx.op("dve", lambda: nc.vector.reciprocal(rs_t[:, 1:2], rs_t[:, 0:1]), writes=[rs_t])
            cx.op("dve", lambda: nc.vector.tensor_scalar(out=r[:, :], in0=r[:, :], scalar1=mv[:, 0:1], scalar2=rs_t[:, 1:2],
                                                         op0=ALU.subtract, op1=ALU.mult), reads=[mv, rs_t], writes=[r])
            cx.op("pool", lambda: nc.gpsimd.tensor_tensor(out=r[:, :], in0=r[:, :], in1=lng[:, :], op=ALU.mult), reads=[lng], writes=[r])
            cx.op("pool", lambda: nc.gpsimd.tensor_tensor(out=dst[:, :], in0=r[:, :], in1=lnb[:, :], op=ALU.add), reads=[r, lnb], writes=[dst])

        if upto >= 7:
          with ExitStack() as ph:
            wo = sb(ph, "wo", [128, 16, D], BF16)
            lng = sb(ph, "ln1g", [128, D])
            lnb = sb(ph, "ln1b", [128, D])
            yfs = [sb(ph, "yfo%d" % i, [128, 16, 512], BF16) for i in range(2)]
            xts = [sb(ph, "xt%d" % i, [128, D]) for i in range(2)]
            rts = [sb(ph, "rt%d" % i, [128, D]) for i in range(1)]
            x1s = [sb(ph, "x1t%d" % i, [128, D]) for i in range(2)]
            h2s = [sb(ph, "h2s%d" % i, [128, 16, 512], BF16) for i in range(1)]
            stats = sb(ph, "stats", [128, 4, 6])
            mv = sb(ph, "mv", [128, 2])
            rs_t = sb(ph, "rs_t", [128, 2])
            pob = [ps(ph, "pob%d" % i, [128, 512]) for i in range(4)]
            ptb = [ps(ph, "ptb%d" % i, [128, 512]) for i in range(4)]
            for a in range(4):
                cx.dma("pool", wo[:, a * 4:(a + 1) * 4, :], wo_d[a * 512:(a + 1) * 512, :].rearrange("(k p) c -> p k c", p=128), pwrites=[wo])
            cx.dma("sp", lng[:, :], ln_d[0], writes=[lng])
            cx.dma("sp", lnb[:, :], ln_d[1], writes=[lnb])
            npt = 0
            for g in range(8):
                yf, h2b = yfs[g % 2], h2s[0]
                for a in range(2):
                    cx.dma("sp", yf[:, a * 8:(a + 1) * 8, :], yfT_d[a * 8:(a + 1) * 8, :, g * 512:(g + 1) * 512].rearrange("c p t -> p c t"), pwrites=[yf])
                for tl in range(4):
                    tt = g * 4 + tl
                    xt, r, x1t = xts[tt % 2], rts[0], x1s[tt % 2]
                    cx.dma("sp", xt[:, :], x_d[tt * 128:(tt + 1) * 128, :], writes=[xt])
                    for cg in range(4):
                        for oc in range(16):
                            cx.op("pe", lambda cg=cg, oc=oc, yf=yf, tl=tl: nc.tensor.matmul(
                                pob[cg][:, :], yf[:, oc, tl * 128:(tl + 1) * 128], wo[:, oc, cg * 512:(cg + 1) * 512],
                                start=(oc == 0), stop=(oc == 15)), reads=[yf, wo], pwrites=[pob[cg]])
                        cx.op("dve", lambda cg=cg, r=r: nc.vector.tensor_tensor(
                            out=r[:, cg * 512:(cg + 1) * 512], in0=pob[cg][:, :], in1=g1bc[:, cg * 512:(cg + 1) * 512], op=ALU.mult),
                            reads=[pob[cg], g1bc], pwrites=[r])
                    cx.op("dve", lambda r=r, xt=xt: nc.vector.scalar_tensor_tensor(
                        out=r[:, :], in0=xt[:, :], scalar=DN_ALPHA, in1=r[:, :], op0=ALU.mult, op1=ALU.add), reads=[xt], writes=[r])
                    layer_norm_tile(r, lng, lnb, x1t, stats, mv, rs_t)
                    cx.dma("sp", x1_d[tt * 128:(tt + 1) * 128, :], x1t[:, :], reads=[x1t])
                    for q4 in range(4):
                        pt = ptb[npt % 4]
                        npt += 1
                        for i in range(4):
                            dc = q4 * 4 + i
                            cx.op("pe", lambda pt=pt, x1t=x1t, i=i, dc=dc: nc.tensor.transpose(
                                pt[:, i * 128:(i + 1) * 128], x1t[:, dc * 128:(dc + 1) * 128], ident[:, :]), reads=[x1t, ident], pwrites=[pt])
                        for i in range(4):
                            dc = q4 * 4 + i
                            cx.op("act", lambda pt=pt, h2b=h2b, i=i, dc=dc, tl=tl: nc.scalar.activation(
                                h2b[:, dc, tl * 128:(tl + 1) * 128], pt[:, i * 128:(i + 1) * 128], AF.Identity,
                                bias=b2[:, dc:dc + 1], scale=s2[:, dc:dc + 1]), reads=[pt, s2, b2], pwrites=[h2b])
                for a in range(2):
                    cx.dma("sp", h2T_d[a * 8:(a + 1) * 8, :, g * 512:(g + 1) * 512].rearrange("c p t -> p c t"), h2b[:, a * 8:(a + 1) * 8, :], reads=[h2b])
            cx.barrier()

        es_g1.close()
        if upto >= 8:
          with ExitStack() as ph:
            wq = sb(ph, "wq", [128, 16, D], BF16)
            keysT = sb(ph, "keysT", [128, 16, 128], BF16)
            h2g = [sb(ph, "h2g%d" % i, [128, 16, 512], BF16) for i in range(2)]
            qTs = [sb(ph, "qTs%d" % i, [128, 16, 512], BF16) for i in range(2)]
            scb = [sb(ph, "scb%d" % i, [128, 8, 2, 128]) for i in range(2)]
            sv = sb(ph, "sv", [128, 8, 2, 16])
            tmp = sb(ph, "tk_tmp", [128, 128])
            c16 = sb(ph, "c16", [128, 8, 16, 16])
            c8 = sb(ph, "c8", [128, 8, 24])
            tmp2 = sb(ph, "tk_tmp2", [128, 256])
            tmp3 = sb(ph, "tk_tmp3", [128, 256])
            d16 = sb(ph, "d16", [128, 8, 16])
            zz = sb(ph, "zz", [128, 8])
            lz = sb(ph, "lz", [128, 8])
            idx = sb(ph, "idx", [128, 8, 16], mybir.dt.uint32)
            pkb = [sb(ph, "pkb%d" % i, [128, 272]) for i in range(2)]
            pq = [ps(ph, "pq%d" % i, [128, 512]) for i in range(4)]
            psc = [ps(ph, "psc%d" % i, [128, 512]) for i in range(4)]
            for a in range(4):
                cx.dma("pool", wq[:, a * 4:(a + 1) * 4, :], wq_d[a * 512:(a + 1) * 512, :].rearrange("(k p) c -> p k c", p=128), pwrites=[wq])
            cx.dma("pool", keysT[:, :, :], keysT_d, writes=[keysT])
            npq = 0
            for g in range(8):
                hg, qt = h2g[g % 2], qTs[g % 2]
                for a in range(2):
                    cx.dma("sp", hg[:, a * 8:(a + 1) * 8, :], h2T_d[a * 8:(a + 1) * 8, :, g * 512:(g + 1) * 512].rearrange("c p t -> p c t"), pwrites=[hg])
                for hp in range(16):
                    p = pq[npq % 4]
                    npq += 1
                    for dc in range(16):
                        cx.op("pe", lambda p=p, hg=hg, dc=dc, hp=hp: nc.tensor.matmul(
                            p[:, :], wq[:, dc, hp * 128:(hp + 1) * 128], hg[:, dc, :], start=(dc == 0), stop=(dc == 15)),
                            reads=[wq, hg], pwrites=[p])
                    cx.op("act", lambda p=p, qt=qt, hp=hp: nc.scalar.copy(qt[:, hp, :], p[:, :]), reads=[p], pwrites=[qt])
                for tl in range(4):
                    tt = g * 4 + tl
                    sc = scb[tt % 2]
                    for bk in range(4):
                        for i in range(4):
                            hp = bk * 4 + i
                            cx.op("pe", lambda bk=bk, i=i, hp=hp, qt=qt, tl=tl: nc.tensor.matmul(
                                psc[bk][:, i * 128:(i + 1) * 128], qt[:, hp, tl * 128:(tl + 1) * 128], keysT[:, hp, :], start=True, stop=True),
                                reads=[qt, keysT], pwrites=[psc[bk]])
                        cx.op("act", lambda bk=bk, sc=sc: nc.scalar.copy(
                            sc[:, bk * 2:(bk + 1) * 2, :, :], psc[bk][:, :].rearrange("p (a b c) -> p a b c", a=2, b=2)),
                            reads=[psc[bk]], pwrites=[sc])
                    cx.dma("sp", scs_d[tt * 128:(tt + 1) * 128, :], sc[:, :, :, :].rearrange("p a b c -> p (a b c)"), reads=[sc])
                    for h in range(8):
                        for p_ in range(2):
                            cx.op("dve", lambda h=h, p_=p_, sc=sc: nc.vector.max(out=sv[:, h, p_, 0:8], in_=sc[:, h, p_, :]), reads=[sc], pwrites=[sv])
                            cx.op("dve", lambda h=h, p_=p_, sc=sc: nc.vector.match_replace(
                                out=tmp[:, :], in_to_replace=sv[:, h, p_, 0:8], in_values=sc[:, h, p_, :], imm_value=-1e30), reads=[sc, sv], writes=[tmp])
                            cx.op("dve", lambda h=h, p_=p_: nc.vector.max(out=sv[:, h, p_, 8:16], in_=tmp[:, :]), reads=[tmp], pwrites=[sv])
                    for h in range(8):
                        for r8 in range(2):
                            cx.op("dve", lambda h=h, r8=r8, sc=sc: nc.vector.max_index(
                                out=idx[:, h, r8 * 8:(r8 + 1) * 8], in_max=sv[:, h, 0, r8 * 8:(r8 + 1) * 8], in_values=sc[:, h, 0, :]),
                                reads=[sv, sc], pwrites=[idx])
                    cx.op("dve", lambda: nc.vector.tensor_tensor(
                        out=c16[:, :, :, :], in0=bcast(sv[:, :, 0, :], 3, [128, 8, 16, 16]), in1=bcast(sv[:, :, 1, :], 2, [128, 8, 16, 16]), op=ALU.add),
                        reads=[sv], writes=[c16])
                    for h in range(8):
                        cflat = c16[:, h, :, :].rearrange("p a b -> p (a b)")
                        cx.op("dve", lambda h=h, cflat=cflat: nc.vector.max(out=c8[:, h, 0:8], in_=cflat), reads=[c16], pwrites=[c8])
                        cx.op("dve", lambda h=h, cflat=cflat: nc.vector.match_replace(
                            out=tmp2[:, :], in_to_replace=c8[:, h, 0:8], in_values=cflat, imm_value=-1e30), reads=[c16, c8], writes=[tmp2])
                        cx.op("dve", lambda h=h: nc.vector.max(out=c8[:, h, 8:16], in_=tmp2[:, :]), reads=[tmp2], pwrites=[c8])
                        cx.op("dve", lambda h=h: nc.vector.match_replace(
                            out=tmp3[:, :], in_to_replace=c8[:, h, 8:16], in_values=tmp2[:, :], imm_value=-1e30), reads=[tmp2, c8], writes=[tmp3])
                        cx.op("dve", lambda h=h: nc.vector.max(out=c8[:, h, 16:24], in_=tmp3[:, :]), reads=[tmp3], pwrites=[c8])
                    cx.op("dve", lambda: nc.vector.tensor_tensor(out=zz[:, :], in0=c8[:, :, 15], in1=c8[:, :, 16], op=ALU.add),
                          reads=[c8], writes=[zz])
                    cx.op("dve", lambda tt=tt: nc.vector.tensor_scalar(out=prm[:, tt, 0:8], in0=zz[:, :], scalar1=0.5, scalar2=None, op0=ALU.mult),
                          reads=[zz], pwrites=[prm])
                    cx.op("dve", lambda: nc.vector.tensor_tensor(out=d16[:, :, :], in0=c8[:, :, 0:16], in1=bcast(c8[:, :, 0], 2, [128, 8, 16]), op=ALU.subtract),
                          reads=[c8], writes=[d16])
                    cx.op("act", lambda: nc.scalar.activation(d16[:, :, :], d16[:, :, :], AF.Exp), writes=[d16])
                    cx.op("dve", lambda: nc.vector.tensor_reduce(out=zz[:, :], in_=d16[:, :, :], axis=AX.X, op=ALU.add), reads=[d16], writes=[zz])
                    cx.op("act", lambda: nc.scalar.activation(lz[:, :], zz[:, :], AF.Ln), reads=[zz], writes=[lz])
                    cx.op("dve", lambda tt=tt: nc.vector.scalar_tensor_tensor(
                        out=prm[:, tt, 8:16], in0=c8[:, :, 0], scalar=-1.0, in1=lz[:, :], op0=ALU.mult, op1=ALU.subtract),
                        reads=[c8, lz], pwrites=[prm])
                    pk = pkb[tt % 2]
                    cx.op("dve", lambda pk=pk, tt=tt: nc.vector.tensor_tensor(
                        out=pk[:, 0:128].rearrange("p (h k) -> p h k", h=8), in0=sv[:, :, 0, :], in1=bcast(prm[:, tt, 8:16], 2, [128, 8, 16]), op=ALU.add),
                        reads=[sv, prm], pwrites=[pk])
                    cx.op("dve", lambda pk=pk: nc.vector.tensor_copy(pk[:, 128:256].rearrange("p (h k) -> p h k", h=8), idx[:, :, :]),
                          reads=[idx], pwrites=[pk])
                    cx.op("dve", lambda pk=pk, tt=tt: nc.vector.tensor_tensor(out=pk[:, 256:264], in0=prm[:, tt, 0:8], in1=prm[:, tt, 8:16], op=ALU.add),
                          reads=[prm], pwrites=[pk])
                    cx.op("dve", lambda pk=pk: nc.vector.memset(pk[:, 264:272], 0.0), pwrites=[pk])
                    cx.dma("sp", pk_d[tt * 128:(tt + 1) * 128, :], pk[:, :], reads=[pk])
            if dbgprm_d is not None:
                cx.dma("sp", dbgprm_d, prm[:, :, :], reads=[prm])
            cx.barrier()

        if upto >= 9:
          with ExitStack() as ph:
            NTT = NT // 2
            FP8 = mybir.dt.float8e4
            lng = sb(ph, "ln2g", [128, D])
            lnb = sb(ph, "ln2b", [128, D])
            iota = sb(ph, "iota", [128, 128])
            hgs = [sb(ph, "h2p%d" % i, [128, 16, 128], BF16) for i in range(2)]
            s2bs = [sb(ph, "s2b%d" % i, [128, 8, 128]) for i in range(2)]
            pks = [sb(ph, "pkp%d" % i, [128, 272]) for i in range(2)]
            x1t = sb(ph, "x1p", [128, D])
            accs = [sb(ph, "acc%d" % i, [128, D]) for i in range(2)]
            cmb = sb(ph, "cmb", [128, 128, 16])
            ee = sb(ph, "eeR", [128, 2048], BF16)
            Rp = sb(ph, "Rp", [128, 128, 16], BF16)
            RTo = [sb(ph, "RTo%d" % i, [128, 16, 128], BF16) for i in range(2)]
            si1T = [sb(ph, "si1T%d" % i, [128, 128]) for i in range(2)]
            O1T = [sb(ph, "O1T%d" % i, [128, 128, 128], FP8) for i in range(2)]
            GTo = sb(ph, "GTo", [128, 128, 16], BF16)
            gqs = [[sb(ph, "gq%d_%d" % (i, k), [128, 2048], BF16) for k in range(2)] for i in range(2)]
            uts = [sb(ph, "ut%d" % i, [128, 16, 512], BF16) for i in range(2)]
            vcs = [sb(ph, "vc%d" % i, [128, 2, D], BF16) for i in range(2)]
            gas = [sb(ph, "ga%d" % i, [128, 512], BF16) for i in range(2)]
            wws = [sb(ph, "ww%d" % i, [128, 512], BF16) for i in range(2)]
            wTs = [sb(ph, "wT%d" % i, [128, 4, 128], BF16) for i in range(2)]
            stats = sb(ph, "stats2", [128, 4, 6])
            mv = sb(ph, "mv2", [128, 2])
            rs_t = sb(ph, "rs_t2", [128, 2])
            pop = [ps(ph, "pop%d" % i, [128, 512]) for i in range(4)]
            pap = [ps(ph, "pap%d" % i, [128, 512]) for i in range(2)]
            pgf = ps(ph, "pgf", [128, 512])
            pgb = ps(ph, "pgb", [128, 1024], BF16)
            cx.dma("sp", lng[:, :], ln_d[2], writes=[lng])
            cx.dma("sp", lnb[:, :], ln_d[3], writes=[lnb])
            cx.dma("sp", iota[:, :], iota_d, writes=[iota])

            def load_small(T_):
                for sub in range(2):
                    tt = 2 * T_ + sub
                    cx.dma("sp", s2bs[sub][:, :, :], scs_d[tt * 128:(tt + 1) * 128, :].rearrange("t (h p n) -> t h p n", h=8, p=2)[:, :, 1, :], writes=[s2bs[sub]])
                    cx.dma("sp", pks[sub][:, :], pk_d[tt * 128:(tt + 1) * 128, :], writes=[pks[sub]])

            def load_hg(T_, sub):
                tt = 2 * T_ + sub
                hg = hgs[sub]
                for a in range(2):
                    cx.dma("sp", hg[:, a * 8:(a + 1) * 8, :], h2T_d[a * 8:(a + 1) * 8, :, tt * 128:(tt + 1) * 128].rearrange("c p t -> p c t"), pwrites=[hg])

            def tile_prep_items(T_):
                items = []
                for sub in range(2):
                    def s_item(sub=sub):
                        cx.op("pe", lambda: nc.tensor.transpose(pgf[:, 0:128], pks[sub][:, 128:256], ident[:, :]), reads=[pks[sub], ident], pwrites=[pgf])
                        cx.op("dve", lambda: nc.vector.tensor_copy(si1T[sub][:, :], pgf[:, 0:128]), reads=[pgf], writes=[si1T[sub]])

                    def o1(sub=sub):
                        cx.op("dve", lambda: nc.vector.tensor_tensor(
                            out=O1T[sub][:, :, :], in0=bcast(si1T[sub][:, :], 2, [128, 128, 128]), in1=bcast(iota[:, :], 1, [128, 128, 128]),
                            op=ALU.is_equal, saturate=False), reads=[si1T[sub], iota], writes=[O1T[sub]])
                    items += [s_item, o1]
                return items

            def oct_items(T_, o):
                j0 = o * 16
                parts = {"elem": [], "rb": [[], []], "pt": [[], []], "gt": [[], []]}
                for sub in range(2):
                    s2b, pk = s2bs[sub], pks[sub]
                    gq = gqs[sub][(T_ * 8 + o) % 2]

                    def elem(s2b=s2b, pk=pk):
                        cx.op("dve", lambda: nc.vector.tensor_tensor(
                            out=cmb[:, :, :].rearrange("p (h k) j -> p h k j", h=8),
                            in0=bcast(pk[:, 0:128].rearrange("p (h k) -> p h k", h=8), 3, [128, 8, 16, 16]),
                            in1=bcast(s2b[:, :, j0:j0 + 16], 2, [128, 8, 16, 16]), op=ALU.add), reads=[pk, s2b], writes=[cmb])
                        cx.op("act", lambda: nc.scalar.activation(ee[:, :], cmb[:, :, :].rearrange("p a b -> p (a b)"), AF.Exp),
                              reads=[cmb], writes=[ee])
                        cx.op("dve", lambda: nc.vector.tensor_tensor(
                            out=Rp[:, :, :].rearrange("p (h k) j -> p h (k j)", h=8), in0=cmb[:, :, :].rearrange("p (h k) j -> p h (k j)", h=8),
                            in1=bcast(pk[:, 256:264], 2, [128, 8, 256]), op=ALU.is_ge), reads=[cmb, pk], writes=[Rp])
                        cx.op("pool", lambda: nc.gpsimd.tensor_tensor(out=Rp[:, :, :].rearrange("p a b -> p (a b)"),
                                                                      in0=Rp[:, :, :].rearrange("p a b -> p (a b)"), in1=ee[:, :], op=ALU.mult),
                              reads=[ee], writes=[Rp])
                    parts["elem"].append(elem)
                    for b_ in range(2):
                        def rb(b_=b_, sub=sub):
                            for i in range(8):
                                cx.op("pe", lambda i=i: nc.tensor.transpose(pgb[:, i * 128:(i + 1) * 128], Rp[:, :, b_ * 8 + i], identb[:, :]),
                                      reads=[Rp, identb], pwrites=[pgb])
                            cx.op("act", lambda: nc.scalar.copy(
                                RTo[sub][:, b_ * 8:b_ * 8 + 8, :], pgb[:, :].rearrange("p (j t) -> p j t", t=128)),
                                reads=[pgb], pwrites=[RTo[sub]])
                        parts["rb"][sub].append(rb)
                    for tb in range(4):
                        def pt(tb=tb, sub=sub):
                            for tl in range(32):
                                t = tb * 32 + tl
                                cx.op("pe", lambda tl=tl, t=t: nc.tensor.matmul(pgf[:, tl * 16:(tl + 1) * 16], O1T[sub][:, t, :], RTo[sub][:, :, t], start=True, stop=True),
                                      reads=[RTo[sub], O1T[sub]], pwrites=[pgf])
                            cx.op("dve", lambda: nc.vector.tensor_copy(
                                GTo[:, tb * 32:(tb + 1) * 32, :], pgf[:, :].rearrange("p (t j) -> p t j", j=16)), reads=[pgf], pwrites=[GTo])
                        parts["pt"][sub].append(pt)
                    for gb in range(2):
                        def gt(gb=gb, gq=gq):
                            for jl in range(8):
                                cx.op("pe", lambda jl=jl: nc.tensor.transpose(pgb[:, jl * 128:(jl + 1) * 128], GTo[:, :, gb * 8 + jl], identb[:, :]),
                                      reads=[GTo, identb], pwrites=[pgb])
                            cx.op("act", lambda: nc.scalar.copy(gq[:, gb * 1024:(gb + 1) * 1024], pgb[:, :]), reads=[pgb], pwrites=[gq])
                        parts["gt"][sub].append(gt)
                return parts

            NEG = NTT * 32
            NU = NEG * 2

            def ucoords(u):
                EG, sub = u // 2, u % 2
                T_, eg = EG // 32, EG % 32
                return EG, sub, T_, eg, eg // 4, eg % 4

            def load_ut(EG):
                eg = EG % 32
                ut = uts[EG % 2]
                cx.dma("sp", ut[:, :, :], utb_d[eg * 512:(eg + 1) * 512, :].rearrange("(p k) c -> p (k c)", k=4).rearrange("p (k c) -> p k c", c=512),
                       reads=[utb_t] if EG < 2 else (), writes=[ut])

            def load_vc(EG, half):
                eg = EG % 32
                vc = vcs[half]
                r0 = eg * 512 + half * 256
                cx.dma("sp", vc[:, :, :], vb16_d[r0:r0 + 256, :].rearrange("(k p) d -> p k d", p=128),
                       reads=[vb16_t] if EG < 1 else (), writes=[vc])

            def a_mm(u, part):
                EG, sub, T_, eg, o, e4 = ucoords(u)
                hg, ut, pa, ga, ww = hgs[sub], uts[EG % 2], pap[u % 2], gas[u % 2], wws[u % 2]
                gq = gqs[sub][(T_ * 8 + o) % 2]
                for dc in range(part * 8, part * 8 + 8):
                    cx.op("pe", lambda dc=dc: nc.tensor.matmul(pa[:, :], hg[:, dc, :], ut[:, dc, :], start=(dc == 0), stop=(dc == 15)),
                          reads=[hg, ut], pwrites=[pa])
                if part == 0:
                    return
                cx.op("act", lambda: nc.scalar.activation(ga[:, :], pa[:, :], AF.Gelu), reads=[pa], writes=[ga])
                cx.op("dve", lambda: nc.vector.tensor_tensor(out=ww[:, :], in0=ga[:, :], in1=gq[:, e4 * 512:(e4 + 1) * 512], op=ALU.mult),
                      reads=[ga, gq], writes=[ww])

            def wtr(u):
                ww, wT = wws[u % 2], wTs[u % 2]
                for k in range(4):
                    cx.op("pe", lambda k=k: nc.tensor.transpose(pgb[:, k * 128:(k + 1) * 128], ww[:, k * 128:(k + 1) * 128], identb[:, :]),
                          reads=[ww, identb], pwrites=[pgb])
                cx.op("act", lambda: nc.scalar.copy(wT[:, :, :], pgb[:, 0:512].rearrange("p (a b) -> p a b", b=128)), reads=[pgb], writes=[wT])

            def ph2(u, part):
                wT = wTs[u % 2]
                for k in range(part * 2, part * 2 + 2):
                    vc = vcs[k // 2]
                    for dq in range(4):
                        cx.op("pe", lambda k=k, dq=dq, vc=vc: nc.tensor.matmul(
                            pop[dq][:, :], wT[:, k, :], vc[:, k % 2, dq * 512:(dq + 1) * 512],
                            start=(k == 0), stop=(k == 3)), reads=[wT, vc], pwrites=[pop[dq]])

            def accadd(u):
                EG, sub, T_, eg, o, e4 = ucoords(u)
                acc = accs[sub]
                for dq in range(4):
                    sl = slice(dq * 512, (dq + 1) * 512)
                    if eg == 0:
                        cx.op("dve", lambda dq=dq, sl=sl: nc.vector.tensor_copy(acc[:, sl], pop[dq][:, :]), reads=[pop[dq]], pwrites=[acc])
                    else:
                        cx.op("dve", lambda dq=dq, sl=sl: nc.vector.tensor_tensor(out=acc[:, sl], in0=pop[dq][:, :], in1=acc[:, sl], op=ALU.add),
                              reads=[pop[dq]], writes=[acc])

            def epilogue(T_, sub):
                tt = 2 * T_ + sub
                acc = accs[sub]
                cx.dma("sp", x1t[:, :], x1_d[tt * 128:(tt + 1) * 128, :], writes=[x1t])
                cx.op("dve", lambda: nc.vector.tensor_tensor(out=acc[:, :], in0=acc[:, :], in1=g2bc[:, :], op=ALU.mult), reads=[g2bc], writes=[acc])
                cx.op("dve", lambda: nc.vector.scalar_tensor_tensor(
                    out=acc[:, :], in0=x1t[:, :], scalar=DN_ALPHA, in1=acc[:, :], op0=ALU.mult, op1=ALU.add), reads=[x1t], writes=[acc])
                layer_norm_tile(acc, lng, lnb, x1t, stats, mv, rs_t)
                cx.dma("sp", out_d[tt * 128:(tt + 1) * 128, :], x1t[:, :], reads=[x1t])

            parts_cache = {}

            def get_parts(T_, o):
                if (T_, o) not in parts_cache:
                    parts_cache[(T_, o)] = oct_items(T_, o)
                return parts_cache[(T_, o)]

            def nxt_oct(T_, o):
                return (T_, o + 1) if o < 7 else (T_ + 1, 0)

            def q_for(T_, o):
                n1 = nxt_oct(T_, o)
                items = []
                if n1[0] < NTT:
                    p = get_parts(*n1)
                    if n1[1] == 0:
                        items += tile_prep_items(n1[0])
                    items += p["rb"][0] + [p["elem"][1]] + p["pt"][0] + p["rb"][1] + p["gt"][0] + p["pt"][1] + p["gt"][1]
                    n2 = nxt_oct(*n1)
                    if n2[0] < NTT:
                        if n2[1] == 0:
                            items.append(lambda: load_small(n2[0]))
                        items.append(get_parts(*n2)["elem"][0])
                return items

            load_small(0)
            load_hg(0, 0)
            load_hg(0, 1)
            get_parts(0, 0)["elem"][0]()
            p0 = get_parts(0, 0)
            for it in tile_prep_items(0) + p0["rb"][0] + [p0["elem"][1]] + p0["pt"][0] + p0["rb"][1] + p0["gt"][0] + p0["pt"][1] + p0["gt"][1] + [get_parts(0, 1)["elem"][0]]:
                it()
            load_ut(0)
            load_ut(1)
            load_vc(0, 0)
            load_vc(0, 1)
            a_mm(0, 0)
            a_mm(0, 1)
            queue = []
            per_slot = 0

            def pop_items(n):
                for _ in range(min(n, len(queue))):
                    queue.pop(0)()

            for u in range(NU):
                EG, sub, T_, eg, o, e4 = ucoords(u)
                last_of_oct = (e4 == 3 and sub == 1)
                if e4 == 0 and sub == 0:
                    queue.extend(q_for(T_, o))
                    per_slot = -(-len(queue) // 8)
                n_left = per_slot
                k1 = (n_left + 3) // 4
                wtr(u)
                if sub == 1 and EG + 2 < NEG:
                    load_ut(EG + 2)
                pop_items(k1); n_left -= k1
                if u + 1 < NU:
                    if (u + 1) % 64 < 2 and u + 1 >= 64:
                        load_hg((u + 1) // 64, (u + 1) % 2)
                    a_mm(u + 1, 0)
                if last_of_oct:
                    pop_items(len(queue))
                else:
                    pop_items(k1); n_left -= k1
                if u + 1 < NU:
                    a_mm(u + 1, 1)
                if u >= 1:
                    ph2(u - 1, 0)
                    if (u - 1) % 2 == 1 and (u - 1) // 2 + 1 < NEG:
                        load_vc((u - 1) // 2 + 1, 0)
                if not last_of_oct:
                    pop_items(k1); n_left -= k1
                if u >= 1:
                    ph2(u - 1, 1)
                    if (u - 1) % 2 == 1 and (u - 1) // 2 + 1 < NEG:
                        load_vc((u - 1) // 2 + 1, 1)
                    accadd(u - 1)
                    pEG, psub, pT, peg, po_, pe4 = ucoords(u - 1)
                    if peg == 31:
                        epilogue(pT, psub)
                if not last_of_oct:
                    pop_items(max(n_left, 0))
            ph2(NU - 1, 0)
            ph2(NU - 1, 1)
            accadd(NU - 1)
            epilogue(NTT - 1, 1)
            cx.barrier()
        cx.barrier()
        print("[kernel] instructions emitted:", cx.nins)
    return nc


def host_layout(inputs):
    f = lambda k: np.asarray(inputs[k])
    shared = {}
    w_in = f("w_in")[0]
    b_in = f("b_in")[0]
    perm = np.concatenate([np.arange(32, 64), np.arange(0, 32)])
    cols = np.concatenate([np.arange(0, 4160), 4096 + perm, np.arange(4160, 8256)])
    wext = w_in[:, cols]
    shared["w_in_l"] = np.ascontiguousarray(wext.reshape(16, 128, 65, 128).transpose(2, 1, 0, 3).reshape(65, 128, 2048))
    shared["b_inT"] = np.ascontiguousarray(b_in[cols].reshape(65, 128).T)
    shared["w_ada"] = np.ascontiguousarray(f("w_ada")[0])
    b_ada = f("b_ada")[0]
    shared["b_adaT"] = np.ascontiguousarray(b_ada.reshape(96, 128).T)
    shared["b_ada_row"] = np.ascontiguousarray(b_ada.reshape(1, -1))
    rb = f("rel_bias")[0]
    p = np.arange(128)[:, None, None]
    j = np.arange(5)[None, :, None]
    c = np.arange(128)[None, None, :]
    rel = 128 * (4 - j) + c - p
    idx = np.clip(rel, -63, 256) + 63
    shared["biasT"] = np.ascontiguousarray(rb[:, idx].transpose(1, 0, 2, 3).reshape(128, 8, 640))
    mask = np.zeros((128, 5, 128), np.float32)
    mask[:64, 0, 64:] = NEGM
    mask[64:, 4, :64] = NEGM
    shared["maskA"] = mask.reshape(128, 640)
    shared["gqT"] = np.ascontiguousarray(f("q_norm_g")[0].reshape(4, 128).T)
    shared["gkvT"] = np.ascontiguousarray(f("kv_norm_g")[0].reshape(4, 128).T)
    w_uq = f("w_uq")[0]
    qcols = []
    for h in range(8):
        base = h * 192
        qcols += [np.arange(base, base + 128), np.arange(base + 128, base + 192), base + 128 + perm]
    qcols = np.concatenate(qcols)
    shared["w_uq_l"] = np.ascontiguousarray(w_uq[:, qcols].reshape(4, 128, 2048).transpose(1, 0, 2))
    w_ukv = f("w_ukv")[0].reshape(512, 8, 256)
    shared["w_uk_l"] = np.ascontiguousarray(w_ukv[:, :, :128].reshape(4, 128, 1024).transpose(1, 0, 2))
    shared["w_uv_l"] = np.ascontiguousarray(w_ukv[:, :, 128:].reshape(4, 128, 1024).transpose(1, 0, 2))
    shared["w_pa"] = np.ascontiguousarray(f("w_pa")[0])
    shared["w_pb"] = np.ascontiguousarray(f("w_pb")[0])
    shared["w_o"] = np.ascontiguousarray(f("w_o")[0])
    shared["ln_bc"] = np.ascontiguousarray(np.stack([np.broadcast_to(f(k)[0][None, :], (128, D)) for k in ("ln1_g", "ln1_b", "ln2_g", "ln2_b")]))
    shared["peer_wq"] = np.ascontiguousarray(f("peer_wq")[0])
    keys = f("peer_keys")[0]
    shared["keysT"] = np.ascontiguousarray(keys.reshape(16, 128, 128).transpose(2, 0, 1))
    U = f("peer_u")[0].reshape(128, 128, D).transpose(1, 0, 2).reshape(16384, D)
    shared["ut_l"] = np.ascontiguousarray(U.reshape(32, 512, 16, 128).transpose(0, 3, 2, 1)).reshape(16384, 2048)
    shared["peer_v"] = np.ascontiguousarray(f("peer_v")[0].reshape(128, 128, D).transpose(1, 0, 2).reshape(16384, D))
    shared["ident"] = np.eye(128, dtype=np.float32)
    shared["iota"] = np.ascontiguousarray(np.broadcast_to(np.arange(128, dtype=np.float32)[None, :], (128, 128)))
    inv_freq = (10000.0 ** (-np.arange(0, 64, 2, dtype=np.float32) / 64.0)).astype(np.float32)
    invf = np.zeros((64, 2), np.float32)
    invf[:, 0] = np.concatenate([inv_freq, inv_freq])
    invf[:32, 1] = -1.0
    invf[32:, 1] = 1.0
    shared["invf"] = invf
    per_core = []
    x = f("x")
    cc = f("c")
    pos = f("positions")
    for b in range(NCORES):
        m = dict(shared)
        m["x"] = np.ascontiguousarray(x[b])
        m["cT"] = np.ascontiguousarray(cc[b].reshape(16, 128).T)
        m["posb"] = np.ascontiguousarray(np.broadcast_to(pos[b][None, :].astype(np.int32), (64, S)))
        per_core.append(m)
    return per_core


_NC_CACHE = {}


def kernel(**inputs):
    maps = host_layout(inputs)
    if "full" not in _NC_CACHE:
        _NC_CACHE["full"] = build()
    nc = _NC_CACHE["full"]
    res = run_bass_kernel_spmd(nc, maps, core_ids=list(range(NCORES)))
    out = np.stack([np.asarray(r["out"]) for r in res.results], axis=0)
    return out.astype(np.float32)
```

```python
import math
from contextlib import ExitStack

import numpy as np
import concourse.bass as bass
import concourse.mybir as mybir
from concourse.bass_utils import run_bass_kernel_spmd

F32 = mybir.dt.float32
BF16 = mybir.dt.bfloat16
I32 = mybir.dt.int32
AF = mybir.ActivationFunctionType
ALU = mybir.AluOpType
AX = mybir.AxisListType

NCORES = 8
S = 4096
D = 2048
NT = S // 128
DN_ALPHA = 2.0 ** 0.25
LN_EPS = 1e-5
RMS_EPS = 1e-6
SCALE_A = 128.0 ** -0.5
SCALE_B = 192.0 ** -0.5
NEGM = -30000.0
MAGIC = 12582912.0
TWO_PI = 2.0 * math.pi
C1 = 6.28125
C2 = TWO_PI - C1
PI_SAFE = 3.14159

SAME_ENG_SYNC = True
CB_ON_POOL = False


class Buf:
    __slots__ = ("w", "r")

    def __init__(self):
        self.w = {}
        self.r = {}


class T:
    def __init__(self, handle):
        self.t = handle
        self.b = Buf()

    def __getitem__(self, k):
        return self.t[k]


class Ctx:
    def __init__(self, nc, es):
        self.nc = nc
        self.eng = {"pe": nc.tensor, "act": nc.scalar, "dve": nc.vector, "pool": nc.gpsimd, "sp": nc.sync}
        self.sems = {}
        self.tot = {}
        for e in ("pe", "act", "dve", "pool"):
            self.sems[e] = es.enter_context(nc.semaphore("s_" + e))
            self.tot[e] = 0
        self.dq = {}
        for q, n in (("sp", 16), ("pool", 8)):
            lst = []
            for i in range(n):
                k = "d_%s%d" % (q, i)
                self.sems[k] = es.enter_context(nc.semaphore(k))
                self.tot[k] = 0
                lst.append(k)
            self.dq[q] = [lst, 0]
        self.seen = {e: {} for e in self.eng}
        self.nins = 0

    def _wait(self, e, deps):
        own = e if e in ("pe", "act", "dve", "pool") else None
        seen = self.seen[e]
        for k, v in deps.items():
            if v <= 0:
                continue
            if k == own and (own == "pe" or not SAME_ENG_SYNC):
                continue
            if seen.get(k, 0) >= v:
                continue
            self.eng[e].wait_ge(self.sems[k], v)
            seen[k] = v

    @staticmethod
    def _merge(d, k, v):
        if d.get(k, 0) < v:
            d[k] = v

    def _deps(self, reads, writes, pwrites):
        deps = {}
        for t in reads:
            for k, v in t.b.w.items():
                self._merge(deps, k, v)
        for t in writes:
            for k, v in t.b.w.items():
                self._merge(deps, k, v)
            for k, v in t.b.r.items():
                self._merge(deps, k, v)
        for t in pwrites:
            for k, v in t.b.r.items():
                self._merge(deps, k, v)
        return deps

    def _update(self, tok, reads, writes, pwrites):
        k, v = tok
        for t in writes:
            t.b.w = {k: v}
            t.b.r = {}
        for t in pwrites:
            if t.b.r:
                t.b.w = {k: v}
                t.b.r = {}
            else:
                self._merge(t.b.w, k, v)
        for t in reads:
            self._merge(t.b.r, k, v)

    def op(self, e, fn, reads=(), writes=(), pwrites=()):
        self._wait(e, self._deps(reads, writes, pwrites))
        ins = fn()
        self.tot[e] += 1
        ins.then_inc(self.sems[e], 1)
        self.nins += 1
        self._update((e, self.tot[e]), reads, writes, pwrites)
        return ins

    def dma(self, q, out, in_, reads=(), writes=(), pwrites=()):
        lst, i = self.dq[q]
        k = lst[i % len(lst)]
        self.dq[q][1] = i + 1
        deps = self._deps(reads, writes, pwrites)
        self._merge(deps, k, self.tot[k])
        self._wait(q, deps)
        ins = self.eng[q].dma_start(out=out, in_=in_)
        self.tot[k] += 16
        ins.then_inc(self.sems[k], 16)
        self.nins += 1
        self._update((k, self.tot[k]), reads, writes, pwrites)
        return ins

    def barrier(self, engines=("pe", "act", "dve", "pool", "sp")):
        for e in engines:
            self._wait(e, dict(self.tot))


def bcast(ap, axis, shape):
    return ap.unsqueeze(axis).broadcast_to(shape)


def build(upto=99, dbg=()):
    nc = bass.Bass("TRN2", target_bir_lowering=False)

    def din(name, shape, dt=F32):
        return nc.dram_tensor(name, list(shape), dt, kind="ExternalInput").ap()

    def dscr(name, shape, dt):
        kind = "ExternalOutput" if name in dbg else "Internal"
        return nc.dram_tensor(name, list(shape), dt, kind=kind).ap()

    x_d = din("x", [S, D])
    cT_d = din("cT", [128, 16])
    pos_d = din("posb", [64, S], I32)
    invf_d = din("invf", [64, 2])
    wada_d = din("w_ada", [D, 6 * D])
    badaT_d = din("b_adaT", [128, 96])
    badar_d = din("b_ada_row", [1, 6 * D])
    win_d = din("w_in_l", [65, 128, 2048])
    binT_d = din("b_inT", [128, 65])
    biasT_d = din("biasT", [128, 8, 640])
    maskA_d = din("maskA", [128, 640])
    gq_d = din("gqT", [128, 4])
    gkv_d = din("gkvT", [128, 4])
    wuq_d = din("w_uq_l", [128, 4, 2048])
    wuk_d = din("w_uk_l", [128, 4, 1024])
    wuv_d = din("w_uv_l", [128, 4, 1024])
    wpa_d = din("w_pa", [1024, D])
    wpb_d = din("w_pb", [1024, D])
    wo_d = din("w_o", [D, D])
    ln_d = din("ln_bc", [4, 128, D])
    wq_d = din("peer_wq", [D, D])
    keysT_d = din("keysT", [128, 16, 128])
    ut_d = din("ut_l", [16384, 2048])
    v_d = din("peer_v", [16384, D])
    ident_d = din("ident", [128, 128])
    iota_d = din("iota", [128, 128])

    out_d = nc.dram_tensor("out", [S, D], F32, kind="ExternalOutput").ap()

    featT_d = dscr("featT", [65, 128, S], BF16)
    yT_d = dscr("yT", [16, 128, S], BF16)
    qnT_d = dscr("qnT", [8, 128, S], BF16)
    qrT_d = dscr("qrT", [8, 64, S], BF16)
    knT_d = dscr("knT", [8, 128, S], BF16)
    krT_d = dscr("krT", [64, S], BF16)
    vbs_d = dscr("vbs", [S, 1024], BF16)
    yfT_d = dscr("yfT", [16, 128, S], BF16)
    x1_d = dscr("x1s", [S, D], F32)
    h2T_d = dscr("h2T", [16, 128, S], BF16)
    scs_d = dscr("scs", [S, 2048], F32)
    pk_d = dscr("pk", [S, 272], F32)
    utb_d = dscr("utb", [16384, 2048], BF16)
    vb16_d = dscr("vb16", [16384, D], BF16)
    dbgmod_d = dscr("dbgmod", [128, 96 + 2], F32) if "dbgmod" in dbg else None
    dbgg_d = dscr("dbgg", [128, 2 * D], F32) if "dbgg" in dbg else None
    dbgprm_d = dscr("dbgprm", [128, NT, 16], F32) if "dbgprm" in dbg else None

    with ExitStack() as es:
        cx = Ctx(nc, es)

        uid = [0]

        def sb(scope, name, shape, dt=F32):
            uid[0] += 1
            return T(scope.enter_context(nc.sbuf_tensor("sb%d_%s" % (uid[0], name), list(shape), dt)))

        def ps(scope, name, shape, dt=F32):
            uid[0] += 1
            return T(scope.enter_context(nc.psum_tensor("ps%d_%s" % (uid[0], name), list(shape), dt)))

        CB_ENG = "pool" if CB_ON_POOL else "dve"
        CB_OBJ = nc.gpsimd if CB_ON_POOL else nc.vector
        ident = sb(es, "ident", [128, 128])
        identb = sb(es, "identb", [128, 128], BF16)
        ones_f = sb(es, "ones_f", [128, 128])
        ones_b = sb(es, "ones_b", [128, 128], BF16)
        s1 = sb(es, "s1", [128, 16])
        b1 = sb(es, "b1", [128, 16])
        s2 = sb(es, "s2", [128, 16])
        b2 = sb(es, "b2", [128, 16])
        g2bc = sb(es, "g2bc", [128, D])
        prm = sb(es, "prm", [128, NT, 16])
        es_g1 = ExitStack()
        g1bc = sb(es_g1, "g1bc", [128, D])

        cx.dma("sp", ident[:, :], ident_d, writes=[ident])
        cx.op("dve", lambda: nc.vector.tensor_copy(identb[:, :], ident[:, :]), reads=[ident], writes=[identb])
        cx.op("dve", lambda: nc.vector.memset(ones_f[:, :], 1.0), writes=[ones_f])
        cx.op("dve", lambda: nc.vector.memset(ones_b[:, :], 1.0), writes=[ones_b])

        utb_t = T(None)
        vb16_t = T(None)

        def cast_tables(i):
            if upto < 6 or i >= 64:
                return
            if i < 32:
                cx.dma("pool", utb_d[i * 512:(i + 1) * 512, :], ut_d[i * 512:(i + 1) * 512, :], pwrites=[utb_t])
            else:
                i -= 32
                cx.dma("pool", vb16_d[i * 512:(i + 1) * 512, :], v_d[i * 512:(i + 1) * 512, :], pwrites=[vb16_t])

        with ExitStack() as ph:
            cT = sb(ph, "cT", [128, 16])
            scT = sb(ph, "scT", [128, 16])
            badaT = sb(ph, "badaT", [128, 96])
            badar = sb(ph, "badar", [1, 6 * D])
            modT = sb(ph, "modT", [128, 96])
            wst = [sb(ph, "wst%d" % i, [128, 16, 512]) for i in range(2)]
            rowsb = [sb(ph, "rowsb%d" % i, [1, 512]) for i in range(2)]
            pm = ps(ph, "pm", [128, 512])
            pr = [ps(ph, "pr%d" % i, [128, 512]) for i in range(2)]
            pbc = [ps(ph, "pbc%d" % i, [128, 512]) for i in range(2)]
            cx.dma("sp", cT[:, :], cT_d, writes=[cT])
            cx.dma("sp", badaT[:, :], badaT_d, writes=[badaT])
            cx.dma("sp", badar[:, :], badar_d, writes=[badar])
            cx.op("act", lambda: nc.scalar.activation(scT[:, :], cT[:, :], AF.Silu), reads=[cT], writes=[scT])
            nrow = 0
            for gi in range(24):
                m = gi // 4
                w = wst[gi % 2]
                cx.dma("sp", w[:, :, :], wada_d[:, gi * 512:(gi + 1) * 512].rearrange("(k p) c -> p k c", p=128), writes=[w])
                if m in (0, 1, 3, 4):
                    for cc in range(4):
                        col = m * 16 + (gi % 4) * 4 + cc
                        for kc in range(16):
                            cx.op("pe", lambda kc=kc, cc=cc, col=col, w=w: nc.tensor.matmul(
                                pm[:, col:col + 1], w[:, kc, cc * 128:(cc + 1) * 128], scT[:, kc:kc + 1],
                                start=(kc == 0), stop=(kc == 15)), reads=[w, scT], pwrites=[pm])
                else:
                    p_r = pr[nrow % 2]
                    rs = rowsb[nrow % 2]
                    p_b = pbc[nrow % 2]
                    nrow += 1
                    for kc in range(16):
                        cx.op("pe", lambda kc=kc, w=w, p_r=p_r: nc.tensor.matmul(
                            p_r[0:1, :], scT[:, kc:kc + 1], w[:, kc, :], start=(kc == 0), stop=(kc == 15)),
                            reads=[w, scT], pwrites=[p_r])
                    cx.op("dve", lambda p_r=p_r, rs=rs, gi=gi: nc.vector.tensor_tensor(
                        out=rs[0:1, :], in0=p_r[0:1, :], in1=badar[0:1, gi * 512:(gi + 1) * 512], op=ALU.add),
                        reads=[p_r, badar], writes=[rs])
                    cx.op("pe", lambda rs=rs, p_b=p_b: nc.tensor.matmul(
                        p_b[:, :], ones_f[0:1, :], rs[0:1, :], start=True, stop=True), reads=[rs, ones_f], pwrites=[p_b])
                    dst = g1bc if m == 2 else g2bc
                    cx.op("act", lambda dst=dst, p_b=p_b, gi=gi: nc.scalar.copy(
                        dst[:, (gi % 4) * 512:(gi % 4 + 1) * 512], p_b[:, :]), reads=[p_b], pwrites=[dst])
            cx.op("dve", lambda: nc.vector.tensor_tensor(out=modT[:, :], in0=pm[:, 0:96], in1=badaT[:, :], op=ALU.add),
                  reads=[pm, badaT], writes=[modT])
            cx.op("dve", lambda: nc.vector.tensor_scalar(out=s1[:, :], in0=modT[:, 16:32], scalar1=1.0, scalar2=None, op0=ALU.add),
                  reads=[modT], writes=[s1])
            cx.op("dve", lambda: nc.vector.tensor_copy(b1[:, :], modT[:, 0:16]), reads=[modT], writes=[b1])
            cx.op("dve", lambda: nc.vector.tensor_scalar(out=s2[:, :], in0=modT[:, 64:80], scalar1=1.0, scalar2=None, op0=ALU.add),
                  reads=[modT], writes=[s2])
            cx.op("dve", lambda: nc.vector.tensor_copy(b2[:, :], modT[:, 48:64]), reads=[modT], writes=[b2])
            if dbgmod_d is not None:
                cx.dma("sp", dbgmod_d[:, 0:96], modT[:, :], reads=[modT])
            if dbgg_d is not None:
                cx.dma("sp", dbgg_d[:, 0:D], g1bc[:, :], reads=[g1bc])
                cx.dma("sp", dbgg_d[:, D:2 * D], g2bc[:, :], reads=[g2bc])
            cx.barrier()

        if upto >= 1:
          with ExitStack() as ph12:
            hT = sb(ph12, "hT", [128, 16, S], BF16)
            with ExitStack() as ph:
                xs = [sb(ph, "xs%d" % i, [128, 2, D]) for i in range(2)]
                ptr = [ps(ph, "ptr%d" % i, [128, 512]) for i in range(4)]
                n = 0
                for g in range(16):
                    xb = xs[g % 2]
                    cx.dma("sp", xb[:, :, :], x_d[g * 256:(g + 1) * 256, :].rearrange("(j p) d -> p j d", p=128), writes=[xb])
                    for dc in range(16):
                        pt = ptr[n % 4]
                        n += 1
                        for j in range(2):
                            cx.op("pe", lambda pt=pt, xb=xb, j=j, dc=dc: nc.tensor.transpose(
                                pt[:, j * 128:(j + 1) * 128], xb[:, j, dc * 128:(dc + 1) * 128], ident[:, :]),
                                reads=[xb, ident], pwrites=[pt])
                        cx.op("act", lambda pt=pt, g=g, dc=dc: nc.scalar.activation(
                            hT[:, dc, g * 256:(g + 1) * 256], pt[:, 0:256], AF.Identity,
                            bias=b1[:, dc:dc + 1], scale=s1[:, dc:dc + 1]), reads=[pt, s1, b1], pwrites=[hT])
                cx.barrier()
            with ExitStack() as ph:
                wc = [sb(ph, "wc%d" % i, [128, 16, 128], BF16) for i in range(3)]
                ost = [sb(ph, "ost%d" % i, [128, S], BF16) for i in range(2)]
                binT = sb(ph, "binT", [128, 65])
                pz = [ps(ph, "pz%d" % i, [128, 512]) for i in range(4)]
                cx.dma("sp", binT[:, :], binT_d, writes=[binT])
                n = 0
                for ch in range(65):
                    w = wc[ch % 3]
                    cx.dma("pool", w[:, :, :], win_d[ch].rearrange("p (k c) -> p k c", c=128), writes=[w])
                    if ch >= 2:
                        cast_tables(ch - 2)
                        if ch == 64:
                            cast_tables(63)
                    o = ost[ch % 2]
                    func = AF.Sigmoid if ch >= 33 else AF.Identity
                    for g in range(8):
                        p = pz[n % 4]
                        n += 1
                        for kc in range(16):
                            cx.op("pe", lambda p=p, w=w, kc=kc, g=g: nc.tensor.matmul(
                                p[:, :], w[:, kc, :], hT[:, kc, g * 512:(g + 1) * 512], start=(kc == 0), stop=(kc == 15)),
                                reads=[w, hT], pwrites=[p])
                        cx.op("act", lambda p=p, o=o, g=g, ch=ch, func=func: nc.scalar.activation(
                            o[:, g * 512:(g + 1) * 512], p[:, :], func, bias=binT[:, ch:ch + 1]),
                            reads=[p, binT], pwrites=[o])
                    cx.dma("sp", featT_d[ch], o[:, :], reads=[o])
                cx.barrier()

        if upto >= 3:
          with ExitStack() as ph:
            biasT = sb(ph, "biasT", [128, 8, 640])
            maskA = sb(ph, "maskA", [128, 640])
            qTs = [sb(ph, "qT%d" % i, [128, S], BF16) for i in range(2)]
            kTs = [sb(ph, "kT%d" % i, [128, S], BF16) for i in range(2)]
            vTs = [sb(ph, "vT%d" % i, [128, S], BF16) for i in range(2)]
            vas = [sb(ph, "va%d" % i, [128, NT, 128], BF16) for i in range(2)]
            ybs = [sb(ph, "yb%d" % i, [128, S], BF16) for i in range(2)]
            t1s = [sb(ph, "t1_%d" % i, [128, 640]) for i in range(2)]
            pTs = [sb(ph, "pT%d" % i, [128, 640], BF16) for i in range(2)]
            rds = [sb(ph, "rd%d" % i, [128, 128]) for i in range(2)]
            pss = [ps(ph, "psA%d" % i, [128, 1024]) for i in range(2)]
            pos_ = [ps(ph, "poA%d" % i, [128, 512]) for i in range(2)]
            ptr = [ps(ph, "ptA%d" % i, [128, 1024], BF16) for i in range(2)]
            cx.dma("sp", biasT[:, :, :], biasT_d, writes=[biasT])
            cx.dma("sp", maskA[:, :], maskA_d, writes=[maskA])
            for h in range(8):
                cx.op("dve", lambda h=h: nc.vector.tensor_tensor(out=biasT[:, h, :], in0=biasT[:, h, :], in1=maskA[:, :], op=ALU.add),
                      reads=[maskA], writes=[biasT])
            nt = 0
            for h in range(8):
                qT, kT, vT, va, yb = qTs[h % 2], kTs[h % 2], vTs[h % 2], vas[h % 2], ybs[h % 2]
                cx.dma("sp", qT[:, :], featT_d[h], writes=[qT])
                cx.dma("sp", kT[:, :], featT_d[8 + h], writes=[kT])
                cx.dma("sp", vT[:, :], featT_d[16 + h], writes=[vT])
                for blk in range(4):
                    pt = ptr[nt % 2]
                    nt += 1
                    for i in range(8):
                        tl = blk * 8 + i
                        cx.op("pe", lambda pt=pt, vT=vT, i=i, tl=tl: nc.tensor.transpose(
                            pt[:, i * 128:(i + 1) * 128], vT[:, tl * 128:(tl + 1) * 128], identb[:, :]),
                            reads=[vT, identb], pwrites=[pt])
                    cx.op("act", lambda pt=pt, va=va, blk=blk: nc.scalar.copy(
                        va[:, blk * 8:(blk + 1) * 8, :], pt[:, :].rearrange("p (a b) -> p a b", b=128)),
                        reads=[pt], pwrites=[va])
                def a_scores(m):
                    j0 = max(0, 4 - m)
                    lo = j0 * 128
                    psm, t1, pT = pss[m % 2], t1s[m % 2], pTs[m % 2]
                    for j in range(j0, 5):
                        kt = m - 4 + j
                        cx.op("pe", lambda j=j, kt=kt: nc.tensor.matmul(
                            psm[:, j * 128:(j + 1) * 128], kT[:, kt * 128:(kt + 1) * 128], qT[:, m * 128:(m + 1) * 128],
                            start=True, stop=True), reads=[kT, qT], pwrites=[psm])
                    cx.op("dve", lambda: nc.vector.scalar_tensor_tensor(
                        out=t1[:, lo:640], in0=psm[:, lo:640], scalar=SCALE_A, in1=biasT[:, h, lo:640],
                        op0=ALU.mult, op1=ALU.add), reads=[psm, biasT], writes=[t1])
                    cx.op("act", lambda: nc.scalar.activation(pT[:, lo:640], t1[:, lo:640], AF.Exp), reads=[t1], writes=[pT])

                def a_pv(m):
                    j0 = max(0, 4 - m)
                    pT, po, rd = pTs[m % 2], pos_[m % 2], rds[m % 2]
                    for j in range(j0, 5):
                        kt = m - 4 + j
                        cx.op("pe", lambda j=j, kt=kt: nc.tensor.matmul(
                            po[:, 0:128], va[:, kt, :], pT[:, j * 128:(j + 1) * 128], start=(j == j0), stop=(j == 4)),
                            reads=[va, pT], pwrites=[po])
                    for j in range(j0, 5):
                        cx.op("pe", lambda j=j: nc.tensor.matmul(
                            po[:, 128:256], ones_b[:, :], pT[:, j * 128:(j + 1) * 128], start=(j == j0), stop=(j == 4)),
                            reads=[ones_b, pT], pwrites=[po])
                    cx.op("dve", lambda: nc.vector.reciprocal(rd[:, :], po[:, 128:256]), reads=[po], writes=[rd])
                    cx.op("dve", lambda: nc.vector.tensor_tensor(
                        out=yb[:, m * 128:(m + 1) * 128], in0=po[:, 0:128], in1=rd[:, :], op=ALU.mult),
                        reads=[po, rd], pwrites=[yb])

                a_scores(0)
                for m in range(NT):
                    if m + 1 < NT:
                        a_scores(m + 1)
                    a_pv(m)
                cx.dma("sp", yT_d[h], yb[:, :], reads=[yb])
            cx.barrier()

        if upto >= 4:
          with ExitStack() as ph:
            cos2 = sb(ph, "cos2", [64, S])
            sinS = sb(ph, "sinS", [64, S])
            invf = sb(ph, "invf", [64, 2])
            cx.dma("sp", invf[:, :], invf_d, writes=[invf])
            with ExitStack() as ph2:
                posi = sb(ph2, "posi", [64, S], I32)
                ang = sb(ph2, "ang", [64, S])
                ta = sb(ph2, "ta", [64, S])
                tb = sb(ph2, "tb", [64, S])
                cx.dma("sp", posi[:, :], pos_d, writes=[posi])
                cx.op("dve", lambda: nc.vector.tensor_copy(ta[:, :], posi[:, :]), reads=[posi], writes=[ta])
                cx.op("dve", lambda: nc.vector.tensor_scalar(out=ang[:, :], in0=ta[:, :], scalar1=invf[:, 0:1], scalar2=None, op0=ALU.mult),
                      reads=[ta, invf], writes=[ang])
                for dst, shift, use_sgn in ((sinS, 0.0, True), (cos2, math.pi / 2.0, False)):
                    src = ang
                    if shift != 0.0:
                        cx.op("dve", lambda: nc.vector.tensor_scalar(out=ta[:, :], in0=ang[:, :], scalar1=shift, scalar2=None, op0=ALU.add),
                              reads=[ang], writes=[ta])
                        src = ta
                    else:
                        cx.op("dve", lambda: nc.vector.tensor_copy(ta[:, :], ang[:, :]), reads=[ang], writes=[ta])
                        src = ta
                    cx.op("dve", lambda: nc.vector.tensor_scalar(out=tb[:, :], in0=ta[:, :], scalar1=1.0 / TWO_PI, scalar2=None, op0=ALU.mult),
                          reads=[ta], writes=[tb])
                    cx.op("dve", lambda: nc.vector.tensor_scalar(out=tb[:, :], in0=tb[:, :], scalar1=MAGIC, scalar2=None, op0=ALU.add),
                          reads=[tb], writes=[tb])
                    cx.op("dve", lambda: nc.vector.tensor_scalar(out=tb[:, :], in0=tb[:, :], scalar1=-MAGIC, scalar2=None, op0=ALU.add),
                          reads=[tb], writes=[tb])
                    cx.op("dve", lambda: nc.vector.scalar_tensor_tensor(out=ta[:, :], in0=tb[:, :], scalar=-C1, in1=ta[:, :], op0=ALU.mult, op1=ALU.add),
                          reads=[tb, ta], writes=[ta])
                    cx.op("dve", lambda: nc.vector.scalar_tensor_tensor(out=ta[:, :], in0=tb[:, :], scalar=-C2, in1=ta[:, :], op0=ALU.mult, op1=ALU.add),
                          reads=[tb, ta], writes=[ta])
                    cx.op("dve", lambda: nc.vector.tensor_scalar(out=ta[:, :], in0=ta[:, :], scalar1=PI_SAFE, scalar2=-PI_SAFE, op0=ALU.min, op1=ALU.max),
                          reads=[ta], writes=[ta])
                    if use_sgn:
                        cx.op("act", lambda dst=dst: nc.scalar.activation(dst[:, :], ta[:, :], AF.Sin, scale=invf[:, 1:2]),
                              reads=[ta, invf], writes=[dst])
                    else:
                        cx.op("act", lambda dst=dst: nc.scalar.activation(dst[:, :], ta[:, :], AF.Sin), reads=[ta], writes=[dst])
                cx.barrier()

            with ExitStack() as ph2:
                lat = sb(ph2, "lat", [128, 4, S], BF16)
                sq = [sb(ph2, "sq%d" % i, [128, 4, 512], BF16) for i in range(2)]
                tmpf = [sb(ph2, "tmpf%d" % i, [128, 512]) for i in range(2)]
                rstd = sb(ph2, "rstd", [128, S])
                gn = sb(ph2, "gn", [128, 8])
                wuq = sb(ph2, "wuq", [128, 4, 2048], BF16)
                wuk = sb(ph2, "wuk", [128, 4, 1024], BF16)
                wuv = sb(ph2, "wuv", [128, 4, 1024], BF16)
                qn_st = [sb(ph2, "qn_st%d" % i, [128, S], BF16) for i in range(2)]
                qr_st = [sb(ph2, "qr_st%d" % i, [64, S], BF16) for i in range(2)]
                v_st = [sb(ph2, "v_st%d" % i, [128, 2, 1024], BF16) for i in range(2)]
                rta = [sb(ph2, "rta%d" % i, [64, 512]) for i in range(2)]
                rtb = [sb(ph2, "rtb%d" % i, [64, 512]) for i in range(2)]
                kr_a, kr_b = qr_st[0], qr_st[1]
                pp = [ps(ph2, "pp%d" % i, [128, 512]) for i in range(6)]
                npp = [0]

                def nextp():
                    p = pp[npp[0] % 6]
                    npp[0] += 1
                    return p

                cx.dma("sp", gn[:, 0:4], gq_d, pwrites=[gn])
                cx.dma("sp", gn[:, 4:8], gkv_d, pwrites=[gn])
                cx.dma("pool", wuq[:, :, :], wuq_d, writes=[wuq])
                cx.dma("pool", wuk[:, :, :], wuk_d, writes=[wuk])
                cx.dma("pool", wuv[:, :, :], wuv_d, writes=[wuv])

                def load_norm(first_chunk, goff):
                    for c in range(4):
                        cx.dma("sp", lat[:, c, :], featT_d[first_chunk + c], pwrites=[lat])
                    for g in range(8):
                        sqb, tf = sq[g % 2], tmpf[g % 2]
                        cx.op("act", lambda sqb=sqb, g=g: nc.scalar.activation(sqb[:, :, :], lat[:, :, g * 512:(g + 1) * 512], AF.Square),
                              reads=[lat], writes=[sqb])
                        p = nextp()
                        for c in range(4):
                            cx.op("pe", lambda p=p, sqb=sqb, c=c: nc.tensor.matmul(p[:, :], ones_b[:, :], sqb[:, c, :], start=(c == 0), stop=(c == 3)),
                                  reads=[sqb, ones_b], pwrites=[p])
                        cx.op("act", lambda p=p, tf=tf: nc.scalar.activation(tf[:, :], p[:, :], AF.Sqrt, scale=1.0 / 512.0, bias=RMS_EPS),
                              reads=[p], writes=[tf])
                        cx.op("dve", lambda tf=tf, g=g: nc.vector.reciprocal(rstd[:, g * 512:(g + 1) * 512], tf[:, :]), reads=[tf], pwrites=[rstd])
                    for c in range(4):
                        for g in range(4):
                            cx.op("dve", lambda c=c, g=g: nc.vector.scalar_tensor_tensor(
                                out=lat[:, c, g * 1024:(g + 1) * 1024], in0=lat[:, c, g * 1024:(g + 1) * 1024],
                                scalar=gn[:, goff + c:goff + c + 1], in1=rstd[:, g * 1024:(g + 1) * 1024], op0=ALU.mult, op1=ALU.mult),
                                reads=[rstd, gn], writes=[lat])

                load_norm(28, 4)
                for h in range(8):
                    st = qn_st[h % 2]
                    for g in range(8):
                        p = nextp()
                        for c in range(4):
                            cx.op("pe", lambda p=p, c=c, h=h, g=g: nc.tensor.matmul(
                                p[:, :], wuk[:, c, h * 128:(h + 1) * 128], lat[:, c, g * 512:(g + 1) * 512], start=(c == 0), stop=(c == 3)),
                                reads=[wuk, lat], pwrites=[p])
                        cx.op("act", lambda p=p, st=st, g=g: nc.scalar.copy(st[:, g * 512:(g + 1) * 512], p[:, :]), reads=[p], pwrites=[st])
                    cx.dma("sp", knT_d[h], st[:, :], reads=[st])
                for tq in range(16):
                    st = v_st[tq % 2]
                    for ti in range(2):
                        tt = tq * 2 + ti
                        for half in range(2):
                            p = nextp()
                            for c in range(4):
                                cx.op("pe", lambda p=p, c=c, tt=tt, half=half: nc.tensor.matmul(
                                    p[:, :], lat[:, c, tt * 128:(tt + 1) * 128], wuv[:, c, half * 512:(half + 1) * 512], start=(c == 0), stop=(c == 3)),
                                    reads=[wuv, lat], pwrites=[p])
                            cx.op("act", lambda p=p, st=st, ti=ti, half=half: nc.scalar.copy(st[:, ti, half * 512:(half + 1) * 512], p[:, :]),
                                  reads=[p], pwrites=[st])
                    cx.dma("sp", vbs_d[tq * 256:(tq + 1) * 256, :].rearrange("(a p) c -> p a c", p=128), st[:, :, :], reads=[st])
                cx.dma("sp", kr_a[:, :], featT_d[32][0:64, :], writes=[kr_a])
                cx.dma("sp", kr_b[:, :], featT_d[32][64:128, :], writes=[kr_b])
                for g in range(8):
                    ra, rb = rta[g % 2], rtb[g % 2]
                    sl = slice(g * 512, (g + 1) * 512)
                    cx.op("dve", lambda ra=ra, sl=sl: nc.vector.tensor_tensor(out=ra[:, :], in0=kr_a[:, sl], in1=cos2[:, sl], op=ALU.mult),
                          reads=[kr_a, cos2], writes=[ra])
                    cx.op("dve", lambda rb=rb, sl=sl: nc.vector.tensor_tensor(out=rb[:, :], in0=kr_b[:, sl], in1=sinS[:, sl], op=ALU.mult),
                          reads=[kr_b, sinS], writes=[rb])
                    cx.op("dve", lambda ra=ra, rb=rb, sl=sl: nc.vector.tensor_tensor(out=kr_a[:, sl], in0=ra[:, :], in1=rb[:, :], op=ALU.add),
                          reads=[ra, rb], writes=[kr_a])
                cx.dma("sp", krT_d, kr_a[:, :], reads=[kr_a])

                load_norm(24, 0)
                for h in range(8):
                    stn, strp = qn_st[h % 2], qr_st[h % 2]
                    for g in range(8):
                        sl = slice(g * 512, (g + 1) * 512)
                        p = nextp()
                        for c in range(4):
                            cx.op("pe", lambda p=p, c=c, h=h, sl=sl: nc.tensor.matmul(
                                p[:, :], wuq[:, c, h * 256:h * 256 + 128], lat[:, c, sl], start=(c == 0), stop=(c == 3)),
                                reads=[wuq, lat], pwrites=[p])
                        cx.op("act", lambda p=p, stn=stn, sl=sl: nc.scalar.copy(stn[:, sl], p[:, :]), reads=[p], pwrites=[stn])
                        p2 = nextp()
                        p3 = nextp()
                        for c in range(4):
                            cx.op("pe", lambda p2=p2, c=c, h=h, sl=sl: nc.tensor.matmul(
                                p2[0:64, :], wuq[:, c, h * 256 + 128:h * 256 + 192], lat[:, c, sl], start=(c == 0), stop=(c == 3)),
                                reads=[wuq, lat], pwrites=[p2])
                        for c in range(4):
                            cx.op("pe", lambda p3=p3, c=c, h=h, sl=sl: nc.tensor.matmul(
                                p3[0:64, :], wuq[:, c, h * 256 + 192:h * 256 + 256], lat[:, c, sl], start=(c == 0), stop=(c == 3)),
                                reads=[wuq, lat], pwrites=[p3])
                        ra, rb = rta[g % 2], rtb[g % 2]
                        cx.op("dve", lambda ra=ra, p2=p2, sl=sl: nc.vector.tensor_tensor(out=ra[:, :], in0=p2[0:64, :], in1=cos2[:, sl], op=ALU.mult),
                              reads=[p2, cos2], writes=[ra])
                        cx.op("dve", lambda rb=rb, p3=p3, sl=sl: nc.vector.tensor_tensor(out=rb[:, :], in0=p3[0:64, :], in1=sinS[:, sl], op=ALU.mult),
                              reads=[p3, sinS], writes=[rb])
                        cx.op("dve", lambda ra=ra, rb=rb, strp=strp, sl=sl: nc.vector.tensor_tensor(out=strp[:, sl], in0=ra[:, :], in1=rb[:, :], op=ALU.add),
                              reads=[ra, rb], pwrites=[strp])
                    cx.dma("sp", qnT_d[h], stn[:, :], reads=[stn])
                    cx.dma("sp", qrT_d[h], strp[:, :], reads=[strp])
                cx.barrier()
            cx.barrier()

        if upto >= 5:
          with ExitStack() as ph:
            knT = sb(ph, "knT", [128, 8, S], BF16)
            vb = sb(ph, "vb", [128, NT, 1024], BF16)
            krT = sb(ph, "krT", [64, S], BF16)
            qns = [sb(ph, "qn%d" % i, [128, 512], BF16) for i in range(2)]
            qrs = [sb(ph, "qr%d" % i, [64, 512], BF16) for i in range(2)]
            pTs = [sb(ph, "pTb%d" % i, [128, 512], BF16) for i in range(3)]
            rds = [sb(ph, "rdb%d" % i, [128, 512]) for i in range(2)]
            ybs = [sb(ph, "ybb%d" % i, [128, S], BF16) for i in range(2)]
            pss = [ps(ph, "psB%d" % i, [128, 512]) for i in range(2)]
            pos_ = [ps(ph, "poB%d" % i, [128, 512]) for i in range(2)]
            pds = [ps(ph, "pdB%d" % i, [128, 512]) for i in range(2)]
            for h in range(8):
                cx.dma("sp", knT[:, h, :], knT_d[h], pwrites=[knT])
            for a in range(4):
                cx.dma("sp", vb[:, a * 8:(a + 1) * 8, :], vbs_d[a * 1024:(a + 1) * 1024, :].rearrange("(a p) c -> p a c", p=128), pwrites=[vb])
            cx.dma("sp", krT[:, :], krT_d, writes=[krT])
            it = 0
            cnt = 0
            for h in range(8):
                yb = ybs[h % 2]
                for Q in range(8):
                    qn, qr, po, pd, rd = qns[it % 2], qrs[it % 2], pos_[it % 2], pds[it % 2], rds[it % 2]
                    it += 1
                    cx.dma("sp", qn[:, :], qnT_d[h][:, Q * 512:(Q + 1) * 512], writes=[qn])
                    cx.dma("sp", qr[:, :], qrT_d[h][:, Q * 512:(Q + 1) * 512], writes=[qr])
                    nk = 4 * (Q + 1)

                    def s_step(kt, cnt_):
                        jj = kt - 4 * Q
                        c0 = max(jj, 0) * 128
                        psm = pss[cnt_ % 2]
                        pT = pTs[cnt_ % 3]
                        ks = slice(kt * 128, (kt + 1) * 128)
                        cx.op("pe", lambda: nc.tensor.matmul(
                            psm[:, c0:512], knT[:, h, ks], qn[:, c0:512], start=True, stop=False), reads=[knT, qn], pwrites=[psm])
                        cx.op("pe", lambda: nc.tensor.matmul(
                            psm[:, c0:512], krT[:, ks], qr[:, c0:512], start=False, stop=True), reads=[krT, qr], pwrites=[psm])
                        cx.op("act", lambda: nc.scalar.activation(pT[:, c0:512], psm[:, c0:512], AF.Exp, scale=SCALE_B),
                              reads=[psm], writes=[pT])
                        if jj >= 0:
                            cx.op("dve", lambda: nc.vector.memset(pT[64:128, c0:c0 + 64], 0.0), writes=[pT])

                    def v_step(kt, cnt_):
                        jj = kt - 4 * Q
                        c0 = max(jj, 0) * 128
                        pT = pTs[cnt_ % 3]
                        cx.op("pe", lambda: nc.tensor.matmul(
                            po[:, c0:512], vb[:, kt, h * 128:(h + 1) * 128], pT[:, c0:512], start=(kt == 0), stop=(kt == nk - 1)),
                            reads=[vb, pT], pwrites=[po])
                        cx.op("pe", lambda: nc.tensor.matmul(
                            pd[:, c0:512], ones_b[:, :], pT[:, c0:512], start=(kt == 0), stop=(kt == nk - 1)),
                            reads=[ones_b, pT], pwrites=[pd])

                    s_step(0, cnt)
                    for kt in range(nk):
                        if kt + 1 < nk:
                            s_step(kt + 1, cnt + kt + 1)
                        v_step(kt, cnt + kt)
                    cnt += nk
                    cx.op("dve", lambda rd=rd, pd=pd: nc.vector.reciprocal(rd[:, :], pd[:, :]), reads=[pd], writes=[rd])
                    cx.op("dve", lambda yb=yb, po=po, rd=rd, Q=Q: nc.vector.tensor_tensor(
                        out=yb[:, Q * 512:(Q + 1) * 512], in0=po[:, :], in1=rd[:, :], op=ALU.mult), reads=[po, rd], pwrites=[yb])
                cx.dma("sp", yT_d[8 + h], yb[:, :], reads=[yb])
            cx.barrier()

        if upto >= 6:
          with ExitStack() as ph:
            wpa = sb(ph, "wpa", [128, 8, D], BF16)
            wpb = sb(ph, "wpb", [128, 8, D], BF16)
            yas = [sb(ph, "ya%d" % i, [128, 8, 256], BF16) for i in range(2)]
            ybs = [sb(ph, "ybm%d" % i, [128, 8, 256], BF16) for i in range(2)]
            gts = [sb(ph, "gt%d" % i, [128, 32, 256], BF16) for i in range(2)]
            yfs = [sb(ph, "yf%d" % i, [128, 16, 256], BF16) for i in range(2)]
            tas = [sb(ph, "tam%d" % i, [128, 256]) for i in range(2)]
            tbs = [sb(ph, "tbm%d" % i, [128, 256]) for i in range(2)]
            ppa = [ps(ph, "ppa%d" % i, [128, 512]) for i in range(2)]
            ppb = [ps(ph, "ppb%d" % i, [128, 512]) for i in range(2)]
            for hh in range(2):
                cx.dma("pool", wpa[:, hh * 4:(hh + 1) * 4, :], wpa_d[hh * 512:(hh + 1) * 512, :].rearrange("(h p) c -> p h c", p=128), pwrites=[wpa])
                cx.dma("pool", wpb[:, hh * 4:(hh + 1) * 4, :], wpb_d[hh * 512:(hh + 1) * 512, :].rearrange("(h p) c -> p h c", p=128), pwrites=[wpb])
            n = 0
            for g in range(16):
                ya, yb, gt, yf = yas[g % 2], ybs[g % 2], gts[g % 2], yfs[g % 2]
                ts = slice(g * 256, (g + 1) * 256)
                cx.dma("sp", ya[:, :, :], yT_d[0:8, :, ts].rearrange("h p t -> p h t"), writes=[ya])
                cx.dma("sp", yb[:, :, :], yT_d[8:16, :, ts].rearrange("h p t -> p h t"), writes=[yb])
                for a in range(4):
                    cx.dma("sp", gt[:, a * 8:(a + 1) * 8, :], featT_d[33 + a * 8:33 + (a + 1) * 8, :, ts].rearrange("c p t -> p c t"), pwrites=[gt])
                for oc in range(16):
                    pa, pb, ta, tb = ppa[n % 2], ppb[n % 2], tas[n % 2], tbs[n % 2]
                    n += 1
                    os_ = slice(oc * 128, (oc + 1) * 128)
                    for h in range(8):
                        cx.op("pe", lambda pa=pa, ya=ya, h=h, os_=os_: nc.tensor.matmul(
                            pa[:, 0:256], wpa[:, h, os_], ya[:, h, :], start=(h == 0), stop=(h == 7)), reads=[wpa, ya], pwrites=[pa])
                    for h in range(8):
                        cx.op("pe", lambda pb=pb, yb=yb, h=h, os_=os_: nc.tensor.matmul(
                            pb[:, 0:256], wpb[:, h, os_], yb[:, h, :], start=(h == 0), stop=(h == 7)), reads=[wpb, yb], pwrites=[pb])
                    cx.op("dve", lambda ta=ta, pa=pa, gt=gt, oc=oc: nc.vector.tensor_tensor(out=ta[:, :], in0=pa[:, 0:256], in1=gt[:, oc, :], op=ALU.mult),
                          reads=[pa, gt], writes=[ta])
                    cx.op("dve", lambda tb=tb, pb=pb, gt=gt, oc=oc: nc.vector.tensor_tensor(out=tb[:, :], in0=pb[:, 0:256], in1=gt[:, 16 + oc, :], op=ALU.mult),
                          reads=[pb, gt], writes=[tb])
                    cx.op("pool", lambda yf=yf, ta=ta, tb=tb, oc=oc: nc.gpsimd.tensor_tensor(out=yf[:, oc, :], in0=ta[:, :], in1=tb[:, :], op=ALU.add),
                          reads=[ta, tb], pwrites=[yf])
                for a in range(2):
                    cx.dma("sp", yfT_d[a * 8:(a + 1) * 8, :, ts].rearrange("c p t -> p c t"), yf[:, a * 8:(a + 1) * 8, :], reads=[yf])
            cx.barrier()

        def layer_norm_tile(r, lng, lnb, dst, stats, mv, rs_t):
            for k in range(4):
                cx.op("dve", lambda k=k: nc.vector.bn_stats(stats[:, k, :], r[:, k * 512:(k + 1) * 512]), reads=[r], pwrites=[stats])
            cx.op("dve", lambda: nc.vector.bn_aggr(mv[:, :], stats[:, :, :].rearrange("p a b -> p (a b)")), reads=[stats], writes=[mv])
            cx.op("act", lambda: nc.scalar.activation(rs_t[:, 0:1], mv[:, 1:2], AF.Sqrt, bias=LN_EPS), reads=[mv], writes=[rs_t])
            cx.op("dve", lambda: nc.vector.reciprocal(rs_t[:, 1:2], rs_t[:, 0:1]), writes=[rs_t])
            cx.op("dve", lambda: nc.vector.tensor_scalar(out=r[:, :], in0=r[:, :], scalar1=mv[:, 0:1], scalar2=rs_t[:, 1:2],
                                                         op0=ALU.subtract, op1=ALU.mult), reads=[mv, rs_t], writes=[r])
            cx.op("dve", lambda: nc.vector.tensor_tensor(out=r[:, :], in0=r[:, :], in1=lng[:, :], op=ALU.mult), reads=[lng], writes=[r])
            cx.op("pool", lambda: nc.gpsimd.tensor_tensor(out=dst[:, :], in0=r[:, :], in1=lnb[:, :], op=ALU.add), reads=[r, lnb], writes=[dst])

        if upto >= 7:
          with ExitStack() as ph:
            wo = sb(ph, "wo", [128, 16, D], BF16)
            lng = sb(ph, "ln1g", [128, D])
            lnb = sb(ph, "ln1b", [128, D])
            yfs = [sb(ph, "yfo%d" % i, [128, 16, 512], BF16) for i in range(2)]
            xts = [sb(ph, "xt%d" % i, [128, D]) for i in range(2)]
            rts = [sb(ph, "rt%d" % i, [128, D]) for i in range(2)]
            x1s = [sb(ph, "x1t%d" % i, [128, D]) for i in range(2)]
            h2s = [sb(ph, "h2s%d" % i, [128, 16, 512], BF16) for i in range(1)]
            stats = sb(ph, "stats", [128, 4, 6])
            mv = sb(ph, "mv", [128, 2])
            rs_t = sb(ph, "rs_t", [128, 2])
            pob = [ps(ph, "pob%d" % i, [128, 512]) for i in range(4)]
            ptb = [ps(ph, "ptb%d" % i, [128, 512]) for i in range(4)]
            for a in range(4):
                cx.dma("pool", wo[:, a * 4:(a + 1) * 4, :], wo_d[a * 512:(a + 1) * 512, :].rearrange("(k p) c -> p k c", p=128), pwrites=[wo])
            cx.dma("sp", lng[:, :], ln_d[0], writes=[lng])
            cx.dma("sp", lnb[:, :], ln_d[1], writes=[lnb])
            npt = [0]

            def st_a(tt):
                g, tl = tt // 4, tt % 4
                yf = yfs[g % 2]
                if tl == 0:
                    for a in range(2):
                        cx.dma("sp", yf[:, a * 8:(a + 1) * 8, :], yfT_d[a * 8:(a + 1) * 8, :, g * 512:(g + 1) * 512].rearrange("c p t -> p c t"), pwrites=[yf])
                xt, r = xts[tt % 2], rts[tt % 2]
                cx.dma("sp", xt[:, :], x_d[tt * 128:(tt + 1) * 128, :], writes=[xt])
                for cg in range(4):
                    for oc in range(16):
                        cx.op("pe", lambda cg=cg, oc=oc: nc.tensor.matmul(
                            pob[cg][:, :], yf[:, oc, tl * 128:(tl + 1) * 128], wo[:, oc, cg * 512:(cg + 1) * 512],
                            start=(oc == 0), stop=(oc == 15)), reads=[yf, wo], pwrites=[pob[cg]])
                    cx.op("dve", lambda cg=cg: nc.vector.tensor_tensor(
                        out=r[:, cg * 512:(cg + 1) * 512], in0=pob[cg][:, :], in1=g1bc[:, cg * 512:(cg + 1) * 512], op=ALU.mult),
                        reads=[pob[cg], g1bc], pwrites=[r])
                cx.op("dve", lambda: nc.vector.scalar_tensor_tensor(
                    out=r[:, :], in0=xt[:, :], scalar=DN_ALPHA, in1=r[:, :], op0=ALU.mult, op1=ALU.add), reads=[xt], writes=[r])

            def st_b(tt):
                r, x1t = rts[tt % 2], x1s[tt % 2]
                layer_norm_tile(r, lng, lnb, x1t, stats, mv, rs_t)
                cx.dma("sp", x1_d[tt * 128:(tt + 1) * 128, :], x1t[:, :], reads=[x1t])

            def st_c(tt):
                g, tl = tt // 4, tt % 4
                x1t, h2b = x1s[tt % 2], h2s[0]
                for q4 in range(4):
                    pt = ptb[npt[0] % 4]
                    npt[0] += 1
                    for i in range(4):
                        dc = q4 * 4 + i
                        cx.op("pe", lambda i=i, dc=dc: nc.tensor.transpose(
                            pt[:, i * 128:(i + 1) * 128], x1t[:, dc * 128:(dc + 1) * 128], ident[:, :]), reads=[x1t, ident], pwrites=[pt])
                    for i in range(4):
                        dc = q4 * 4 + i
                        cx.op("act", lambda i=i, dc=dc: nc.scalar.activation(
                            h2b[:, dc, tl * 128:(tl + 1) * 128], pt[:, i * 128:(i + 1) * 128], AF.Identity,
                            bias=b2[:, dc:dc + 1], scale=s2[:, dc:dc + 1]), reads=[pt, s2, b2], pwrites=[h2b])
                if tl == 3:
                    for a in range(2):
                        cx.dma("sp", h2T_d[a * 8:(a + 1) * 8, :, g * 512:(g + 1) * 512].rearrange("c p t -> p c t"), h2b[:, a * 8:(a + 1) * 8, :], reads=[h2b])

            st_a(0)
            st_b(0)
            for tt in range(NT):
                if tt + 1 < NT:
                    st_a(tt + 1)
                st_c(tt)
                if tt + 1 < NT:
                    st_b(tt + 1)
            cx.barrier()

        es_g1.close()
        if upto >= 8:
          with ExitStack() as ph:
            wq = sb(ph, "wq", [128, 16, D], BF16)
            keysT = sb(ph, "keysT", [128, 16, 128], BF16)
            h2g = [sb(ph, "h2g%d" % i, [128, 16, 512], BF16) for i in range(2)]
            qTs = [sb(ph, "qTs%d" % i, [128, 16, 512], BF16) for i in range(2)]
            scb = [sb(ph, "scb%d" % i, [128, 8, 2, 128]) for i in range(2)]
            sv = sb(ph, "sv", [128, 8, 2, 16])
            tmpA = sb(ph, "tk_tmpA", [128, 16, 128])
            tmpB = sb(ph, "tk_tmpB", [128, 8, 256])
            tmpC = sb(ph, "tk_tmpC", [128, 8, 256])
            c16 = sb(ph, "c16", [128, 8, 16, 16])
            c8 = sb(ph, "c8", [128, 8, 24])
            d16 = sb(ph, "d16", [128, 8, 16])
            zz = sb(ph, "zz", [128, 8])
            lz = sb(ph, "lz", [128, 8])
            idx = sb(ph, "idx", [128, 8, 16], mybir.dt.uint32)
            pkb = [sb(ph, "pkb%d" % i, [128, 272]) for i in range(2)]
            pq = [ps(ph, "pq%d" % i, [128, 512]) for i in range(4)]
            psc = [ps(ph, "psc%d" % i, [128, 512]) for i in range(4)]
            for a in range(4):
                cx.dma("pool", wq[:, a * 4:(a + 1) * 4, :], wq_d[a * 512:(a + 1) * 512, :].rearrange("(k p) c -> p k c", p=128), pwrites=[wq])
            cx.dma("pool", keysT[:, :, :], keysT_d, writes=[keysT])
            npq = 0
            for g in range(8):
                hg, qt = h2g[g % 2], qTs[g % 2]
                for a in range(2):
                    cx.dma("sp", hg[:, a * 8:(a + 1) * 8, :], h2T_d[a * 8:(a + 1) * 8, :, g * 512:(g + 1) * 512].rearrange("c p t -> p c t"), pwrites=[hg])
                for hp in range(16):
                    p = pq[npq % 4]
                    npq += 1
                    for dc in range(16):
                        cx.op("pe", lambda p=p, hg=hg, dc=dc, hp=hp: nc.tensor.matmul(
                            p[:, :], wq[:, dc, hp * 128:(hp + 1) * 128], hg[:, dc, :], start=(dc == 0), stop=(dc == 15)),
                            reads=[wq, hg], pwrites=[p])
                    cx.op("act", lambda p=p, qt=qt, hp=hp: nc.scalar.copy(qt[:, hp, :], p[:, :]), reads=[p], pwrites=[qt])
                for tl in range(4):
                    tt = g * 4 + tl
                    sc = scb[tt % 2]
                    for bk in range(4):
                        for i in range(4):
                            hp = bk * 4 + i
                            cx.op("pe", lambda bk=bk, i=i, hp=hp, qt=qt, tl=tl: nc.tensor.matmul(
                                psc[bk][:, i * 128:(i + 1) * 128], qt[:, hp, tl * 128:(tl + 1) * 128], keysT[:, hp, :], start=True, stop=True),
                                reads=[qt, keysT], pwrites=[psc[bk]])
                        cx.op("act", lambda bk=bk, sc=sc: nc.scalar.copy(
                            sc[:, bk * 2:(bk + 1) * 2, :, :], psc[bk][:, :].rearrange("p (a b c) -> p a b c", a=2, b=2)),
                            reads=[psc[bk]], pwrites=[sc])
                    cx.dma("sp", scs_d[tt * 128:(tt + 1) * 128, :], sc[:, :, :, :].rearrange("p a b c -> p (a b c)"), reads=[sc])
                    for h in range(8):
                        for p_ in range(2):
                            cx.op("dve", lambda h=h, p_=p_, sc=sc: nc.vector.max(out=sv[:, h, p_, 0:8], in_=sc[:, h, p_, :]), reads=[sc], pwrites=[sv])
                    for h in range(8):
                        for p_ in range(2):
                            cx.op("dve", lambda h=h, p_=p_, sc=sc: nc.vector.match_replace(
                                out=tmpA[:, h * 2 + p_, :], in_to_replace=sv[:, h, p_, 0:8], in_values=sc[:, h, p_, :], imm_value=-1e30),
                                reads=[sc, sv], pwrites=[tmpA])
                    for h in range(8):
                        for p_ in range(2):
                            cx.op("dve", lambda h=h, p_=p_: nc.vector.max(out=sv[:, h, p_, 8:16], in_=tmpA[:, h * 2 + p_, :]), reads=[tmpA], pwrites=[sv])
                    for h in range(8):
                        for r8 in range(2):
                            cx.op("dve", lambda h=h, r8=r8, sc=sc: nc.vector.max_index(
                                out=idx[:, h, r8 * 8:(r8 + 1) * 8], in_max=sv[:, h, 0, r8 * 8:(r8 + 1) * 8], in_values=sc[:, h, 0, :]),
                                reads=[sv, sc], pwrites=[idx])
                    cx.op("dve", lambda: nc.vector.tensor_tensor(
                        out=c16[:, :, :, :], in0=bcast(sv[:, :, 0, :], 3, [128, 8, 16, 16]), in1=bcast(sv[:, :, 1, :], 2, [128, 8, 16, 16]), op=ALU.add),
                        reads=[sv], writes=[c16])
                    for h in range(8):
                        cx.op("dve", lambda h=h: nc.vector.max(out=c8[:, h, 0:8], in_=c16[:, h, :, :].rearrange("p a b -> p (a b)")), reads=[c16], pwrites=[c8])
                    for h in range(8):
                        cx.op("dve", lambda h=h: nc.vector.match_replace(
                            out=tmpB[:, h, :], in_to_replace=c8[:, h, 0:8], in_values=c16[:, h, :, :].rearrange("p a b -> p (a b)"), imm_value=-1e30),
                            reads=[c16, c8], pwrites=[tmpB])
                    for h in range(8):
                        cx.op("dve", lambda h=h: nc.vector.max(out=c8[:, h, 8:16], in_=tmpB[:, h, :]), reads=[tmpB], pwrites=[c8])
                    for h in range(8):
                        cx.op("dve", lambda h=h: nc.vector.match_replace(
                            out=tmpC[:, h, :], in_to_replace=c8[:, h, 8:16], in_values=tmpB[:, h, :], imm_value=-1e30), reads=[tmpB, c8], pwrites=[tmpC])
                    for h in range(8):
                        cx.op("dve", lambda h=h: nc.vector.max(out=c8[:, h, 16:24], in_=tmpC[:, h, :]), reads=[tmpC], pwrites=[c8])
                    cx.op("dve", lambda: nc.vector.tensor_tensor(out=zz[:, :], in0=c8[:, :, 15], in1=c8[:, :, 16], op=ALU.add),
                          reads=[c8], writes=[zz])
                    cx.op("dve", lambda tt=tt: nc.vector.tensor_scalar(out=prm[:, tt, 0:8], in0=zz[:, :], scalar1=0.5, scalar2=None, op0=ALU.mult),
                          reads=[zz], pwrites=[prm])
                    cx.op("dve", lambda: nc.vector.tensor_tensor(out=d16[:, :, :], in0=c8[:, :, 0:16], in1=bcast(c8[:, :, 0], 2, [128, 8, 16]), op=ALU.subtract),
                          reads=[c8], writes=[d16])
                    cx.op("act", lambda: nc.scalar.activation(d16[:, :, :], d16[:, :, :], AF.Exp), writes=[d16])
                    cx.op("dve", lambda: nc.vector.tensor_reduce(out=zz[:, :], in_=d16[:, :, :], axis=AX.X, op=ALU.add), reads=[d16], writes=[zz])
                    cx.op("act", lambda: nc.scalar.activation(lz[:, :], zz[:, :], AF.Ln), reads=[zz], writes=[lz])
                    cx.op("dve", lambda tt=tt: nc.vector.scalar_tensor_tensor(
                        out=prm[:, tt, 8:16], in0=c8[:, :, 0], scalar=-1.0, in1=lz[:, :], op0=ALU.mult, op1=ALU.subtract),
                        reads=[c8, lz], pwrites=[prm])
                    pk = pkb[tt % 2]
                    cx.op("dve", lambda pk=pk, tt=tt: nc.vector.tensor_tensor(
                        out=pk[:, 0:128].rearrange("p (h k) -> p h k", h=8), in0=sv[:, :, 0, :], in1=bcast(prm[:, tt, 8:16], 2, [128, 8, 16]), op=ALU.add),
                        reads=[sv, prm], pwrites=[pk])
                    cx.op("dve", lambda pk=pk: nc.vector.tensor_copy(pk[:, 128:256].rearrange("p (h k) -> p h k", h=8), idx[:, :, :]),
                          reads=[idx], pwrites=[pk])
                    cx.op("dve", lambda pk=pk, tt=tt: nc.vector.tensor_tensor(out=pk[:, 256:264], in0=prm[:, tt, 0:8], in1=prm[:, tt, 8:16], op=ALU.add),
                          reads=[prm], pwrites=[pk])
                    cx.op("dve", lambda pk=pk: nc.vector.memset(pk[:, 264:272], 0.0), pwrites=[pk])
                    cx.dma("sp", pk_d[tt * 128:(tt + 1) * 128, :], pk[:, :], reads=[pk])
            if dbgprm_d is not None:
                cx.dma("sp", dbgprm_d, prm[:, :, :], reads=[prm])
            cx.barrier()

        if upto >= 9:
          with ExitStack() as ph:
            NTT = NT // 2
            FP8 = mybir.dt.float8e4
            lng = sb(ph, "ln2g", [128, D])
            lnb = sb(ph, "ln2b", [128, D])
            iota = sb(ph, "iota", [128, 128])
            hgs = [sb(ph, "h2p%d" % i, [128, 16, 128], BF16) for i in range(2)]
            s2bs = [sb(ph, "s2b%d" % i, [128, 8, 128]) for i in range(2)]
            pks = [sb(ph, "pkp%d" % i, [128, 272]) for i in range(2)]
            x1t = sb(ph, "x1p", [128, D])
            accs = [sb(ph, "acc%d" % i, [128, D]) for i in range(2)]
            cmb = sb(ph, "cmb", [128, 128, 16])
            ee = sb(ph, "eeR", [128, 2048], BF16)
            Rp = sb(ph, "Rp", [128, 128, 16], BF16)
            RTo = [sb(ph, "RTo%d" % i, [128, 16, 128], BF16) for i in range(2)]
            si1T = [sb(ph, "si1T%d" % i, [128, 128]) for i in range(2)]
            O1T = [sb(ph, "O1T%d" % i, [128, 128, 128], FP8) for i in range(2)]
            GTo = sb(ph, "GTo", [128, 128, 16], BF16)
            gqs = [[sb(ph, "gq%d_%d" % (i, k), [128, 2048], BF16) for k in range(2)] for i in range(2)]
            uts = [sb(ph, "ut%d" % i, [128, 16, 512], BF16) for i in range(2)]
            vcs = [sb(ph, "vc%d" % i, [128, 2, D], BF16) for i in range(2)]
            gas = [sb(ph, "ga%d" % i, [128, 512], BF16) for i in range(2)]
            wws = [sb(ph, "ww%d" % i, [128, 512], BF16) for i in range(2)]
            wTs = [sb(ph, "wT%d" % i, [128, 4, 128], BF16) for i in range(2)]
            stats = sb(ph, "stats2", [128, 4, 6])
            mv = sb(ph, "mv2", [128, 2])
            rs_t = sb(ph, "rs_t2", [128, 2])
            pop = [ps(ph, "pop%d" % i, [128, 512]) for i in range(4)]
            pap = [ps(ph, "pap%d" % i, [128, 512]) for i in range(2)]
            pgf = ps(ph, "pgf", [128, 512])
            pgb = ps(ph, "pgb", [128, 1024], BF16)
            cx.dma("sp", lng[:, :], ln_d[2], writes=[lng])
            cx.dma("sp", lnb[:, :], ln_d[3], writes=[lnb])
            cx.dma("sp", iota[:, :], iota_d, writes=[iota])

            def load_small(T_):
                for sub in range(2):
                    tt = 2 * T_ + sub
                    cx.dma("sp", s2bs[sub][:, :, :], scs_d[tt * 128:(tt + 1) * 128, :].rearrange("t (h p n) -> t h p n", h=8, p=2)[:, :, 1, :], writes=[s2bs[sub]])
                    cx.dma("sp", pks[sub][:, :], pk_d[tt * 128:(tt + 1) * 128, :], writes=[pks[sub]])

            def load_hg(T_, sub):
                tt = 2 * T_ + sub
                hg = hgs[sub]
                for a in range(2):
                    cx.dma("sp", hg[:, a * 8:(a + 1) * 8, :], h2T_d[a * 8:(a + 1) * 8, :, tt * 128:(tt + 1) * 128].rearrange("c p t -> p c t"), pwrites=[hg])

            def tile_prep_items(T_):
                items = []
                for sub in range(2):
                    def s_item(sub=sub):
                        cx.op("pe", lambda: nc.tensor.transpose(pgf[:, 0:128], pks[sub][:, 128:256], ident[:, :]), reads=[pks[sub], ident], pwrites=[pgf])
                        cx.op("dve", lambda: nc.vector.tensor_copy(si1T[sub][:, :], pgf[:, 0:128]), reads=[pgf], writes=[si1T[sub]])

                    def o1(sub=sub):
                        cx.op("dve", lambda: nc.vector.tensor_tensor(
                            out=O1T[sub][:, :, :], in0=bcast(si1T[sub][:, :], 2, [128, 128, 128]), in1=bcast(iota[:, :], 1, [128, 128, 128]),
                            op=ALU.is_equal, saturate=False), reads=[si1T[sub], iota], writes=[O1T[sub]])
                    items += [s_item, o1]
                return items

            def oct_items(T_, o):
                j0 = o * 16
                parts = {"elem": [], "rb": [[], []], "pt": [[], []], "gt": [[], []]}
                for sub in range(2):
                    s2b, pk = s2bs[sub], pks[sub]
                    gq = gqs[sub][(T_ * 8 + o) % 2]

                    def elem(s2b=s2b, pk=pk):
                        cx.op("dve", lambda: nc.vector.tensor_tensor(
                            out=cmb[:, :, :].rearrange("p (h k) j -> p h k j", h=8),
                            in0=bcast(pk[:, 0:128].rearrange("p (h k) -> p h k", h=8), 3, [128, 8, 16, 16]),
                            in1=bcast(s2b[:, :, j0:j0 + 16], 2, [128, 8, 16, 16]), op=ALU.add), reads=[pk, s2b], writes=[cmb])
                        cx.op("act", lambda: nc.scalar.activation(ee[:, :], cmb[:, :, :].rearrange("p a b -> p (a b)"), AF.Exp),
                              reads=[cmb], writes=[ee])
                        cx.op("dve", lambda: nc.vector.tensor_tensor(
                            out=Rp[:, :, :].rearrange("p (h k) j -> p h (k j)", h=8), in0=cmb[:, :, :].rearrange("p (h k) j -> p h (k j)", h=8),
                            in1=bcast(pk[:, 256:264], 2, [128, 8, 256]), op=ALU.is_ge), reads=[cmb, pk], writes=[Rp])
                        cx.op("pool", lambda: nc.gpsimd.tensor_tensor(out=Rp[:, :, :].rearrange("p a b -> p (a b)"),
                                                                      in0=Rp[:, :, :].rearrange("p a b -> p (a b)"), in1=ee[:, :], op=ALU.mult),
                              reads=[ee], writes=[Rp])
                    parts["elem"].append(elem)
                    for b_ in range(2):
                        def rb(b_=b_, sub=sub):
                            for i in range(8):
                                cx.op("pe", lambda i=i: nc.tensor.transpose(pgb[:, i * 128:(i + 1) * 128], Rp[:, :, b_ * 8 + i], identb[:, :]),
                                      reads=[Rp, identb], pwrites=[pgb])
                            cx.op("act", lambda: nc.scalar.copy(
                                RTo[sub][:, b_ * 8:b_ * 8 + 8, :], pgb[:, :].rearrange("p (j t) -> p j t", t=128)),
                                reads=[pgb], pwrites=[RTo[sub]])
                        parts["rb"][sub].append(rb)
                    for tb in range(4):
                        def pt(tb=tb, sub=sub):
                            for tl in range(32):
                                t = tb * 32 + tl
                                cx.op("pe", lambda tl=tl, t=t: nc.tensor.matmul(pgf[:, tl * 16:(tl + 1) * 16], O1T[sub][:, t, :], RTo[sub][:, :, t], start=True, stop=True),
                                      reads=[RTo[sub], O1T[sub]], pwrites=[pgf])
                            cx.op("dve", lambda: nc.vector.tensor_copy(
                                GTo[:, tb * 32:(tb + 1) * 32, :], pgf[:, :].rearrange("p (t j) -> p t j", j=16)), reads=[pgf], pwrites=[GTo])
                        parts["pt"][sub].append(pt)
                    for gb in range(2):
                        def gt(gb=gb, gq=gq):
                            for jl in range(8):
                                cx.op("pe", lambda jl=jl: nc.tensor.transpose(pgb[:, jl * 128:(jl + 1) * 128], GTo[:, :, gb * 8 + jl], identb[:, :]),
                                      reads=[GTo, identb], pwrites=[pgb])
                            cx.op("act", lambda: nc.scalar.copy(gq[:, gb * 1024:(gb + 1) * 1024], pgb[:, :]), reads=[pgb], pwrites=[gq])
                        parts["gt"][sub].append(gt)
                return parts

            NEG = NTT * 32
            NU = NEG * 2

            def ucoords(u):
                EG, sub = u // 2, u % 2
                T_, eg = EG // 32, EG % 32
                return EG, sub, T_, eg, eg // 4, eg % 4

            def load_ut(EG):
                eg = EG % 32
                ut = uts[EG % 2]
                cx.dma("sp", ut[:, :, :], utb_d[eg * 512:(eg + 1) * 512, :].rearrange("(p k) c -> p (k c)", k=4).rearrange("p (k c) -> p k c", c=512),
                       reads=[utb_t] if EG < 2 else (), writes=[ut])

            def load_vc(EG, half):
                eg = EG % 32
                vc = vcs[half]
                r0 = eg * 512 + half * 256
                cx.dma("sp", vc[:, :, :], vb16_d[r0:r0 + 256, :].rearrange("(k p) d -> p k d", p=128),
                       reads=[vb16_t] if EG < 1 else (), writes=[vc])

            def a_mm(u, part):
                EG, sub, T_, eg, o, e4 = ucoords(u)
                hg, ut, pa, ga, ww = hgs[sub], uts[EG % 2], pap[u % 2], gas[u % 2], wws[u % 2]
                gq = gqs[sub][(T_ * 8 + o) % 2]
                for dc in range(part * 8, part * 8 + 8):
                    cx.op("pe", lambda dc=dc: nc.tensor.matmul(pa[:, :], hg[:, dc, :], ut[:, dc, :], start=(dc == 0), stop=(dc == 15)),
                          reads=[hg, ut], pwrites=[pa])
                if part == 0:
                    return
                cx.op("act", lambda: nc.scalar.activation(ga[:, :], pa[:, :], AF.Gelu), reads=[pa], writes=[ga])
                cx.op("dve", lambda: nc.vector.tensor_tensor(out=ww[:, :], in0=ga[:, :], in1=gq[:, e4 * 512:(e4 + 1) * 512], op=ALU.mult),
                      reads=[ga, gq], writes=[ww])

            def wtr(u):
                ww, wT = wws[u % 2], wTs[u % 2]
                for k in range(4):
                    cx.op("pe", lambda k=k: nc.tensor.transpose(pgb[:, k * 128:(k + 1) * 128], ww[:, k * 128:(k + 1) * 128], identb[:, :]),
                          reads=[ww, identb], pwrites=[pgb])
                cx.op("act", lambda: nc.scalar.copy(wT[:, :, :], pgb[:, 0:512].rearrange("p (a b) -> p a b", b=128)), reads=[pgb], writes=[wT])

            def ph2(u, part):
                wT = wTs[u % 2]
                for k in range(part * 2, part * 2 + 2):
                    vc = vcs[k // 2]
                    for dq in range(4):
                        cx.op("pe", lambda k=k, dq=dq, vc=vc: nc.tensor.matmul(
                            pop[dq][:, :], wT[:, k, :], vc[:, k % 2, dq * 512:(dq + 1) * 512],
                            start=(k == 0), stop=(k == 3)), reads=[wT, vc], pwrites=[pop[dq]])

            def accadd(u):
                EG, sub, T_, eg, o, e4 = ucoords(u)
                acc = accs[sub]
                for dq in range(4):
                    sl = slice(dq * 512, (dq + 1) * 512)
                    if eg == 0:
                        cx.op("dve", lambda dq=dq, sl=sl: nc.vector.tensor_copy(acc[:, sl], pop[dq][:, :]), reads=[pop[dq]], pwrites=[acc])
                    else:
                        cx.op("dve", lambda dq=dq, sl=sl: nc.vector.tensor_tensor(out=acc[:, sl], in0=pop[dq][:, :], in1=acc[:, sl], op=ALU.add),
                              reads=[pop[dq]], writes=[acc])

            def epilogue(T_, sub):
                tt = 2 * T_ + sub
                acc = accs[sub]
                cx.dma("sp", x1t[:, :], x1_d[tt * 128:(tt + 1) * 128, :], writes=[x1t])
                cx.op("dve", lambda: nc.vector.tensor_tensor(out=acc[:, :], in0=acc[:, :], in1=g2bc[:, :], op=ALU.mult), reads=[g2bc], writes=[acc])
                cx.op("dve", lambda: nc.vector.scalar_tensor_tensor(
                    out=acc[:, :], in0=x1t[:, :], scalar=DN_ALPHA, in1=acc[:, :], op0=ALU.mult, op1=ALU.add), reads=[x1t], writes=[acc])
                layer_norm_tile(acc, lng, lnb, x1t, stats, mv, rs_t)
                cx.dma("sp", out_d[tt * 128:(tt + 1) * 128, :], x1t[:, :], reads=[x1t])

            parts_cache = {}

            def get_parts(T_, o):
                if (T_, o) not in parts_cache:
                    parts_cache[(T_, o)] = oct_items(T_, o)
                return parts_cache[(T_, o)]

            def nxt_oct(T_, o):
                return (T_, o + 1) if o < 7 else (T_ + 1, 0)

            def q_for(T_, o):
                n1 = nxt_oct(T_, o)
                items = []
                if n1[0] < NTT:
                    p = get_parts(*n1)
                    if n1[1] == 0:
                        items += tile_prep_items(n1[0])
                    items += p["rb"][0] + [p["elem"][1]] + p["pt"][0] + p["rb"][1] + p["gt"][0] + p["pt"][1] + p["gt"][1]
                    n2 = nxt_oct(*n1)
                    if n2[0] < NTT:
                        if n2[1] == 0:
                            items.append(lambda: load_small(n2[0]))
                        items.append(get_parts(*n2)["elem"][0])
                return items

            load_small(0)
            load_hg(0, 0)
            load_hg(0, 1)
            get_parts(0, 0)["elem"][0]()
            p0 = get_parts(0, 0)
            for it in tile_prep_items(0) + p0["rb"][0] + [p0["elem"][1]] + p0["pt"][0] + p0["rb"][1] + p0["gt"][0] + p0["pt"][1] + p0["gt"][1] + [get_parts(0, 1)["elem"][0]]:
                it()
            load_ut(0)
            load_ut(1)
            load_vc(0, 0)
            load_vc(0, 1)
            a_mm(0, 0)
            a_mm(0, 1)
            queue = []
            per_slot = 0

            def pop_items(n):
                for _ in range(min(n, len(queue))):
                    queue.pop(0)()

            for u in range(NU):
                EG, sub, T_, eg, o, e4 = ucoords(u)
                last_of_oct = (e4 == 3 and sub == 1)
                if e4 == 0 and sub == 0:
                    queue.extend(q_for(T_, o))
                    per_slot = -(-len(queue) // 8)
                n_left = per_slot
                k1 = (n_left + 3) // 4
                wtr(u)
                if sub == 1 and EG + 2 < NEG:
                    load_ut(EG + 2)
                pop_items(k1); n_left -= k1
                if u + 1 < NU:
                    if (u + 1) % 64 < 2 and u + 1 >= 64:
                        load_hg((u + 1) // 64, (u + 1) % 2)
                    a_mm(u + 1, 0)
                if last_of_oct:
                    pop_items(len(queue))
                else:
                    pop_items(k1); n_left -= k1
                if u + 1 < NU:
                    a_mm(u + 1, 1)
                if u >= 1:
                    ph2(u - 1, 0)
                    if (u - 1) % 2 == 1 and (u - 1) // 2 + 1 < NEG:
                        load_vc((u - 1) // 2 + 1, 0)
                if not last_of_oct:
                    pop_items(k1); n_left -= k1
                if u >= 1:
                    ph2(u - 1, 1)
                    if (u - 1) % 2 == 1 and (u - 1) // 2 + 1 < NEG:
                        load_vc((u - 1) // 2 + 1, 1)
                    accadd(u - 1)
                    pEG, psub, pT, peg, po_, pe4 = ucoords(u - 1)
                    if peg == 31:
                        epilogue(pT, psub)
                if not last_of_oct:
                    pop_items(max(n_left, 0))
            ph2(NU - 1, 0)
            ph2(NU - 1, 1)
            accadd(NU - 1)
            epilogue(NTT - 1, 1)
            cx.barrier()
        cx.barrier()
        print("[kernel] instructions emitted:", cx.nins)
    return nc


def host_layout(inputs):
    f = lambda k: np.asarray(inputs[k])
    shared = {}
    w_in = f("w_in")[0]
    b_in = f("b_in")[0]
    perm = np.concatenate([np.arange(32, 64), np.arange(0, 32)])
    cols = np.concatenate([np.arange(0, 4160), 4096 + perm, np.arange(4160, 8256)])
    wext = w_in[:, cols]
    shared["w_in_l"] = np.ascontiguousarray(wext.reshape(16, 128, 65, 128).transpose(2, 1, 0, 3).reshape(65, 128, 2048))
    shared["b_inT"] = np.ascontiguousarray(b_in[cols].reshape(65, 128).T)
    shared["w_ada"] = np.ascontiguousarray(f("w_ada")[0])
    b_ada = f("b_ada")[0]
    shared["b_adaT"] = np.ascontiguousarray(b_ada.reshape(96, 128).T)
    shared["b_ada_row"] = np.ascontiguousarray(b_ada.reshape(1, -1))
    rb = f("rel_bias")[0]
    p = np.arange(128)[:, None, None]
    j = np.arange(5)[None, :, None]
    c = np.arange(128)[None, None, :]
    rel = 128 * (4 - j) + c - p
    idx = np.clip(rel, -63, 256) + 63
    shared["biasT"] = np.ascontiguousarray(rb[:, idx].transpose(1, 0, 2, 3).reshape(128, 8, 640))
    mask = np.zeros((128, 5, 128), np.float32)
    mask[:64, 0, 64:] = NEGM
    mask[64:, 4, :64] = NEGM
    shared["maskA"] = mask.reshape(128, 640)
    shared["gqT"] = np.ascontiguousarray(f("q_norm_g")[0].reshape(4, 128).T)
    shared["gkvT"] = np.ascontiguousarray(f("kv_norm_g")[0].reshape(4, 128).T)
    w_uq = f("w_uq")[0]
    qcols = []
    for h in range(8):
        base = h * 192
        qcols += [np.arange(base, base + 128), np.arange(base + 128, base + 192), base + 128 + perm]
    qcols = np.concatenate(qcols)
    shared["w_uq_l"] = np.ascontiguousarray(w_uq[:, qcols].reshape(4, 128, 2048).transpose(1, 0, 2))
    w_ukv = f("w_ukv")[0].reshape(512, 8, 256)
    shared["w_uk_l"] = np.ascontiguousarray(w_ukv[:, :, :128].reshape(4, 128, 1024).transpose(1, 0, 2))
    shared["w_uv_l"] = np.ascontiguousarray(w_ukv[:, :, 128:].reshape(4, 128, 1024).transpose(1, 0, 2))
    shared["w_pa"] = np.ascontiguousarray(f("w_pa")[0])
    shared["w_pb"] = np.ascontiguousarray(f("w_pb")[0])
    shared["w_o"] = np.ascontiguousarray(f("w_o")[0])
    shared["ln_bc"] = np.ascontiguousarray(np.stack([np.broadcast_to(f(k)[0][None, :], (128, D)) for k in ("ln1_g", "ln1_b", "ln2_g", "ln2_b")]))
    shared["peer_wq"] = np.ascontiguousarray(f("peer_wq")[0])
    keys = f("peer_keys")[0]
    shared["keysT"] = np.ascontiguousarray(keys.reshape(16, 128, 128).transpose(2, 0, 1))
    U = f("peer_u")[0].reshape(128, 128, D).transpose(1, 0, 2).reshape(16384, D)
    shared["ut_l"] = np.ascontiguousarray(U.reshape(32, 512, 16, 128).transpose(0, 3, 2, 1)).reshape(16384, 2048)
    shared["peer_v"] = np.ascontiguousarray(f("peer_v")[0].reshape(128, 128, D).transpose(1, 0, 2).reshape(16384, D))
    shared["ident"] = np.eye(128, dtype=np.float32)
    shared["iota"] = np.ascontiguousarray(np.broadcast_to(np.arange(128, dtype=np.float32)[None, :], (128, 128)))
    inv_freq = (10000.0 ** (-np.arange(0, 64, 2, dtype=np.float32) / 64.0)).astype(np.float32)
    invf = np.zeros((64, 2), np.float32)
    invf[:, 0] = np.concatenate([inv_freq, inv_freq])
    invf[:32, 1] = -1.0
    invf[32:, 1] = 1.0
    shared["invf"] = invf
    per_core = []
    x = f("x")
    cc = f("c")
    pos = f("positions")
    for b in range(NCORES):
        m = dict(shared)
        m["x"] = np.ascontiguousarray(x[b])
        m["cT"] = np.ascontiguousarray(cc[b].reshape(16, 128).T)
        m["posb"] = np.ascontiguousarray(np.broadcast_to(pos[b][None, :].astype(np.int32), (64, S)))
        per_core.append(m)
    return per_core


_NC_CACHE = {}


def kernel(**inputs):
    maps = host_layout(inputs)
    if "full" not in _NC_CACHE:
        _NC_CACHE["full"] = build()
    nc = _NC_CACHE["full"]
    res = run_bass_kernel_spmd(nc, maps, core_ids=list(range(NCORES)))
    out = np.stack([np.asarray(r["out"]) for r in res.results], axis=0)
    return out.astype(np.float32)
```

```python
import math
from contextlib import ExitStack

import numpy as np
import concourse.bass as bass
import concourse.mybir as mybir
from concourse.bass_utils import run_bass_kernel_spmd

F32 = mybir.dt.float32
BF16 = mybir.dt.bfloat16
I32 = mybir.dt.int32
AF = mybir.ActivationFunctionType
ALU = mybir.AluOpType
AX = mybir.AxisListType

NCORES = 8
S = 4096
D = 2048
NT = S // 128
DN_ALPHA = 2.0 ** 0.25
LN_EPS = 1e-5
RMS_EPS = 1e-6
SCALE_A = 128.0 ** -0.5
SCALE_B = 192.0 ** -0.5
NEGM = -30000.0
MAGIC = 12582912.0
TWO_PI = 2.0 * math.pi
C1 = 6.28125
C2 = TWO_PI - C1
PI_SAFE = 3.14159

SAME_ENG_SYNC = True
CB_ON_POOL = False


class Buf:
    __slots__ = ("w", "r")

    def __init__(self):
        self.w = {}
        self.r = {}


class T:
    def __init__(self, handle):
        self.t = handle
        self.b = Buf()

    def __getitem__(self, k):
        return self.t[k]


class Ctx:
    def __init__(self, nc, es):
        self.nc = nc
        self.eng = {"pe": nc.tensor, "act": nc.scalar, "dve": nc.vector, "pool": nc.gpsimd, "sp": nc.sync}
        self.sems = {}
        self.tot = {}
        for e in ("pe", "act", "dve", "pool"):
            self.sems[e] = es.enter_context(nc.semaphore("s_" + e))
            self.tot[e] = 0
        self.dq = {}
        for q, n in (("sp", 16), ("pool", 8)):
            lst = []
            for i in range(n):
                k = "d_%s%d" % (q, i)
                self.sems[k] = es.enter_context(nc.semaphore(k))
                self.tot[k] = 0
                lst.append(k)
            self.dq[q] = [lst, 0]
        self.seen = {e: {} for e in self.eng}
        self.nins = 0

    def _wait(self, e, deps):
        own = e if e in ("pe", "act", "dve", "pool") else None
        seen = self.seen[e]
        for k, v in deps.items():
            if v <= 0:
                continue
            if k == own and (own == "pe" or not SAME_ENG_SYNC):
                continue
            if seen.get(k, 0) >= v:
                continue
            self.eng[e].wait_ge(self.sems[k], v)
            seen[k] = v

    @staticmethod
    def _merge(d, k, v):
        if d.get(k, 0) < v:
            d[k] = v

    def _deps(self, reads, writes, pwrites):
        deps = {}
        for t in reads:
            for k, v in t.b.w.items():
                self._merge(deps, k, v)
        for t in writes:
            for k, v in t.b.w.items():
                self._merge(deps, k, v)
            for k, v in t.b.r.items():
                self._merge(deps, k, v)
        for t in pwrites:
            for k, v in t.b.r.items():
                self._merge(deps, k, v)
        return deps

    def _update(self, tok, reads, writes, pwrites):
        k, v = tok
        for t in writes:
            t.b.w = {k: v}
            t.b.r = {}
        for t in pwrites:
            if t.b.r:
                t.b.w = {k: v}
                t.b.r = {}
            else:
                self._merge(t.b.w, k, v)
        for t in reads:
            self._merge(t.b.r, k, v)

    def op(self, e, fn, reads=(), writes=(), pwrites=()):
        self._wait(e, self._deps(reads, writes, pwrites))
        ins = fn()
        self.tot[e] += 1
        ins.then_inc(self.sems[e], 1)
        self.nins += 1
        self._update((e, self.tot[e]), reads, writes, pwrites)
        return ins

    def dma(self, q, out, in_, reads=(), writes=(), pwrites=()):
        lst, i = self.dq[q]
        k = lst[i % len(lst)]
        self.dq[q][1] = i + 1
        deps = self._deps(reads, writes, pwrites)
        self._merge(deps, k, self.tot[k])
        self._wait(q, deps)
        ins = self.eng[q].dma_start(out=out, in_=in_)
        self.tot[k] += 16
        ins.then_inc(self.sems[k], 16)
        self.nins += 1
        self._update((k, self.tot[k]), reads, writes, pwrites)
        return ins

    def barrier(self, engines=("pe", "act", "dve", "pool", "sp")):
        for e in engines:
            self._wait(e, dict(self.tot))


def bcast(ap, axis, shape):
    return ap.unsqueeze(axis).broadcast_to(shape)


def build(upto=99, dbg=()):
    nc = bass.Bass("TRN2", target_bir_lowering=False)

    def din(name, shape, dt=F32):
        return nc.dram_tensor(name, list(shape), dt, kind="ExternalInput").ap()

    def dscr(name, shape, dt):
        kind = "ExternalOutput" if name in dbg else "Internal"
        return nc.dram_tensor(name, list(shape), dt, kind=kind).ap()

    x_d = din("x", [S, D])
    cT_d = din("cT", [128, 16])
    pos_d = din("posb", [64, S], I32)
    invf_d = din("invf", [64, 2])
    wada_d = din("w_ada", [D, 6 * D])
    badaT_d = din("b_adaT", [128, 96])
    badar_d = din("b_ada_row", [1, 6 * D])
    win_d = din("w_in_l", [65, 128, 2048])
    binT_d = din("b_inT", [128, 65])
    biasT_d = din("biasT", [128, 8, 640])
    maskA_d = din("maskA", [128, 640])
    gq_d = din("gqT", [128, 4])
    gkv_d = din("gkvT", [128, 4])
    wuq_d = din("w_uq_l", [128, 4, 2048])
    wuk_d = din("w_uk_l", [128, 4, 1024])
    wuv_d = din("w_uv_l", [128, 4, 1024])
    wpa_d = din("w_pa", [1024, D])
    wpb_d = din("w_pb", [1024, D])
    wo_d = din("w_o", [D, D])
    ln_d = din("ln_bc", [4, 128, D])
    wq_d = din("peer_wq", [D, D])
    keysT_d = din("keysT", [128, 16, 128])
    ut_d = din("ut_l", [16384, 2048])
    v_d = din("peer_v", [16384, D])
    ident_d = din("ident", [128, 128])
    iota_d = din("iota", [128, 128])

    out_d = nc.dram_tensor("out", [S, D], F32, kind="ExternalOutput").ap()

    featT_d = dscr("featT", [65, 128, S], BF16)
    yT_d = dscr("yT", [16, 128, S], BF16)
    qnT_d = dscr("qnT", [8, 128, S], BF16)
    qrT_d = dscr("qrT", [8, 64, S], BF16)
    knT_d = dscr("knT", [8, 128, S], BF16)
    krT_d = dscr("krT", [64, S], BF16)
    vbs_d = dscr("vbs", [S, 1024], BF16)
    yfT_d = dscr("yfT", [16, 128, S], BF16)
    x1_d = dscr("x1s", [S, D], F32)
    h2T_d = dscr("h2T", [16, 128, S], BF16)
    scs_d = dscr("scs", [S, 2048], F32)
    pk_d = dscr("pk", [S, 272], F32)
    utb_d = dscr("utb", [16384, 2048], BF16)
    vb16_d = dscr("vb16", [16384, D], BF16)
    dbgmod_d = dscr("dbgmod", [128, 96 + 2], F32) if "dbgmod" in dbg else None
    dbgg_d = dscr("dbgg", [128, 2 * D], F32) if "dbgg" in dbg else None
    dbgprm_d = dscr("dbgprm", [128, NT, 16], F32) if "dbgprm" in dbg else None

    with ExitStack() as es:
        cx = Ctx(nc, es)

        uid = [0]

        def sb(scope, name, shape, dt=F32):
            uid[0] += 1
            return T(scope.enter_context(nc.sbuf_tensor("sb%d_%s" % (uid[0], name), list(shape), dt)))

        def ps(scope, name, shape, dt=F32):
            uid[0] += 1
            return T(scope.enter_context(nc.psum_tensor("ps%d_%s" % (uid[0], name), list(shape), dt)))

        CB_ENG = "pool" if CB_ON_POOL else "dve"
        CB_OBJ = nc.gpsimd if CB_ON_POOL else nc.vector
        ident = sb(es, "ident", [128, 128])
        identb = sb(es, "identb", [128, 128], BF16)
        ones_f = sb(es, "ones_f", [128, 128])
        ones_b = sb(es, "ones_b", [128, 128], BF16)
        s1 = sb(es, "s1", [128, 16])
        b1 = sb(es, "b1", [128, 16])
        s2 = sb(es, "s2", [128, 16])
        b2 = sb(es, "b2", [128, 16])
        g2bc = sb(es, "g2bc", [128, D])
        prm = sb(es, "prm", [128, NT, 16])
        es_g1 = ExitStack()
        g1bc = sb(es_g1, "g1bc", [128, D])

        cx.dma("sp", ident[:, :], ident_d, writes=[ident])
        cx.op("dve", lambda: nc.vector.tensor_copy(identb[:, :], ident[:, :]), reads=[ident], writes=[identb])
        cx.op("dve", lambda: nc.vector.memset(ones_f[:, :], 1.0), writes=[ones_f])
        cx.op("dve", lambda: nc.vector.memset(ones_b[:, :], 1.0), writes=[ones_b])

        utb_t = T(None)
        vb16_t = T(None)

        def cast_tables(i):
            if upto < 6 or i >= 64:
                return
            if i < 32:
                cx.dma("pool", utb_d[i * 512:(i + 1) * 512, :], ut_d[i * 512:(i + 1) * 512, :], pwrites=[utb_t])
            else:
                i -= 32
                cx.dma("pool", vb16_d[i * 512:(i + 1) * 512, :], v_d[i * 512:(i + 1) * 512, :], pwrites=[vb16_t])

        with ExitStack() as ph:
            cT = sb(ph, "cT", [128, 16])
            scT = sb(ph, "scT", [128, 16])
            badaT = sb(ph, "badaT", [128, 96])
            badar = sb(ph, "badar", [1, 6 * D])
            modT = sb(ph, "modT", [128, 96])
            wst = [sb(ph, "wst%d" % i, [128, 16, 512]) for i in range(2)]
            rowsb = [sb(ph, "rowsb%d" % i, [1, 512]) for i in range(2)]
            pm = ps(ph, "pm", [128, 512])
            pr = [ps(ph, "pr%d" % i, [128, 512]) for i in range(2)]
            pbc = [ps(ph, "pbc%d" % i, [128, 512]) for i in range(2)]
            cx.dma("sp", cT[:, :], cT_d, writes=[cT])
            cx.dma("sp", badaT[:, :], badaT_d, writes=[badaT])
            cx.dma("sp", badar[:, :], badar_d, writes=[badar])
            cx.op("act", lambda: nc.scalar.activation(scT[:, :], cT[:, :], AF.Silu), reads=[cT], writes=[scT])
            nrow = 0
            for gi in range(24):
                m = gi // 4
                w = wst[gi % 2]
                cx.dma("sp", w[:, :, :], wada_d[:, gi * 512:(gi + 1) * 512].rearrange("(k p) c -> p k c", p=128), writes=[w])
                if m in (0, 1, 3, 4):
                    for cc in range(4):
                        col = m * 16 + (gi % 4) * 4 + cc
                        for kc in range(16):
                            cx.op("pe", lambda kc=kc, cc=cc, col=col, w=w: nc.tensor.matmul(
                                pm[:, col:col + 1], w[:, kc, cc * 128:(cc + 1) * 128], scT[:, kc:kc + 1],
                                start=(kc == 0), stop=(kc == 15)), reads=[w, scT], pwrites=[pm])
                else:
                    p_r = pr[nrow % 2]
                    rs = rowsb[nrow % 2]
                    p_b = pbc[nrow % 2]
                    nrow += 1
                    for kc in range(16):
                        cx.op("pe", lambda kc=kc, w=w, p_r=p_r: nc.tensor.matmul(
                            p_r[0:1, :], scT[:, kc:kc + 1], w[:, kc, :], start=(kc == 0), stop=(kc == 15)),
                            reads=[w, scT], pwrites=[p_r])
                    cx.op("dve", lambda p_r=p_r, rs=rs, gi=gi: nc.vector.tensor_tensor(
                        out=rs[0:1, :], in0=p_r[0:1, :], in1=badar[0:1, gi * 512:(gi + 1) * 512], op=ALU.add),
                        reads=[p_r, badar], writes=[rs])
                    cx.op("pe", lambda rs=rs, p_b=p_b: nc.tensor.matmul(
                        p_b[:, :], ones_f[0:1, :], rs[0:1, :], start=True, stop=True), reads=[rs, ones_f], pwrites=[p_b])
                    dst = g1bc if m == 2 else g2bc
                    cx.op("act", lambda dst=dst, p_b=p_b, gi=gi: nc.scalar.copy(
                        dst[:, (gi % 4) * 512:(gi % 4 + 1) * 512], p_b[:, :]), reads=[p_b], pwrites=[dst])
            cx.op("dve", lambda: nc.vector.tensor_tensor(out=modT[:, :], in0=pm[:, 0:96], in1=badaT[:, :], op=ALU.add),
                  reads=[pm, badaT], writes=[modT])
            cx.op("dve", lambda: nc.vector.tensor_scalar(out=s1[:, :], in0=modT[:, 16:32], scalar1=1.0, scalar2=None, op0=ALU.add),
                  reads=[modT], writes=[s1])
            cx.op("dve", lambda: nc.vector.tensor_copy(b1[:, :], modT[:, 0:16]), reads=[modT], writes=[b1])
            cx.op("dve", lambda: nc.vector.tensor_scalar(out=s2[:, :], in0=modT[:, 64:80], scalar1=1.0, scalar2=None, op0=ALU.add),
                  reads=[modT], writes=[s2])
            cx.op("dve", lambda: nc.vector.tensor_copy(b2[:, :], modT[:, 48:64]), reads=[modT], writes=[b2])
            if dbgmod_d is not None:
                cx.dma("sp", dbgmod_d[:, 0:96], modT[:, :], reads=[modT])
            if dbgg_d is not None:
                cx.dma("sp", dbgg_d[:, 0:D], g1bc[:, :], reads=[g1bc])
                cx.dma("sp", dbgg_d[:, D:2 * D], g2bc[:, :], reads=[g2bc])
            cx.barrier()

        if upto >= 1:
          with ExitStack() as ph12:
            hT = sb(ph12, "hT", [128, 16, S], BF16)
            with ExitStack() as ph:
                xs = [sb(ph, "xs%d" % i, [128, 2, D]) for i in range(2)]
                ptr = [ps(ph, "ptr%d" % i, [128, 512]) for i in range(4)]
                n = 0
                for g in range(16):
                    xb = xs[g % 2]
                    cx.dma("sp", xb[:, :, :], x_d[g * 256:(g + 1) * 256, :].rearrange("(j p) d -> p j d", p=128), writes=[xb])
                    for dc in range(16):
                        pt = ptr[n % 4]
                        n += 1
                        for j in range(2):
                            cx.op("pe", lambda pt=pt, xb=xb, j=j, dc=dc: nc.tensor.transpose(
                                pt[:, j * 128:(j + 1) * 128], xb[:, j, dc * 128:(dc + 1) * 128], ident[:, :]),
                                reads=[xb, ident], pwrites=[pt])
                        cx.op("act", lambda pt=pt, g=g, dc=dc: nc.scalar.activation(
                            hT[:, dc, g * 256:(g + 1) * 256], pt[:, 0:256], AF.Identity,
                            bias=b1[:, dc:dc + 1], scale=s1[:, dc:dc + 1]), reads=[pt, s1, b1], pwrites=[hT])
                cx.barrier()
            with ExitStack() as ph:
                wc = [sb(ph, "wc%d" % i, [128, 16, 128], BF16) for i in range(3)]
                ost = [sb(ph, "ost%d" % i, [128, S], BF16) for i in range(2)]
                binT = sb(ph, "binT", [128, 65])
                pz = [ps(ph, "pz%d" % i, [128, 512]) for i in range(4)]
                cx.dma("sp", binT[:, :], binT_d, writes=[binT])
                n = 0
                for ch in range(65):
                    w = wc[ch % 3]
                    cx.dma("pool", w[:, :, :], win_d[ch].rearrange("p (k c) -> p k c", c=128), writes=[w])
                    if ch >= 2:
                        cast_tables(ch - 2)
                        if ch == 64:
                            cast_tables(63)
                    o = ost[ch % 2]
                    func = AF.Sigmoid if ch >= 33 else AF.Identity
                    for g in range(8):
                        p = pz[n % 4]
                        n += 1
                        for kc in range(16):
                            cx.op("pe", lambda p=p, w=w, kc=kc, g=g: nc.tensor.matmul(
                                p[:, :], w[:, kc, :], hT[:, kc, g * 512:(g + 1) * 512], start=(kc == 0), stop=(kc == 15)),
                                reads=[w, hT], pwrites=[p])
                        cx.op("act", lambda p=p, o=o, g=g, ch=ch, func=func: nc.scalar.activation(
                            o[:, g * 512:(g + 1) * 512], p[:, :], func, bias=binT[:, ch:ch + 1]),
                            reads=[p, binT], pwrites=[o])
                    cx.dma("sp", featT_d[ch], o[:, :], reads=[o])
                cx.barrier()

        if upto >= 3:
          with ExitStack() as ph:
            biasT = sb(ph, "biasT", [128, 8, 640])
            maskA = sb(ph, "maskA", [128, 640])
            qTs = [sb(ph, "qT%d" % i, [128, S], BF16) for i in range(2)]
            kTs = [sb(ph, "kT%d" % i, [128, S], BF16) for i in range(2)]
            vTs = [sb(ph, "vT%d" % i, [128, S], BF16) for i in range(2)]
            vas = [sb(ph, "va%d" % i, [128, NT, 128], BF16) for i in range(2)]
            ybs = [sb(ph, "yb%d" % i, [128, S], BF16) for i in range(2)]
            t1s = [sb(ph, "t1_%d" % i, [128, 640]) for i in range(2)]
            pTs = [sb(ph, "pT%d" % i, [128, 640], BF16) for i in range(2)]
            rds = [sb(ph, "rd%d" % i, [128, 128]) for i in range(2)]
            pss = [ps(ph, "psA%d" % i, [128, 1024]) for i in range(2)]
            pos_ = [ps(ph, "poA%d" % i, [128, 512]) for i in range(2)]
            ptr = [ps(ph, "ptA%d" % i, [128, 1024], BF16) for i in range(2)]
            cx.dma("sp", biasT[:, :, :], biasT_d, writes=[biasT])
            cx.dma("sp", maskA[:, :], maskA_d, writes=[maskA])
            for h in range(8):
                cx.op("dve", lambda h=h: nc.vector.tensor_tensor(out=biasT[:, h, :], in0=biasT[:, h, :], in1=maskA[:, :], op=ALU.add),
                      reads=[maskA], writes=[biasT])
            nt = 0
            for h in range(8):
                qT, kT, vT, va, yb = qTs[h % 2], kTs[h % 2], vTs[h % 2], vas[h % 2], ybs[h % 2]
                cx.dma("sp", qT[:, :], featT_d[h], writes=[qT])
                cx.dma("sp", kT[:, :], featT_d[8 + h], writes=[kT])
                cx.dma("sp", vT[:, :], featT_d[16 + h], writes=[vT])
                for blk in range(4):
                    pt = ptr[nt % 2]
                    nt += 1
                    for i in range(8):
                        tl = blk * 8 + i
                        cx.op("pe", lambda pt=pt, vT=vT, i=i, tl=tl: nc.tensor.transpose(
                            pt[:, i * 128:(i + 1) * 128], vT[:, tl * 128:(tl + 1) * 128], identb[:, :]),
                            reads=[vT, identb], pwrites=[pt])
                    cx.op("act", lambda pt=pt, va=va, blk=blk: nc.scalar.copy(
                        va[:, blk * 8:(blk + 1) * 8, :], pt[:, :].rearrange("p (a b) -> p a b", b=128)),
                        reads=[pt], pwrites=[va])
                def a_scores(m):
                    j0 = max(0, 4 - m)
                    lo = j0 * 128
                    psm, t1, pT = pss[m % 2], t1s[m % 2], pTs[m % 2]
                    for j in range(j0, 5):
                        kt = m - 4 + j
                        cx.op("pe", lambda j=j, kt=kt: nc.tensor.matmul(
                            psm[:, j * 128:(j + 1) * 128], kT[:, kt * 128:(kt + 1) * 128], qT[:, m * 128:(m + 1) * 128],
                            start=True, stop=True), reads=[kT, qT], pwrites=[psm])
                    cx.op("dve", lambda: nc.vector.scalar_tensor_tensor(
                        out=t1[:, lo:640], in0=psm[:, lo:640], scalar=SCALE_A, in1=biasT[:, h, lo:640],
                        op0=ALU.mult, op1=ALU.add), reads=[psm, biasT], writes=[t1])
                    cx.op("act", lambda: nc.scalar.activation(pT[:, lo:640], t1[:, lo:640], AF.Exp), reads=[t1], writes=[pT])

                def a_pv(m):
                    j0 = max(0, 4 - m)
                    pT, po, rd = pTs[m % 2], pos_[m % 2], rds[m % 2]
                    for j in range(j0, 5):
                        kt = m - 4 + j
                        cx.op("pe", lambda j=j, kt=kt: nc.tensor.matmul(
                            po[:, 0:128], va[:, kt, :], pT[:, j * 128:(j + 1) * 128], start=(j == j0), stop=(j == 4)),
                            reads=[va, pT], pwrites=[po])
                    for j in range(j0, 5):
                        cx.op("pe", lambda j=j: nc.tensor.matmul(
                            po[:, 128:256], ones_b[:, :], pT[:, j * 128:(j + 1) * 128], start=(j == j0), stop=(j == 4)),
                            reads=[ones_b, pT], pwrites=[po])
                    cx.op("dve", lambda: nc.vector.reciprocal(rd[:, :], po[:, 128:256]), reads=[po], writes=[rd])
                    cx.op("dve", lambda: nc.vector.tensor_tensor(
                        out=yb[:, m * 128:(m + 1) * 128], in0=po[:, 0:128], in1=rd[:, :], op=ALU.mult),
                        reads=[po, rd], pwrites=[yb])

                a_scores(0)
                for m in range(NT):
                    if m + 1 < NT:
                        a_scores(m + 1)
                    a_pv(m)
                cx.dma("sp", yT_d[h], yb[:, :], reads=[yb])
            cx.barrier()

        if upto >= 4:
          with ExitStack() as ph:
            cos2 = sb(ph, "cos2", [64, S])
            sinS = sb(ph, "sinS", [64, S])
            invf = sb(ph, "invf", [64, 2])
            cx.dma("sp", invf[:, :], invf_d, writes=[invf])
            with ExitStack() as ph2:
                posi = sb(ph2, "posi", [64, S], I32)
                ang = sb(ph2, "ang", [64, S])
                ta = sb(ph2, "ta", [64, S])
                tb = sb(ph2, "tb", [64, S])
                cx.dma("sp", posi[:, :], pos_d, writes=[posi])
                cx.op("dve", lambda: nc.vector.tensor_copy(ta[:, :], posi[:, :]), reads=[posi], writes=[ta])
                cx.op("dve", lambda: nc.vector.tensor_scalar(out=ang[:, :], in0=ta[:, :], scalar1=invf[:, 0:1], scalar2=None, op0=ALU.mult),
                      reads=[ta, invf], writes=[ang])
                for dst, shift, use_sgn in ((sinS, 0.0, True), (cos2, math.pi / 2.0, False)):
                    src = ang
                    if shift != 0.0:
                        cx.op("dve", lambda: nc.vector.tensor_scalar(out=ta[:, :], in0=ang[:, :], scalar1=shift, scalar2=None, op0=ALU.add),
                              reads=[ang], writes=[ta])
                        src = ta
                    else:
                        cx.op("dve", lambda: nc.vector.tensor_copy(ta[:, :], ang[:, :]), reads=[ang], writes=[ta])
                        src = ta
                    cx.op("dve", lambda: nc.vector.tensor_scalar(out=tb[:, :], in0=ta[:, :], scalar1=1.0 / TWO_PI, scalar2=None, op0=ALU.mult),
                          reads=[ta], writes=[tb])
                    cx.op("dve", lambda: nc.vector.tensor_scalar(out=tb[:, :], in0=tb[:, :], scalar1=MAGIC, scalar2=None, op0=ALU.add),
                          reads=[tb], writes=[tb])
                    cx.op("dve", lambda: nc.vector.tensor_scalar(out=tb[:, :], in0=tb[:, :], scalar1=-MAGIC, scalar2=None, op0=ALU.add),
                          reads=[tb], writes=[tb])
                    cx.op("dve", lambda: nc.vector.scalar_tensor_tensor(out=ta[:, :], in0=tb[:, :], scalar=-C1, in1=ta[:, :], op0=ALU.mult, op1=ALU.add),
                          reads=[tb, ta], writes=[ta])
                    cx.op("dve", lambda: nc.vector.scalar_tensor_tensor(out=ta[:, :], in0=tb[:, :], scalar=-C2, in1=ta[:, :], op0=ALU.mult, op1=ALU.add),
                          reads=[tb, ta], writes=[ta])
                    cx.op("dve", lambda: nc.vector.tensor_scalar(out=ta[:, :], in0=ta[:, :], scalar1=PI_SAFE, scalar2=-PI_SAFE, op0=ALU.min, op1=ALU.max),
                          reads=[ta], writes=[ta])
                    if use_sgn:
                        cx.op("act", lambda dst=dst: nc.scalar.activation(dst[:, :], ta[:, :], AF.Sin, scale=invf[:, 1:2]),
                              reads=[ta, invf], writes=[dst])
                    else:
                        cx.op("act", lambda dst=dst: nc.scalar.activation(dst[:, :], ta[:, :], AF.Sin), reads=[ta], writes=[dst])
                cx.barrier()

            with ExitStack() as ph2:
                lat = sb(ph2, "lat", [128, 4, S], BF16)
                sq = [sb(ph2, "sq%d" % i, [128, 4, 512], BF16) for i in range(2)]
                tmpf = [sb(ph2, "tmpf%d" % i, [128, 512]) for i in range(2)]
                rstd = sb(ph2, "rstd", [128, S])
                gn = sb(ph2, "gn", [128, 8])
                wuq = sb(ph2, "wuq", [128, 4, 2048], BF16)
                wuk = sb(ph2, "wuk", [128, 4, 1024], BF16)
                wuv = sb(ph2, "wuv", [128, 4, 1024], BF16)
                qn_st = [sb(ph2, "qn_st%d" % i, [128, S], BF16) for i in range(2)]
                qr_st = [sb(ph2, "qr_st%d" % i, [64, S], BF16) for i in range(2)]
                v_st = [sb(ph2, "v_st%d" % i, [128, 2, 1024], BF16) for i in range(2)]
                rta = [sb(ph2, "rta%d" % i, [64, 512]) for i in range(2)]
                rtb = [sb(ph2, "rtb%d" % i, [64, 512]) for i in range(2)]
                kr_a, kr_b = qr_st[0], qr_st[1]
                pp = [ps(ph2, "pp%d" % i, [128, 512]) for i in range(6)]
                npp = [0]

                def nextp():
                    p = pp[npp[0] % 6]
                    npp[0] += 1
                    return p

                cx.dma("sp", gn[:, 0:4], gq_d, pwrites=[gn])
                cx.dma("sp", gn[:, 4:8], gkv_d, pwrites=[gn])
                cx.dma("pool", wuq[:, :, :], wuq_d, writes=[wuq])
                cx.dma("pool", wuk[:, :, :], wuk_d, writes=[wuk])
                cx.dma("pool", wuv[:, :, :], wuv_d, writes=[wuv])

                def load_norm(first_chunk, goff):
                    for c in range(4):
                        cx.dma("sp", lat[:, c, :], featT_d[first_chunk + c], pwrites=[lat])
                    for g in range(8):
                        sqb, tf = sq[g % 2], tmpf[g % 2]
                        cx.op("act", lambda sqb=sqb, g=g: nc.scalar.activation(sqb[:, :, :], lat[:, :, g * 512:(g + 1) * 512], AF.Square),
                              reads=[lat], writes=[sqb])
                        p = nextp()
                        for c in range(4):
                            cx.op("pe", lambda p=p, sqb=sqb, c=c: nc.tensor.matmul(p[:, :], ones_b[:, :], sqb[:, c, :], start=(c == 0), stop=(c == 3)),
                                  reads=[sqb, ones_b], pwrites=[p])
                        cx.op("act", lambda p=p, tf=tf: nc.scalar.activation(tf[:, :], p[:, :], AF.Sqrt, scale=1.0 / 512.0, bias=RMS_EPS),
                              reads=[p], writes=[tf])
                        cx.op("dve", lambda tf=tf, g=g: nc.vector.reciprocal(rstd[:, g * 512:(g + 1) * 512], tf[:, :]), reads=[tf], pwrites=[rstd])
                    for c in range(4):
                        for g in range(4):
                            cx.op("dve", lambda c=c, g=g: nc.vector.scalar_tensor_tensor(
                                out=lat[:, c, g * 1024:(g + 1) * 1024], in0=lat[:, c, g * 1024:(g + 1) * 1024],
                                scalar=gn[:, goff + c:goff + c + 1], in1=rstd[:, g * 1024:(g + 1) * 1024], op0=ALU.mult, op1=ALU.mult),
                                reads=[rstd, gn], writes=[lat])

                load_norm(28, 4)
                for h in range(8):
                    st = qn_st[h % 2]
                    for g in range(8):
                        p = nextp()
                        for c in range(4):
                            cx.op("pe", lambda p=p, c=c, h=h, g=g: nc.tensor.matmul(
                                p[:, :], wuk[:, c, h * 128:(h + 1) * 128], lat[:, c, g * 512:(g + 1) * 512], start=(c == 0), stop=(c == 3)),
                                reads=[wuk, lat], pwrites=[p])
                        cx.op("act", lambda p=p, st=st, g=g: nc.scalar.copy(st[:, g * 512:(g + 1) * 512], p[:, :]), reads=[p], pwrites=[st])
                    cx.dma("sp", knT_d[h], st[:, :], reads=[st])
                for tq in range(16):
                    st = v_st[tq % 2]
                    for ti in range(2):
                        tt = tq * 2 + ti
                        for half in range(2):
                            p = nextp()
                            for c in range(4):
                                cx.op("pe", lambda p=p, c=c, tt=tt, half=half: nc.tensor.matmul(
                                    p[:, :], lat[:, c, tt * 128:(tt + 1) * 128], wuv[:, c, half * 512:(half + 1) * 512], start=(c == 0), stop=(c == 3)),
                                    reads=[wuv, lat], pwrites=[p])
                            cx.op("act", lambda p=p, st=st, ti=ti, half=half: nc.scalar.copy(st[:, ti, half * 512:(half + 1) * 512], p[:, :]),
                                  reads=[p], pwrites=[st])
                    cx.dma("sp", vbs_d[tq * 256:(tq + 1) * 256, :].rearrange("(a p) c -> p a c", p=128), st[:, :, :], reads=[st])
                cx.dma("sp", kr_a[:, :], featT_d[32][0:64, :], writes=[kr_a])
                cx.dma("sp", kr_b[:, :], featT_d[32][64:128, :], writes=[kr_b])
                for g in range(8):
                    ra, rb = rta[g % 2], rtb[g % 2]
                    sl = slice(g * 512, (g + 1) * 512)
                    cx.op("dve", lambda ra=ra, sl=sl: nc.vector.tensor_tensor(out=ra[:, :], in0=kr_a[:, sl], in1=cos2[:, sl], op=ALU.mult),
                          reads=[kr_a, cos2], writes=[ra])
                    cx.op("dve", lambda rb=rb, sl=sl: nc.vector.tensor_tensor(out=rb[:, :], in0=kr_b[:, sl], in1=sinS[:, sl], op=ALU.mult),
                          reads=[kr_b, sinS], writes=[rb])
                    cx.op("dve", lambda ra=ra, rb=rb, sl=sl: nc.vector.tensor_tensor(out=kr_a[:, sl], in0=ra[:, :], in1=rb[:, :], op=ALU.add),
                          reads=[ra, rb], writes=[kr_a])
                cx.dma("sp", krT_d, kr_a[:, :], reads=[kr_a])

                load_norm(24, 0)
                for h in range(8):
                    stn, strp = qn_st[h % 2], qr_st[h % 2]
                    for g in range(8):
                        sl = slice(g * 512, (g + 1) * 512)
                        p = nextp()
                        for c in range(4):
                            cx.op("pe", lambda p=p, c=c, h=h, sl=sl: nc.tensor.matmul(
                                p[:, :], wuq[:, c, h * 256:h * 256 + 128], lat[:, c, sl], start=(c == 0), stop=(c == 3)),
                                reads=[wuq, lat], pwrites=[p])
                        cx.op("act", lambda p=p, stn=stn, sl=sl: nc.scalar.copy(stn[:, sl], p[:, :]), reads=[p], pwrites=[stn])
                        p2 = nextp()
                        p3 = nextp()
                        for c in range(4):
                            cx.op("pe", lambda p2=p2, c=c, h=h, sl=sl: nc.tensor.matmul(
                                p2[0:64, :], wuq[:, c, h * 256 + 128:h * 256 + 192], lat[:, c, sl], start=(c == 0), stop=(c == 3)),
                                reads=[wuq, lat], pwrites=[p2])
                        for c in range(4):
                            cx.op("pe", lambda p3=p3, c=c, h=h, sl=sl: nc.tensor.matmul(
                                p3[0:64, :], wuq[:, c, h * 256 + 192:h * 256 + 256], lat[:, c, sl], start=(c == 0), stop=(c == 3)),
                                reads=[wuq, lat], pwrites=[p3])
                        ra, rb = rta[g % 2], rtb[g % 2]
                        cx.op("dve", lambda ra=ra, p2=p2, sl=sl: nc.vector.tensor_tensor(out=ra[:, :], in0=p2[0:64, :], in1=cos2[:, sl], op=ALU.mult),
                              reads=[p2, cos2], writes=[ra])
                        cx.op("dve", lambda rb=rb, p3=p3, sl=sl: nc.vector.tensor_tensor(out=rb[:, :], in0=p3[0:64, :], in1=sinS[:, sl], op=ALU.mult),
                              reads=[p3, sinS], writes=[rb])
                        cx.op("dve", lambda ra=ra, rb=rb, strp=strp, sl=sl: nc.vector.tensor_tensor(out=strp[:, sl], in0=ra[:, :], in1=rb[:, :], op=ALU.add),
                              reads=[ra, rb], pwrites=[strp])
                    cx.dma("sp", qnT_d[h], stn[:, :], reads=[stn])
                    cx.dma("sp", qrT_d[h], strp[:, :], reads=[strp])
                cx.barrier()
            cx.barrier()

        if upto >= 5:
          with ExitStack() as ph:
            knT = sb(ph, "knT", [128, 8, S], BF16)
            vb = sb(ph, "vb", [128, NT, 1024], BF16)
            krT = sb(ph, "krT", [64, S], BF16)
            qns = [sb(ph, "qn%d" % i, [128, 512], BF16) for i in range(2)]
            qrs = [sb(ph, "qr%d" % i, [64, 512], BF16) for i in range(2)]
            pTs = [sb(ph, "pTb%d" % i, [128, 512], BF16) for i in range(3)]
            rds = [sb(ph, "rdb%d" % i, [128, 512]) for i in range(2)]
            ybs = [sb(ph, "ybb%d" % i, [128, S], BF16) for i in range(2)]
            pss = [ps(ph, "psB%d" % i, [128, 512]) for i in range(2)]
            pos_ = [ps(ph, "poB%d" % i, [128, 512]) for i in range(2)]
            pds = [ps(ph, "pdB%d" % i, [128, 512]) for i in range(2)]
            for h in range(8):
                cx.dma("sp", knT[:, h, :], knT_d[h], pwrites=[knT])
            for a in range(4):
                cx.dma("sp", vb[:, a * 8:(a + 1) * 8, :], vbs_d[a * 1024:(a + 1) * 1024, :].rearrange("(a p) c -> p a c", p=128), pwrites=[vb])
            cx.dma("sp", krT[:, :], krT_d, writes=[krT])
            it = 0
            cnt = 0
            for h in range(8):
                yb = ybs[h % 2]
                for Q in range(8):
                    qn, qr, po, pd, rd = qns[it % 2], qrs[it % 2], pos_[it % 2], pds[it % 2], rds[it % 2]
                    it += 1
                    cx.dma("sp", qn[:, :], qnT_d[h][:, Q * 512:(Q + 1) * 512], writes=[qn])
                    cx.dma("sp", qr[:, :], qrT_d[h][:, Q * 512:(Q + 1) * 512], writes=[qr])
                    nk = 4 * (Q + 1)

                    def s_step(kt, cnt_):
                        jj = kt - 4 * Q
                        c0 = max(jj, 0) * 128
                        psm = pss[cnt_ % 2]
                        pT = pTs[cnt_ % 3]
                        ks = slice(kt * 128, (kt + 1) * 128)
                        cx.op("pe", lambda: nc.tensor.matmul(
                            psm[:, c0:512], knT[:, h, ks], qn[:, c0:512], start=True, stop=False), reads=[knT, qn], pwrites=[psm])
                        cx.op("pe", lambda: nc.tensor.matmul(
                            psm[:, c0:512], krT[:, ks], qr[:, c0:512], start=False, stop=True), reads=[krT, qr], pwrites=[psm])
                        cx.op("act", lambda: nc.scalar.activation(pT[:, c0:512], psm[:, c0:512], AF.Exp, scale=SCALE_B),
                              reads=[psm], writes=[pT])
                        if jj >= 0:
                            cx.op("dve", lambda: nc.vector.memset(pT[64:128, c0:c0 + 64], 0.0), writes=[pT])

                    def v_step(kt, cnt_):
                        jj = kt - 4 * Q
                        c0 = max(jj, 0) * 128
                        pT = pTs[cnt_ % 3]
                        cx.op("pe", lambda: nc.tensor.matmul(
                            po[:, c0:512], vb[:, kt, h * 128:(h + 1) * 128], pT[:, c0:512], start=(kt == 0), stop=(kt == nk - 1)),
                            reads=[vb, pT], pwrites=[po])
                        cx.op("pe", lambda: nc.tensor.matmul(
                            pd[:, c0:512], ones_b[:, :], pT[:, c0:512], start=(kt == 0), stop=(kt == nk - 1)),
                            reads=[ones_b, pT], pwrites=[pd])

                    s_step(0, cnt)
                    for kt in range(nk):
                        if kt + 1 < nk:
                            s_step(kt + 1, cnt + kt + 1)
                        v_step(kt, cnt + kt)
                    cnt += nk
                    cx.op("dve", lambda rd=rd, pd=pd: nc.vector.reciprocal(rd[:, :], pd[:, :]), reads=[pd], writes=[rd])
                    cx.op("dve", lambda yb=yb, po=po, rd=rd, Q=Q: nc.vector.tensor_tensor(
                        out=yb[:, Q * 512:(Q + 1) * 512], in0=po[:, :], in1=rd[:, :], op=ALU.mult), reads=[po, rd], pwrites=[yb])
                cx.dma("sp", yT_d[8 + h], yb[:, :], reads=[yb])
            cx.barrier()

        if upto >= 6:
          with ExitStack() as ph:
            wpa = sb(ph, "wpa", [128, 8, D], BF16)
            wpb = sb(ph, "wpb", [128, 8, D], BF16)
            yas = [sb(ph, "ya%d" % i, [128, 8, 256], BF16) for i in range(2)]
            ybs = [sb(ph, "ybm%d" % i, [128, 8, 256], BF16) for i in range(2)]
            gts = [sb(ph, "gt%d" % i, [128, 32, 256], BF16) for i in range(2)]
            yfs = [sb(ph, "yf%d" % i, [128, 16, 256], BF16) for i in range(2)]
            tas = [sb(ph, "tam%d" % i, [128, 256]) for i in range(2)]
            tbs = [sb(ph, "tbm%d" % i, [128, 256]) for i in range(2)]
            ppa = [ps(ph, "ppa%d" % i, [128, 512]) for i in range(2)]
            ppb = [ps(ph, "ppb%d" % i, [128, 512]) for i in range(2)]
            for hh in range(2):
                cx.dma("pool", wpa[:, hh * 4:(hh + 1) * 4, :], wpa_d[hh * 512:(hh + 1) * 512, :].rearrange("(h p) c -> p h c", p=128), pwrites=[wpa])
                cx.dma("pool", wpb[:, hh * 4:(hh + 1) * 4, :], wpb_d[hh * 512:(hh + 1) * 512, :].rearrange("(h p) c -> p h c", p=128), pwrites=[wpb])
            n = 0
            for g in range(16):
                ya, yb, gt, yf = yas[g % 2], ybs[g % 2], gts[g % 2], yfs[g % 2]
                ts = slice(g * 256, (g + 1) * 256)
                cx.dma("sp", ya[:, :, :], yT_d[0:8, :, ts].rearrange("h p t -> p h t"), writes=[ya])
                cx.dma("sp", yb[:, :, :], yT_d[8:16, :, ts].rearrange("h p t -> p h t"), writes=[yb])
                for a in range(4):
                    cx.dma("sp", gt[:, a * 8:(a + 1) * 8, :], featT_d[33 + a * 8:33 + (a + 1) * 8, :, ts].rearrange("c p t -> p c t"), pwrites=[gt])
                for oc in range(16):
                    pa, pb, ta, tb = ppa[n % 2], ppb[n % 2], tas[n % 2], tbs[n % 2]
                    n += 1
                    os_ = slice(oc * 128, (oc + 1) * 128)
                    for h in range(8):
                        cx.op("pe", lambda pa=pa, ya=ya, h=h, os_=os_: nc.tensor.matmul(
                            pa[:, 0:256], wpa[:, h, os_], ya[:, h, :], start=(h == 0), stop=(h == 7)), reads=[wpa, ya], pwrites=[pa])
                    for h in range(8):
                        cx.op("pe", lambda pb=pb, yb=yb, h=h, os_=os_: nc.tensor.matmul(
                            pb[:, 0:256], wpb[:, h, os_], yb[:, h, :], start=(h == 0), stop=(h == 7)), reads=[wpb, yb], pwrites=[pb])
                    cx.op("dve", lambda ta=ta, pa=pa, gt=gt, oc=oc: nc.vector.tensor_tensor(out=ta[:, :], in0=pa[:, 0:256], in1=gt[:, oc, :], op=ALU.mult),
                          reads=[pa, gt], writes=[ta])
                    cx.op("dve", lambda tb=tb, pb=pb, gt=gt, oc=oc: nc.vector.tensor_tensor(out=tb[:, :], in0=pb[:, 0:256], in1=gt[:, 16 + oc, :], op=ALU.mult),
                          reads=[pb, gt], writes=[tb])
                    cx.op("pool", lambda yf=yf, ta=ta, tb=tb, oc=oc: nc.gpsimd.tensor_tensor(out=yf[:, oc, :], in0=ta[:, :], in1=tb[:, :], op=ALU.add),
                          reads=[ta, tb], pwrites=[yf])
                for a in range(2):
                    cx.dma("sp", yfT_d[a * 8:(a + 1) * 8, :, ts].rearrange("c p t -> p c t"), yf[:, a * 8:(a + 1) * 8, :], reads=[yf])
            cx.barrier()

        def layer_norm_tile(r, lng, lnb, dst, stats, mv, rs_t):
            for k in range(4):
                cx.op("dve", lambda k=k: nc.vector.bn_stats(stats[:, k, :], r[:, k * 512:(k + 1) * 512]), reads=[r], pwrites=[stats])
            cx.op("dve", lambda: nc.vector.bn_aggr(mv[:, :], stats[:, :, :].rearrange("p a b -> p (a b)")), reads=[stats], writes=[mv])
            cx.op("act", lambda: nc.scalar.activation(rs_t[:, 0:1], mv[:, 1:2], AF.Sqrt, bias=LN_EPS), reads=[mv], writes=[rs_t])
            cx.op("dve", lambda: nc.vector.reciprocal(rs_t[:, 1:2], rs_t[:, 0:1]), writes=[rs_t])
            cx.op("dve", lambda: nc.vector.tensor_scalar(out=r[:, :], in0=r[:, :], scalar1=mv[:, 0:1], scalar2=rs_t[:, 1:2],
                                                         op0=ALU.subtract, op1=ALU.mult), reads=[mv, rs_t], writes=[r])
            cx.op("dve", lambda: nc.vector.tensor_tensor(out=r[:, :], in0=r[:, :], in1=lng[:, :], op=ALU.mult), reads=[lng], writes=[r])
            cx.op("pool", lambda: nc.gpsimd.tensor_tensor(out=dst[:, :], in0=r[:, :], in1=lnb[:, :], op=ALU.add), reads=[r, lnb], writes=[dst])

        if upto >= 7:
          with ExitStack() as ph:
            wo = sb(ph, "wo", [128, 16, D], BF16)
            lng = sb(ph, "ln1g", [128, D])
            lnb = sb(ph, "ln1b", [128, D])
            yfs = [sb(ph, "yfo%d" % i, [128, 16, 512], BF16) for i in range(2)]
            xts = [sb(ph, "xt%d" % i, [128, D]) for i in range(2)]
            rts = [sb(ph, "rt%d" % i, [128, D]) for i in range(2)]
            x1s = [sb(ph, "x1t%d" % i, [128, D]) for i in range(2)]
            h2s = [sb(ph, "h2s%d" % i, [128, 16, 512], BF16) for i in range(1)]
            stats = sb(ph, "stats", [128, 4, 6])
            mv = sb(ph, "mv", [128, 2])
            rs_t = sb(ph, "rs_t", [128, 2])
            pob = [ps(ph, "pob%d" % i, [128, 512]) for i in range(4)]
            ptb = [ps(ph, "ptb%d" % i, [128, 512]) for i in range(4)]
            for a in range(4):
                cx.dma("pool", wo[:, a * 4:(a + 1) * 4, :], wo_d[a * 512:(a + 1) * 512, :].rearrange("(k p) c -> p k c", p=128), pwrites=[wo])
            cx.dma("sp", lng[:, :], ln_d[0], writes=[lng])
            cx.dma("sp", lnb[:, :], ln_d[1], writes=[lnb])
            npt = [0]

            def st_a(tt):
                g, tl = tt // 4, tt % 4
                yf = yfs[g % 2]
                if tl == 0:
                    for a in range(2):
                        cx.dma("sp", yf[:, a * 8:(a + 1) * 8, :], yfT_d[a * 8:(a + 1) * 8, :, g * 512:(g + 1) * 512].rearrange("c p t -> p c t"), pwrites=[yf])
                xt, r = xts[tt % 2], rts[tt % 2]
                cx.dma("sp", xt[:, :], x_d[tt * 128:(tt + 1) * 128, :], writes=[xt])
                for cg in range(4):
                    for oc in range(16):
                        cx.op("pe", lambda cg=cg, oc=oc: nc.tensor.matmul(
                            pob[cg][:, :], yf[:, oc, tl * 128:(tl + 1) * 128], wo[:, oc, cg * 512:(cg + 1) * 512],
                            start=(oc == 0), stop=(oc == 15)), reads=[yf, wo], pwrites=[pob[cg]])
                    cx.op("dve", lambda cg=cg: nc.vector.tensor_tensor(
                        out=r[:, cg * 512:(cg + 1) * 512], in0=pob[cg][:, :], in1=g1bc[:, cg * 512:(cg + 1) * 512], op=ALU.mult),
                        reads=[pob[cg], g1bc], pwrites=[r])
                cx.op("dve", lambda: nc.vector.scalar_tensor_tensor(
                    out=r[:, :], in0=xt[:, :], scalar=DN_ALPHA, in1=r[:, :], op0=ALU.mult, op1=ALU.add), reads=[xt], writes=[r])

            def st_b(tt):
                r, x1t = rts[tt % 2], x1s[tt % 2]
                layer_norm_tile(r, lng, lnb, x1t, stats, mv, rs_t)
                cx.dma("sp", x1_d[tt * 128:(tt + 1) * 128, :], x1t[:, :], reads=[x1t])

            def st_c(tt):
                g, tl = tt // 4, tt % 4
                x1t, h2b = x1s[tt % 2], h2s[0]
                for q4 in range(4):
                    pt = ptb[npt[0] % 4]
                    npt[0] += 1
                    for i in range(4):
                        dc = q4 * 4 + i
                        cx.op("pe", lambda i=i, dc=dc: nc.tensor.transpose(
                            pt[:, i * 128:(i + 1) * 128], x1t[:, dc * 128:(dc + 1) * 128], ident[:, :]), reads=[x1t, ident], pwrites=[pt])
                    for i in range(4):
                        dc = q4 * 4 + i
                        cx.op("act", lambda i=i, dc=dc: nc.scalar.activation(
                            h2b[:, dc, tl * 128:(tl + 1) * 128], pt[:, i * 128:(i + 1) * 128], AF.Identity,
                            bias=b2[:, dc:dc + 1], scale=s2[:, dc:dc + 1]), reads=[pt, s2, b2], pwrites=[h2b])
                if tl == 3:
                    for a in range(2):
                        cx.dma("sp", h2T_d[a * 8:(a + 1) * 8, :, g * 512:(g + 1) * 512].rearrange("c p t -> p c t"), h2b[:, a * 8:(a + 1) * 8, :], reads=[h2b])

            st_a(0)
            st_b(0)
            for tt in range(NT):
                if tt + 1 < NT:
                    st_a(tt + 1)
                st_c(tt)
                if tt + 1 < NT:
                    st_b(tt + 1)
            cx.barrier()

        es_g1.close()
        if upto >= 8:
          with ExitStack() as ph:
            wq = sb(ph, "wq", [128, 16, D], BF16)
            keysT = sb(ph, "keysT", [128, 16, 128], BF16)
            h2g = [sb(ph, "h2g%d" % i, [128, 16, 512], BF16) for i in range(2)]
            qTs = [sb(ph, "qTs%d" % i, [128, 16, 512], BF16) for i in range(2)]
            scb = [sb(ph, "scb%d" % i, [128, 8, 2, 128]) for i in range(2)]
            sv = sb(ph, "sv", [128, 8, 2, 16])
            tmpA = sb(ph, "tk_tmpA", [128, 16, 128])
            tmpB = sb(ph, "tk_tmpB", [128, 8, 256])
            tmpC = sb(ph, "tk_tmpC", [128, 8, 256])
            c16 = sb(ph, "c16", [128, 8, 16, 16])
            c8 = sb(ph, "c8", [128, 8, 24])
            d16 = sb(ph, "d16", [128, 8, 16])
            zz = sb(ph, "zz", [128, 8])
            lz = sb(ph, "lz", [128, 8])
            idx = sb(ph, "idx", [128, 8, 16], mybir.dt.uint32)
            pkb = [sb(ph, "pkb%d" % i, [128, 272]) for i in range(2)]
            pq = [ps(ph, "pq%d" % i, [128, 512]) for i in range(4)]
            psc = [ps(ph, "psc%d" % i, [128, 512]) for i in range(4)]
            for a in range(4):
                cx.dma("pool", wq[:, a * 4:(a + 1) * 4, :], wq_d[a * 512:(a + 1) * 512, :].rearrange("(k p) c -> p k c", p=128), pwrites=[wq])
            cx.dma("pool", keysT[:, :, :], keysT_d, writes=[keysT])
            npq = 0
            for g in range(8):
                hg, qt = h2g[g % 2], qTs[g % 2]
                for a in range(2):
                    cx.dma("sp", hg[:, a * 8:(a + 1) * 8, :], h2T_d[a * 8:(a + 1) * 8, :, g * 512:(g + 1) * 512].rearrange("c p t -> p c t"), pwrites=[hg])
                for hp in range(16):
                    p = pq[npq % 4]
                    npq += 1
                    for dc in range(16):
                        cx.op("pe", lambda p=p, hg=hg, dc=dc, hp=hp: nc.tensor.matmul(
                            p[:, :], wq[:, dc, hp * 128:(hp + 1) * 128], hg[:, dc, :], start=(dc == 0), stop=(dc == 15)),
                            reads=[wq, hg], pwrites=[p])
                    cx.op("act", lambda p=p, qt=qt, hp=hp: nc.scalar.copy(qt[:, hp, :], p[:, :]), reads=[p], pwrites=[qt])
                for tl in range(4):
                    tt = g * 4 + tl
                    sc = scb[tt % 2]
                    for bk in range(4):
                        for i in range(4):
                            hp = bk * 4 + i
                            cx.op("pe", lambda bk=bk, i=i, hp=hp, qt=qt, tl=tl: nc.tensor.matmul(
                                psc[bk][:, i * 128:(i + 1) * 128], qt[:, hp, tl * 128:(tl + 1) * 128], keysT[:, hp, :], start=True, stop=True),
                                reads=[qt, keysT], pwrites=[psc[bk]])
                        cx.op("act", lambda bk=bk, sc=sc: nc.scalar.copy(
                            sc[:, bk * 2:(bk + 1) * 2, :, :], psc[bk][:, :].rearrange("p (a b c) -> p a b c", a=2, b=2)),
                            reads=[psc[bk]], pwrites=[sc])
                    cx.dma("sp", scs_d[tt * 128:(tt + 1) * 128, :], sc[:, :, :, :].rearrange("p a b c -> p (a b c)"), reads=[sc])
                    for h in range(8):
                        for p_ in range(2):
                            cx.op("dve", lambda h=h, p_=p_, sc=sc: nc.vector.max(out=sv[:, h, p_, 0:8], in_=sc[:, h, p_, :]), reads=[sc], pwrites=[sv])
                    for h in range(8):
                        for p_ in range(2):
                            cx.op("dve", lambda h=h, p_=p_, sc=sc: nc.vector.match_replace(
                                out=tmpA[:, h * 2 + p_, :], in_to_replace=sv[:, h, p_, 0:8], in_values=sc[:, h, p_, :], imm_value=-1e30),
                                reads=[sc, sv], pwrites=[tmpA])
                    for h in range(8):
                        for p_ in range(2):
                            cx.op("dve", lambda h=h, p_=p_: nc.vector.max(out=sv[:, h, p_, 8:16], in_=tmpA[:, h * 2 + p_, :]), reads=[tmpA], pwrites=[sv])
                    for h in range(8):
                        for r8 in range(2):
                            cx.op("dve", lambda h=h, r8=r8, sc=sc: nc.vector.max_index(
                                out=idx[:, h, r8 * 8:(r8 + 1) * 8], in_max=sv[:, h, 0, r8 * 8:(r8 + 1) * 8], in_values=sc[:, h, 0, :]),
                                reads=[sv, sc], pwrites=[idx])
                    cx.op("dve", lambda: nc.vector.tensor_tensor(
                        out=c16[:, :, :, :], in0=bcast(sv[:, :, 0, :], 3, [128, 8, 16, 16]), in1=bcast(sv[:, :, 1, :], 2, [128, 8, 16, 16]), op=ALU.add),
                        reads=[sv], writes=[c16])
                    for h in range(8):
                        cx.op("dve", lambda h=h: nc.vector.max(out=c8[:, h, 0:8], in_=c16[:, h, :, :].rearrange("p a b -> p (a b)")), reads=[c16], pwrites=[c8])
                    for h in range(8):
                        cx.op("dve", lambda h=h: nc.vector.match_replace(
                            out=tmpB[:, h, :], in_to_replace=c8[:, h, 0:8], in_values=c16[:, h, :, :].rearrange("p a b -> p (a b)"), imm_value=-1e30),
                            reads=[c16, c8], pwrites=[tmpB])
                    for h in range(8):
                        cx.op("dve", lambda h=h: nc.vector.max(out=c8[:, h, 8:16], in_=tmpB[:, h, :]), reads=[tmpB], pwrites=[c8])
                    for h in range(8):
                        cx.op("dve", lambda h=h: nc.vector.match_replace(
                            out=tmpC[:, h, :], in_to_replace=c8[:, h, 8:16], in_values=tmpB[:, h, :], imm_value=-1e30), reads=[tmpB, c8], pwrites=[tmpC])
                    for h in range(8):
                        cx.op("dve", lambda h=h: nc.vector.max(out=c8[:, h, 16:24], in_=tmpC[:, h, :]), reads=[tmpC], pwrites=[c8])
                    cx.op("dve", lambda: nc.vector.tensor_tensor(out=zz[:, :], in0=c8[:, :, 15], in1=c8[:, :, 16], op=ALU.add),
                          reads=[c8], writes=[zz])
                    cx.op("dve", lambda tt=tt: nc.vector.tensor_scalar(out=prm[:, tt, 0:8], in0=zz[:, :], scalar1=0.5, scalar2=None, op0=ALU.mult),
                          reads=[zz], pwrites=[prm])
                    cx.op("dve", lambda: nc.vector.tensor_tensor(out=d16[:, :, :], in0=c8[:, :, 0:16], in1=bcast(c8[:, :, 0], 2, [128, 8, 16]), op=ALU.subtract),
                          reads=[c8], writes=[d16])
                    cx.op("act", lambda: nc.scalar.activation(d16[:, :, :], d16[:, :, :], AF.Exp), writes=[d16])
                    cx.op("dve", lambda: nc.vector.tensor_reduce(out=zz[:, :], in_=d16[:, :, :], axis=AX.X, op=ALU.add), reads=[d16], writes=[zz])
                    cx.op("act", lambda: nc.scalar.activation(lz[:, :], zz[:, :], AF.Ln), reads=[zz], writes=[lz])
                    cx.op("dve", lambda tt=tt: nc.vector.scalar_tensor_tensor(
                        out=prm[:, tt, 8:16], in0=c8[:, :, 0], scalar=-1.0, in1=lz[:, :], op0=ALU.mult, op1=ALU.subtract),
                        reads=[c8, lz], pwrites=[prm])
                    pk = pkb[tt % 2]
                    cx.op("dve", lambda pk=pk, tt=tt: nc.vector.tensor_tensor(
                        out=pk[:, 0:128].rearrange("p (h k) -> p h k", h=8), in0=sv[:, :, 0, :], in1=bcast(prm[:, tt, 8:16], 2, [128, 8, 16]), op=ALU.add),
                        reads=[sv, prm], pwrites=[pk])
                    cx.op("dve", lambda pk=pk: nc.vector.tensor_copy(pk[:, 128:256].rearrange("p (h k) -> p h k", h=8), idx[:, :, :]),
                          reads=[idx], pwrites=[pk])
                    cx.op("dve", lambda pk=pk, tt=tt: nc.vector.tensor_tensor(out=pk[:, 256:264], in0=prm[:, tt, 0:8], in1=prm[:, tt, 8:16], op=ALU.add),
                          reads=[prm], pwrites=[pk])
                    cx.op("dve", lambda pk=pk: nc.vector.memset(pk[:, 264:272], 0.0), pwrites=[pk])
                    cx.dma("sp", pk_d[tt * 128:(tt + 1) * 128, :], pk[:, :], reads=[pk])
            if dbgprm_d is not None:
                cx.dma("sp", dbgprm_d, prm[:, :, :], reads=[prm])
            cx.barrier()

        if upto >= 9:
          with ExitStack() as ph:
            NTT = NT // 2
            FP8 = mybir.dt.float8e4
            lng = sb(ph, "ln2g", [128, D])
            lnb = sb(ph, "ln2b", [128, D])
            iota = sb(ph, "iota", [128, 128])
            hgs = [sb(ph, "h2p%d" % i, [128, 16, 128], BF16) for i in range(2)]
            s2bs = [sb(ph, "s2b%d" % i, [128, 8, 128]) for i in range(2)]
            pks = [sb(ph, "pkp%d" % i, [128, 272]) for i in range(2)]
            x1t = sb(ph, "x1p", [128, D])
            accs = [sb(ph, "acc%d" % i, [128, D]) for i in range(2)]
            cmb = sb(ph, "cmb", [128, 128, 16])
            ee = sb(ph, "eeR", [128, 2048], BF16)
            Rp = sb(ph, "Rp", [128, 128, 16], BF16)
            RTo = [sb(ph, "RTo%d" % i, [128, 16, 128], BF16) for i in range(2)]
            si1T = [sb(ph, "si1T%d" % i, [128, 128]) for i in range(2)]
            O1T = [sb(ph, "O1T%d" % i, [128, 128, 128], FP8) for i in range(2)]
            GTo = sb(ph, "GTo", [128, 128, 16], BF16)
            gqs = [[sb(ph, "gq%d_%d" % (i, k), [128, 2048], BF16) for k in range(2)] for i in range(2)]
            uts = [sb(ph, "ut%d" % i, [128, 16, 512], BF16) for i in range(2)]
            vcs = [sb(ph, "vc%d" % i, [128, 2, D], BF16) for i in range(2)]
            gas = [sb(ph, "ga%d" % i, [128, 512], BF16) for i in range(2)]
            wws = [sb(ph, "ww%d" % i, [128, 512], BF16) for i in range(2)]
            wTs = [sb(ph, "wT%d" % i, [128, 4, 128], BF16) for i in range(2)]
            stats = sb(ph, "stats2", [128, 4, 6])
            mv = sb(ph, "mv2", [128, 2])
            rs_t = sb(ph, "rs_t2", [128, 2])
            pop = [ps(ph, "pop%d" % i, [128, 512]) for i in range(4)]
            pap = [ps(ph, "pap%d" % i, [128, 512]) for i in range(1)] * 2
            pgf = ps(ph, "pgf", [128, 512])
            pgbs = [ps(ph, "pgb%d" % i, [128, 1024], BF16) for i in range(2)]
            rr = {"b": 0}

            def next_pgb():
                rr["b"] += 1
                return pgbs[rr["b"] % 2]
            cx.dma("sp", lng[:, :], ln_d[2], writes=[lng])
            cx.dma("sp", lnb[:, :], ln_d[3], writes=[lnb])
            cx.dma("sp", iota[:, :], iota_d, writes=[iota])

            def load_small(T_):
                for sub in range(2):
                    tt = 2 * T_ + sub
                    cx.dma("sp", s2bs[sub][:, :, :], scs_d[tt * 128:(tt + 1) * 128, :].rearrange("t (h p n) -> t h p n", h=8, p=2)[:, :, 1, :], writes=[s2bs[sub]])
                    cx.dma("sp", pks[sub][:, :], pk_d[tt * 128:(tt + 1) * 128, :], writes=[pks[sub]])

            def load_hg(T_, sub):
                tt = 2 * T_ + sub
                hg = hgs[sub]
                for a in range(2):
                    cx.dma("sp", hg[:, a * 8:(a + 1) * 8, :], h2T_d[a * 8:(a + 1) * 8, :, tt * 128:(tt + 1) * 128].rearrange("c p t -> p c t"), pwrites=[hg])

            def tile_prep_items(T_):
                items = []
                for sub in range(2):
                    def s_item(sub=sub):
                        cx.op("pe", lambda: nc.tensor.transpose(pgf[:, 0:128], pks[sub][:, 128:256], ident[:, :]), reads=[pks[sub], ident], pwrites=[pgf])
                        cx.op("dve", lambda: nc.vector.tensor_copy(si1T[sub][:, :], pgf[:, 0:128]), reads=[pgf], writes=[si1T[sub]])

                    def o1(sub=sub):
                        cx.op("dve", lambda: nc.vector.tensor_tensor(
                            out=O1T[sub][:, :, :], in0=bcast(si1T[sub][:, :], 2, [128, 128, 128]), in1=bcast(iota[:, :], 1, [128, 128, 128]),
                            op=ALU.is_equal, saturate=False), reads=[si1T[sub], iota], writes=[O1T[sub]])
                    items += [s_item, o1]
                return items

            def oct_items(T_, o):
                j0 = o * 16
                parts = {"elem": [], "rb": [[], []], "pt": [[], []], "gt": [[], []]}
                for sub in range(2):
                    s2b, pk = s2bs[sub], pks[sub]
                    gq = gqs[sub][(T_ * 8 + o) % 2]

                    def elem(s2b=s2b, pk=pk):
                        cx.op("dve", lambda: nc.vector.tensor_tensor(
                            out=cmb[:, :, :].rearrange("p (h k) j -> p h k j", h=8),
                            in0=bcast(pk[:, 0:128].rearrange("p (h k) -> p h k", h=8), 3, [128, 8, 16, 16]),
                            in1=bcast(s2b[:, :, j0:j0 + 16], 2, [128, 8, 16, 16]), op=ALU.add), reads=[pk, s2b], writes=[cmb])
                        cx.op("act", lambda: nc.scalar.activation(ee[:, :], cmb[:, :, :].rearrange("p a b -> p (a b)"), AF.Exp),
                              reads=[cmb], writes=[ee])
                        cx.op("dve", lambda: nc.vector.tensor_tensor(
                            out=Rp[:, :, :].rearrange("p (h k) j -> p h (k j)", h=8), in0=cmb[:, :, :].rearrange("p (h k) j -> p h (k j)", h=8),
                            in1=bcast(pk[:, 256:264], 2, [128, 8, 256]), op=ALU.is_ge), reads=[cmb, pk], writes=[Rp])
                        cx.op("pool", lambda: nc.gpsimd.tensor_tensor(out=Rp[:, :, :].rearrange("p a b -> p (a b)"),
                                                                      in0=Rp[:, :, :].rearrange("p a b -> p (a b)"), in1=ee[:, :], op=ALU.mult),
                              reads=[ee], writes=[Rp])
                    parts["elem"].append(elem)
                    for b_ in range(2):
                        def rb(b_=b_, sub=sub):
                            pgb = next_pgb()
                            for i in range(8):
                                cx.op("pe", lambda i=i: nc.tensor.transpose(pgb[:, i * 128:(i + 1) * 128], Rp[:, :, b_ * 8 + i], identb[:, :]),
                                      reads=[Rp, identb], pwrites=[pgb])
                            cx.op("act", lambda: nc.scalar.copy(
                                RTo[sub][:, b_ * 8:b_ * 8 + 8, :], pgb[:, :].rearrange("p (j t) -> p j t", t=128)),
                                reads=[pgb], pwrites=[RTo[sub]])
                        parts["rb"][sub].append(rb)
                    for tb in range(4):
                        def pt(tb=tb, sub=sub):
                            for tl in range(32):
                                t = tb * 32 + tl
                                cx.op("pe", lambda tl=tl, t=t: nc.tensor.matmul(pgf[:, tl * 16:(tl + 1) * 16], O1T[sub][:, t, :], RTo[sub][:, :, t], start=True, stop=True),
                                      reads=[RTo[sub], O1T[sub]], pwrites=[pgf])
                            cx.op("dve", lambda: nc.vector.tensor_copy(
                                GTo[:, tb * 32:(tb + 1) * 32, :], pgf[:, :].rearrange("p (t j) -> p t j", j=16)), reads=[pgf], pwrites=[GTo])
                        parts["pt"][sub].append(pt)
                    for gb in range(2):
                        def gt(gb=gb, gq=gq):
                            pgb = next_pgb()
                            for jl in range(8):
                                cx.op("pe", lambda jl=jl: nc.tensor.transpose(pgb[:, jl * 128:(jl + 1) * 128], GTo[:, :, gb * 8 + jl], identb[:, :]),
                                      reads=[GTo, identb], pwrites=[pgb])
                            cx.op("act", lambda: nc.scalar.copy(gq[:, gb * 1024:(gb + 1) * 1024], pgb[:, :]), reads=[pgb], pwrites=[gq])
                        parts["gt"][sub].append(gt)
                return parts

            NEG = NTT * 32
            NU = NEG * 2

            def ucoords(u):
                EG, sub = u // 2, u % 2
                T_, eg = EG // 32, EG % 32
                return EG, sub, T_, eg, eg // 4, eg % 4

            def load_ut(EG):
                eg = EG % 32
                ut = uts[EG % 2]
                cx.dma("sp", ut[:, :, :], utb_d[eg * 512:(eg + 1) * 512, :].rearrange("(p k) c -> p (k c)", k=4).rearrange("p (k c) -> p k c", c=512),
                       reads=[utb_t] if EG < 2 else (), writes=[ut])

            def load_vc(EG, half):
                eg = EG % 32
                vc = vcs[half]
                r0 = eg * 512 + half * 256
                cx.dma("sp", vc[:, :, :], vb16_d[r0:r0 + 256, :].rearrange("(k p) d -> p k d", p=128),
                       reads=[vb16_t] if EG < 1 else (), writes=[vc])

            def a_mm(u, part):
                EG, sub, T_, eg, o, e4 = ucoords(u)
                hg, ut, pa, ga, ww = hgs[sub], uts[EG % 2], pap[u % 2], gas[u % 2], wws[u % 2]
                gq = gqs[sub][(T_ * 8 + o) % 2]
                for dc in range(part * 8, part * 8 + 8):
                    cx.op("pe", lambda dc=dc: nc.tensor.matmul(pa[:, :], hg[:, dc, :], ut[:, dc, :], start=(dc == 0), stop=(dc == 15)),
                          reads=[hg, ut], pwrites=[pa])
                if part == 0:
                    return
                cx.op("act", lambda: nc.scalar.activation(ga[:, :], pa[:, :], AF.Gelu), reads=[pa], writes=[ga])
                cx.op("dve", lambda: nc.vector.tensor_tensor(out=ww[:, :], in0=ga[:, :], in1=gq[:, e4 * 512:(e4 + 1) * 512], op=ALU.mult),
                      reads=[ga, gq], writes=[ww])

            def wtr(u):
                ww, wT = wws[u % 2], wTs[u % 2]
                pgb = next_pgb()
                for k in range(4):
                    cx.op("pe", lambda k=k: nc.tensor.transpose(pgb[:, k * 128:(k + 1) * 128], ww[:, k * 128:(k + 1) * 128], identb[:, :]),
                          reads=[ww, identb], pwrites=[pgb])
                cx.op("act", lambda: nc.scalar.copy(wT[:, :, :], pgb[:, 0:512].rearrange("p (a b) -> p a b", b=128)), reads=[pgb], writes=[wT])

            def ph2(u, part):
                wT = wTs[u % 2]
                for k in range(part * 2, part * 2 + 2):
                    vc = vcs[k // 2]
                    for dq in range(4):
                        cx.op("pe", lambda k=k, dq=dq, vc=vc: nc.tensor.matmul(
                            pop[dq][:, :], wT[:, k, :], vc[:, k % 2, dq * 512:(dq + 1) * 512],
                            start=(k == 0), stop=(k == 3)), reads=[wT, vc], pwrites=[pop[dq]])

            def accadd(u):
                EG, sub, T_, eg, o, e4 = ucoords(u)
                acc = accs[sub]
                for dq in range(4):
                    sl = slice(dq * 512, (dq + 1) * 512)
                    if eg == 0:
                        cx.op("dve", lambda dq=dq, sl=sl: nc.vector.tensor_copy(acc[:, sl], pop[dq][:, :]), reads=[pop[dq]], pwrites=[acc])
                    else:
                        cx.op("dve", lambda dq=dq, sl=sl: nc.vector.tensor_tensor(out=acc[:, sl], in0=pop[dq][:, :], in1=acc[:, sl], op=ALU.add),
                              reads=[pop[dq]], writes=[acc])

            def epilogue(T_, sub):
                tt = 2 * T_ + sub
                acc = accs[sub]
                cx.dma("sp", x1t[:, :], x1_d[tt * 128:(tt + 1) * 128, :], writes=[x1t])
                cx.op("dve", lambda: nc.vector.tensor_tensor(out=acc[:, :], in0=acc[:, :], in1=g2bc[:, :], op=ALU.mult), reads=[g2bc], writes=[acc])
                cx.op("dve", lambda: nc.vector.scalar_tensor_tensor(
                    out=acc[:, :], in0=x1t[:, :], scalar=DN_ALPHA, in1=acc[:, :], op0=ALU.mult, op1=ALU.add), reads=[x1t], writes=[acc])
                layer_norm_tile(acc, lng, lnb, x1t, stats, mv, rs_t)
                cx.dma("sp", out_d[tt * 128:(tt + 1) * 128, :], x1t[:, :], reads=[x1t])

            parts_cache = {}

            def get_parts(T_, o):
                if (T_, o) not in parts_cache:
                    parts_cache[(T_, o)] = oct_items(T_, o)
                return parts_cache[(T_, o)]

            def nxt_oct(T_, o):
                return (T_, o + 1) if o < 7 else (T_ + 1, 0)

            def q_for(T_, o):
                n1 = nxt_oct(T_, o)
                items = []
                if n1[0] < NTT:
                    p = get_parts(*n1)
                    if n1[1] == 0:
                        items += tile_prep_items(n1[0])
                    items += p["rb"][0] + [p["elem"][1]] + p["pt"][0] + p["rb"][1] + p["gt"][0] + p["pt"][1] + p["gt"][1]
                    n2 = nxt_oct(*n1)
                    if n2[0] < NTT:
                        if n2[1] == 0:
                            items.append(lambda: load_small(n2[0]))
                        items.append(get_parts(*n2)["elem"][0])
                return items

            load_small(0)
            load_hg(0, 0)
            load_hg(0, 1)
            get_parts(0, 0)["elem"][0]()
            p0 = get_parts(0, 0)
            for it in tile_prep_items(0) + p0["rb"][0] + [p0["elem"][1]] + p0["pt"][0] + p0["rb"][1] + p0["gt"][0] + p0["pt"][1] + p0["gt"][1] + [get_parts(0, 1)["elem"][0]]:
                it()
            load_ut(0)
            load_ut(1)
            load_vc(0, 0)
            load_vc(0, 1)
            a_mm(0, 0)
            a_mm(0, 1)
            queue = []
            per_slot = 0

            def pop_items(n):
                for _ in range(min(n, len(queue))):
                    queue.pop(0)()

            for u in range(NU):
                EG, sub, T_, eg, o, e4 = ucoords(u)
                last_of_oct = (e4 == 3 and sub == 1)
                if e4 == 0 and sub == 0:
                    queue.extend(q_for(T_, o))
                    per_slot = -(-len(queue) // 8)
                n_left = per_slot
                k1 = (n_left + 3) // 4
                wtr(u)
                if sub == 1 and EG + 2 < NEG:
                    load_ut(EG + 2)
                pop_items(k1); n_left -= k1
                if u + 1 < NU:
                    if (u + 1) % 64 < 2 and u + 1 >= 64:
                        load_hg((u + 1) // 64, (u + 1) % 2)
                    a_mm(u + 1, 0)
                if last_of_oct:
                    pop_items(len(queue))
                else:
                    pop_items(k1); n_left -= k1
                if u + 1 < NU:
                    a_mm(u + 1, 1)
                if u >= 1:
                    ph2(u - 1, 0)
                    if (u - 1) % 2 == 1 and (u - 1) // 2 + 1 < NEG:
                        load_vc((u - 1) // 2 + 1, 0)
                if not last_of_oct:
                    pop_items(k1); n_left -= k1
                if u >= 1:
                    ph2(u - 1, 1)
                    if (u - 1) % 2 == 1 and (u - 1) // 2 + 1 < NEG:
                        load_vc((u - 1) // 2 + 1, 1)
                    accadd(u - 1)
                    pEG, psub, pT, peg, po_, pe4 = ucoords(u - 1)
                    if peg == 31:
                        epilogue(pT, psub)
                if not last_of_oct:
                    pop_items(max(n_left, 0))
            ph2(NU - 1, 0)
            ph2(NU - 1, 1)
            accadd(NU - 1)
            epilogue(NTT - 1, 1)
            cx.barrier()
        cx.barrier()
        print("[kernel] instructions emitted:", cx.nins)
    return nc


def host_layout(inputs):
    f = lambda k: np.asarray(inputs[k])
    shared = {}
    w_in = f("w_in")[0]
    b_in = f("b_in")[0]
    perm = np.concatenate([np.arange(32, 64), np.arange(0, 32)])
    cols = np.concatenate([np.arange(0, 4160), 4096 + perm, np.arange(4160, 8256)])
    wext = w_in[:, cols]
    shared["w_in_l"] = np.ascontiguousarray(wext.reshape(16, 128, 65, 128).transpose(2, 1, 0, 3).reshape(65, 128, 2048))
    shared["b_inT"] = np.ascontiguousarray(b_in[cols].reshape(65, 128).T)
    shared["w_ada"] = np.ascontiguousarray(f("w_ada")[0])
    b_ada = f("b_ada")[0]
    shared["b_adaT"] = np.ascontiguousarray(b_ada.reshape(96, 128).T)
    shared["b_ada_row"] = np.ascontiguousarray(b_ada.reshape(1, -1))
    rb = f("rel_bias")[0]
    p = np.arange(128)[:, None, None]
    j = np.arange(5)[None, :, None]
    c = np.arange(128)[None, None, :]
    rel = 128 * (4 - j) + c - p
    idx = np.clip(rel, -63, 256) + 63
    shared["biasT"] = np.ascontiguousarray(rb[:, idx].transpose(1, 0, 2, 3).reshape(128, 8, 640))
    mask = np.zeros((128, 5, 128), np.float32)
    mask[:64, 0, 64:] = NEGM
    mask[64:, 4, :64] = NEGM
    shared["maskA"] = mask.reshape(128, 640)
    shared["gqT"] = np.ascontiguousarray(f("q_norm_g")[0].reshape(4, 128).T)
    shared["gkvT"] = np.ascontiguousarray(f("kv_norm_g")[0].reshape(4, 128).T)
    w_uq = f("w_uq")[0]
    qcols = []
    for h in range(8):
        base = h * 192
        qcols += [np.arange(base, base + 128), np.arange(base + 128, base + 192), base + 128 + perm]
    qcols = np.concatenate(qcols)
    shared["w_uq_l"] = np.ascontiguousarray(w_uq[:, qcols].reshape(4, 128, 2048).transpose(1, 0, 2))
    w_ukv = f("w_ukv")[0].reshape(512, 8, 256)
    shared["w_uk_l"] = np.ascontiguousarray(w_ukv[:, :, :128].reshape(4, 128, 1024).transpose(1, 0, 2))
    shared["w_uv_l"] = np.ascontiguousarray(w_ukv[:, :, 128:].reshape(4, 128, 1024).transpose(1, 0, 2))
    shared["w_pa"] = np.ascontiguousarray(f("w_pa")[0])
    shared["w_pb"] = np.ascontiguousarray(f("w_pb")[0])
    shared["w_o"] = np.ascontiguousarray(f("w_o")[0])
    shared["ln_bc"] = np.ascontiguousarray(np.stack([np.broadcast_to(f(k)[0][None, :], (128, D)) for k in ("ln1_g", "ln1_b", "ln2_g", "ln2_b")]))
    shared["peer_wq"] = np.ascontiguousarray(f("peer_wq")[0])
    keys = f("peer_keys")[0]
    shared["keysT"] = np.ascontiguousarray(keys.reshape(16, 128, 128).transpose(2, 0, 1))
    U = f("peer_u")[0].reshape(128, 128, D).transpose(1, 0, 2).reshape(16384, D)
    shared["ut_l"] = np.ascontiguousarray(U.reshape(32, 512, 16, 128).transpose(0, 3, 2, 1)).reshape(16384, 2048)
    shared["peer_v"] = np.ascontiguousarray(f("peer_v")[0].reshape(128, 128, D).transpose(1, 0, 2).reshape(16384, D))
    shared["ident"] = np.eye(128, dtype=np.float32)
    shared["iota"] = np.ascontiguousarray(np.broadcast_to(np.arange(128, dtype=np.float32)[None, :], (128, 128)))
    inv_freq = (10000.0 ** (-np.arange(0, 64, 2, dtype=np.float32) / 64.0)).astype(np.float32)
    invf = np.zeros((64, 2), np.float32)
    invf[:, 0] = np.concatenate([inv_freq, inv_freq])
    invf[:32, 1] = -1.0
    invf[32:, 1] = 1.0
    shared["invf"] = invf
    per_core = []
    x = f("x")
    cc = f("c")
    pos = f("positions")
    for b in range(NCORES):
        m = dict(shared)
        m["x"] = np.ascontiguousarray(x[b])
        m["cT"] = np.ascontiguousarray(cc[b].reshape(16, 128).T)
        m["posb"] = np.ascontiguousarray(np.broadcast_to(pos[b][None, :].astype(np.int32), (64, S)))
        per_core.append(m)
    return per_core


_NC_CACHE = {}


def kernel(**inputs):
    maps = host_layout(inputs)
    if "full" not in _NC_CACHE:
        _NC_CACHE["full"] = build()
    nc = _NC_CACHE["full"]
    res = run_bass_kernel_spmd(nc, maps, core_ids=list(range(NCORES)))
    out = np.stack([np.asarray(r["out"]) for r in res.results], axis=0)
    return out.astype(np.float32)
```
